# Optimizing a Trainium2 kernel written in Bass

```python
import math
import jax
import jax.numpy as jnp
from jax import lax
import numpy as np

D_MODEL = 1024
BATCH = 8
SEQ = 4096
DEPTH = 2

N_BRANCH = 3
BRANCH_WIDTH = D_MODEL // 2

DA_HEADS = 4
DA_HEAD_DIM = BRANCH_WIDTH // (2 * DA_HEADS)
DA_V_DIM = 2 * DA_HEAD_DIM
DA_QK = DA_HEADS * 2 * DA_HEAD_DIM
DA_V = DA_HEADS * DA_V_DIM
Q_BLOCK = 128
ALIBI_MAX_EXP = 8.0

S5_GROUP_SIZE = 16
S5_GROUPS = BRANCH_WIDTH // S5_GROUP_SIZE
S5_STATE = 64
S5_DT_MIN = 1e-3
S5_DT_MAX = 1e-1

GLA_HEADS = 4
GLA_DV = BRANCH_WIDTH // GLA_HEADS
GLA_DK = GLA_DV // 2
GLA_K = GLA_HEADS * GLA_DK
GLA_V = GLA_HEADS * GLA_DV
GLA_GATE_RANK = 16
GLA_TAU = 16.0
GLA_CHUNK = 64

SPLIT_SIZES = (DA_QK, DA_QK, DA_V, BRANCH_WIDTH, GLA_K, GLA_K, GLA_V, GLA_V, GLA_GATE_RANK, GLA_GATE_RANK, N_BRANCH * D_MODEL)
N_IN = sum(SPLIT_SIZES)

N_EXPERTS = 16
N_EXPERT_GROUPS = 4
EXPERTS_PER_GROUP = N_EXPERTS // N_EXPERT_GROUPS
TOP_K = 2
GROUP_SCORE_K = 2
EXPERT_FF = D_MODEL // 2

DN_ALPHA = (2.0 * DEPTH) ** 0.25
DN_BETA = (8.0 * DEPTH) ** -0.25
NORM_EPS = 1e-5

kernel_name = 'hybrid_diffattn_s5_gla_moe_encoder'


def layer_norm(x, g, b):
    xf = x.astype(jnp.float32)
    mu = jnp.mean(xf, axis=-1, keepdims=True)
    xc = xf - mu
    var = jnp.mean(xc * xc, axis=-1, keepdims=True)
    return (xc * lax.rsqrt(var + NORM_EPS) * g.astype(jnp.float32) + b.astype(jnp.float32)).astype(x.dtype)


def rms_norm(x, g):
    xf = x.astype(jnp.float32)
    ms = jnp.mean(xf * xf, axis=-1, keepdims=True)
    return (xf * lax.rsqrt(ms + NORM_EPS) * g.astype(jnp.float32)).astype(x.dtype)


def diff_attention(q, k, v, lam):
    b, s, h, _, dh = q.shape
    n_blk = s // Q_BLOCK
    scale = dh ** -0.5
    q_blocks = q.astype(jnp.float32).reshape(b, n_blk, Q_BLOCK, h, 2, dh).transpose(1, 0, 3, 4, 2, 5)
    k_t = k.astype(jnp.float32).transpose(0, 2, 3, 1, 4)
    v_t = v.astype(jnp.float32).transpose(0, 2, 1, 3)
    slopes = 2.0 ** (-ALIBI_MAX_EXP * jnp.arange(1, h + 1, dtype=jnp.float32) / h)
    pos_k = jnp.arange(s, dtype=jnp.int32)

    def block(args):
        q_blk, start = args
        logits = jnp.einsum('bhiqd,bhikd->bhiqk', q_blk, k_t) * scale
        pos_q = start + jnp.arange(Q_BLOCK, dtype=jnp.int32)
        dist = jnp.abs(pos_q[:, None] - pos_k[None, :]).astype(jnp.float32)
        alibi = -slopes[:, None, None] * dist
        probs = jax.nn.softmax(logits + alibi[None, :, None], axis=-1)
        weights = probs[:, :, 0] - lam * probs[:, :, 1]
        return jnp.einsum('bhqk,bhkv->bhqv', weights, v_t)

    starts = jnp.arange(n_blk, dtype=jnp.int32) * Q_BLOCK
    out = lax.map(block, (q_blocks, starts))
    return out.transpose(1, 0, 3, 2, 4).reshape(b, s, h, 2 * dh).astype(v.dtype)


def _ssm_combine(left, right):
    a_l, b_l = left
    a_r, b_r = right
    return a_r * a_l, a_r * b_l + b_r


def s5_bidirectional(u, a_re, a_im, log_dt, b_re, b_im, c_re, c_im, d_skip):
    bsz, s, w = u.shape
    uf = u.astype(jnp.float32).reshape(bsz, s, S5_GROUPS, S5_GROUP_SIZE)
    uc = uf.astype(jnp.complex64)
    y = uf * d_skip.astype(jnp.float32).reshape(S5_GROUPS, S5_GROUP_SIZE)
    for direction, rev in ((0, False), (1, True)):
        lam = lax.complex(a_re[direction].astype(jnp.float32), a_im[direction].astype(jnp.float32))
        dt = jnp.exp(log_dt[direction].astype(jnp.float32))[:, None]
        a_bar = jnp.exp(lam * dt)
        b_mat = lax.complex(b_re[direction].astype(jnp.float32), b_im[direction].astype(jnp.float32))
        b_bar = ((a_bar - 1.0) / lam)[..., None] * b_mat
        c_mat = lax.complex(c_re[direction].astype(jnp.float32), c_im[direction].astype(jnp.float32))
        bu = jnp.einsum('bsgc,gpc->bsgp', uc, b_bar)
        a_seq = jnp.broadcast_to(a_bar, (s,) + a_bar.shape)
        states = jax.vmap(lambda e: lax.associative_scan(_ssm_combine, (a_seq, e), reverse=rev)[1])(bu)
        y = y + jnp.real(jnp.einsum('bsgp,gcp->bsgc', states, c_mat))
    return y.reshape(bsz, s, w).astype(u.dtype)


def gla_chunked(q, k, v, log_a, inclusive):
    b, s, h, dk = q.shape
    dv = v.shape[-1]
    c = GLA_CHUNK
    n = s // c
    qf = q.astype(jnp.float32).reshape(b, n, c, h, dk)
    kf = k.astype(jnp.float32).reshape(b, n, c, h, dk)
    vf = v.astype(jnp.float32).reshape(b, n, c, h, dv)
    cum = jnp.cumsum(log_a.astype(jnp.float32).reshape(b, n, c, h, dk), axis=2)
    ref = cum[:, :, c // 2][:, :, None]
    last = cum[:, :, -1]
    scores = jnp.einsum('bnchd,bnshd->bnhcs', qf * jnp.exp(cum - ref), kf * jnp.exp(ref - cum))
    mask = jnp.tril(jnp.ones((c, c), dtype=bool), 0 if inclusive else -1)
    scores = jnp.where(mask, scores, 0.0)
    o_intra = jnp.einsum('bnhcs,bnshv->bnchv', scores, vf)
    d_state = jnp.einsum('bnchd,bnchv->bnhdv', kf * jnp.exp(last[:, :, None] - cum), vf)
    chunk_decay = jnp.exp(last)

    def step(state, inp):
        ds, dec = inp
        return dec[..., None] * state + ds, state

    init = jnp.zeros((b, h, dk, dv), jnp.float32)
    _, s_in = lax.scan(step, init, (jnp.moveaxis(d_state, 1, 0), jnp.moveaxis(chunk_decay, 1, 0)))
    s_in = jnp.moveaxis(s_in, 0, 1)
    o_inter = jnp.einsum('bnchd,bnhdv->bnchv', qf * jnp.exp(cum), s_in)
    return (o_intra + o_inter).reshape(b, s, h, dv)


def _flip_seq(t):
    return jnp.flip(t, axis=1)


def hybrid_mixer(x, layer, w_in, da_lambda, da_norm_g, s5_a_re, s5_a_im, s5_log_dt, s5_b_re, s5_b_im,
                 s5_c_re, s5_c_im, s5_d, s5_w_glu, s5_b_glu, gla_w_gate, gla_b_gate, gla_norm_g,
                 merge_w_up, merge_b, w_out):
    b, s, d = x.shape
    split_points = np.cumsum(SPLIT_SIZES)[:-1].tolist()
    (q_a, k_a, v_a, u_s5, q_g, k_g, v_g, r_g, z_f, z_b, gate_logits) = jnp.split(x @ w_in, split_points, axis=-1)

    lam_init = 0.8 - 0.6 * math.exp(-0.3 * layer)
    lv = da_lambda.astype(jnp.float32)
    lam = jnp.exp(jnp.sum(lv[0] * lv[1])) - jnp.exp(jnp.sum(lv[2] * lv[3])) + lam_init
    o_a = diff_attention(q_a.reshape(b, s, DA_HEADS, 2, DA_HEAD_DIM),
                         k_a.reshape(b, s, DA_HEADS, 2, DA_HEAD_DIM),
                         v_a.reshape(b, s, DA_HEADS, DA_V_DIM), lam)
    o_a = (rms_norm(o_a, da_norm_g) * (1.0 - lam_init)).reshape(b, s, BRANCH_WIDTH)

    y_s5 = jax.nn.gelu(s5_bidirectional(u_s5, s5_a_re, s5_a_im, s5_log_dt, s5_b_re, s5_b_im, s5_c_re, s5_c_im, s5_d))
    o_b = y_s5 * jax.nn.sigmoid(y_s5 @ s5_w_glu + s5_b_glu)

    qg = q_g.reshape(b, s, GLA_HEADS, GLA_DK) * (GLA_DK ** -0.5)
    kg = k_g.reshape(b, s, GLA_HEADS, GLA_DK)
    vg = v_g.reshape(b, s, GLA_HEADS, GLA_DV)
    log_f = (jax.nn.log_sigmoid((z_f @ gla_w_gate[0] + gla_b_gate[0]).astype(jnp.float32)) / GLA_TAU).reshape(b, s, GLA_HEADS, GLA_DK)
    log_b = (jax.nn.log_sigmoid((z_b @ gla_w_gate[1] + gla_b_gate[1]).astype(jnp.float32)) / GLA_TAU).reshape(b, s, GLA_HEADS, GLA_DK)
    o_fwd = gla_chunked(qg, kg, vg, log_f, True)
    o_bwd = _flip_seq(gla_chunked(_flip_seq(qg), _flip_seq(kg), _flip_seq(vg), _flip_seq(log_b), False))
    o_c = rms_norm((o_fwd + o_bwd).astype(x.dtype), gla_norm_g) * jax.nn.silu(r_g).reshape(b, s, GLA_HEADS, GLA_DV)
    o_c = o_c.reshape(b, s, BRANCH_WIDTH)

    gate_logits = gate_logits.reshape(b, s, N_BRANCH, d)
    merged = jnp.zeros_like(x)
    for n, o_n in enumerate((o_a, o_b, o_c)):
        gate = jax.nn.sigmoid(gate_logits[:, :, n] + merge_b[n])
        merged = merged + gate * (o_n @ merge_w_up[n])
    return merged @ w_out


def grouped_moe(x, router_w, router_bias, w_gate, w_up, w_down):
    b, s, d = x.shape
    t = x.reshape(b * s, d)
    scores = jax.nn.sigmoid((t @ router_w).astype(jnp.float32))
    biased = (scores + router_bias.astype(jnp.float32)).reshape(-1, N_EXPERT_GROUPS, EXPERTS_PER_GROUP)
    group_score = jnp.sum(lax.top_k(biased, GROUP_SCORE_K)[0], axis=-1)
    group_sel = jnp.argmax(group_score, axis=-1)
    in_group = jnp.take_along_axis(biased, group_sel[:, None, None], axis=1)[:, 0]
    _, local = lax.top_k(in_group, TOP_K)
    expert_idx = group_sel[:, None] * EXPERTS_PER_GROUP + local
    w_sel = jnp.take_along_axis(scores, expert_idx, axis=1)
    w_sel = w_sel / jnp.sum(w_sel, axis=-1, keepdims=True)
    combine = jnp.sum(jax.nn.one_hot(expert_idx, N_EXPERTS, dtype=jnp.float32) * w_sel[..., None], axis=1).astype(t.dtype)
    out = jnp.zeros_like(t)
    for e in range(N_EXPERTS):
        h = jax.nn.silu(t @ w_gate[e]) * (t @ w_up[e])
        out = out + combine[:, e:e + 1] * (h @ w_down[e])
    return out.reshape(b, s, d)


def setup_inputs(seed: int = 0) -> dict:
    key = jax.random.key(seed)
    ks = jax.random.split(key, 32)
    f32 = jnp.float32

    def nrm(i, shape, std):
        return jax.random.normal(ks[i], shape, f32) * std

    W, G, P, C = BRANCH_WIDTH, S5_GROUPS, S5_STATE, S5_GROUP_SIZE
    n_idx = jnp.arange(P, dtype=f32)
    return {
        'x': nrm(0, (BATCH, SEQ, D_MODEL), 1.0),
        'ln0_g': 1.0 + nrm(1, (D_MODEL,), 0.02),
        'ln0_b': nrm(2, (D_MODEL,), 0.02),
        'w_in': nrm(3, (DEPTH, D_MODEL, N_IN), D_MODEL ** -0.5),
        'da_lambda': nrm(4, (DEPTH, 4, DA_HEAD_DIM), 0.1),
        'da_norm_g': 1.0 + nrm(5, (DEPTH, DA_V_DIM), 0.02),
        's5_a_re': -0.5 + nrm(6, (DEPTH, 2, G, P), 0.01),
        's5_a_im': math.pi * n_idx + nrm(7, (DEPTH, 2, G, P), 0.01),
        's5_log_dt': jax.random.uniform(ks[8], (DEPTH, 2, G), f32, math.log(S5_DT_MIN), math.log(S5_DT_MAX)),
        's5_b_re': nrm(9, (DEPTH, 2, G, P, C), (2.0 * C) ** -0.5),
        's5_b_im': nrm(10, (DEPTH, 2, G, P, C), (2.0 * C) ** -0.5),
        's5_c_re': nrm(11, (DEPTH, 2, G, C, P), (2.0 * P) ** -0.5),
        's5_c_im': nrm(12, (DEPTH, 2, G, C, P), (2.0 * P) ** -0.5),
        's5_d': nrm(13, (DEPTH, W), 1.0),
        's5_w_glu': nrm(14, (DEPTH, W, W), W ** -0.5),
        's5_b_glu': nrm(15, (DEPTH, W), 0.02),
        'gla_w_gate': nrm(16, (DEPTH, 2, GLA_GATE_RANK, GLA_K), GLA_GATE_RANK ** -0.5),
        'gla_b_gate': nrm(17, (DEPTH, 2, GLA_K), 0.1),
        'gla_norm_g': 1.0 + nrm(18, (DEPTH, GLA_DV), 0.02),
        'merge_w_up': nrm(19, (DEPTH, N_BRANCH, W, D_MODEL), (W ** -0.5) * DN_BETA),
        'merge_b': nrm(20, (DEPTH, N_BRANCH, D_MODEL), 0.02),
        'w_out': nrm(21, (DEPTH, D_MODEL, D_MODEL), (D_MODEL ** -0.5) * DN_BETA),
        'ln1_g': 1.0 + nrm(22, (DEPTH, D_MODEL), 0.02),
        'ln1_b': nrm(23, (DEPTH, D_MODEL), 0.02),
        'router_w': nrm(24, (D_MODEL, N_EXPERTS), D_MODEL ** -0.5),
        'router_bias': nrm(25, (N_EXPERTS,), 0.01),
        'moe_w_gate': nrm(26, (DEPTH, N_EXPERTS, D_MODEL, EXPERT_FF), (D_MODEL ** -0.5) * DN_BETA),
        'moe_w_up': nrm(27, (DEPTH, N_EXPERTS, D_MODEL, EXPERT_FF), (D_MODEL ** -0.5) * DN_BETA),
        'moe_w_down': nrm(28, (DEPTH, N_EXPERTS, EXPERT_FF, D_MODEL), (EXPERT_FF ** -0.5) * DN_BETA),
        'ln2_g': 1.0 + nrm(29, (DEPTH, D_MODEL), 0.02),
        'ln2_b': nrm(30, (DEPTH, D_MODEL), 0.02),
    }


def reference(x, ln0_g, ln0_b, w_in, da_lambda, da_norm_g, s5_a_re, s5_a_im, s5_log_dt, s5_b_re, s5_b_im,
              s5_c_re, s5_c_im, s5_d, s5_w_glu, s5_b_glu, gla_w_gate, gla_b_gate, gla_norm_g, merge_w_up,
              merge_b, w_out, ln1_g, ln1_b, router_w, router_bias, moe_w_gate, moe_w_up, moe_w_down,
              ln2_g, ln2_b):
    x = layer_norm(x, ln0_g, ln0_b)
    for l in range(DEPTH):
        mix = hybrid_mixer(x, l, w_in[l], da_lambda[l], da_norm_g[l], s5_a_re[l], s5_a_im[l], s5_log_dt[l],
                           s5_b_re[l], s5_b_im[l], s5_c_re[l], s5_c_im[l], s5_d[l], s5_w_glu[l], s5_b_glu[l],
                           gla_w_gate[l], gla_b_gate[l], gla_norm_g[l], merge_w_up[l], merge_b[l], w_out[l])
        x = layer_norm(DN_ALPHA * x + mix, ln1_g[l], ln1_b[l])
        ffn = grouped_moe(x, router_w, router_bias, moe_w_gate[l], moe_w_up[l], moe_w_down[l])
        x = layer_norm(DN_ALPHA * x + ffn, ln2_g[l], ln2_b[l])
    return x
```

```python
import math
from contextlib import ExitStack
import numpy as np
import ml_dtypes
import concourse.bass as bass
import concourse.mybir as mybir
from concourse.bass_utils import run_bass_kernel_spmd

F32 = mybir.dt.float32
BF16 = mybir.dt.bfloat16
AF = mybir.ActivationFunctionType
ALU = mybir.AluOpType
AX = mybir.AxisListType

ENGS = ("pe", "act", "dve", "pool", "sp")
NDMA_SEM = 6


class Buf:
    __slots__ = ("name", "w", "r", "excl")

    def __init__(self, name=""):
        self.name = name
        self.w = None
        self.r = {}
        self.excl = False


class Tl:
    def __init__(self, t, name, nb=0):
        self.t = t
        self.b = Buf(name)
        self.bs = [Buf(f"{name}{i}") for i in range(nb)]

    def __getitem__(self, k):
        return self.t[k]


class Sub(Tl):
    def __init__(self, ap, buf):
        self.t = ap
        self.b = buf
        self.bs = []


class Prog:
    def __init__(self, nc):
        self.nc = nc
        self.stack = ExitStack()
        self.lists = {e: [] for e in ENGS}
        self.cnt = {e: 0 for e in ENGS}
        self.known = {e: {} for e in ENGS}
        self.sem = {}
        for e in ("pe", "act", "dve", "pool"):
            self.sem["c_" + e] = self.stack.enter_context(nc.semaphore("c_" + e))
        self.dq = {}
        for q in ("sp", "pool", "act"):
            names = []
            for i in range(NDMA_SEM):
                n = f"d_{q}{i}"
                self.sem[n] = self.stack.enter_context(nc.semaphore(n))
                names.append(n)
            self.dq[q] = dict(sems=names, issued=[0] * NDMA_SEM, rr=0)
        self.nwaits = 0

    uid = 0

    def sb(self, name, shape, dtype, stack=None, nb=0):
        Prog.uid += 1
        name = f"{name}_{Prog.uid}"
        t = (stack or self.stack).enter_context(self.nc.sbuf_tensor(name, list(shape), dtype))
        return Tl(t, name, nb)

    def ps(self, name, shape, dtype=F32, stack=None, nb=0):
        t = (stack or self.stack).enter_context(self.nc.psum_tensor(name, list(shape), dtype))
        tl = Tl(t, name, nb)
        tl.b.excl = True
        return tl

    def dram(self, name, shape, dtype, kind="Internal", nb=0):
        t = self.nc.dram_tensor(name, list(shape), dtype, kind=kind).ap()
        return Tl(t, name, nb)

    @staticmethod
    def _bufs(xs):
        out = []
        for x in xs:
            if x is None:
                continue
            out.append(x.b if isinstance(x, Tl) else x)
        return out

    def _need(self, reads, writes):
        need = {}

        def add(sv):
            s, v = sv
            if need.get(s, 0) < v:
                need[s] = v

        for b in reads:
            if b.w:
                add(b.w)
        for b in writes:
            if b.w:
                add(b.w)
            for sv in b.r.items():
                add(sv)
        return need

    def _waits(self, e, need, skip=None):
        kn = self.known[e]
        for s, v in need.items():
            if s == skip:
                continue
            if kn.get(s, 0) < v:
                self.lists[e].append(("wait", s, v))
                kn[s] = v
                self.nwaits += 1

    def op(self, e, fn, reads=(), writes=(), serial=False):
        reads = self._bufs(reads)
        writes = self._bufs(writes)
        ex = [b for b in reads if b.excl and b not in writes]
        if ex:
            reads = [b for b in reads if not b.excl]
            writes = writes + ex
        s = "c_" + e
        self._waits(e, self._need(reads, writes), skip=(s if (e == "pe" and not serial) else None))
        self.cnt[e] += 1
        v = self.cnt[e]
        self.lists[e].append(("op", fn, s))
        for b in reads:
            b.r[s] = v
        for b in writes:
            b.w = (s, v)
            b.r = {}

    def dma(self, q, out, in_, reads=(), writes=(), **kw):
        reads = self._bufs(reads)
        writes = self._bufs(writes)
        d = self.dq[q]
        i = d["rr"]
        d["rr"] = (i + 1) % NDMA_SEM
        s = d["sems"][i]
        need = self._need(reads, writes)
        if d["issued"][i] > 0:
            pv = 16 * d["issued"][i]
            if need.get(s, 0) < pv:
                need[s] = pv
        self._waits(q, need)
        d["issued"][i] += 1
        v = 16 * d["issued"][i]
        self.lists[q].append(("dma", out, in_, s, kw))
        for b in reads:
            b.r[s] = v
        for b in writes:
            b.w = (s, v)
            b.r = {}

    def barrier(self, engines=ENGS):
        need = {}
        for e in ("pe", "act", "dve", "pool"):
            if self.cnt[e]:
                need["c_" + e] = self.cnt[e]
        for q, d in self.dq.items():
            for s, n in zip(d["sems"], d["issued"]):
                if n:
                    need[s] = 16 * n
        for e in engines:
            self._waits(e, dict(need), skip=("c_" + e if e in ("pe",) else None))

    def emit(self):
        nc = self.nc
        self.barrier(engines=("sp",))
        lists = self.lists
        sem = self.sem

        def run(eng, lst):
            for it in lst:
                if it[0] == "wait":
                    eng.wait_ge(sem[it[1]], it[2])
                elif it[0] == "op":
                    it[1](eng).then_inc(sem[it[2]], 1)
                else:
                    eng.dma_start(out=it[1], in_=it[2], **it[4]).then_inc(sem[it[3]], 16)

        with nc.Block() as block:
            @block.tensor
            def _(eng):
                run(eng, lists["pe"])

            @block.scalar
            def _(eng):
                run(eng, lists["act"])

            @block.vector
            def _(eng):
                run(eng, lists["dve"])

            @block.gpsimd
            def _(eng):
                run(eng, lists["pool"])

            @block.sync
            def _(eng):
                run(eng, lists["sp"])
        self.stack.close()


S = 4096
D = 1024
NT = 32
DEPTH = 2
N_IN = 6688
COL = dict(qa=0, ka=512, va=1024, u=1536, qc=2048, kc=2304, vc=2560, rc=3072, zf=3584, zb=3600, gate=3616)
ALPHA = (2.0 * DEPTH) ** 0.25
EPS = 1e-5
PI = math.pi

PARAM_SHAPES = dict(
    ln0_g=[1024], ln0_b=[1024], w_in=[2, 1024, 6688], da_lambda=[2, 4, 64], da_norm_g=[2, 128],
    s5_a_re=[2, 2, 32, 64], s5_a_im=[2, 2, 32, 64], s5_log_dt=[2, 2, 32], s5_b_re=[2, 2, 32, 64, 16],
    s5_b_im=[2, 2, 32, 64, 16], s5_c_re=[2, 2, 32, 16, 64], s5_c_im=[2, 2, 32, 16, 64], s5_d=[2, 512],
    s5_w_glu=[2, 512, 512], s5_b_glu=[2, 512], gla_w_gate=[2, 2, 16, 256], gla_b_gate=[2, 2, 256],
    gla_norm_g=[2, 128], merge_w_up=[2, 3, 512, 1024], merge_b=[2, 3, 1024], w_out=[2, 1024, 1024],
    ln1_g=[2, 1024], ln1_b=[2, 1024], router_w=[1024, 16], router_bias=[16],
    moe_w_gate=[2, 16, 1024, 512], moe_w_up=[2, 16, 1024, 512], moe_w_down=[2, 16, 512, 1024],
    ln2_g=[2, 1024], ln2_b=[2, 1024])


def host_consts():
    c = {}
    c["identf"] = np.eye(128, dtype=np.float32)
    i = np.arange(128)
    c["absdiff"] = np.abs(i[:, None] - i[None, :]).astype(np.float32)
    pos = np.arange(S)
    hi, lo = pos // 64, pos % 64
    c["qaug"] = np.stack([64.0 * hi, lo, np.ones(S), np.ones(S)]).astype(ml_dtypes.bfloat16)
    ka = np.zeros((4, 2, 4, S), np.float32)
    for h in range(4):
        sl = 2.0 ** (-2.0 * (h + 1))
        plus = np.stack([np.full(S, sl), np.full(S, sl), -sl * 64.0 * hi, -sl * lo])
        ka[h, 0] = plus
        ka[h, 1] = -plus
    c["kaug"] = ka.astype(ml_dtypes.bfloat16)
    j = np.arange(64)
    c["gmask"] = np.stack([(j[:, None] <= j[None, :]), (j[:, None] > j[None, :])]).astype(np.float32)
    jj = np.repeat(np.arange(8), 16)
    c["s5mask"] = np.stack([(jj[None, :] >= jj[:, None]), (jj[None, :] <= jj[:, None])]).astype(np.float32)
    jx = np.zeros((128, 128), np.float32)
    for q in range(64):
        jx[q, 64 + q] = 1.0
        jx[64 + q, q] = 1.0
    c["jx"] = jx
    c["rowmask"] = (np.arange(128)[:, None] // 16 == np.arange(8)[None, :]).astype(np.float32)
    sg = np.ones((128, 2), np.float32)
    sg[:64, 0] = -1.0
    sg[64:, 1] = -1.0
    c["sgn"] = sg
    return c


CONST_SHAPES = dict(jx=([128, 128], F32), rowmask=([128, 8], F32), sgn=([128, 2], F32), identf=([128, 128], F32), absdiff=([128, 128], F32), qaug=([4, S], BF16), kaug=([4, 2, 4, S], BF16),
                    gmask=([2, 64, 64], F32), s5mask=([2, 128, 128], F32))


class Model:
    def __init__(self, dbg=None):
        self.dbg = dbg or {}
        nc = bass.Bass("TRN2", target_bir_lowering=False)
        self.nc = nc
        p = Prog(nc)
        self.p = p
        kinds = self.dbg.get("kinds", {})
        self.x_in = p.dram("x", [S, D], F32, kind="ExternalInput")
        self.out = p.dram("out", [S, D], F32, kind="ExternalOutput", nb=NT)
        self.W = {k: p.dram(k, shp, F32, kind="ExternalInput") for k, shp in PARAM_SHAPES.items()}
        self.C = {k: p.dram(k, shp, dt, kind="ExternalInput") for k, (shp, dt) in CONST_SHAPES.items()}
        self.xres = p.dram("xres", [S, D], F32, kind=kinds.get("xres", "Internal"), nb=NT)
        self.oT = p.dram("oT", [3, 4, 128, S], BF16, kind=kinds.get("oT", "Internal"), nb=12)
        self.mT = p.dram("mT", [128, 8, S], BF16, kind=kinds.get("mT", "Internal"), nb=16)
        if self.dbg.get("s5y"):
            self.dbgy = p.dram("dbgy", [8, 128, S], F32, kind="ExternalOutput")
        self.xT = p.sb("xT", [128, 8, S], BF16, nb=NT)
        self.identf = p.sb("identf_sb", [128, 128], F32)
        self.call = p.sb("call", [128, NT, 16], F32, nb=NT)
        self.PS = [p.ps(f"ps{i}", [128, 512], F32) for i in range(8)]
        self.psi = 0
        p.dma("sp", self.identf[:], self.C["identf"][:, :], writes=[self.identf])

    def dump(self, name, tl, ap, shape):
        if not self.dbg.get("dump"):
            return
        d = self.p.dram("dump_" + name, list(shape), F32, kind="ExternalOutput")
        self.p.dma("pool", d.t, ap, reads=[tl], writes=[d])

    def bank(self, lo=0, hi=8):
        n = hi - lo
        b = self.PS[lo + (self.psi % n)]
        self.psi += 1
        return b

    def mm(self, out, lhsT, rhs, start, stop, reads, writes, serial=False):
        self.p.op("pe", lambda e: e.matmul(out, lhsT, rhs, start=start, stop=stop, skip_group_check=True), reads=reads, writes=writes, serial=serial)

    def ln_tile(self, z, xn, stats, mv, rstd):
        p = self.p
        for k in range(2):
            p.op("dve", lambda e, k=k: e.bn_stats(out=stats[:, k, :], in_=z[:, k * 512:(k + 1) * 512]), reads=[z], writes=[stats])
        p.op("dve", lambda e: e.bn_aggr(out=mv[:, :], in_=stats[:, :, :].rearrange("p a b -> p (a b)")), reads=[stats], writes=[mv])
        self.rsqrt(rstd, rstd[:, :], mv, mv[:, 1:2], 1.0)
        p.op("dve", lambda e: e.tensor_scalar(xn[:, :], z[:, :], mv[:, 0:1], rstd[:, 0:1], ALU.subtract, ALU.mult), reads=[z, mv, rstd], writes=[xn])
        p.op("pool", lambda e, g_=self.gbc: e.tensor_tensor(out=xn[:, :], in0=xn[:, :], in1=g_[:, :], op=ALU.mult), reads=[xn, self.gbc], writes=[xn])
        p.op("pool", lambda e, b_=self.bbc: e.tensor_tensor(out=xn[:, :], in0=xn[:, :], in1=b_[:, :], op=ALU.add), reads=[xn, self.bbc], writes=[xn])

    def rsqrt(self, dst_tl, dst, src_tl, src, scale):
        p = self.p
        p.op("dve", lambda e: e.tensor_scalar(dst, src, scale, EPS, ALU.mult, ALU.add), reads=[src_tl], writes=[dst_tl])
        p.op("act", lambda e: e.sqrt(out=dst, in_=dst), reads=[dst_tl], writes=[dst_tl])
        p.op("dve", lambda e: e.reciprocal(out=dst, in_=dst), reads=[dst_tl], writes=[dst_tl])

    def load_ln_params(self, g_ap, b_ap, st):
        p = self.p
        self.gbc = p.sb("gbc", [128, D], F32, st)
        self.bbc = p.sb("bbc", [128, D], F32, st)
        p.dma("sp", self.gbc[:], g_ap.partition_broadcast(128), writes=[self.gbc])
        p.dma("sp", self.bbc[:], b_ap.partition_broadcast(128), writes=[self.bbc])

    def to_xT(self, xn, t, xT32=None):
        p = self.p
        for c0 in (0, 4):
            ps = self.bank(0, 4)
            for j in range(4):
                c = c0 + j
                p.op("pe", lambda e, ps=ps, j=j, c=c: e.transpose(ps[:, j * 128:(j + 1) * 128], xn[:, c * 128:(c + 1) * 128], self.identf[:, :]),
                     reads=[xn, self.identf], writes=[ps])
            src = ps[:, :].rearrange("p (j n) -> p j n", j=4)
            p.op("act", lambda e, src=src, c0=c0: e.copy(out=self.xT[:, c0:c0 + 4, t * 128:(t + 1) * 128], in_=src), reads=[ps], writes=[self.xT.bs[t]])
            if xT32 is not None:
                p.op("dve", lambda e, src=src, c0=c0: e.tensor_copy(out=xT32[:, c0:c0 + 4, :], in_=src), reads=[ps], writes=[xT32])

    def phase_ln0(self):
        p = self.p
        st = ExitStack()
        self.load_ln_params(self.W["ln0_g"].t.rearrange("(o n) -> o n", o=1), self.W["ln0_b"].t.rearrange("(o n) -> o n", o=1), st)
        zs = [p.sb(f"l0z{i}", [128, D], F32, st) for i in range(2)]
        stats = p.sb("l0stats", [128, 2, 6], F32, st)
        mv = p.sb("l0mv", [128, 2], F32, st)
        rstd = p.sb("l0rstd", [128, 1], F32, st)
        for t in range(NT):
            z = zs[t % 2]
            p.dma("sp", z[:], self.x_in[t * 128:(t + 1) * 128, :], writes=[z])
            self.ln_tile(z, z, stats, mv, rstd)
            p.dma("pool", self.xres[t * 128:(t + 1) * 128, :], z[:], reads=[z], writes=[self.xres.bs[t]])
            self.to_xT(z, t)
        p.barrier()
        st.close()

    def wload(self, stg, dst_tl, dst_ap, src_ap, kc, ncols, cast="pool", wbuf=None):
        p = self.p
        cap = stg[0].t.shape[1]
        kcp = max(1, min(kc, cap // ncols))
        wb = [wbuf if wbuf is not None else dst_tl]
        for k0 in range(0, kc, kcp):
            st = stg[self.wl_i % len(stg)]
            self.wl_i += 1
            view = st[:, 0:kcp * ncols].rearrange("p (c n) -> p c n", c=kcp)
            p.dma("sp", view, src_ap[k0 * 128:(k0 + kcp) * 128, :].rearrange("(c p) n -> p c n", p=128), writes=[st])
            p.op(cast, lambda e, view=view, k0=k0: e.tensor_copy(out=dst_ap[:, k0:k0 + kcp, :], in_=view), reads=[st], writes=wb)

    wl_i = 0

    def phase_attn(self, l):
        p = self.p
        W = self.W
        st = ExitStack()
        stg = [p.sb(f"a_stg{i}", [128, 8 * 128], F32, st) for i in range(2)]
        wA = [p.sb(f"a_w{i}", [128, 8, 384], BF16, st) for i in range(2)]
        QT = [p.sb(f"a_qt{m}", [68, S], BF16, st) for m in range(2)]
        KTp = [p.sb(f"a_ktp{m}", [68, S], BF16, st) for m in range(2)]
        KTm = [p.sb(f"a_ktm{m}", [68, S], BF16, st) for m in range(2)]
        V = p.sb("a_v", [128, NT, 129], BF16, st)
        PT = [p.sb(f"a_pt{i}", [128, 512], BF16, st) for i in range(4)]
        oaT = [p.sb(f"a_oat{i}", [128, S], BF16, st) for i in range(1)]
        on = [p.sb(f"a_on{m}", [128, 4, 128], F32, st) for m in range(2)]
        rs = p.sb("a_rs", [128, 4], F32, st)
        diff = p.sb("a_diff", [128, 4, 128], F32, st)
        sq = p.sb("a_sq", [128, 4, 128], F32, st)
        ss = p.sb("a_ss", [128, 4], F32, st)
        rstd = p.sb("a_rstd", [128, 4], F32, st)
        oo = p.sb("a_oo", [128, 4, 128], F32, st)
        absd = p.sb("a_absd", [128, 128], F32, st)
        lamt = p.sb("a_lamt", [128, 256], F32, st)
        lsm = p.sb("a_lsm", [128, 8], F32, st)
        gA = p.sb("a_gA", [128, 128], F32, st)
        lam_init = 0.8 - 0.6 * math.exp(-0.3 * l)
        p.dma("sp", absd[:], self.C["absdiff"][:, :], writes=[absd])
        for m in range(2):
            p.dma("sp", QT[m][64:68, :], self.C["qaug"][:, :], writes=[QT[m]])
        p.dma("sp", lamt[:], W["da_lambda"][l:l + 1, :, :].rearrange("o a b -> o (a b)").partition_broadcast(128), writes=[lamt])
        p.dma("sp", gA[:], W["da_norm_g"][l:l + 1, :].partition_broadcast(128), writes=[gA])
        p.op("dve", lambda e: e.tensor_scalar(gA[:, :], gA[:, :], 1.0 - lam_init, None, ALU.mult), reads=[gA], writes=[gA])
        p.op("dve", lambda e: e.tensor_tensor(out=lamt[:, 0:64], in0=lamt[:, 0:64], in1=lamt[:, 64:128], op=ALU.mult), reads=[lamt], writes=[lamt])
        p.op("dve", lambda e: e.tensor_tensor(out=lamt[:, 128:192], in0=lamt[:, 128:192], in1=lamt[:, 192:256], op=ALU.mult), reads=[lamt], writes=[lamt])
        p.op("dve", lambda e: e.tensor_reduce(out=lsm[:, 0:1], in_=lamt[:, 0:64], axis=AX.X, op=ALU.add), reads=[lamt], writes=[lsm])
        p.op("dve", lambda e: e.tensor_reduce(out=lsm[:, 1:2], in_=lamt[:, 128:192], axis=AX.X, op=ALU.add), reads=[lamt], writes=[lsm])
        p.op("act", lambda e: e.activation(out=lsm[:, 2:4], in_=lsm[:, 0:2], func=AF.Exp), reads=[lsm], writes=[lsm])
        p.op("dve", lambda e: e.tensor_tensor(out=lsm[:, 4:5], in0=lsm[:, 3:4], in1=lsm[:, 2:3], op=ALU.subtract), reads=[lsm], writes=[lsm])
        p.op("dve", lambda e: e.tensor_scalar(lsm[:, 5:6], lsm[:, 4:5], -lam_init, None, ALU.add), reads=[lsm], writes=[lsm])
        p.op("dve", lambda e: e.memset(V[:, :, 128:129], 1.0), writes=[V])
        xT = self.xT
        xTr = list(xT.bs)

        def load_head_w(h):
            w = wA[h % 2]
            for k, nm in enumerate(("qa", "ka", "va")):
                c0 = COL[nm] + h * 128
                self.wload(stg, w, w[:, :, k * 128:(k + 1) * 128], W["w_in"][l, :, c0:c0 + 128], 8, 128)

        load_head_w(0)
        for h in range(self.dbg.get("heads", 4)):
            slope = 2.0 ** (-2.0 * (h + 1))
            w = wA[h % 2]
            if h + 1 < self.dbg.get("heads", 4):
                load_head_w(h + 1)
            for m in range(2):
                p.dma("sp", KTp[m][64:68, :], self.C["kaug"][h, 0, :, :], writes=[KTp[m]])
                p.dma("sp", KTm[m][64:68, :], self.C["kaug"][h, 1, :, :], writes=[KTm[m]])
            for m in range(2):
                for tb in range(8):
                    ps = self.bank(0, 4)
                    for c in range(8):
                        self.mm(ps[0:64, :], w[:, c, m * 64:(m + 1) * 64], xT[:, c, tb * 512:(tb + 1) * 512], c == 0, c == 7, [w] + xTr[tb * 4:tb * 4 + 4], [ps])
                    p.op("act", lambda e, ps=ps, m=m, tb=tb: e.mul(out=QT[m][0:64, tb * 512:(tb + 1) * 512], in_=ps[0:64, :], mul=0.125), reads=[ps], writes=[QT[m]])
                    ps = self.bank(0, 4)
                    for c in range(8):
                        self.mm(ps[0:64, :], w[:, c, 128 + m * 64:128 + (m + 1) * 64], xT[:, c, tb * 512:(tb + 1) * 512], c == 0, c == 7, [w] + xTr[tb * 4:tb * 4 + 4], [ps])
                    p.op("act", lambda e, ps=ps, m=m, tb=tb: e.copy(out=KTp[m][0:64, tb * 512:(tb + 1) * 512], in_=ps[0:64, :]), reads=[ps], writes=[KTp[m]])
                    p.op("dve", lambda e, ps=ps, m=m, tb=tb: e.tensor_copy(out=KTm[m][0:64, tb * 512:(tb + 1) * 512], in_=ps[0:64, :]), reads=[ps], writes=[KTm[m]])
            for t4 in range(8):
                ps = self.bank(0, 4)
                for j in range(4):
                    t = t4 * 4 + j
                    for c in range(8):
                        self.mm(ps[:, j * 128:(j + 1) * 128], xT[:, c, t * 128:(t + 1) * 128], w[:, c, 256:384], c == 0, c == 7, [w, xTr[t]], [ps])
                p.op("act", lambda e, ps=ps, t4=t4: e.copy(out=V[:, t4 * 4:(t4 + 1) * 4, 0:128], in_=ps[:, :].rearrange("p (j n) -> p j n", j=4)), reads=[ps], writes=[V])
            oa = oaT[0]
            pti = 0
            for Q in range(self.dbg.get("nQ", 8)):
                for m in range(2):
                    OB = (self.PS[4 + 2 * m], self.PS[5 + 2 * m])
                    for kt in range(NT):
                        ps = self.bank(0, 4)
                        rel = kt - 4 * Q
                        ksl = slice(kt * 128, (kt + 1) * 128)
                        if rel < 0:
                            self.mm(ps[:, :], KTm[m][0:68, ksl], QT[m][0:68, Q * 512:(Q + 1) * 512], True, True, [KTm[m], QT[m]], [ps])
                        elif rel > 3:
                            self.mm(ps[:, :], KTp[m][0:68, ksl], QT[m][0:68, Q * 512:(Q + 1) * 512], True, True, [KTp[m], QT[m]], [ps])
                        else:
                            q0 = Q * 512
                            first = True
                            if rel > 0:
                                self.mm(ps[:, 0:rel * 128], KTp[m][0:68, ksl], QT[m][0:68, q0:q0 + rel * 128], first, True, [KTp[m], QT[m]], [ps])
                                first = False
                            self.mm(ps[:, rel * 128:(rel + 1) * 128], KTp[m][0:64, ksl], QT[m][0:64, q0 + rel * 128:q0 + (rel + 1) * 128], first, True, [KTp[m], QT[m]], [ps])
                            if rel < 3:
                                self.mm(ps[:, (rel + 1) * 128:512], KTm[m][0:68, ksl], QT[m][0:68, q0 + (rel + 1) * 128:q0 + 512], False, True, [KTm[m], QT[m]], [ps])
                            p.op("dve", lambda e, ps=ps, rel=rel, slope=slope: e.scalar_tensor_tensor(
                                out=ps[:, rel * 128:(rel + 1) * 128], in0=absd[:, :], scalar=-slope, in1=ps[:, rel * 128:(rel + 1) * 128], op0=ALU.mult, op1=ALU.add),
                                reads=[absd, ps], writes=[ps])
                        pt = PT[pti % 4]
                        pti += 1
                        p.op("act", lambda e, ps=ps, pt=pt: e.activation(out=pt[:, :], in_=ps[:, :], func=AF.Exp), reads=[ps], writes=[pt])
                        for j in range(4):
                            ob = OB[j // 2]
                            oc = (j % 2) * 256
                            self.mm(ob[:, oc:oc + 129], pt[:, j * 128:(j + 1) * 128], V[:, kt, :], (kt == 0 and j % 2 == 0), kt == NT - 1, [pt, V], [ob])
                    for j in range(4):
                        ob = OB[j // 2]
                        oc = (j % 2) * 256
                        p.op("dve", lambda e, ob=ob, oc=oc, j=j: e.reciprocal(out=rs[:, j:j + 1], in_=ob[:, oc + 128:oc + 129]), reads=[ob], writes=[rs])
                        p.op("dve", lambda e, ob=ob, oc=oc, j=j, m=m: e.tensor_scalar(on[m][:, j, :], ob[:, oc:oc + 128], rs[:, j:j + 1], None, ALU.mult), reads=[ob, rs], writes=[on[m]])
                p.op("dve", lambda e: e.scalar_tensor_tensor(out=diff[:, :, :], in0=on[1][:, :, :], scalar=lsm[:, 5:6], in1=on[0][:, :, :], op0=ALU.mult, op1=ALU.add),
                     reads=[on[0], on[1], lsm], writes=[diff])
                p.op("pool", lambda e: e.tensor_tensor(out=sq[:, :, :], in0=diff[:, :, :], in1=diff[:, :, :], op=ALU.mult), reads=[diff], writes=[sq])
                p.op("dve", lambda e: e.tensor_reduce(out=ss[:, :], in_=sq[:, :, :], axis=AX.X, op=ALU.add), reads=[sq], writes=[ss])
                self.rsqrt(rstd, rstd[:, :], ss, ss[:, :], 1.0 / 128.0)
                for j in range(4):
                    p.op("dve", lambda e, j=j: e.scalar_tensor_tensor(out=oo[:, j, :], in0=diff[:, j, :], scalar=rstd[:, j:j + 1], in1=gA[:, :], op0=ALU.mult, op1=ALU.mult),
                         reads=[diff, rstd, gA], writes=[oo])
                ps = self.bank(0, 4)
                for j in range(4):
                    p.op("pe", lambda e, ps=ps, j=j: e.transpose(ps[:, j * 128:(j + 1) * 128], oo[:, j, :], self.identf[:, :]), reads=[oo, self.identf], writes=[ps])
                p.op("act", lambda e, ps=ps, Q=Q, oa=oa: e.copy(out=oa[:, Q * 512:(Q + 1) * 512], in_=ps[:, :]), reads=[ps], writes=[oa])
            p.dma("pool", self.oT[0, h, :, :], oa[:, :], reads=[oa], writes=[self.oT.bs[h]])
        p.barrier()
        st.close()

    def phase_merge1(self, l):
        for dh in range(2):
            self._merge1_dh(l, dh)

    def _merge1_dh(self, l, dh):
        p = self.p
        W = self.W
        xT = self.xT
        if True:
            st = ExitStack()
            stg = [p.sb(f"m_stg{i}", [128, 2048], F32, st) for i in range(2)]
            wg = p.sb("m_wg", [128, 8, 1536], BF16, st, nb=3)
            wup = p.sb("m_wup", [128, 12, 512], BF16, st, nb=3)
            mb = p.sb("m_mb", [128, 3, 512], F32, st)
            ot = [p.sb(f"m_ot{i}", [128, 12, 512], BF16, st) for i in range(2)]
            sg = [p.sb(f"m_sg{i}", [128, 512], F32, st) for i in range(2)]
            acc = [p.sb(f"m_acc{i}", [128, 512], F32, st) for i in range(2)]
            tmp = [p.sb(f"m_tmp{i}", [128, 512], F32, st) for i in range(2)]
            mtb = [p.sb(f"m_mtb{i}", [128, 4, 512], BF16, st) for i in range(2)]
            d0 = dh * 512
            for n in range(3):
                c0 = COL["gate"] + n * 1024 + d0
                self.wload(stg, wg, wg[:, :, n * 512:(n + 1) * 512], W["w_in"][l, :, c0:c0 + 512], 8, 512, wbuf=wg.bs[n])
                self.wload(stg, wup, wup[:, n * 4:(n + 1) * 4, :], W["merge_w_up"][l, n, :, d0:d0 + 512], 4, 512, wbuf=wup.bs[n])
            p.dma("sp", mb[:], W["merge_b"][l:l + 1, :, d0:d0 + 512].partition_broadcast(128), writes=[mb])
            k = 0
            for tb in range(8):
                o = ot[tb % 2]
                p.dma("sp", o[:], self.oT[:, :, :, tb * 512:(tb + 1) * 512].rearrange("n c p t -> p (n c) t"), reads=self.oT.bs, writes=[o])
                mt = mtb[tb % 2]
                for tt in range(4):
                    t = tb * 4 + tt
                    a = acc[k % 2]
                    for n in range(3):
                        g = sg[(k * 3 + n) % 2]
                        psg = self.bank(0, 3)
                        for c in range(8):
                            self.mm(psg[:, :], xT[:, c, t * 128:(t + 1) * 128], wg[:, c, n * 512:(n + 1) * 512], c == 0, c == 7, [xT.bs[t], wg.bs[n]], [psg])
                        p.op("dve", lambda e, g=g, psg=psg, n=n, mb=mb: e.tensor_tensor(out=g[:, :], in0=psg[:, :], in1=mb[:, n, :], op=ALU.add), reads=[psg, mb], writes=[g])
                        p.op("act", lambda e, g=g: e.activation(out=g[:, :], in_=g[:, :], func=AF.Sigmoid), reads=[g], writes=[g])
                        psu = self.bank(3, 6)
                        for c in range(4):
                            self.mm(psu[:, :], o[:, n * 4 + c, tt * 128:(tt + 1) * 128], wup[:, n * 4 + c, :], c == 0, c == 3, [o, wup.bs[n]], [psu])
                        if n == 0:
                            p.op("dve", lambda e, a=a, g=g, psu=psu: e.tensor_tensor(out=a[:, :], in0=g[:, :], in1=psu[:, :], op=ALU.mult), reads=[g, psu], writes=[a])
                        else:
                            tm = tmp[n % 2]
                            p.op("dve", lambda e, tm=tm, g=g, psu=psu: e.tensor_tensor(out=tm[:, :], in0=g[:, :], in1=psu[:, :], op=ALU.mult), reads=[g, psu], writes=[tm])
                            p.op("pool", lambda e, tm=tm, a=a: e.tensor_tensor(out=a[:, :], in0=a[:, :], in1=tm[:, :], op=ALU.add), reads=[a, tm], writes=[a])
                    pst = self.bank(6, 8)
                    for j in range(4):
                        p.op("pe", lambda e, pst=pst, j=j, a=a: e.transpose(pst[:, j * 128:(j + 1) * 128], a[:, j * 128:(j + 1) * 128], self.identf[:, :]), reads=[a, self.identf], writes=[pst])
                    p.op("act", lambda e, pst=pst, mt=mt, tt=tt: e.copy(out=mt[:, :, tt * 128:(tt + 1) * 128], in_=pst[:, :].rearrange("p (j n) -> p j n", j=4)), reads=[pst], writes=[mt])
                    k += 1
                p.dma("pool", self.mT[:, dh * 4:(dh + 1) * 4, tb * 512:(tb + 1) * 512], mt[:, :, :], reads=[mt], writes=[self.mT.bs[dh * 8 + tb]])
            p.barrier()
            st.close()

    def phase_merge2(self, l):
        p = self.p
        W = self.W
        st = ExitStack()
        stg = [p.sb(f"n_stg{i}", [128, 2048], F32, st) for i in range(2)]
        wo = p.sb("n_wo", [128, 8, 1024], BF16, st)
        rw = p.sb("n_rw", [128, 8, 16], F32, st)
        rb = p.sb("n_rb", [128, 16], F32, st)
        mtl = [p.sb(f"n_mt{i}", [128, 8, 512], BF16, st) for i in range(2)]
        xr = [p.sb(f"n_xr{i}", [128, D], F32, st) for i in range(2)]
        z = [p.sb(f"n_z{i}", [128, D], F32, st) for i in range(2)]
        xT32 = p.sb("n_xT32", [128, 8, 128], F32, st)
        stats = p.sb("n_stats", [128, 2, 6], F32, st)
        mv = p.sb("n_mv", [128, 2], F32, st)
        rstd = p.sb("n_rstd", [128, 1], F32, st)
        R = {k: p.sb("n_r_" + k, shp, F32, st) for k, shp in dict(sc=[128, 16], bi=[128, 16], m1=[128, 4], eq=[128, 16], t2=[128, 16], m2=[128, 4],
                                                                  gs=[128, 4], gm=[128, 1], gsel=[128, 4], ge=[128, 16], w=[128, 16], ws=[128, 1]).items()}
        for h2 in range(2):
            self.wload(stg, wo, wo[:, :, h2 * 512:(h2 + 1) * 512], W["w_out"][l, :, h2 * 512:(h2 + 1) * 512], 8, 512)
        p.dma("sp", rw[:], W["router_w"].t.rearrange("(c p) n -> p c n", p=128), writes=[rw])
        p.dma("sp", rb[:], W["router_bias"].t.rearrange("(o n) -> o n", o=1).partition_broadcast(128), writes=[rb])
        self.load_ln_params(W["ln1_g"][l:l + 1, :], W["ln1_b"][l:l + 1, :], st)
        for tb in range(8):
            mt = mtl[tb % 2]
            p.dma("sp", mt[:], self.mT[:, :, tb * 512:(tb + 1) * 512], reads=[self.mT.bs[tb], self.mT.bs[8 + tb]], writes=[mt])
            for tt in range(4):
                t = tb * 4 + tt
                x_ = xr[t % 2]
                z_ = z[t % 2]
                p.dma("sp", x_[:], self.xres[t * 128:(t + 1) * 128, :], reads=[self.xres.bs[t]], writes=[x_])
                for h2 in range(2):
                    ps = self.bank(0, 4)
                    for c in range(8):
                        self.mm(ps[:, :], mt[:, c, tt * 128:(tt + 1) * 128], wo[:, c, h2 * 512:(h2 + 1) * 512], c == 0, c == 7, [mt, wo], [ps])
                    p.op("dve", lambda e, ps=ps, x_=x_, z_=z_, h2=h2: e.scalar_tensor_tensor(out=z_[:, h2 * 512:(h2 + 1) * 512], in0=x_[:, h2 * 512:(h2 + 1) * 512], scalar=ALPHA,
                                                                                    in1=ps[:, :], op0=ALU.mult, op1=ALU.add), reads=[ps, x_], writes=[z_])
                self.ln_tile(z_, z_, stats, mv, rstd)
                p.dma("pool", self.xres[t * 128:(t + 1) * 128, :], z_[:], reads=[z_], writes=[self.xres.bs[t]])
                self.to_xT(z_, t, xT32=xT32)
                self.router(t, xT32, rw, rb, R)
        p.barrier()
        st.close()

    def router(self, t, xT32, rw, rb, R):
        p = self.p
        ps = self.bank(4, 8)
        for c in range(8):
            self.mm(ps[:, 0:16], xT32[:, c, :], rw[:, c, :], c == 0, c == 7, [xT32, rw], [ps])
        sc, bi, m1, eq, t2, m2, gs, gm, gsel, ge, w, ws = (R[k] for k in ("sc", "bi", "m1", "eq", "t2", "m2", "gs", "gm", "gsel", "ge", "w", "ws"))
        v3 = lambda tl: tl[:, :].rearrange("p (g e) -> p g e", g=4)
        b3 = lambda tl: tl[:, :].unsqueeze(2).to_broadcast([128, 4, 4])
        p.op("act", lambda e: e.activation(out=sc[:, :], in_=ps[:, 0:16], func=AF.Sigmoid), reads=[ps], writes=[sc])
        p.op("dve", lambda e: e.tensor_tensor(out=bi[:, :], in0=sc[:, :], in1=rb[:, :], op=ALU.add), reads=[sc, rb], writes=[bi])
        p.op("dve", lambda e: e.tensor_reduce(out=m1[:, :], in_=v3(bi), axis=AX.X, op=ALU.max), reads=[bi], writes=[m1])
        p.op("dve", lambda e: e.tensor_tensor(out=v3(eq), in0=v3(bi), in1=b3(m1), op=ALU.is_equal), reads=[bi, m1], writes=[eq])
        p.op("dve", lambda e: e.scalar_tensor_tensor(out=t2[:, :], in0=eq[:, :], scalar=-1e30, in1=bi[:, :], op0=ALU.mult, op1=ALU.add), reads=[eq, bi], writes=[t2])
        p.op("dve", lambda e: e.tensor_reduce(out=m2[:, :], in_=v3(t2), axis=AX.X, op=ALU.max), reads=[t2], writes=[m2])
        p.op("dve", lambda e: e.tensor_tensor(out=gs[:, :], in0=m1[:, :], in1=m2[:, :], op=ALU.add), reads=[m1, m2], writes=[gs])
        p.op("dve", lambda e: e.tensor_reduce(out=gm[:, :], in_=gs[:, :], axis=AX.X, op=ALU.max), reads=[gs], writes=[gm])
        p.op("dve", lambda e: e.tensor_scalar(gsel[:, :], gs[:, :], gm[:, 0:1], None, ALU.is_equal), reads=[gs, gm], writes=[gsel])
        p.op("dve", lambda e: e.tensor_tensor(out=v3(ge), in0=v3(bi), in1=b3(m2), op=ALU.is_ge), reads=[bi, m2], writes=[ge])
        p.op("dve", lambda e: e.tensor_tensor(out=v3(ge), in0=v3(ge), in1=b3(gsel), op=ALU.mult), reads=[ge, gsel], writes=[ge])
        p.op("dve", lambda e: e.tensor_tensor(out=w[:, :], in0=ge[:, :], in1=sc[:, :], op=ALU.mult), reads=[ge, sc], writes=[w])
        p.op("dve", lambda e: e.tensor_reduce(out=ws[:, :], in_=w[:, :], axis=AX.X, op=ALU.add), reads=[w], writes=[ws])
        p.op("dve", lambda e: e.reciprocal(out=ws[:, :], in_=ws[:, :]), reads=[ws], writes=[ws])
        p.op("dve", lambda e: e.tensor_scalar(self.call[:, t, :], w[:, :], ws[:, 0:1], None, ALU.mult), reads=[w, ws], writes=[self.call.bs[t]])

    def phase_moe(self, l, last):
        p = self.p
        W = self.W
        xT = self.xT
        st = ExitStack()
        stg = [p.sb(f"e_stg{i}", [128, 2048], F32, st) for i in range(2)]
        wg = [p.sb(f"e_wg{i}", [128, 8, 512], BF16, st) for i in range(2)]
        wu = [p.sb(f"e_wu{i}", [128, 8, 512], BF16, st) for i in range(2)]
        wd = [p.sb(f"e_wd{i}", [128, 4, 1024], BF16, st) for i in range(2)]
        yacc = p.sb("e_yacc", [128, 8, D], F32, st, nb=8)
        hT = [p.sb(f"e_hT{i}", [128, 4, 512], BF16, st) for i in range(2)]
        sgl = [p.sb(f"e_sg{i}", [128, 512], F32, st) for i in range(2)]
        xr = [p.sb(f"e_xr{i}", [128, D], F32, st) for i in range(1)]
        stats = p.sb("e_stats", [128, 2, 6], F32, st)
        mv = p.sb("e_mv", [128, 2], F32, st)
        rstd = p.sb("e_rstd", [128, 1], F32, st)
        self.load_ln_params(W["ln2_g"][l:l + 1, :], W["ln2_b"][l:l + 1, :], st)
        cast_i = 0
        k = 0
        for q4 in range(4):
            for ex in range(16):
                g_, u_, d_ = wg[ex % 2], wu[ex % 2], wd[ex % 2]
                for h2 in range(2):
                    self.wload(stg, g_, g_[:, h2 * 4:(h2 + 1) * 4, :], W["moe_w_gate"][l, ex, h2 * 512:(h2 + 1) * 512, :], 4, 512, cast=("pool", "dve")[h2])
                    self.wload(stg, u_, u_[:, h2 * 4:(h2 + 1) * 4, :], W["moe_w_up"][l, ex, h2 * 512:(h2 + 1) * 512, :], 4, 512, cast=("pool", "dve")[h2])
                    self.wload(stg, d_, d_[:, :, h2 * 512:(h2 + 1) * 512], W["moe_w_down"][l, ex, :, h2 * 512:(h2 + 1) * 512], 4, 512, cast=("pool", "dve")[h2])
                for tb2 in range(2):
                    tb = q4 * 2 + tb2
                    h_ = hT[k % 2]
                    k += 1
                    xr_ = [xT.bs[tb * 4 + i] for i in range(4)]
                    for fc in range(4):
                        pg = self.bank(0, 2)
                        for c in range(8):
                            self.mm(pg[:, :], g_[:, c, fc * 128:(fc + 1) * 128], xT[:, c, tb * 512:(tb + 1) * 512], c == 0, c == 7, [g_] + xr_, [pg])
                        pu = self.bank(2, 4)
                        for c in range(8):
                            self.mm(pu[:, :], u_[:, c, fc * 128:(fc + 1) * 128], xT[:, c, tb * 512:(tb + 1) * 512], c == 0, c == 7, [u_] + xr_, [pu])
                        s_ = sgl[fc % 2]
                        p.op("act", lambda e, s_=s_, pg=pg: e.activation(out=s_[:, :], in_=pg[:, :], func=AF.Silu), reads=[pg], writes=[s_])
                        p.op("dve", lambda e, s_=s_, pu=pu, h_=h_, fc=fc: e.tensor_tensor(out=h_[:, fc, :], in0=s_[:, :], in1=pu[:, :], op=ALU.mult), reads=[s_, pu], writes=[h_])
                    for tt in range(4):
                        t = tb * 4 + tt
                        tl = tb2 * 4 + tt
                        for h2 in range(2):
                            py = self.bank(4, 8)
                            for fc in range(4):
                                self.mm(py[:, :], h_[:, fc, tt * 128:(tt + 1) * 128], d_[:, fc, h2 * 512:(h2 + 1) * 512], fc == 0, fc == 3, [h_, d_], [py])
                            ya = yacc[:, tl, h2 * 512:(h2 + 1) * 512]
                            if ex == 0:
                                p.op("dve", lambda e, ya=ya, py=py, t=t, ex=ex: e.tensor_scalar(ya, py[:, :], self.call[:, t, ex:ex + 1], None, ALU.mult),
                                     reads=[py, self.call.bs[t]], writes=[yacc.bs[tl]])
                            else:
                                p.op("dve", lambda e, ya=ya, py=py, t=t, ex=ex: e.scalar_tensor_tensor(out=ya, in0=py[:, :], scalar=self.call[:, t, ex:ex + 1], in1=ya, op0=ALU.mult, op1=ALU.add),
                                     reads=[py, self.call.bs[t]], writes=[yacc.bs[tl]])
            for tl in range(8):
                t = q4 * 8 + tl
                x_ = xr[0]
                yv = Sub(yacc[:, tl, :], yacc.bs[tl])
                p.dma("sp", x_[:], self.xres[t * 128:(t + 1) * 128, :], reads=[self.xres.bs[t]], writes=[x_])
                p.op("dve", lambda e, x_=x_, tl=tl: e.scalar_tensor_tensor(out=yacc[:, tl, :], in0=x_[:, :], scalar=ALPHA, in1=yacc[:, tl, :], op0=ALU.mult, op1=ALU.add),
                     reads=[x_, yacc.bs[tl]], writes=[yacc.bs[tl]])
                self.ln_tile(yv, yv, stats, mv, rstd)
                if last:
                    p.dma("pool", self.out[t * 128:(t + 1) * 128, :], yv[:, :], reads=[yv], writes=[self.out.bs[t]])
                else:
                    p.dma("pool", self.xres[t * 128:(t + 1) * 128, :], yv[:, :], reads=[yv], writes=[self.xres.bs[t]])
                    self.to_xT(yv, t)
        p.barrier()
        st.close()

    def phase_gla(self, l):
        for hp in range(2):
            self._gla_hp(l, hp)

    def _gla_hp(self, l, hp):
        p = self.p
        W = self.W
        xT = self.xT
        xTr = list(xT.bs)
        if True:
            st = ExitStack()
            stg = [p.sb(f"g_stg{i}", [128, 1024], F32, st) for i in range(2)]
            wq = p.sb("g_wq", [128, 8, 128], BF16, st)
            wk = p.sb("g_wk", [128, 8, 128], BF16, st)
            wv = p.sb("g_wv", [128, 8, 256], BF16, st)
            wr = p.sb("g_wr", [128, 8, 256], BF16, st)
            wz = p.sb("g_wz", [128, 8, 32], BF16, st)
            wgf = p.sb("g_wgf", [16, 2, 128], F32, st)
            wgt = p.sb("g_wgt", [16, 2, 128], BF16, st)
            bg = p.sb("g_bg", [128, 2], F32, st)
            gG = p.sb("g_gG", [128, 128], F32, st)
            ones = p.sb("g_ones", [128, 1], F32, st)
            m4 = p.sb("g_m4", [128, 4, 64], F32, st)
            msk = p.sb("g_msk", [128, 8, 64], F32, st)
            qA1 = p.sb("g_qA1", [128, S], BF16, st)
            kA1 = p.sb("g_kA1", [128, S], BF16, st)
            qi1 = p.sb("g_qi1", [128, S], BF16, st)
            qA0 = [p.sb(f"g_qA0{i}", [128, 512], BF16, st) for i in range(2)]
            kA0 = [p.sb(f"g_kA0{i}", [128, 512], BF16, st) for i in range(2)]
            qi0 = [p.sb(f"g_qi0{i}", [128, 512], BF16, st) for i in range(2)]
            dec = [p.sb(f"g_dec{d}", [128, 64], F32, st) for d in range(2)]
            sts1 = p.sb("g_st1", [128, 64, 128], BF16, st)
            sts0 = [p.sb(f"g_st0{i}", [128, 8, 128], BF16, st) for i in range(2)]
            S32 = [p.sb(f"g_S32{d}", [128, 128], F32, st) for d in range(2)]
            v = p.sb("g_v", [128, NT, 256], BF16, st)
            zt = p.sb("g_zt", [16, 512], BF16, st)
            T1 = p.sb("g_T1", [128, 512], F32, st)
            T2 = p.sb("g_T2", [128, 512], F32, st)
            T3 = p.sb("g_T3", [128, 512], F32, st)
            T4 = p.sb("g_T4", [128, 512], F32, st)
            T5 = p.sb("g_T5", [128, 512], F32, st)
            klt = p.sb("g_klt", [128, 4, 128], BF16, st)
            scT = [p.sb(f"g_scT{i}", [128, 4, 64], BF16, st) for i in range(2)]
            sr = p.sb("g_sr", [128, 256], F32, st)
            sq = p.sb("g_sq", [128, 2, 128], F32, st)
            ssq = p.sb("g_ssq", [128, 2], F32, st)
            oc = p.sb("g_oc", [128, 256], F32, st)
            ocT = [p.sb(f"g_ocT{i}", [128, 2, 512], BF16, st) for i in range(2)]
            c0 = COL["qc"] + hp * 128
            self.wload(stg, wq, wq[:, :, :], W["w_in"][l, :, c0:c0 + 128], 8, 128)
            p.op("pool", lambda e: e.tensor_scalar(wq[:, :, :], wq[:, :, :], 0.125, None, ALU.mult), reads=[wq], writes=[wq])
            c0 = COL["kc"] + hp * 128
            self.wload(stg, wk, wk[:, :, :], W["w_in"][l, :, c0:c0 + 128], 8, 128)
            for k2 in range(2):
                c0 = COL["vc"] + hp * 256 + k2 * 128
                self.wload(stg, wv, wv[:, :, k2 * 128:(k2 + 1) * 128], W["w_in"][l, :, c0:c0 + 128], 8, 128)
                c0 = COL["rc"] + hp * 256 + k2 * 128
                self.wload(stg, wr, wr[:, :, k2 * 128:(k2 + 1) * 128], W["w_in"][l, :, c0:c0 + 128], 8, 128)
            self.wload(stg, wz, wz[:, :, :], W["w_in"][l, :, COL["zf"]:COL["zf"] + 32], 8, 32)
            p.dma("sp", wgf[:], W["gla_w_gate"][l, :, :, hp * 128:(hp + 1) * 128].rearrange("d r n -> r d n"), writes=[wgf])
            p.op("dve", lambda e: e.tensor_copy(out=wgt[:, :, :], in_=wgf[:, :, :]), reads=[wgf], writes=[wgt])
            p.dma("sp", bg[:], W["gla_b_gate"][l, :, hp * 128:(hp + 1) * 128].rearrange("d n -> n d"), writes=[bg], allow_slow_non_contiguous=True)
            p.dma("sp", gG[:], W["gla_norm_g"][l:l + 1, :].partition_broadcast(128), writes=[gG])
            p.op("pool", lambda e: e.memset(ones[:, :], 1.0), writes=[ones])
            for d in range(2):
                for hh in range(2):
                    for half in range(2):
                        p.dma("sp", m4[half * 64:(half + 1) * 64, d * 2 + hh, :], self.C["gmask"][d, :, :], writes=[m4])
            p.op("pool", lambda e: e.memset(msk[:, :, :], 1.0), writes=[msk])
            p.op("pool", lambda e: e.memset(msk[:, :, 0:1], 0.0), reads=[msk], writes=[msk])
            for t in range(NT):
                ps = self.bank(0, 4)
                for c in range(8):
                    self.mm(ps[:, 0:256], xT[:, c, t * 128:(t + 1) * 128], wv[:, c, :], c == 0, c == 7, [wv, xTr[t]], [ps])
                p.op("act", lambda e, ps=ps, t=t: e.copy(out=v[:, t, :], in_=ps[:, 0:256]), reads=[ps], writes=[v])
            def arr(d, tb):
                if d == 1:
                    sl = slice(tb * 512, (tb + 1) * 512)
                    return (qA1, qA1[:, sl]), (kA1, kA1[:, sl]), (qi1, qi1[:, sl])
                i = tb % 2
                return (qA0[i], qA0[i][:, :]), (kA0[i], kA0[i][:, :]), (qi0[i], qi0[i][:, :])

            def chunk_view(d, which, n, hh):
                hs = slice(hh * 64, (hh + 1) * 64)
                if d == 1:
                    tl = (qA1, kA1, qi1)[which]
                    return tl, tl[hs, n * 64:(n + 1) * 64]
                tl = (qA0, kA0, qi0)[which][(n // 8) % 2]
                return tl, tl[hs, (n % 8) * 64:(n % 8 + 1) * 64]

            def state_view(d, n, hh):
                hs = slice(hh * 64, (hh + 1) * 64)
                if d == 1:
                    return sts1, sts1[hs, n, :]
                tl = sts0[(n // 8) % 2]
                return tl, tl[hs, n % 8, :]

            def sweep_block(d, tb):
                xr_ = xTr[tb * 4:tb * 4 + 4]
                tsl = slice(tb * 512, (tb + 1) * 512)
                (qA_t, qA_ap), (kA_t, kA_ap), (qi_t, qi_ap) = arr(d, tb)
                pz = self.bank(0, 4)
                for c in range(8):
                    self.mm(pz[0:16, :], wz[:, c, d * 16:(d + 1) * 16], xT[:, c, tsl], c == 0, c == 7, [wz] + xr_, [pz])
                p.op("act", lambda e: e.copy(out=zt[:, :], in_=pz[0:16, :]), reads=[pz], writes=[zt])
                pg = self.bank(0, 4)
                self.mm(pg[:, :], wgt[0:16, d, :], zt[0:16, :], True, True, [wgt, zt], [pg])
                pq = self.bank(4, 6)
                for c in range(8):
                    self.mm(pq[:, :], wq[:, c, :], xT[:, c, tsl], c == 0, c == 7, [wq] + xr_, [pq])
                pk = self.bank(6, 8)
                for c in range(8):
                    self.mm(pk[:, :], wk[:, c, :], xT[:, c, tsl], c == 0, c == 7, [wk] + xr_, [pk])
                p.op("dve", lambda e: e.tensor_scalar(T1[:, :], pg[:, :], bg[:, d:d + 1], None, ALU.add), reads=[pg, bg], writes=[T1])
                p.op("dve", lambda e: e.scalar_tensor_tensor(out=T2[:, :], in0=T1[:, :], scalar=-1.0, in1=T1[:, :], op0=ALU.mult, op1=ALU.max), reads=[T1], writes=[T2])
                p.op("act", lambda e: e.activation(out=T2[:, :], in_=T2[:, :], func=AF.Exp, scale=-1.0), reads=[T2], writes=[T2])
                p.op("act", lambda e: e.activation(out=T2[:, :], in_=T2[:, :], func=AF.Ln, bias=ones[:, 0:1]), reads=[T2, ones], writes=[T2])
                p.op("dve", lambda e: e.scalar_tensor_tensor(out=T1[:, :], in0=T1[:, :], scalar=0.0, in1=T2[:, :], op0=ALU.min, op1=ALU.subtract), reads=[T1, T2], writes=[T1])
                p.op("pool", lambda e: e.tensor_scalar(T1[:, :], T1[:, :], 1.0 / 16.0, None, ALU.mult), reads=[T1], writes=[T1])
                p.op("dve", lambda e: e.tensor_tensor_scan(T3[:, :], msk[:, :, :].rearrange("p a b -> p (a b)"), T1[:, :], 0.0, ALU.mult, ALU.add), reads=[msk, T1], writes=[T3])
                c3 = T3[:, :].rearrange("p (a b) -> p a b", a=8)
                v4 = lambda tl: tl[:, :].rearrange("p (a b) -> p a b", a=8)
                if d == 1:
                    p.op("dve", lambda e: e.tensor_tensor(out=v4(T2), in0=c3[:, :, 63:64].to_broadcast([128, 8, 64]), in1=c3, op=ALU.subtract), reads=[T3], writes=[T2])
                    p.op("dve", lambda e: e.tensor_tensor(out=T3[:, :], in0=T2[:, :], in1=T1[:, :], op=ALU.add), reads=[T2, T1], writes=[T3])
                    ref, last = c3[:, :, 31:32], c3[:, :, 0:1]
                else:
                    ref, last = c3[:, :, 32:33], c3[:, :, 63:64]
                refb = ref.to_broadcast([128, 8, 64])
                lastb = last.to_broadcast([128, 8, 64])
                p.op("act", lambda e: e.activation(out=dec[d][:, tb * 8:(tb + 1) * 8].unsqueeze(2), in_=last, func=AF.Exp), reads=[T3], writes=[dec[d]])
                p.op("dve", lambda e: e.tensor_tensor(out=v4(T4), in0=c3, in1=refb, op=ALU.subtract), reads=[T3], writes=[T4])
                p.op("act", lambda e: e.activation(out=T5[:, :], in_=T4[:, :], func=AF.Exp), reads=[T4], writes=[T5])
                p.op("dve", lambda e: e.tensor_tensor(out=qA_ap, in0=pq[:, :], in1=T5[:, :], op=ALU.mult), reads=[pq, T5], writes=[qA_t])
                p.op("act", lambda e: e.activation(out=T5[:, :], in_=T4[:, :], func=AF.Exp, scale=-1.0), reads=[T4], writes=[T5])
                p.op("dve", lambda e: e.tensor_tensor(out=kA_ap, in0=pk[:, :], in1=T5[:, :], op=ALU.mult), reads=[pk, T5], writes=[kA_t])
                p.op("act", lambda e: e.activation(out=T5[:, :], in_=T3[:, :], func=AF.Exp), reads=[T3], writes=[T5])
                p.op("dve", lambda e: e.tensor_tensor(out=qi_ap, in0=pq[:, :], in1=T5[:, :], op=ALU.mult), reads=[pq, T5], writes=[qi_t])
                p.op("dve", lambda e: e.tensor_tensor(out=v4(T4), in0=lastb, in1=c3, op=ALU.subtract), reads=[T3], writes=[T4])
                p.op("act", lambda e: e.activation(out=T5[:, :], in_=T4[:, :], func=AF.Exp), reads=[T4], writes=[T5])
                p.op("dve", lambda e: e.tensor_tensor(out=T4[:, :], in0=pk[:, :], in1=T5[:, :], op=ALU.mult), reads=[pk, T5], writes=[T4])
                pt = self.bank(0, 4)
                for j in range(4):
                    p.op("pe", lambda e, j=j: e.transpose(pt[:, j * 128:(j + 1) * 128], T4[:, j * 128:(j + 1) * 128], self.identf[:, :]), reads=[T4, self.identf], writes=[pt])
                p.op("act", lambda e: e.copy(out=klt[:, :, :], in_=pt[:, :].rearrange("p (j n) -> p j n", j=4)), reads=[pt], writes=[klt])
                cs = range(8) if d == 0 else range(7, -1, -1)
                for ci in cs:
                    n = tb * 8 + ci
                    tt, half = ci // 2, ci % 2
                    t = tb * 4 + tt
                    stl, _ = state_view(d, n, 0)
                    sap = sts1[:, n, :] if d == 1 else stl[:, n % 8, :]
                    p.op("act", lambda e, sap=sap: e.copy(out=sap, in_=S32[d][:, :]), reads=[S32[d]], writes=[stl])
                    pd = self.bank(0, 4)
                    for hh in range(2):
                        self.mm(pd[hh * 64:(hh + 1) * 64, 0:128], klt[half * 64:(half + 1) * 64, tt, hh * 64:(hh + 1) * 64],
                                v[half * 64:(half + 1) * 64, t, hh * 128:(hh + 1) * 128], True, True, [klt, v], [pd], serial=True)
                    p.op("dve", lambda e, pd=pd, n=n: e.scalar_tensor_tensor(out=S32[d][:, :], in0=S32[d][:, :], scalar=dec[d][:, n:n + 1], in1=pd[:, 0:128], op0=ALU.mult, op1=ALU.add),
                         reads=[pd, dec[d], S32[d]], writes=[S32[d]])

            def out_block(tb):
                ot_ = ocT[tb % 2]
                for tt in range(4):
                    t = tb * 4 + tt
                    pss = self.bank(0, 2)
                    for half in range(2):
                        n = t * 2 + half
                        first = True
                        for d in range(2):
                            for hh in range(2):
                                kt_, kap = chunk_view(d, 1, n, hh)
                                qt_, qap = chunk_view(d, 0, n, hh)
                                self.mm(pss[half * 64:(half + 1) * 64, (d * 2 + hh) * 64:(d * 2 + hh + 1) * 64], kap, qap, first, True, [kt_, qt_], [pss], serial=True)
                                first = False
                    sc_ = scT[t % 2]
                    p.op("dve", lambda e, pss=pss, sc_=sc_: e.tensor_tensor(out=sc_[:, :, :], in0=pss[:, 0:256].rearrange("p (a b) -> p a b", a=4), in1=m4[:, :, :], op=ALU.mult), reads=[pss, m4], writes=[sc_])
                    po = self.bank(2, 4)
                    for half in range(2):
                        n = t * 2 + half
                        hs = slice(half * 64, (half + 1) * 64)
                        first = True
                        for hh in range(2):
                            osl = po[hs, hh * 128:(hh + 1) * 128]
                            for d in range(2):
                                self.mm(osl, sc_[hs, d * 2 + hh, :], v[hs, t, hh * 128:(hh + 1) * 128], first, False, [sc_, v], [po], serial=True)
                                first = False
                            for d in range(2):
                                it_, iap = chunk_view(d, 2, n, hh)
                                st_, sap = state_view(d, n, hh)
                                self.mm(osl, iap, sap, False, d == 1, [it_, st_], [po], serial=True)
                    pr = self.bank(4, 8)
                    for c in range(8):
                        self.mm(pr[:, 0:256], xT[:, c, t * 128:(t + 1) * 128], wr[:, c, :], c == 0, c == 7, [wr, xTr[t]], [pr])
                    p.op("act", lambda e, pr=pr: e.activation(out=sr[:, :], in_=pr[:, 0:256], func=AF.Silu), reads=[pr], writes=[sr])
                    po3 = po[:, 0:256].rearrange("p (a b) -> p a b", a=2)
                    p.op("act", lambda e, po3=po3: e.activation(out=sq[:, :, :], in_=po3, func=AF.Square), reads=[po], writes=[sq])
                    p.op("dve", lambda e: e.tensor_reduce(out=ssq[:, :], in_=sq[:, :, :], axis=AX.X, op=ALU.add), reads=[sq], writes=[ssq])
                    self.rsqrt(ssq, ssq[:, :], ssq, ssq[:, :], 1.0 / 128.0)
                    for hh in range(2):
                        p.op("dve", lambda e, po=po, hh=hh: e.scalar_tensor_tensor(out=oc[:, hh * 128:(hh + 1) * 128], in0=po[:, hh * 128:(hh + 1) * 128], scalar=ssq[:, hh:hh + 1], in1=gG[:, :],
                                                                                  op0=ALU.mult, op1=ALU.mult), reads=[po, ssq, gG], writes=[oc])
                    p.op("pool", lambda e: e.tensor_tensor(out=oc[:, :], in0=oc[:, :], in1=sr[:, :], op=ALU.mult), reads=[oc, sr], writes=[oc])
                    pt = self.bank(4, 8)
                    for hh in range(2):
                        p.op("pe", lambda e, pt=pt, hh=hh: e.transpose(pt[:, hh * 128:(hh + 1) * 128], oc[:, hh * 128:(hh + 1) * 128], self.identf[:, :]), reads=[oc, self.identf], writes=[pt])
                    p.op("act", lambda e, pt=pt, tt=tt: e.copy(out=ot_[:, :, tt * 128:(tt + 1) * 128], in_=pt[:, 0:256].rearrange("p (a b) -> p a b", a=2)), reads=[pt], writes=[ot_])
                p.dma("pool", self.oT[2, hp * 2:(hp + 1) * 2, :, tb * 512:(tb + 1) * 512].rearrange("c p t -> p c t"), ot_[:, :, :], reads=[ot_], writes=[self.oT.bs[8 + hp * 2], self.oT.bs[8 + hp * 2 + 1]])

            for d in (1, 0):
                p.op("pool", lambda e, d=d: e.memset(S32[d][:, :], 0.0), writes=[S32[d]])
            for tb in range(7, -1, -1):
                sweep_block(1, tb)
            for tb in range(8):
                sweep_block(0, tb)
                out_block(tb)
            p.barrier()
            st.close()

    def phase_s5(self, l):
        p = self.p
        W = self.W
        xT = self.xT
        xTr = list(xT.bs)
        st = ExitStack()
        sm = lambda n, shp=(128, 32): p.sb("s_" + n, list(shp), F32, st)
        stg = [p.sb(f"s_stg{i}", [128, 1024], F32, st) for i in range(2)]
        identb = p.sb("s_identb", [128, 128], BF16, st)
        jx = sm("jx", (128, 128))
        rowm = sm("rowm", (128, 8))
        sgn = sm("sgn", (128, 2))
        hpi = sm("hpi", (128, 1))
        wu = p.sb("s_wu", [128, 8, 128], BF16, st)
        wglu = p.sb("s_wglu", [128, 4, 512], BF16, st)
        bglu = sm("bglu", (128, 4))
        dcol = sm("dcol", (128, 4))
        uT = p.sb("s_uT", [128, S], BF16, st)
        X = [p.sb(f"s_X{i}", [128, S], BF16, st, nb=8) for i in range(2)]
        yacc = p.sb("s_yacc", [128, S], F32, st, nb=8)
        ygT = p.sb("s_ygT", [128, 4, S], BF16, st)
        Mk = [p.sb(f"s_Mk{i}", [128, 128], BF16, st) for i in range(4)]
        Mt = [p.sb(f"s_Mt{i}", [128, 128], F32, st) for i in range(2)]
        Mt2 = [p.sb(f"s_Mt2{i}", [128, 128], F32, st) for i in range(2)]
        LB = [p.sb(f"s_LB{i}", [128, 128], BF16, st) for i in range(2)]
        LC = [p.sb(f"s_LC{i}", [128, 128], BF16, st) for i in range(2)]
        yt = [p.sb(f"s_yt{i}", [128, 512], F32, st) for i in range(2)]
        ob = [p.sb(f"s_ob{i}", [128, 512], BF16, st) for i in range(2)]
        p.dma("sp", jx[:], self.C["jx"][:, :], writes=[jx])
        p.dma("sp", rowm[:], self.C["rowmask"][:, :], writes=[rowm])
        p.dma("sp", sgn[:], self.C["sgn"][:, :], writes=[sgn])
        p.op("dve", lambda e: e.tensor_copy(out=identb[:, :], in_=self.identf[:, :]), reads=[self.identf], writes=[identb])
        p.op("pool", lambda e: e.memset(hpi[:, :], PI / 2), writes=[hpi])
        for i in range(2):
            p.op("pool", lambda e, i=i: e.memset(LC[i][:, :], 0.0), writes=[LC[i]])
        for b in range(4):
            self.wload(stg, wglu, wglu[:, b:b + 1, :], W["s5_w_glu"][l, b * 128:(b + 1) * 128, :], 1, 512)
        p.dma("sp", bglu[:], W["s5_b_glu"][l, :].rearrange("(m q) -> q m", q=128), writes=[bglu], allow_slow_non_contiguous=True)
        p.dma("sp", dcol[:], W["s5_d"][l, :].rearrange("(m q) -> q m", q=128), writes=[dcol], allow_slow_non_contiguous=True)

        PR, PQ, BST = [], [], []
        Cn = sm("Cn", (128, 128))
        CST = [[p.sb(f"s_cst{d}{b}", [128, 128], F32, st) for b in range(4)] for d in range(2)]
        BT = [[p.sb(f"s_bt{d}{b}", [128, 128], F32, st) for b in range(4)] for d in range(2)]
        dv = lambda fn, r, w: p.op("dve", fn, reads=r, writes=w)
        for d in range(2):
            are, aim, dt, lr, li, m1, cc, ss, t1, t2, nr, ni, den, cr, ci = (sm(f"{n}{d}") for n in
                                                                             ("are", "aim", "dt", "lr", "li", "m1", "cc", "ss", "t1", "t2", "nr", "ni", "den", "cr", "ci"))
            Xb = sm(f"Xb{d}", (128, 32, 16))
            Yb = sm(f"Yb{d}", (128, 32, 16))
            Bst = sm(f"Bst{d}", (128, 32, 16))
            Tb = sm(f"Tb{d}", (128, 32, 16))
            pr = sm(f"pr{d}", (128, 12, 32))
            pq = sm(f"pq{d}", (128, 12, 32))
            for hf in range(2):
                hs = slice(hf * 64, (hf + 1) * 64)
                p.dma("sp", are[hs, :], W["s5_a_re"][l, d, :, :].rearrange("g q -> q g"), writes=[are], allow_slow_non_contiguous=True)
                p.dma("sp", aim[hs, :], W["s5_a_im"][l, d, :, :].rearrange("g q -> q g"), writes=[aim], allow_slow_non_contiguous=True)
                own, oth = ("s5_b_re", "s5_b_im") if hf == 0 else ("s5_b_im", "s5_b_re")
                p.dma("sp", Xb[hs, :, :], W[own][l, d, :, :, :].rearrange("g q c -> q g c"), writes=[Xb])
                p.dma("sp", Yb[hs, :, :], W[oth][l, d, :, :, :].rearrange("g q c -> q g c"), writes=[Yb])
            p.dma("sp", dt[:], W["s5_log_dt"][l, d:d + 1, :].partition_broadcast(128), writes=[dt])
            p.op("act", lambda e, dt=dt: e.activation(out=dt[:, :], in_=dt[:, :], func=AF.Exp), reads=[dt], writes=[dt])
            TT = lambda o, a, b_, op: (lambda e: e.tensor_tensor(out=o[:, :], in0=a[:, :], in1=b_[:, :], op=op))
            dv(TT(lr, are, dt, ALU.mult), [are, dt], [lr])
            dv(TT(li, aim, dt, ALU.mult), [aim, dt], [li])
            TS = lambda o, a, s1, s2, o0, o1=None: (lambda e: e.tensor_scalar(o[:, :], a[:, :], s1, s2, o0, o1) if o1 is not None else e.tensor_scalar(o[:, :], a[:, :], s1, None, o0))
            dv(TS(m1, lr, 0.25, 1.0, ALU.mult, ALU.add), [lr], [m1])
            dv(TT(m1, m1, lr, ALU.mult), [m1, lr], [m1])
            dv(TS(m1, m1, 1.0 / 3.0, 1.0, ALU.mult, ALU.add), [m1], [m1])
            dv(TT(m1, m1, lr, ALU.mult), [m1, lr], [m1])
            dv(TS(m1, m1, 0.5, 1.0, ALU.mult, ALU.add), [m1], [m1])
            dv(TT(m1, m1, lr, ALU.mult), [m1, lr], [m1])
            dv(TS(m1, m1, 1.0, None, ALU.add), [m1], [m1])
            dv(TS(nr, li, 1.0 / 256.0, None, ALU.mult), [li], [nr])
            dv(TT(t1, nr, nr, ALU.mult), [nr], [t1])
            dv(TS(ss, t1, 1.0 / 120.0, -1.0 / 6.0, ALU.mult, ALU.add), [t1], [ss])
            dv(TT(ss, ss, t1, ALU.mult), [ss, t1], [ss])
            dv(TS(ss, ss, 1.0, None, ALU.add), [ss], [ss])
            dv(TT(ss, ss, nr, ALU.mult), [ss, nr], [ss])
            dv(TS(cc, t1, -1.0 / 720.0, 1.0 / 24.0, ALU.mult, ALU.add), [t1], [cc])
            dv(TT(cc, cc, t1, ALU.mult), [cc, t1], [cc])
            dv(TS(cc, cc, -0.5, None, ALU.add), [cc], [cc])
            dv(TT(cc, cc, t1, ALU.mult), [cc, t1], [cc])
            dv(TS(cc, cc, 1.0, None, ALU.add), [cc], [cc])
            for _ in range(8):
                dv(TT(t1, cc, cc, ALU.mult), [cc], [t1])
                dv(TT(t2, ss, ss, ALU.mult), [ss], [t2])
                dv(lambda e, cc=cc, ss=ss: e.scalar_tensor_tensor(out=ss[:, :], in0=cc[:, :], scalar=2.0, in1=ss[:, :], op0=ALU.mult, op1=ALU.mult), [cc, ss], [ss])
                dv(TT(cc, t1, t2, ALU.subtract), [t1, t2], [cc])
            dv(TT(t1, cc, cc, ALU.mult), [cc], [t1])
            dv(TT(t2, ss, ss, ALU.mult), [ss], [t2])
            dv(TT(t1, t1, t2, ALU.add), [t1, t2], [t1])
            dv(TS(t1, t1, -0.5, 1.5, ALU.mult, ALU.add), [t1], [t1])
            dv(TT(cc, cc, t1, ALU.mult), [cc, t1], [cc])
            dv(TT(ss, ss, t1, ALU.mult), [ss, t1], [ss])
            dv(lambda e, pr=pr, m1=m1, cc=cc: e.tensor_tensor(out=pr[:, 0, :], in0=m1[:, :], in1=cc[:, :], op=ALU.mult), [m1, cc], [pr])
            dv(TT(ni, m1, ss, ALU.mult), [m1, ss], [ni])
            dv(lambda e, pq=pq, ni=ni: e.tensor_scalar(pq[:, 0, :], ni[:, :], sgn[:, 1:2], None, ALU.mult), [ni, sgn], [pq])
            dv(lambda e, nr=nr, pr=pr: e.tensor_scalar(nr[:, :], pr[:, 0, :], -1.0, None, ALU.add), [pr], [nr])
            dv(TT(t1, are, are, ALU.mult), [are], [t1])
            dv(TT(t2, aim, aim, ALU.mult), [aim], [t2])
            dv(TT(den, t1, t2, ALU.add), [t1, t2], [den])
            dv(lambda e, den=den: e.reciprocal(out=den[:, :], in_=den[:, :]), [den], [den])
            dv(TT(t1, nr, are, ALU.mult), [nr, are], [t1])
            dv(TT(t2, ni, aim, ALU.mult), [ni, aim], [t2])
            dv(TT(cr, t1, t2, ALU.add), [t1, t2], [cr])
            dv(TT(cr, cr, den, ALU.mult), [cr, den], [cr])
            dv(TT(t1, ni, are, ALU.mult), [ni, are], [t1])
            dv(TT(t2, nr, aim, ALU.mult), [nr, aim], [t2])
            dv(TT(ci, t1, t2, ALU.subtract), [t1, t2], [ci])
            dv(TT(ci, ci, den, ALU.mult), [ci, den], [ci])
            dv(lambda e, ci=ci: e.tensor_scalar(ci[:, :], ci[:, :], sgn[:, 0:1], None, ALU.mult), [ci, sgn], [ci])
            bc = lambda t_: t_[:, :].unsqueeze(2).to_broadcast([128, 32, 16])
            dv(lambda e, Bst=Bst, Xb=Xb, cr=cr: e.tensor_tensor(out=Bst[:, :, :], in0=Xb[:, :, :], in1=bc(cr), op=ALU.mult), [Xb, cr], [Bst])
            dv(lambda e, Tb=Tb, Yb=Yb, ci=ci: e.tensor_tensor(out=Tb[:, :, :], in0=Yb[:, :, :], in1=bc(ci), op=ALU.mult), [Yb, ci], [Tb])
            dv(lambda e, Bst=Bst, Tb=Tb: e.tensor_tensor(out=Bst[:, :, :], in0=Bst[:, :, :], in1=Tb[:, :, :], op=ALU.add), [Bst, Tb], [Bst])
            for k in range(11):
                dv(lambda e, k=k, pr=pr, t1=t1: e.tensor_tensor(out=t1[:, :], in0=pr[:, k, :], in1=pr[:, k, :], op=ALU.mult), [pr], [t1])
                dv(lambda e, k=k, pq=pq, t2=t2: e.tensor_tensor(out=t2[:, :], in0=pq[:, k, :], in1=pq[:, k, :], op=ALU.mult), [pq], [t2])
                dv(lambda e, k=k, pr=pr, pq=pq: e.scalar_tensor_tensor(out=pq[:, k + 1, :], in0=pr[:, k, :], scalar=2.0, in1=pq[:, k, :], op0=ALU.mult, op1=ALU.mult), [pr, pq], [pq])
                dv(lambda e, k=k, pr=pr, t1=t1, t2=t2: e.tensor_tensor(out=pr[:, k + 1, :], in0=t1[:, :], in1=t2[:, :], op=ALU.subtract), [t1, t2], [pr])
            PR.append(pr)
            PQ.append(pq)
            if d == 0:
                for nm, tl_ in (("are", are), ("aim", aim), ("dt", dt), ("m1", m1), ("cc", cc), ("ss", ss), ("cr", cr), ("ci", ci)):
                    self.dump(nm, tl_, tl_[:, :], [128, 32])
                self.dump("pr", pr, pr[:, :, :], [128, 12, 32])
                self.dump("pq", pq, pq[:, :, :], [128, 12, 32])
                self.dump("Bst", Bst, Bst[:, :, :], [128, 32, 16])
            for b in range(4):
                ps = self.bank(0, 4)
                p.op("pe", lambda e, ps=ps, Bst=Bst, b=b: e.transpose(ps[:, 0:128], Bst[:, b * 8:(b + 1) * 8, :].rearrange("q g c -> q (g c)"), self.identf[:, :]), reads=[Bst, self.identf], writes=[ps])
                p.op("act", lambda e, ps=ps, d=d, b=b: e.copy(out=BT[d][b][:, :], in_=ps[:, 0:128]), reads=[ps], writes=[BT[d][b]])
                p.dma("sp", Cn[:, 0:64], W["s5_c_re"][l, d, b * 8:(b + 1) * 8, :, :].rearrange("g c q -> (g c) q"), writes=[Cn])
                p.dma("sp", Cn[:, 64:128], W["s5_c_im"][l, d, b * 8:(b + 1) * 8, :, :].rearrange("g c q -> (g c) q"), writes=[Cn])
                ps = self.bank(0, 4)
                p.op("pe", lambda e, ps=ps: e.transpose(ps[:, 0:128], Cn[:, :], self.identf[:, :]), reads=[Cn, self.identf], writes=[ps])
                p.op("dve", lambda e, ps=ps, d=d, b=b: e.tensor_scalar(CST[d][b][:, :], ps[:, 0:128], sgn[:, 1:2], None, ALU.mult), reads=[ps, sgn], writes=[CST[d][b]])

        for b_ in (0, 3):
            self.dump(f"BT{b_}", BT[0][b_], BT[0][b_][:, :], [128, 128])
            self.dump(f"CST{b_}", CST[0][b_], CST[0][b_][:, :], [128, 128])
        mki = 0
        gi = 0
        for b in self.dbg.get("s5_tiles", range(4)):
            c0 = COL["u"] + b * 128
            self.wload(stg, wu, wu[:, :, :], W["w_in"][l, :, c0:c0 + 128], 8, 128)
            for ct in range(8):
                ps = self.bank(0, 4)
                for c in range(8):
                    self.mm(ps[:, :], wu[:, c, :], xT[:, c, ct * 512:(ct + 1) * 512], c == 0, c == 7, [wu] + xTr[ct * 4:ct * 4 + 4], [ps])
                p.op("act", lambda e, ps=ps, ct=ct: e.copy(out=uT[:, ct * 512:(ct + 1) * 512], in_=ps[:, :]), reads=[ps], writes=[uT])
            first_acc = True
            for gl in range(8):
                g = b * 8 + gl
                for d in self.dbg.get("s5_dirs", (0, 1)):
                    lb = LB[gi % 2]
                    lc = LC[gi % 2]
                    gi += 1
                    p.op("dve", lambda e, lb=lb, d=d, gl=gl, b=b: e.tensor_scalar(lb[:, :], BT[d][b][:, :], rowm[:, gl:gl + 1], None, ALU.mult), reads=[BT[d][b], rowm], writes=[lb])
                    p.op("pool", lambda e, lc=lc: e.memset(lc[:, :], 0.0), writes=[lc])
                    p.op("pool", lambda e, lc=lc, d=d, gl=gl, b=b: e.tensor_copy(out=lc[:, gl * 16:(gl + 1) * 16], in_=CST[d][b][:, gl * 16:(gl + 1) * 16]), reads=[CST[d][b], lc], writes=[lc])
                    cur = 0
                    for ct in range(8):
                        ps = self.bank(0, 4)
                        self.mm(ps[:, :], lb[:, :], uT[:, ct * 512:(ct + 1) * 512], True, True, [lb, uT], [ps])
                        p.op("act", lambda e, ps=ps, ct=ct, cur=cur: e.copy(out=X[cur][:, ct * 512:(ct + 1) * 512], in_=ps[:, :]), reads=[ps], writes=[X[cur].bs[ct]])
                    for k in range(12):
                        sh = 1 << k
                        mt = Mt[mki % 2]
                        mk = Mk[mki % 4]
                        mki += 1
                        p.op("dve", lambda e, mt=mt, d=d, k=k, g=g: e.tensor_scalar(mt[:, :], jx[:, :], PQ[d][:, k, g:g + 1], None, ALU.mult), reads=[jx, PQ[d]], writes=[mt])
                        p.op("dve", lambda e, mt=mt, mk=mk, d=d, k=k, g=g: e.scalar_tensor_tensor(out=mk[:, :], in0=self.identf[:, :], scalar=PR[d][:, k, g:g + 1], in1=mt[:, :], op0=ALU.mult, op1=ALU.add),
                             reads=[self.identf, PR[d], mt], writes=[mk])
                        Xs, Xd = X[cur], X[1 - cur]
                        for ct in range(8):
                            t0 = ct * 512
                            if d == 0:
                                lo = max(0, sh - t0)
                                hi = 512
                                s0 = t0 + lo - sh
                            else:
                                lo = 0
                                hi = min(512, S - sh - t0)
                                s0 = t0 + sh
                            has = hi > lo
                            srcb = []
                            if has:
                                a0, a1 = s0, s0 + (hi - lo)
                                srcb = [Xs.bs[i] for i in range(a0 // 512, (a1 - 1) // 512 + 1)]
                            if ct % 2 == 0 or not has:
                                ps = self.bank(0, 4) if ct % 2 == 0 else self.bank(4, 8)
                                self.mm(ps[:, :], identb[:, :], Xs[:, t0:t0 + 512], True, not has, [identb, Xs.bs[ct]], [ps])
                                if has:
                                    self.mm(ps[:, lo:hi], mk[:, :], Xs[:, s0:s0 + hi - lo], False, True, [mk] + srcb, [ps])
                                p.op("act", lambda e, ps=ps, Xd=Xd, t0=t0: e.copy(out=Xd[:, t0:t0 + 512], in_=ps[:, :]), reads=[ps], writes=[Xd.bs[ct]])
                            else:
                                ps = self.bank(4, 8)
                                self.mm(ps[:, lo:hi], mk[:, :], Xs[:, s0:s0 + hi - lo], True, True, [mk] + srcb, [ps])
                                p.op("dve", lambda e, ps=ps, Xd=Xd, Xs=Xs, t0=t0, lo=lo, hi=hi: e.tensor_tensor(out=Xd[:, t0 + lo:t0 + hi], in0=ps[:, lo:hi], in1=Xs[:, t0 + lo:t0 + hi], op=ALU.add),
                                     reads=[ps, Xs.bs[ct]], writes=[Xd.bs[ct]])
                                if lo > 0:
                                    p.op("pool", lambda e, Xd=Xd, Xs=Xs, t0=t0, lo=lo: e.tensor_copy(out=Xd[:, t0:t0 + lo], in_=Xs[:, t0:t0 + lo]), reads=[Xs.bs[ct]], writes=[Xd.bs[ct]])
                                if hi < 512:
                                    p.op("pool", lambda e, Xd=Xd, Xs=Xs, t0=t0, hi=hi: e.tensor_copy(out=Xd[:, t0 + hi:t0 + 512], in_=Xs[:, t0 + hi:t0 + 512]), reads=[Xs.bs[ct]], writes=[Xd.bs[ct]])
                        cur = 1 - cur
                    for ct in range(8):
                        ps = self.bank(0, 8)
                        self.mm(ps[:, :], lc[:, :], X[cur][:, ct * 512:(ct + 1) * 512], True, True, [lc, X[cur].bs[ct]], [ps])
                        ya = yacc[:, ct * 512:(ct + 1) * 512]
                        if first_acc:
                            p.op("dve", lambda e, ps=ps, ya=ya: e.tensor_copy(out=ya, in_=ps[:, :]), reads=[ps], writes=[yacc.bs[ct]])
                        else:
                            p.op("dve", lambda e, ps=ps, ya=ya: e.tensor_tensor(out=ya, in0=ps[:, :], in1=ya, op=ALU.add), reads=[ps], writes=[yacc.bs[ct]])
                    first_acc = False
            for ct in range(8):
                y_ = yt[ct % 2]
                sl = slice(ct * 512, (ct + 1) * 512)
                p.op("dve", lambda e, y_=y_, sl=sl, b=b: e.scalar_tensor_tensor(out=y_[:, :], in0=uT[:, sl], scalar=dcol[:, b:b + 1], in1=yacc[:, sl], op0=ALU.mult, op1=ALU.add),
                     reads=[uT, dcol, yacc.bs[ct]], writes=[y_])
                if self.dbg.get("s5y"):
                    p.dma("pool", self.dbgy[b, :, sl], y_[:, :], reads=[y_], writes=[self.dbgy])
                    p.dma("pool", self.dbgy[4 + b, :, sl], yacc[:, sl], reads=[yacc.bs[ct]], writes=[self.dbgy])
                p.op("act", lambda e, y_=y_, sl=sl, b=b: e.activation(out=ygT[:, b, sl], in_=y_[:, :], func=AF.Gelu_apprx_tanh), reads=[y_], writes=[ygT])
        for m in range(4):
            for ct in range(8):
                sl = slice(ct * 512, (ct + 1) * 512)
                ps = self.bank(0, 8)
                for b in range(4):
                    self.mm(ps[:, :], wglu[:, b, m * 128:(m + 1) * 128], ygT[:, b, sl], b == 0, b == 3, [wglu, ygT], [ps])
                y_ = yt[ct % 2]
                o_ = ob[ct % 2]
                p.op("act", lambda e, ps=ps, y_=y_, m=m: e.activation(out=y_[:, :], in_=ps[:, :], func=AF.Sigmoid, bias=bglu[:, m:m + 1]), reads=[ps, bglu], writes=[y_])
                p.op("dve", lambda e, y_=y_, o_=o_, m=m, sl=sl: e.tensor_tensor(out=o_[:, :], in0=ygT[:, m, sl], in1=y_[:, :], op=ALU.mult), reads=[ygT, y_], writes=[o_])
                p.dma("pool", self.oT[1, m, :, sl], o_[:, :], reads=[o_], writes=[self.oT.bs[4 + m]])
        p.barrier()
        st.close()


def build_model():
    M = Model()
    M.phase_ln0()
    for l in range(DEPTH):
        M.phase_attn(l)
        M.phase_s5(l)
        M.phase_gla(l)
        M.phase_merge1(l)
        M.phase_merge2(l)
        M.phase_moe(l, last=(l == DEPTH - 1))
    M.p.emit()
    return M


def kernel(**inputs):
    M = build_model()
    consts = host_consts()
    shared = {k: np.ascontiguousarray(np.asarray(inputs[k], dtype=np.float32)) for k in PARAM_SHAPES}
    shared.update(consts)
    x = np.asarray(inputs["x"], dtype=np.float32)
    in_maps = []
    for b in range(8):
        m = dict(shared)
        m["x"] = np.ascontiguousarray(x[b])
        in_maps.append(m)
    res = run_bass_kernel_spmd(M.nc, in_maps, core_ids=list(range(8)))
    return np.stack([np.asarray(r["out"], dtype=np.float32) for r in res.results], axis=0)
```

```python
import math
from contextlib import ExitStack
import numpy as np
import ml_dtypes
import concourse.bass as bass
import concourse.mybir as mybir
from concourse.bass_utils import run_bass_kernel_spmd

F32 = mybir.dt.float32
BF16 = mybir.dt.bfloat16
AF = mybir.ActivationFunctionType
ALU = mybir.AluOpType
AX = mybir.AxisListType

ENGS = ("pe", "act", "dve", "pool", "sp")
NDMA_SEM = 6


class Buf:
    __slots__ = ("name", "w", "r", "excl")

    def __init__(self, name=""):
        self.name = name
        self.w = None
        self.r = {}
        self.excl = False


class Tl:
    def __init__(self, t, name, nb=0):
        self.t = t
        self.b = Buf(name)
        self.bs = [Buf(f"{name}{i}") for i in range(nb)]

    def __getitem__(self, k):
        return self.t[k]


class Sub(Tl):
    def __init__(self, ap, buf):
        self.t = ap
        self.b = buf
        self.bs = []


class Prog:
    def __init__(self, nc):
        self.nc = nc
        self.stack = ExitStack()
        self.lists = {e: [] for e in ENGS}
        self.cnt = {e: 0 for e in ENGS}
        self.known = {e: {} for e in ENGS}
        self.sem = {}
        for e in ("pe", "act", "dve", "pool"):
            self.sem["c_" + e] = self.stack.enter_context(nc.semaphore("c_" + e))
        self.dq = {}
        for q in ("sp", "pool", "act"):
            names = []
            for i in range(NDMA_SEM):
                n = f"d_{q}{i}"
                self.sem[n] = self.stack.enter_context(nc.semaphore(n))
                names.append(n)
            self.dq[q] = dict(sems=names, issued=[0] * NDMA_SEM, rr=0)
        self.nwaits = 0

    uid = 0

    def sb(self, name, shape, dtype, stack=None, nb=0):
        Prog.uid += 1
        name = f"{name}_{Prog.uid}"
        t = (stack or self.stack).enter_context(self.nc.sbuf_tensor(name, list(shape), dtype))
        return Tl(t, name, nb)

    def ps(self, name, shape, dtype=F32, stack=None, nb=0):
        t = (stack or self.stack).enter_context(self.nc.psum_tensor(name, list(shape), dtype))
        tl = Tl(t, name, nb)
        tl.b.excl = True
        return tl

    def dram(self, name, shape, dtype, kind="Internal", nb=0):
        t = self.nc.dram_tensor(name, list(shape), dtype, kind=kind).ap()
        return Tl(t, name, nb)

    @staticmethod
    def _bufs(xs):
        out = []
        for x in xs:
            if x is None:
                continue
            out.append(x.b if isinstance(x, Tl) else x)
        return out

    def _need(self, reads, writes):
        need = {}

        def add(sv):
            s, v = sv
            if need.get(s, 0) < v:
                need[s] = v

        for b in reads:
            if b.w:
                add(b.w)
        for b in writes:
            if b.w:
                add(b.w)
            for sv in b.r.items():
                add(sv)
        return need

    def _waits(self, e, need, skip=None):
        kn = self.known[e]
        for s, v in need.items():
            if s == skip:
                continue
            if kn.get(s, 0) < v:
                self.lists[e].append(("wait", s, v))
                kn[s] = v
                self.nwaits += 1

    def op(self, e, fn, reads=(), writes=(), serial=False):
        reads = self._bufs(reads)
        writes = self._bufs(writes)
        ex = [b for b in reads if b.excl and b not in writes]
        if ex:
            reads = [b for b in reads if not b.excl]
            writes = writes + ex
        s = "c_" + e
        self._waits(e, self._need(reads, writes), skip=(s if (e == "pe" and not serial) else None))
        self.cnt[e] += 1
        v = self.cnt[e]
        self.lists[e].append(("op", fn, s))
        for b in reads:
            b.r[s] = v
        for b in writes:
            b.w = (s, v)
            b.r = {}

    def dma(self, q, out, in_, reads=(), writes=(), **kw):
        reads = self._bufs(reads)
        writes = self._bufs(writes)
        d = self.dq[q]
        i = d["rr"]
        d["rr"] = (i + 1) % NDMA_SEM
        s = d["sems"][i]
        need = self._need(reads, writes)
        if d["issued"][i] > 0:
            pv = 16 * d["issued"][i]
            if need.get(s, 0) < pv:
                need[s] = pv
        self._waits(q, need)
        d["issued"][i] += 1
        v = 16 * d["issued"][i]
        self.lists[q].append(("dma", out, in_, s, kw))
        for b in reads:
            b.r[s] = v
        for b in writes:
            b.w = (s, v)
            b.r = {}

    def barrier(self, engines=ENGS):
        need = {}
        for e in ("pe", "act", "dve", "pool"):
            if self.cnt[e]:
                need["c_" + e] = self.cnt[e]
        for q, d in self.dq.items():
            for s, n in zip(d["sems"], d["issued"]):
                if n:
                    need[s] = 16 * n
        for e in engines:
            self._waits(e, dict(need), skip=("c_" + e if e in ("pe",) else None))

    def emit(self):
        nc = self.nc
        self.barrier(engines=("sp",))
        lists = self.lists
        sem = self.sem

        def run(eng, lst):
            for it in lst:
                if it[0] == "wait":
                    eng.wait_ge(sem[it[1]], it[2])
                elif it[0] == "op":
                    it[1](eng).then_inc(sem[it[2]], 1)
                else:
                    eng.dma_start(out=it[1], in_=it[2], **it[4]).then_inc(sem[it[3]], 16)

        with nc.Block() as block:
            @block.tensor
            def _(eng):
                run(eng, lists["pe"])

            @block.scalar
            def _(eng):
                run(eng, lists["act"])

            @block.vector
            def _(eng):
                run(eng, lists["dve"])

            @block.gpsimd
            def _(eng):
                run(eng, lists["pool"])

            @block.sync
            def _(eng):
                run(eng, lists["sp"])
        self.stack.close()


S = 4096
D = 1024
NT = 32
DEPTH = 2
N_IN = 6688
COL = dict(qa=0, ka=512, va=1024, u=1536, qc=2048, kc=2304, vc=2560, rc=3072, zf=3584, zb=3600, gate=3616)
ALPHA = (2.0 * DEPTH) ** 0.25
EPS = 1e-5
PI = math.pi

PARAM_SHAPES = dict(
    ln0_g=[1024], ln0_b=[1024], w_in=[2, 1024, 6688], da_lambda=[2, 4, 64], da_norm_g=[2, 128],
    s5_a_re=[2, 2, 32, 64], s5_a_im=[2, 2, 32, 64], s5_log_dt=[2, 2, 32], s5_b_re=[2, 2, 32, 64, 16],
    s5_b_im=[2, 2, 32, 64, 16], s5_c_re=[2, 2, 32, 16, 64], s5_c_im=[2, 2, 32, 16, 64], s5_d=[2, 512],
    s5_w_glu=[2, 512, 512], s5_b_glu=[2, 512], gla_w_gate=[2, 2, 16, 256], gla_b_gate=[2, 2, 256],
    gla_norm_g=[2, 128], merge_w_up=[2, 3, 512, 1024], merge_b=[2, 3, 1024], w_out=[2, 1024, 1024],
    ln1_g=[2, 1024], ln1_b=[2, 1024], router_w=[1024, 16], router_bias=[16],
    moe_w_gate=[2, 16, 1024, 512], moe_w_up=[2, 16, 1024, 512], moe_w_down=[2, 16, 512, 1024],
    ln2_g=[2, 1024], ln2_b=[2, 1024])


def host_consts():
    c = {}
    c["identf"] = np.eye(128, dtype=np.float32)
    i = np.arange(128)
    c["absdiff"] = np.abs(i[:, None] - i[None, :]).astype(np.float32)
    pos = np.arange(S)
    hi, lo = pos // 64, pos % 64
    c["qaug"] = np.stack([64.0 * hi, lo, np.ones(S), np.ones(S)]).astype(ml_dtypes.bfloat16)
    ka = np.zeros((4, 2, 4, S), np.float32)
    for h in range(4):
        sl = 2.0 ** (-2.0 * (h + 1))
        plus = np.stack([np.full(S, sl), np.full(S, sl), -sl * 64.0 * hi, -sl * lo])
        ka[h, 0] = plus
        ka[h, 1] = -plus
    c["kaug"] = ka.astype(ml_dtypes.bfloat16)
    j = np.arange(64)
    c["gmask"] = np.stack([(j[:, None] <= j[None, :]), (j[:, None] > j[None, :])]).astype(np.float32)
    jj = np.repeat(np.arange(8), 16)
    c["s5mask"] = np.stack([(jj[None, :] >= jj[:, None]), (jj[None, :] <= jj[:, None])]).astype(np.float32)
    jx = np.zeros((128, 128), np.float32)
    for q in range(64):
        jx[q, 64 + q] = 1.0
        jx[64 + q, q] = 1.0
    c["jx"] = jx
    c["rowmask"] = (np.arange(128)[:, None] // 16 == np.arange(8)[None, :]).astype(np.float32)
    sg = np.ones((128, 2), np.float32)
    sg[:64, 0] = -1.0
    sg[64:, 1] = -1.0
    c["sgn"] = sg
    return c


CONST_SHAPES = dict(jx=([128, 128], F32), rowmask=([128, 8], F32), sgn=([128, 2], F32), identf=([128, 128], F32), absdiff=([128, 128], F32), qaug=([4, S], BF16), kaug=([4, 2, 4, S], BF16),
                    gmask=([2, 64, 64], F32), s5mask=([2, 128, 128], F32))


class Model:
    def __init__(self, dbg=None):
        self.dbg = dbg or {}
        nc = bass.Bass("TRN2", target_bir_lowering=False)
        self.nc = nc
        p = Prog(nc)
        self.p = p
        kinds = self.dbg.get("kinds", {})
        self.x_in = p.dram("x", [S, D], F32, kind="ExternalInput")
        self.out = p.dram("out", [S, D], F32, kind="ExternalOutput", nb=NT)
        self.W = {k: p.dram(k, shp, F32, kind="ExternalInput") for k, shp in PARAM_SHAPES.items()}
        self.C = {k: p.dram(k, shp, dt, kind="ExternalInput") for k, (shp, dt) in CONST_SHAPES.items()}
        self.xres = p.dram("xres", [S, D], F32, kind=kinds.get("xres", "Internal"), nb=NT)
        self.oT = p.dram("oT", [3, 4, 128, S], BF16, kind=kinds.get("oT", "Internal"), nb=12)
        self.mT = p.dram("mT", [128, 8, S], BF16, kind=kinds.get("mT", "Internal"), nb=16)
        if self.dbg.get("s5y"):
            self.dbgy = p.dram("dbgy", [8, 128, S], F32, kind="ExternalOutput")
        self.xT = p.sb("xT", [128, 8, S], BF16, nb=NT)
        self.identf = p.sb("identf_sb", [128, 128], F32)
        self.call = p.sb("call", [128, NT, 16], F32, nb=NT)
        self.PS = [p.ps(f"ps{i}", [128, 512], F32) for i in range(8)]
        self.psi = 0
        p.dma("sp", self.identf[:], self.C["identf"][:, :], writes=[self.identf])

    def dump(self, name, tl, ap, shape):
        if not self.dbg.get("dump"):
            return
        d = self.p.dram("dump_" + name, list(shape), F32, kind="ExternalOutput")
        self.p.dma("pool", d.t, ap, reads=[tl], writes=[d])

    def bank(self, lo=0, hi=8):
        n = hi - lo
        b = self.PS[lo + (self.psi % n)]
        self.psi += 1
        return b

    def mm(self, out, lhsT, rhs, start, stop, reads, writes, serial=False):
        self.p.op("pe", lambda e: e.matmul(out, lhsT, rhs, start=start, stop=stop, skip_group_check=True), reads=reads, writes=writes, serial=serial)

    def ln_tile(self, z, xn, stats, mv, rstd):
        p = self.p
        for k in range(2):
            p.op("dve", lambda e, k=k: e.bn_stats(out=stats[:, k, :], in_=z[:, k * 512:(k + 1) * 512]), reads=[z], writes=[stats])
        p.op("dve", lambda e: e.bn_aggr(out=mv[:, :], in_=stats[:, :, :].rearrange("p a b -> p (a b)")), reads=[stats], writes=[mv])
        self.rsqrt(rstd, rstd[:, :], mv, mv[:, 1:2], 1.0)
        p.op("dve", lambda e: e.tensor_scalar(xn[:, :], z[:, :], mv[:, 0:1], rstd[:, 0:1], ALU.subtract, ALU.mult), reads=[z, mv, rstd], writes=[xn])
        p.op("pool", lambda e, g_=self.gbc: e.tensor_tensor(out=xn[:, :], in0=xn[:, :], in1=g_[:, :], op=ALU.mult), reads=[xn, self.gbc], writes=[xn])
        p.op("pool", lambda e, b_=self.bbc: e.tensor_tensor(out=xn[:, :], in0=xn[:, :], in1=b_[:, :], op=ALU.add), reads=[xn, self.bbc], writes=[xn])

    def rsqrt(self, dst_tl, dst, src_tl, src, scale):
        p = self.p
        p.op("dve", lambda e: e.tensor_scalar(dst, src, scale, EPS, ALU.mult, ALU.add), reads=[src_tl], writes=[dst_tl])
        p.op("act", lambda e: e.sqrt(out=dst, in_=dst), reads=[dst_tl], writes=[dst_tl])
        p.op("dve", lambda e: e.reciprocal(out=dst, in_=dst), reads=[dst_tl], writes=[dst_tl])

    def load_ln_params(self, g_ap, b_ap, st):
        p = self.p
        self.gbc = p.sb("gbc", [128, D], F32, st)
        self.bbc = p.sb("bbc", [128, D], F32, st)
        p.dma("sp", self.gbc[:], g_ap.partition_broadcast(128), writes=[self.gbc])
        p.dma("sp", self.bbc[:], b_ap.partition_broadcast(128), writes=[self.bbc])

    def to_xT(self, xn, t, xT32=None):
        p = self.p
        for c0 in (0, 4):
            ps = self.bank(0, 4)
            for j in range(4):
                c = c0 + j
                p.op("pe", lambda e, ps=ps, j=j, c=c: e.transpose(ps[:, j * 128:(j + 1) * 128], xn[:, c * 128:(c + 1) * 128], self.identf[:, :]),
                     reads=[xn, self.identf], writes=[ps])
            src = ps[:, :].rearrange("p (j n) -> p j n", j=4)
            p.op("act", lambda e, src=src, c0=c0: e.copy(out=self.xT[:, c0:c0 + 4, t * 128:(t + 1) * 128], in_=src), reads=[ps], writes=[self.xT.bs[t]])
            if xT32 is not None:
                p.op("dve", lambda e, src=src, c0=c0: e.tensor_copy(out=xT32[:, c0:c0 + 4, :], in_=src), reads=[ps], writes=[xT32])

    def phase_ln0(self):
        p = self.p
        st = ExitStack()
        self.load_ln_params(self.W["ln0_g"].t.rearrange("(o n) -> o n", o=1), self.W["ln0_b"].t.rearrange("(o n) -> o n", o=1), st)
        zs = [p.sb(f"l0z{i}", [128, D], F32, st) for i in range(2)]
        stats = p.sb("l0stats", [128, 2, 6], F32, st)
        mv = p.sb("l0mv", [128, 2], F32, st)
        rstd = p.sb("l0rstd", [128, 1], F32, st)
        for t in range(NT):
            z = zs[t % 2]
            p.dma("sp", z[:], self.x_in[t * 128:(t + 1) * 128, :], writes=[z])
            self.ln_tile(z, z, stats, mv, rstd)
            p.dma("pool", self.xres[t * 128:(t + 1) * 128, :], z[:], reads=[z], writes=[self.xres.bs[t]])
            self.to_xT(z, t)
        p.barrier()
        st.close()

    def wload(self, stg, dst_tl, dst_ap, src_ap, kc, ncols, cast="pool", wbuf=None):
        p = self.p
        cap = stg[0].t.shape[1]
        kcp = max(1, min(kc, cap // ncols))
        wb = [wbuf if wbuf is not None else dst_tl]
        for k0 in range(0, kc, kcp):
            st = stg[self.wl_i % len(stg)]
            self.wl_i += 1
            view = st[:, 0:kcp * ncols].rearrange("p (c n) -> p c n", c=kcp)
            p.dma("sp", view, src_ap[k0 * 128:(k0 + kcp) * 128, :].rearrange("(c p) n -> p c n", p=128), writes=[st])
            p.op(cast, lambda e, view=view, k0=k0: e.tensor_copy(out=dst_ap[:, k0:k0 + kcp, :], in_=view), reads=[st], writes=wb)

    wl_i = 0

    def phase_attn(self, l):
        p = self.p
        W = self.W
        st = ExitStack()
        stg = [p.sb(f"a_stg{i}", [128, 8 * 128], F32, st) for i in range(2)]
        wA = [p.sb(f"a_w{i}", [128, 8, 384], BF16, st) for i in range(2)]
        QT = [p.sb(f"a_qt{m}", [68, S], BF16, st) for m in range(2)]
        KTp = [p.sb(f"a_ktp{m}", [68, S], BF16, st) for m in range(2)]
        KTm = [p.sb(f"a_ktm{m}", [68, S], BF16, st) for m in range(2)]
        V = p.sb("a_v", [128, NT, 129], BF16, st)
        PT = [p.sb(f"a_pt{i}", [128, 512], BF16, st) for i in range(4)]
        oaT = [p.sb(f"a_oat{i}", [128, S], BF16, st) for i in range(1)]
        on = [p.sb(f"a_on{m}", [128, 4, 128], F32, st) for m in range(2)]
        rs = p.sb("a_rs", [128, 4], F32, st)
        diff = p.sb("a_diff", [128, 4, 128], F32, st)
        sq = p.sb("a_sq", [128, 4, 128], F32, st)
        ss = p.sb("a_ss", [128, 4], F32, st)
        rstd = p.sb("a_rstd", [128, 4], F32, st)
        oo = p.sb("a_oo", [128, 4, 128], F32, st)
        absd = p.sb("a_absd", [128, 128], F32, st)
        lamt = p.sb("a_lamt", [128, 256], F32, st)
        lsm = p.sb("a_lsm", [128, 8], F32, st)
        gA = p.sb("a_gA", [128, 128], F32, st)
        lam_init = 0.8 - 0.6 * math.exp(-0.3 * l)
        p.dma("sp", absd[:], self.C["absdiff"][:, :], writes=[absd])
        for m in range(2):
            p.dma("sp", QT[m][64:68, :], self.C["qaug"][:, :], writes=[QT[m]])
        p.dma("sp", lamt[:], W["da_lambda"][l:l + 1, :, :].rearrange("o a b -> o (a b)").partition_broadcast(128), writes=[lamt])
        p.dma("sp", gA[:], W["da_norm_g"][l:l + 1, :].partition_broadcast(128), writes=[gA])
        p.op("dve", lambda e: e.tensor_scalar(gA[:, :], gA[:, :], 1.0 - lam_init, None, ALU.mult), reads=[gA], writes=[gA])
        p.op("dve", lambda e: e.tensor_tensor(out=lamt[:, 0:64], in0=lamt[:, 0:64], in1=lamt[:, 64:128], op=ALU.mult), reads=[lamt], writes=[lamt])
        p.op("dve", lambda e: e.tensor_tensor(out=lamt[:, 128:192], in0=lamt[:, 128:192], in1=lamt[:, 192:256], op=ALU.mult), reads=[lamt], writes=[lamt])
        p.op("dve", lambda e: e.tensor_reduce(out=lsm[:, 0:1], in_=lamt[:, 0:64], axis=AX.X, op=ALU.add), reads=[lamt], writes=[lsm])
        p.op("dve", lambda e: e.tensor_reduce(out=lsm[:, 1:2], in_=lamt[:, 128:192], axis=AX.X, op=ALU.add), reads=[lamt], writes=[lsm])
        p.op("act", lambda e: e.activation(out=lsm[:, 2:4], in_=lsm[:, 0:2], func=AF.Exp), reads=[lsm], writes=[lsm])
        p.op("dve", lambda e: e.tensor_tensor(out=lsm[:, 4:5], in0=lsm[:, 3:4], in1=lsm[:, 2:3], op=ALU.subtract), reads=[lsm], writes=[lsm])
        p.op("dve", lambda e: e.tensor_scalar(lsm[:, 5:6], lsm[:, 4:5], -lam_init, None, ALU.add), reads=[lsm], writes=[lsm])
        p.op("dve", lambda e: e.memset(V[:, :, 128:129], 1.0), writes=[V])
        xT = self.xT
        xTr = list(xT.bs)

        def load_head_w(h):
            w = wA[h % 2]
            for k, nm in enumerate(("qa", "ka", "va")):
                c0 = COL[nm] + h * 128
                self.wload(stg, w, w[:, :, k * 128:(k + 1) * 128], W["w_in"][l, :, c0:c0 + 128], 8, 128)

        load_head_w(0)
        for h in range(self.dbg.get("heads", 4)):
            slope = 2.0 ** (-2.0 * (h + 1))
            w = wA[h % 2]
            if h + 1 < self.dbg.get("heads", 4):
                load_head_w(h + 1)
            for m in range(2):
                p.dma("sp", KTp[m][64:68, :], self.C["kaug"][h, 0, :, :], writes=[KTp[m]])
                p.dma("sp", KTm[m][64:68, :], self.C["kaug"][h, 1, :, :], writes=[KTm[m]])
            for m in range(2):
                for tb in range(8):
                    ps = self.bank(0, 4)
                    for c in range(8):
                        self.mm(ps[0:64, :], w[:, c, m * 64:(m + 1) * 64], xT[:, c, tb * 512:(tb + 1) * 512], c == 0, c == 7, [w] + xTr[tb * 4:tb * 4 + 4], [ps])
                    p.op("act", lambda e, ps=ps, m=m, tb=tb: e.mul(out=QT[m][0:64, tb * 512:(tb + 1) * 512], in_=ps[0:64, :], mul=0.125), reads=[ps], writes=[QT[m]])
                    ps = self.bank(0, 4)
                    for c in range(8):
                        self.mm(ps[0:64, :], w[:, c, 128 + m * 64:128 + (m + 1) * 64], xT[:, c, tb * 512:(tb + 1) * 512], c == 0, c == 7, [w] + xTr[tb * 4:tb * 4 + 4], [ps])
                    p.op("act", lambda e, ps=ps, m=m, tb=tb: e.copy(out=KTp[m][0:64, tb * 512:(tb + 1) * 512], in_=ps[0:64, :]), reads=[ps], writes=[KTp[m]])
                    p.op("dve", lambda e, ps=ps, m=m, tb=tb: e.tensor_copy(out=KTm[m][0:64, tb * 512:(tb + 1) * 512], in_=ps[0:64, :]), reads=[ps], writes=[KTm[m]])
            for t4 in range(8):
                ps = self.bank(0, 4)
                for j in range(4):
                    t = t4 * 4 + j
                    for c in range(8):
                        self.mm(ps[:, j * 128:(j + 1) * 128], xT[:, c, t * 128:(t + 1) * 128], w[:, c, 256:384], c == 0, c == 7, [w, xTr[t]], [ps])
                p.op("act", lambda e, ps=ps, t4=t4: e.copy(out=V[:, t4 * 4:(t4 + 1) * 4, 0:128], in_=ps[:, :].rearrange("p (j n) -> p j n", j=4)), reads=[ps], writes=[V])
            oa = oaT[0]
            pti = 0
            for Q in range(self.dbg.get("nQ", 8)):
                stages = [(m, kt) for m in range(2) for kt in range(NT)]
                stbank = {}
                ptbuf = {}

                def st_S(i, Q=Q):
                    m, kt = stages[i]
                    ps = self.PS[i % 4]
                    stbank[i] = ps
                    rel = kt - 4 * Q
                    ksl = slice(kt * 128, (kt + 1) * 128)
                    if rel < 0:
                        self.mm(ps[:, :], KTm[m][0:68, ksl], QT[m][0:68, Q * 512:(Q + 1) * 512], True, True, [KTm[m], QT[m]], [ps])
                    elif rel > 3:
                        self.mm(ps[:, :], KTp[m][0:68, ksl], QT[m][0:68, Q * 512:(Q + 1) * 512], True, True, [KTp[m], QT[m]], [ps])
                    else:
                        q0 = Q * 512
                        first = True
                        if rel > 0:
                            self.mm(ps[:, 0:rel * 128], KTp[m][0:68, ksl], QT[m][0:68, q0:q0 + rel * 128], first, True, [KTp[m], QT[m]], [ps])
                            first = False
                        self.mm(ps[:, rel * 128:(rel + 1) * 128], KTp[m][0:64, ksl], QT[m][0:64, q0 + rel * 128:q0 + (rel + 1) * 128], first, True, [KTp[m], QT[m]], [ps])
                        if rel < 3:
                            self.mm(ps[:, (rel + 1) * 128:512], KTm[m][0:68, ksl], QT[m][0:68, q0 + (rel + 1) * 128:q0 + 512], False, True, [KTm[m], QT[m]], [ps])
                        p.op("dve", lambda e, ps=ps, rel=rel, slope=slope: e.scalar_tensor_tensor(
                            out=ps[:, rel * 128:(rel + 1) * 128], in0=absd[:, :], scalar=-slope, in1=ps[:, rel * 128:(rel + 1) * 128], op0=ALU.mult, op1=ALU.add),
                            reads=[absd, ps], writes=[ps])

                def st_E(i):
                    ps = stbank[i]
                    pt = PT[i % 4]
                    ptbuf[i] = pt
                    p.op("act", lambda e, ps=ps, pt=pt: e.activation(out=pt[:, :], in_=ps[:, :], func=AF.Exp), reads=[ps], writes=[pt])

                def st_P(i):
                    m, kt = stages[i]
                    pt = ptbuf[i]
                    OB = (self.PS[4 + 2 * m], self.PS[5 + 2 * m])
                    for j in range(4):
                        ob = OB[j // 2]
                        oc = (j % 2) * 256
                        self.mm(ob[:, oc:oc + 129], pt[:, j * 128:(j + 1) * 128], V[:, kt, :], (kt == 0 and j % 2 == 0), kt == NT - 1, [pt, V], [ob])
                    if kt == NT - 1:
                        for j in range(4):
                            ob = OB[j // 2]
                            oc = (j % 2) * 256
                            p.op("dve", lambda e, ob=ob, oc=oc, j=j: e.reciprocal(out=rs[:, j:j + 1], in_=ob[:, oc + 128:oc + 129]), reads=[ob], writes=[rs])
                            p.op("dve", lambda e, ob=ob, oc=oc, j=j, m=m: e.tensor_scalar(on[m][:, j, :], ob[:, oc:oc + 128], rs[:, j:j + 1], None, ALU.mult), reads=[ob, rs], writes=[on[m]])

                NS = len(stages)
                st_S(0)
                st_S(1)
                for i in range(NS):
                    st_E(i)
                    if i + 2 < NS:
                        st_S(i + 2)
                    st_P(i)
                p.op("dve", lambda e: e.scalar_tensor_tensor(out=diff[:, :, :], in0=on[1][:, :, :], scalar=lsm[:, 5:6], in1=on[0][:, :, :], op0=ALU.mult, op1=ALU.add),
                     reads=[on[0], on[1], lsm], writes=[diff])
                p.op("pool", lambda e: e.tensor_tensor(out=sq[:, :, :], in0=diff[:, :, :], in1=diff[:, :, :], op=ALU.mult), reads=[diff], writes=[sq])
                p.op("dve", lambda e: e.tensor_reduce(out=ss[:, :], in_=sq[:, :, :], axis=AX.X, op=ALU.add), reads=[sq], writes=[ss])
                self.rsqrt(rstd, rstd[:, :], ss, ss[:, :], 1.0 / 128.0)
                for j in range(4):
                    p.op("dve", lambda e, j=j: e.scalar_tensor_tensor(out=oo[:, j, :], in0=diff[:, j, :], scalar=rstd[:, j:j + 1], in1=gA[:, :], op0=ALU.mult, op1=ALU.mult),
                         reads=[diff, rstd, gA], writes=[oo])
                ps = self.bank(0, 4)
                for j in range(4):
                    p.op("pe", lambda e, ps=ps, j=j: e.transpose(ps[:, j * 128:(j + 1) * 128], oo[:, j, :], self.identf[:, :]), reads=[oo, self.identf], writes=[ps])
                p.op("act", lambda e, ps=ps, Q=Q, oa=oa: e.copy(out=oa[:, Q * 512:(Q + 1) * 512], in_=ps[:, :]), reads=[ps], writes=[oa])
            p.dma("pool", self.oT[0, h, :, :], oa[:, :], reads=[oa], writes=[self.oT.bs[h]])
        p.barrier()
        st.close()

    def phase_merge1(self, l):
        for dh in range(2):
            self._merge1_dh(l, dh)

    def _merge1_dh(self, l, dh):
        p = self.p
        W = self.W
        xT = self.xT
        if True:
            st = ExitStack()
            stg = [p.sb(f"m_stg{i}", [128, 2048], F32, st) for i in range(2)]
            wg = p.sb("m_wg", [128, 8, 1536], BF16, st, nb=3)
            wup = p.sb("m_wup", [128, 12, 512], BF16, st, nb=3)
            mb = p.sb("m_mb", [128, 3, 512], F32, st)
            ot = [p.sb(f"m_ot{i}", [128, 12, 512], BF16, st) for i in range(2)]
            sg = [p.sb(f"m_sg{i}", [128, 512], F32, st) for i in range(2)]
            acc = [p.sb(f"m_acc{i}", [128, 512], F32, st) for i in range(2)]
            tmp = [p.sb(f"m_tmp{i}", [128, 512], F32, st) for i in range(2)]
            mtb = [p.sb(f"m_mtb{i}", [128, 4, 512], BF16, st) for i in range(2)]
            d0 = dh * 512
            for n in range(3):
                c0 = COL["gate"] + n * 1024 + d0
                self.wload(stg, wg, wg[:, :, n * 512:(n + 1) * 512], W["w_in"][l, :, c0:c0 + 512], 8, 512, wbuf=wg.bs[n])
                self.wload(stg, wup, wup[:, n * 4:(n + 1) * 4, :], W["merge_w_up"][l, n, :, d0:d0 + 512], 4, 512, wbuf=wup.bs[n])
            p.dma("sp", mb[:], W["merge_b"][l:l + 1, :, d0:d0 + 512].partition_broadcast(128), writes=[mb])
            k = 0
            for tb in range(8):
                o = ot[tb % 2]
                p.dma("sp", o[:], self.oT[:, :, :, tb * 512:(tb + 1) * 512].rearrange("n c p t -> p (n c) t"), reads=self.oT.bs, writes=[o])
                mt = mtb[tb % 2]
                for tt in range(4):
                    t = tb * 4 + tt
                    a = acc[k % 2]
                    for n in range(3):
                        g = sg[(k * 3 + n) % 2]
                        psg = self.bank(0, 3)
                        for c in range(8):
                            self.mm(psg[:, :], xT[:, c, t * 128:(t + 1) * 128], wg[:, c, n * 512:(n + 1) * 512], c == 0, c == 7, [xT.bs[t], wg.bs[n]], [psg])
                        p.op("dve", lambda e, g=g, psg=psg, n=n, mb=mb: e.tensor_tensor(out=g[:, :], in0=psg[:, :], in1=mb[:, n, :], op=ALU.add), reads=[psg, mb], writes=[g])
                        p.op("act", lambda e, g=g: e.activation(out=g[:, :], in_=g[:, :], func=AF.Sigmoid), reads=[g], writes=[g])
                        psu = self.bank(3, 6)
                        for c in range(4):
                            self.mm(psu[:, :], o[:, n * 4 + c, tt * 128:(tt + 1) * 128], wup[:, n * 4 + c, :], c == 0, c == 3, [o, wup.bs[n]], [psu])
                        if n == 0:
                            p.op("dve", lambda e, a=a, g=g, psu=psu: e.tensor_tensor(out=a[:, :], in0=g[:, :], in1=psu[:, :], op=ALU.mult), reads=[g, psu], writes=[a])
                        else:
                            tm = tmp[n % 2]
                            p.op("dve", lambda e, tm=tm, g=g, psu=psu: e.tensor_tensor(out=tm[:, :], in0=g[:, :], in1=psu[:, :], op=ALU.mult), reads=[g, psu], writes=[tm])
                            p.op("pool", lambda e, tm=tm, a=a: e.tensor_tensor(out=a[:, :], in0=a[:, :], in1=tm[:, :], op=ALU.add), reads=[a, tm], writes=[a])
                    pst = self.bank(6, 8)
                    for j in range(4):
                        p.op("pe", lambda e, pst=pst, j=j, a=a: e.transpose(pst[:, j * 128:(j + 1) * 128], a[:, j * 128:(j + 1) * 128], self.identf[:, :]), reads=[a, self.identf], writes=[pst])
                    p.op("act", lambda e, pst=pst, mt=mt, tt=tt: e.copy(out=mt[:, :, tt * 128:(tt + 1) * 128], in_=pst[:, :].rearrange("p (j n) -> p j n", j=4)), reads=[pst], writes=[mt])
                    k += 1
                p.dma("pool", self.mT[:, dh * 4:(dh + 1) * 4, tb * 512:(tb + 1) * 512], mt[:, :, :], reads=[mt], writes=[self.mT.bs[dh * 8 + tb]])
            p.barrier()
            st.close()

    def phase_merge2(self, l):
        p = self.p
        W = self.W
        st = ExitStack()
        stg = [p.sb(f"n_stg{i}", [128, 2048], F32, st) for i in range(2)]
        wo = p.sb("n_wo", [128, 8, 1024], BF16, st)
        rw = p.sb("n_rw", [128, 8, 16], F32, st)
        rb = p.sb("n_rb", [128, 16], F32, st)
        mtl = [p.sb(f"n_mt{i}", [128, 8, 512], BF16, st) for i in range(2)]
        xr = [p.sb(f"n_xr{i}", [128, D], F32, st) for i in range(2)]
        z = [p.sb(f"n_z{i}", [128, D], F32, st) for i in range(2)]
        xT32 = p.sb("n_xT32", [128, 8, 128], F32, st)
        stats = p.sb("n_stats", [128, 2, 6], F32, st)
        mv = p.sb("n_mv", [128, 2], F32, st)
        rstd = p.sb("n_rstd", [128, 1], F32, st)
        R = {k: p.sb("n_r_" + k, shp, F32, st) for k, shp in dict(sc=[128, 16], bi=[128, 16], m1=[128, 4], eq=[128, 16], t2=[128, 16], m2=[128, 4],
                                                                  gs=[128, 4], gm=[128, 1], gsel=[128, 4], ge=[128, 16], w=[128, 16], ws=[128, 1]).items()}
        for h2 in range(2):
            self.wload(stg, wo, wo[:, :, h2 * 512:(h2 + 1) * 512], W["w_out"][l, :, h2 * 512:(h2 + 1) * 512], 8, 512)
        p.dma("sp", rw[:], W["router_w"].t.rearrange("(c p) n -> p c n", p=128), writes=[rw])
        p.dma("sp", rb[:], W["router_bias"].t.rearrange("(o n) -> o n", o=1).partition_broadcast(128), writes=[rb])
        self.load_ln_params(W["ln1_g"][l:l + 1, :], W["ln1_b"][l:l + 1, :], st)
        for tb in range(8):
            mt = mtl[tb % 2]
            p.dma("sp", mt[:], self.mT[:, :, tb * 512:(tb + 1) * 512], reads=[self.mT.bs[tb], self.mT.bs[8 + tb]], writes=[mt])
            for tt in range(4):
                t = tb * 4 + tt
                x_ = xr[t % 2]
                z_ = z[t % 2]
                p.dma("sp", x_[:], self.xres[t * 128:(t + 1) * 128, :], reads=[self.xres.bs[t]], writes=[x_])
                for h2 in range(2):
                    ps = self.bank(0, 4)
                    for c in range(8):
                        self.mm(ps[:, :], mt[:, c, tt * 128:(tt + 1) * 128], wo[:, c, h2 * 512:(h2 + 1) * 512], c == 0, c == 7, [mt, wo], [ps])
                    p.op("dve", lambda e, ps=ps, x_=x_, z_=z_, h2=h2: e.scalar_tensor_tensor(out=z_[:, h2 * 512:(h2 + 1) * 512], in0=x_[:, h2 * 512:(h2 + 1) * 512], scalar=ALPHA,
                                                                                    in1=ps[:, :], op0=ALU.mult, op1=ALU.add), reads=[ps, x_], writes=[z_])
                self.ln_tile(z_, z_, stats, mv, rstd)
                p.dma("pool", self.xres[t * 128:(t + 1) * 128, :], z_[:], reads=[z_], writes=[self.xres.bs[t]])
                self.to_xT(z_, t, xT32=xT32)
                self.router(t, xT32, rw, rb, R)
        p.barrier()
        st.close()

    def router(self, t, xT32, rw, rb, R):
        p = self.p
        ps = self.bank(4, 8)
        for c in range(8):
            self.mm(ps[:, 0:16], xT32[:, c, :], rw[:, c, :], c == 0, c == 7, [xT32, rw], [ps])
        sc, bi, m1, eq, t2, m2, gs, gm, gsel, ge, w, ws = (R[k] for k in ("sc", "bi", "m1", "eq", "t2", "m2", "gs", "gm", "gsel", "ge", "w", "ws"))
        v3 = lambda tl: tl[:, :].rearrange("p (g e) -> p g e", g=4)
        b3 = lambda tl: tl[:, :].unsqueeze(2).to_broadcast([128, 4, 4])
        p.op("act", lambda e: e.activation(out=sc[:, :], in_=ps[:, 0:16], func=AF.Sigmoid), reads=[ps], writes=[sc])
        p.op("dve", lambda e: e.tensor_tensor(out=bi[:, :], in0=sc[:, :], in1=rb[:, :], op=ALU.add), reads=[sc, rb], writes=[bi])
        p.op("dve", lambda e: e.tensor_reduce(out=m1[:, :], in_=v3(bi), axis=AX.X, op=ALU.max), reads=[bi], writes=[m1])
        p.op("dve", lambda e: e.tensor_tensor(out=v3(eq), in0=v3(bi), in1=b3(m1), op=ALU.is_equal), reads=[bi, m1], writes=[eq])
        p.op("dve", lambda e: e.scalar_tensor_tensor(out=t2[:, :], in0=eq[:, :], scalar=-1e30, in1=bi[:, :], op0=ALU.mult, op1=ALU.add), reads=[eq, bi], writes=[t2])
        p.op("dve", lambda e: e.tensor_reduce(out=m2[:, :], in_=v3(t2), axis=AX.X, op=ALU.max), reads=[t2], writes=[m2])
        p.op("dve", lambda e: e.tensor_tensor(out=gs[:, :], in0=m1[:, :], in1=m2[:, :], op=ALU.add), reads=[m1, m2], writes=[gs])
        p.op("dve", lambda e: e.tensor_reduce(out=gm[:, :], in_=gs[:, :], axis=AX.X, op=ALU.max), reads=[gs], writes=[gm])
        p.op("dve", lambda e: e.tensor_scalar(gsel[:, :], gs[:, :], gm[:, 0:1], None, ALU.is_equal), reads=[gs, gm], writes=[gsel])
        p.op("dve", lambda e: e.tensor_tensor(out=v3(ge), in0=v3(bi), in1=b3(m2), op=ALU.is_ge), reads=[bi, m2], writes=[ge])
        p.op("dve", lambda e: e.tensor_tensor(out=v3(ge), in0=v3(ge), in1=b3(gsel), op=ALU.mult), reads=[ge, gsel], writes=[ge])
        p.op("dve", lambda e: e.tensor_tensor(out=w[:, :], in0=ge[:, :], in1=sc[:, :], op=ALU.mult), reads=[ge, sc], writes=[w])
        p.op("dve", lambda e: e.tensor_reduce(out=ws[:, :], in_=w[:, :], axis=AX.X, op=ALU.add), reads=[w], writes=[ws])
        p.op("dve", lambda e: e.reciprocal(out=ws[:, :], in_=ws[:, :]), reads=[ws], writes=[ws])
        p.op("dve", lambda e: e.tensor_scalar(self.call[:, t, :], w[:, :], ws[:, 0:1], None, ALU.mult), reads=[w, ws], writes=[self.call.bs[t]])

    def phase_moe(self, l, last):
        p = self.p
        W = self.W
        xT = self.xT
        st = ExitStack()
        stg = [p.sb(f"e_stg{i}", [128, 2048], F32, st) for i in range(2)]
        wg = [p.sb(f"e_wg{i}", [128, 8, 512], BF16, st) for i in range(2)]
        wu = [p.sb(f"e_wu{i}", [128, 8, 512], BF16, st) for i in range(2)]
        wd = [p.sb(f"e_wd{i}", [128, 4, 1024], BF16, st) for i in range(2)]
        yacc = p.sb("e_yacc", [128, 8, D], F32, st, nb=8)
        hT = [p.sb(f"e_hT{i}", [128, 4, 512], BF16, st) for i in range(2)]
        sgl = [p.sb(f"e_sg{i}", [128, 512], F32, st) for i in range(2)]
        xr = [p.sb(f"e_xr{i}", [128, D], F32, st) for i in range(1)]
        stats = p.sb("e_stats", [128, 2, 6], F32, st)
        mv = p.sb("e_mv", [128, 2], F32, st)
        rstd = p.sb("e_rstd", [128, 1], F32, st)
        self.load_ln_params(W["ln2_g"][l:l + 1, :], W["ln2_b"][l:l + 1, :], st)
        cast_i = 0
        k = 0
        for q4 in range(4):
            for ex in range(16):
                g_, u_, d_ = wg[ex % 2], wu[ex % 2], wd[ex % 2]
                for h2 in range(2):
                    self.wload(stg, g_, g_[:, h2 * 4:(h2 + 1) * 4, :], W["moe_w_gate"][l, ex, h2 * 512:(h2 + 1) * 512, :], 4, 512, cast=("pool", "dve")[h2])
                    self.wload(stg, u_, u_[:, h2 * 4:(h2 + 1) * 4, :], W["moe_w_up"][l, ex, h2 * 512:(h2 + 1) * 512, :], 4, 512, cast=("pool", "dve")[h2])
                    self.wload(stg, d_, d_[:, :, h2 * 512:(h2 + 1) * 512], W["moe_w_down"][l, ex, :, h2 * 512:(h2 + 1) * 512], 4, 512, cast=("pool", "dve")[h2])
                for tb2 in range(2):
                    tb = q4 * 2 + tb2
                    h_ = hT[k % 2]
                    k += 1
                    xr_ = [xT.bs[tb * 4 + i] for i in range(4)]
                    for fc in range(4):
                        pg = self.bank(0, 2)
                        for c in range(8):
                            self.mm(pg[:, :], g_[:, c, fc * 128:(fc + 1) * 128], xT[:, c, tb * 512:(tb + 1) * 512], c == 0, c == 7, [g_] + xr_, [pg])
                        pu = self.bank(2, 4)
                        for c in range(8):
                            self.mm(pu[:, :], u_[:, c, fc * 128:(fc + 1) * 128], xT[:, c, tb * 512:(tb + 1) * 512], c == 0, c == 7, [u_] + xr_, [pu])
                        s_ = sgl[fc % 2]
                        p.op("act", lambda e, s_=s_, pg=pg: e.activation(out=s_[:, :], in_=pg[:, :], func=AF.Silu), reads=[pg], writes=[s_])
                        p.op("dve", lambda e, s_=s_, pu=pu, h_=h_, fc=fc: e.tensor_tensor(out=h_[:, fc, :], in0=s_[:, :], in1=pu[:, :], op=ALU.mult), reads=[s_, pu], writes=[h_])
                    for tt in range(4):
                        t = tb * 4 + tt
                        tl = tb2 * 4 + tt
                        for h2 in range(2):
                            py = self.bank(4, 8)
                            for fc in range(4):
                                self.mm(py[:, :], h_[:, fc, tt * 128:(tt + 1) * 128], d_[:, fc, h2 * 512:(h2 + 1) * 512], fc == 0, fc == 3, [h_, d_], [py])
                            ya = yacc[:, tl, h2 * 512:(h2 + 1) * 512]
                            if ex == 0:
                                p.op("dve", lambda e, ya=ya, py=py, t=t, ex=ex: e.tensor_scalar(ya, py[:, :], self.call[:, t, ex:ex + 1], None, ALU.mult),
                                     reads=[py, self.call.bs[t]], writes=[yacc.bs[tl]])
                            else:
                                p.op("dve", lambda e, ya=ya, py=py, t=t, ex=ex: e.scalar_tensor_tensor(out=ya, in0=py[:, :], scalar=self.call[:, t, ex:ex + 1], in1=ya, op0=ALU.mult, op1=ALU.add),
                                     reads=[py, self.call.bs[t]], writes=[yacc.bs[tl]])
            for tl in range(8):
                t = q4 * 8 + tl
                x_ = xr[0]
                yv = Sub(yacc[:, tl, :], yacc.bs[tl])
                p.dma("sp", x_[:], self.xres[t * 128:(t + 1) * 128, :], reads=[self.xres.bs[t]], writes=[x_])
                p.op("dve", lambda e, x_=x_, tl=tl: e.scalar_tensor_tensor(out=yacc[:, tl, :], in0=x_[:, :], scalar=ALPHA, in1=yacc[:, tl, :], op0=ALU.mult, op1=ALU.add),
                     reads=[x_, yacc.bs[tl]], writes=[yacc.bs[tl]])
                self.ln_tile(yv, yv, stats, mv, rstd)
                if last:
                    p.dma("pool", self.out[t * 128:(t + 1) * 128, :], yv[:, :], reads=[yv], writes=[self.out.bs[t]])
                else:
                    p.dma("pool", self.xres[t * 128:(t + 1) * 128, :], yv[:, :], reads=[yv], writes=[self.xres.bs[t]])
                    self.to_xT(yv, t)
        p.barrier()
        st.close()

    def phase_gla(self, l):
        for hp in range(2):
            self._gla_hp(l, hp)

    def _gla_hp(self, l, hp):
        p = self.p
        W = self.W
        xT = self.xT
        xTr = list(xT.bs)
        if True:
            st = ExitStack()
            stg = [p.sb(f"g_stg{i}", [128, 1024], F32, st) for i in range(2)]
            wq = p.sb("g_wq", [128, 8, 128], BF16, st)
            wk = p.sb("g_wk", [128, 8, 128], BF16, st)
            wv = p.sb("g_wv", [128, 8, 256], BF16, st)
            wr = p.sb("g_wr", [128, 8, 256], BF16, st)
            wz = p.sb("g_wz", [128, 8, 32], BF16, st)
            wgf = p.sb("g_wgf", [16, 2, 128], F32, st)
            wgt = p.sb("g_wgt", [16, 2, 128], BF16, st)
            bg = p.sb("g_bg", [128, 2], F32, st)
            gG = p.sb("g_gG", [128, 128], F32, st)
            ones = p.sb("g_ones", [128, 1], F32, st)
            m4 = p.sb("g_m4", [128, 4, 64], F32, st)
            msk = p.sb("g_msk", [128, 8, 64], F32, st)
            qA1 = p.sb("g_qA1", [128, S], BF16, st)
            kA1 = p.sb("g_kA1", [128, S], BF16, st)
            qi1 = p.sb("g_qi1", [128, S], BF16, st)
            qA0 = [p.sb(f"g_qA0{i}", [128, 512], BF16, st) for i in range(2)]
            kA0 = [p.sb(f"g_kA0{i}", [128, 512], BF16, st) for i in range(2)]
            qi0 = [p.sb(f"g_qi0{i}", [128, 512], BF16, st) for i in range(2)]
            dec = [p.sb(f"g_dec{d}", [128, 64], F32, st) for d in range(2)]
            sts1 = p.sb("g_st1", [128, 64, 128], BF16, st)
            sts0 = [p.sb(f"g_st0{i}", [128, 8, 128], BF16, st) for i in range(2)]
            S32 = [p.sb(f"g_S32{d}", [128, 128], F32, st) for d in range(2)]
            v = p.sb("g_v", [128, NT, 256], BF16, st)
            zt = p.sb("g_zt", [16, 512], BF16, st)
            T1 = p.sb("g_T1", [128, 512], F32, st)
            T2 = p.sb("g_T2", [128, 512], F32, st)
            T3 = p.sb("g_T3", [128, 512], F32, st)
            T4 = p.sb("g_T4", [128, 512], F32, st)
            T5 = p.sb("g_T5", [128, 512], F32, st)
            klt = p.sb("g_klt", [128, 4, 128], BF16, st)
            scT = [p.sb(f"g_scT{i}", [128, 4, 64], BF16, st) for i in range(2)]
            sr = p.sb("g_sr", [128, 256], F32, st)
            sq = p.sb("g_sq", [128, 2, 128], F32, st)
            ssq = p.sb("g_ssq", [128, 2], F32, st)
            oc = p.sb("g_oc", [128, 256], F32, st)
            ocT = [p.sb(f"g_ocT{i}", [128, 2, 512], BF16, st) for i in range(2)]
            c0 = COL["qc"] + hp * 128
            self.wload(stg, wq, wq[:, :, :], W["w_in"][l, :, c0:c0 + 128], 8, 128)
            p.op("pool", lambda e: e.tensor_scalar(wq[:, :, :], wq[:, :, :], 0.125, None, ALU.mult), reads=[wq], writes=[wq])
            c0 = COL["kc"] + hp * 128
            self.wload(stg, wk, wk[:, :, :], W["w_in"][l, :, c0:c0 + 128], 8, 128)
            for k2 in range(2):
                c0 = COL["vc"] + hp * 256 + k2 * 128
                self.wload(stg, wv, wv[:, :, k2 * 128:(k2 + 1) * 128], W["w_in"][l, :, c0:c0 + 128], 8, 128)
                c0 = COL["rc"] + hp * 256 + k2 * 128
                self.wload(stg, wr, wr[:, :, k2 * 128:(k2 + 1) * 128], W["w_in"][l, :, c0:c0 + 128], 8, 128)
            self.wload(stg, wz, wz[:, :, :], W["w_in"][l, :, COL["zf"]:COL["zf"] + 32], 8, 32)
            p.dma("sp", wgf[:], W["gla_w_gate"][l, :, :, hp * 128:(hp + 1) * 128].rearrange("d r n -> r d n"), writes=[wgf])
            p.op("dve", lambda e: e.tensor_copy(out=wgt[:, :, :], in_=wgf[:, :, :]), reads=[wgf], writes=[wgt])
            p.dma("sp", bg[:], W["gla_b_gate"][l, :, hp * 128:(hp + 1) * 128].rearrange("d n -> n d"), writes=[bg], allow_slow_non_contiguous=True)
            p.dma("sp", gG[:], W["gla_norm_g"][l:l + 1, :].partition_broadcast(128), writes=[gG])
            p.op("pool", lambda e: e.memset(ones[:, :], 1.0), writes=[ones])
            for d in range(2):
                for hh in range(2):
                    for half in range(2):
                        p.dma("sp", m4[half * 64:(half + 1) * 64, d * 2 + hh, :], self.C["gmask"][d, :, :], writes=[m4])
            p.op("pool", lambda e: e.memset(msk[:, :, :], 1.0), writes=[msk])
            p.op("pool", lambda e: e.memset(msk[:, :, 0:1], 0.0), reads=[msk], writes=[msk])
            for t in range(NT):
                ps = self.bank(0, 4)
                for c in range(8):
                    self.mm(ps[:, 0:256], xT[:, c, t * 128:(t + 1) * 128], wv[:, c, :], c == 0, c == 7, [wv, xTr[t]], [ps])
                p.op("act", lambda e, ps=ps, t=t: e.copy(out=v[:, t, :], in_=ps[:, 0:256]), reads=[ps], writes=[v])
            def arr(d, tb):
                if d == 1:
                    sl = slice(tb * 512, (tb + 1) * 512)
                    return (qA1, qA1[:, sl]), (kA1, kA1[:, sl]), (qi1, qi1[:, sl])
                i = tb % 2
                return (qA0[i], qA0[i][:, :]), (kA0[i], kA0[i][:, :]), (qi0[i], qi0[i][:, :])

            def chunk_view(d, which, n, hh):
                hs = slice(hh * 64, (hh + 1) * 64)
                if d == 1:
                    tl = (qA1, kA1, qi1)[which]
                    return tl, tl[hs, n * 64:(n + 1) * 64]
                tl = (qA0, kA0, qi0)[which][(n // 8) % 2]
                return tl, tl[hs, (n % 8) * 64:(n % 8 + 1) * 64]

            def state_view(d, n, hh):
                hs = slice(hh * 64, (hh + 1) * 64)
                if d == 1:
                    return sts1, sts1[hs, n, :]
                tl = sts0[(n // 8) % 2]
                return tl, tl[hs, n % 8, :]

            def sweep_block(d, tb):
                xr_ = xTr[tb * 4:tb * 4 + 4]
                tsl = slice(tb * 512, (tb + 1) * 512)
                (qA_t, qA_ap), (kA_t, kA_ap), (qi_t, qi_ap) = arr(d, tb)
                pz = self.bank(0, 4)
                for c in range(8):
                    self.mm(pz[0:16, :], wz[:, c, d * 16:(d + 1) * 16], xT[:, c, tsl], c == 0, c == 7, [wz] + xr_, [pz])
                p.op("act", lambda e: e.copy(out=zt[:, :], in_=pz[0:16, :]), reads=[pz], writes=[zt])
                pg = self.bank(0, 4)
                self.mm(pg[:, :], wgt[0:16, d, :], zt[0:16, :], True, True, [wgt, zt], [pg])
                pq = self.bank(4, 6)
                for c in range(8):
                    self.mm(pq[:, :], wq[:, c, :], xT[:, c, tsl], c == 0, c == 7, [wq] + xr_, [pq])
                pk = self.bank(6, 8)
                for c in range(8):
                    self.mm(pk[:, :], wk[:, c, :], xT[:, c, tsl], c == 0, c == 7, [wk] + xr_, [pk])
                p.op("dve", lambda e: e.tensor_scalar(T1[:, :], pg[:, :], bg[:, d:d + 1], None, ALU.add), reads=[pg, bg], writes=[T1])
                p.op("dve", lambda e: e.scalar_tensor_tensor(out=T2[:, :], in0=T1[:, :], scalar=-1.0, in1=T1[:, :], op0=ALU.mult, op1=ALU.max), reads=[T1], writes=[T2])
                p.op("act", lambda e: e.activation(out=T2[:, :], in_=T2[:, :], func=AF.Exp, scale=-1.0), reads=[T2], writes=[T2])
                p.op("act", lambda e: e.activation(out=T2[:, :], in_=T2[:, :], func=AF.Ln, bias=ones[:, 0:1]), reads=[T2, ones], writes=[T2])
                p.op("dve", lambda e: e.scalar_tensor_tensor(out=T1[:, :], in0=T1[:, :], scalar=0.0, in1=T2[:, :], op0=ALU.min, op1=ALU.subtract), reads=[T1, T2], writes=[T1])
                p.op("pool", lambda e: e.tensor_scalar(T1[:, :], T1[:, :], 1.0 / 16.0, None, ALU.mult), reads=[T1], writes=[T1])
                p.op("dve", lambda e: e.tensor_tensor_scan(T3[:, :], msk[:, :, :].rearrange("p a b -> p (a b)"), T1[:, :], 0.0, ALU.mult, ALU.add), reads=[msk, T1], writes=[T3])
                c3 = T3[:, :].rearrange("p (a b) -> p a b", a=8)
                v4 = lambda tl: tl[:, :].rearrange("p (a b) -> p a b", a=8)
                if d == 1:
                    p.op("dve", lambda e: e.tensor_tensor(out=v4(T2), in0=c3[:, :, 63:64].to_broadcast([128, 8, 64]), in1=c3, op=ALU.subtract), reads=[T3], writes=[T2])
                    p.op("dve", lambda e: e.tensor_tensor(out=T3[:, :], in0=T2[:, :], in1=T1[:, :], op=ALU.add), reads=[T2, T1], writes=[T3])
                    ref, last = c3[:, :, 31:32], c3[:, :, 0:1]
                else:
                    ref, last = c3[:, :, 32:33], c3[:, :, 63:64]
                refb = ref.to_broadcast([128, 8, 64])
                lastb = last.to_broadcast([128, 8, 64])
                p.op("act", lambda e: e.activation(out=dec[d][:, tb * 8:(tb + 1) * 8].unsqueeze(2), in_=last, func=AF.Exp), reads=[T3], writes=[dec[d]])
                p.op("dve", lambda e: e.tensor_tensor(out=v4(T4), in0=c3, in1=refb, op=ALU.subtract), reads=[T3], writes=[T4])
                p.op("act", lambda e: e.activation(out=T5[:, :], in_=T4[:, :], func=AF.Exp), reads=[T4], writes=[T5])
                p.op("dve", lambda e: e.tensor_tensor(out=qA_ap, in0=pq[:, :], in1=T5[:, :], op=ALU.mult), reads=[pq, T5], writes=[qA_t])
                p.op("act", lambda e: e.activation(out=T5[:, :], in_=T4[:, :], func=AF.Exp, scale=-1.0), reads=[T4], writes=[T5])
                p.op("dve", lambda e: e.tensor_tensor(out=kA_ap, in0=pk[:, :], in1=T5[:, :], op=ALU.mult), reads=[pk, T5], writes=[kA_t])
                p.op("act", lambda e: e.activation(out=T5[:, :], in_=T3[:, :], func=AF.Exp), reads=[T3], writes=[T5])
                p.op("dve", lambda e: e.tensor_tensor(out=qi_ap, in0=pq[:, :], in1=T5[:, :], op=ALU.mult), reads=[pq, T5], writes=[qi_t])
                p.op("dve", lambda e: e.tensor_tensor(out=v4(T4), in0=lastb, in1=c3, op=ALU.subtract), reads=[T3], writes=[T4])
                p.op("act", lambda e: e.activation(out=T5[:, :], in_=T4[:, :], func=AF.Exp), reads=[T4], writes=[T5])
                p.op("dve", lambda e: e.tensor_tensor(out=T4[:, :], in0=pk[:, :], in1=T5[:, :], op=ALU.mult), reads=[pk, T5], writes=[T4])
                pt = self.bank(0, 4)
                for j in range(4):
                    p.op("pe", lambda e, j=j: e.transpose(pt[:, j * 128:(j + 1) * 128], T4[:, j * 128:(j + 1) * 128], self.identf[:, :]), reads=[T4, self.identf], writes=[pt])
                p.op("act", lambda e: e.copy(out=klt[:, :, :], in_=pt[:, :].rearrange("p (j n) -> p j n", j=4)), reads=[pt], writes=[klt])
                cs = range(8) if d == 0 else range(7, -1, -1)
                for ci in cs:
                    n = tb * 8 + ci
                    tt, half = ci // 2, ci % 2
                    t = tb * 4 + tt
                    stl, _ = state_view(d, n, 0)
                    sap = sts1[:, n, :] if d == 1 else stl[:, n % 8, :]
                    p.op("act", lambda e, sap=sap: e.copy(out=sap, in_=S32[d][:, :]), reads=[S32[d]], writes=[stl])
                    pd = self.bank(0, 4)
                    for hh in range(2):
                        self.mm(pd[hh * 64:(hh + 1) * 64, 0:128], klt[half * 64:(half + 1) * 64, tt, hh * 64:(hh + 1) * 64],
                                v[half * 64:(half + 1) * 64, t, hh * 128:(hh + 1) * 128], True, True, [klt, v], [pd], serial=True)
                    p.op("dve", lambda e, pd=pd, n=n: e.scalar_tensor_tensor(out=S32[d][:, :], in0=S32[d][:, :], scalar=dec[d][:, n:n + 1], in1=pd[:, 0:128], op0=ALU.mult, op1=ALU.add),
                         reads=[pd, dec[d], S32[d]], writes=[S32[d]])

            def out_block(tb):
                ot_ = ocT[tb % 2]
                for tt in range(4):
                    t = tb * 4 + tt
                    pss = self.bank(0, 2)
                    for half in range(2):
                        n = t * 2 + half
                        first = True
                        for d in range(2):
                            for hh in range(2):
                                kt_, kap = chunk_view(d, 1, n, hh)
                                qt_, qap = chunk_view(d, 0, n, hh)
                                self.mm(pss[half * 64:(half + 1) * 64, (d * 2 + hh) * 64:(d * 2 + hh + 1) * 64], kap, qap, first, True, [kt_, qt_], [pss], serial=True)
                                first = False
                    sc_ = scT[t % 2]
                    p.op("dve", lambda e, pss=pss, sc_=sc_: e.tensor_tensor(out=sc_[:, :, :], in0=pss[:, 0:256].rearrange("p (a b) -> p a b", a=4), in1=m4[:, :, :], op=ALU.mult), reads=[pss, m4], writes=[sc_])
                    po = self.bank(2, 4)
                    for half in range(2):
                        n = t * 2 + half
                        hs = slice(half * 64, (half + 1) * 64)
                        first = True
                        for hh in range(2):
                            osl = po[hs, hh * 128:(hh + 1) * 128]
                            for d in range(2):
                                self.mm(osl, sc_[hs, d * 2 + hh, :], v[hs, t, hh * 128:(hh + 1) * 128], first, False, [sc_, v], [po], serial=True)
                                first = False
                            for d in range(2):
                                it_, iap = chunk_view(d, 2, n, hh)
                                st_, sap = state_view(d, n, hh)
                                self.mm(osl, iap, sap, False, d == 1, [it_, st_], [po], serial=True)
                    pr = self.bank(4, 8)
                    for c in range(8):
                        self.mm(pr[:, 0:256], xT[:, c, t * 128:(t + 1) * 128], wr[:, c, :], c == 0, c == 7, [wr, xTr[t]], [pr])
                    p.op("act", lambda e, pr=pr: e.activation(out=sr[:, :], in_=pr[:, 0:256], func=AF.Silu), reads=[pr], writes=[sr])
                    po3 = po[:, 0:256].rearrange("p (a b) -> p a b", a=2)
                    p.op("act", lambda e, po3=po3: e.activation(out=sq[:, :, :], in_=po3, func=AF.Square), reads=[po], writes=[sq])
                    p.op("dve", lambda e: e.tensor_reduce(out=ssq[:, :], in_=sq[:, :, :], axis=AX.X, op=ALU.add), reads=[sq], writes=[ssq])
                    self.rsqrt(ssq, ssq[:, :], ssq, ssq[:, :], 1.0 / 128.0)
                    for hh in range(2):
                        p.op("dve", lambda e, po=po, hh=hh: e.scalar_tensor_tensor(out=oc[:, hh * 128:(hh + 1) * 128], in0=po[:, hh * 128:(hh + 1) * 128], scalar=ssq[:, hh:hh + 1], in1=gG[:, :],
                                                                                  op0=ALU.mult, op1=ALU.mult), reads=[po, ssq, gG], writes=[oc])
                    p.op("pool", lambda e: e.tensor_tensor(out=oc[:, :], in0=oc[:, :], in1=sr[:, :], op=ALU.mult), reads=[oc, sr], writes=[oc])
                    pt = self.bank(4, 8)
                    for hh in range(2):
                        p.op("pe", lambda e, pt=pt, hh=hh: e.transpose(pt[:, hh * 128:(hh + 1) * 128], oc[:, hh * 128:(hh + 1) * 128], self.identf[:, :]), reads=[oc, self.identf], writes=[pt])
                    p.op("act", lambda e, pt=pt, tt=tt: e.copy(out=ot_[:, :, tt * 128:(tt + 1) * 128], in_=pt[:, 0:256].rearrange("p (a b) -> p a b", a=2)), reads=[pt], writes=[ot_])
                p.dma("pool", self.oT[2, hp * 2:(hp + 1) * 2, :, tb * 512:(tb + 1) * 512].rearrange("c p t -> p c t"), ot_[:, :, :], reads=[ot_], writes=[self.oT.bs[8 + hp * 2], self.oT.bs[8 + hp * 2 + 1]])

            for d in (1, 0):
                p.op("pool", lambda e, d=d: e.memset(S32[d][:, :], 0.0), writes=[S32[d]])
            for tb in range(7, -1, -1):
                sweep_block(1, tb)
            for tb in range(8):
                sweep_block(0, tb)
                out_block(tb)
            p.barrier()
            st.close()

    def phase_s5(self, l):
        p = self.p
        W = self.W
        xT = self.xT
        xTr = list(xT.bs)
        st = ExitStack()
        sm = lambda n, shp=(128, 32): p.sb("s_" + n, list(shp), F32, st)
        stg = [p.sb(f"s_stg{i}", [128, 1024], F32, st) for i in range(2)]
        identb = p.sb("s_identb", [128, 128], BF16, st)
        jx = sm("jx", (128, 128))
        rowm = sm("rowm", (128, 8))
        sgn = sm("sgn", (128, 2))
        hpi = sm("hpi", (128, 1))
        wu = p.sb("s_wu", [128, 8, 128], BF16, st)
        wglu = p.sb("s_wglu", [128, 4, 512], BF16, st)
        bglu = sm("bglu", (128, 4))
        dcol = sm("dcol", (128, 4))
        CST = [[p.sb(f"s_cst{d}{b}", [128, 128], F32, st) for b in range(4)] for d in range(2)]
        BT = [[p.sb(f"s_bt{d}{b}", [128, 128], F32, st) for b in range(4)] for d in range(2)]
        PRt = [sm(f"pr{d}", (128, 12, 32)) for d in range(2)]
        PQt = [sm(f"pq{d}", (128, 12, 32)) for d in range(2)]
        pst = ExitStack()
        smp = lambda n, shp=(128, 32): p.sb("s_" + n, list(shp), F32, pst)
        p.dma("sp", jx[:], self.C["jx"][:, :], writes=[jx])
        p.dma("sp", rowm[:], self.C["rowmask"][:, :], writes=[rowm])
        p.dma("sp", sgn[:], self.C["sgn"][:, :], writes=[sgn])
        p.op("dve", lambda e: e.tensor_copy(out=identb[:, :], in_=self.identf[:, :]), reads=[self.identf], writes=[identb])
        p.op("pool", lambda e: e.memset(hpi[:, :], PI / 2), writes=[hpi])
        for b in range(4):
            self.wload(stg, wglu, wglu[:, b:b + 1, :], W["s5_w_glu"][l, b * 128:(b + 1) * 128, :], 1, 512)
        p.dma("sp", bglu[:], W["s5_b_glu"][l, :].rearrange("(m q) -> q m", q=128), writes=[bglu], allow_slow_non_contiguous=True)
        p.dma("sp", dcol[:], W["s5_d"][l, :].rearrange("(m q) -> q m", q=128), writes=[dcol], allow_slow_non_contiguous=True)

        PR, PQ, BST = [], [], []
        Cn = smp("Cn", (128, 128))
        dv = lambda fn, r, w: p.op("dve", fn, reads=r, writes=w)
        for d in range(2):
            are, aim, dt, lr, li, m1, cc, ss, t1, t2, nr, ni, den, cr, ci = (smp(f"{n}{d}") for n in
                                                                             ("are", "aim", "dt", "lr", "li", "m1", "cc", "ss", "t1", "t2", "nr", "ni", "den", "cr", "ci"))
            Xb = smp(f"Xb{d}", (128, 32, 16))
            Yb = smp(f"Yb{d}", (128, 32, 16))
            Bst = smp(f"Bst{d}", (128, 32, 16))
            Tb = smp(f"Tb{d}", (128, 32, 16))
            pr = PRt[d]
            pq = PQt[d]
            for hf in range(2):
                hs = slice(hf * 64, (hf + 1) * 64)
                p.dma("sp", are[hs, :], W["s5_a_re"][l, d, :, :].rearrange("g q -> q g"), writes=[are], allow_slow_non_contiguous=True)
                p.dma("sp", aim[hs, :], W["s5_a_im"][l, d, :, :].rearrange("g q -> q g"), writes=[aim], allow_slow_non_contiguous=True)
                own, oth = ("s5_b_re", "s5_b_im") if hf == 0 else ("s5_b_im", "s5_b_re")
                p.dma("sp", Xb[hs, :, :], W[own][l, d, :, :, :].rearrange("g q c -> q g c"), writes=[Xb])
                p.dma("sp", Yb[hs, :, :], W[oth][l, d, :, :, :].rearrange("g q c -> q g c"), writes=[Yb])
            p.dma("sp", dt[:], W["s5_log_dt"][l, d:d + 1, :].partition_broadcast(128), writes=[dt])
            p.op("act", lambda e, dt=dt: e.activation(out=dt[:, :], in_=dt[:, :], func=AF.Exp), reads=[dt], writes=[dt])
            TT = lambda o, a, b_, op: (lambda e: e.tensor_tensor(out=o[:, :], in0=a[:, :], in1=b_[:, :], op=op))
            dv(TT(lr, are, dt, ALU.mult), [are, dt], [lr])
            dv(TT(li, aim, dt, ALU.mult), [aim, dt], [li])
            TS = lambda o, a, s1, s2, o0, o1=None: (lambda e: e.tensor_scalar(o[:, :], a[:, :], s1, s2, o0, o1) if o1 is not None else e.tensor_scalar(o[:, :], a[:, :], s1, None, o0))
            dv(TS(m1, lr, 0.25, 1.0, ALU.mult, ALU.add), [lr], [m1])
            dv(TT(m1, m1, lr, ALU.mult), [m1, lr], [m1])
            dv(TS(m1, m1, 1.0 / 3.0, 1.0, ALU.mult, ALU.add), [m1], [m1])
            dv(TT(m1, m1, lr, ALU.mult), [m1, lr], [m1])
            dv(TS(m1, m1, 0.5, 1.0, ALU.mult, ALU.add), [m1], [m1])
            dv(TT(m1, m1, lr, ALU.mult), [m1, lr], [m1])
            dv(TS(m1, m1, 1.0, None, ALU.add), [m1], [m1])
            dv(TS(nr, li, 1.0 / 256.0, None, ALU.mult), [li], [nr])
            dv(TT(t1, nr, nr, ALU.mult), [nr], [t1])
            dv(TS(ss, t1, 1.0 / 120.0, -1.0 / 6.0, ALU.mult, ALU.add), [t1], [ss])
            dv(TT(ss, ss, t1, ALU.mult), [ss, t1], [ss])
            dv(TS(ss, ss, 1.0, None, ALU.add), [ss], [ss])
            dv(TT(ss, ss, nr, ALU.mult), [ss, nr], [ss])
            dv(TS(cc, t1, -1.0 / 720.0, 1.0 / 24.0, ALU.mult, ALU.add), [t1], [cc])
            dv(TT(cc, cc, t1, ALU.mult), [cc, t1], [cc])
            dv(TS(cc, cc, -0.5, None, ALU.add), [cc], [cc])
            dv(TT(cc, cc, t1, ALU.mult), [cc, t1], [cc])
            dv(TS(cc, cc, 1.0, None, ALU.add), [cc], [cc])
            for _ in range(8):
                dv(TT(t1, cc, cc, ALU.mult), [cc], [t1])
                dv(TT(t2, ss, ss, ALU.mult), [ss], [t2])
                dv(lambda e, cc=cc, ss=ss: e.scalar_tensor_tensor(out=ss[:, :], in0=cc[:, :], scalar=2.0, in1=ss[:, :], op0=ALU.mult, op1=ALU.mult), [cc, ss], [ss])
                dv(TT(cc, t1, t2, ALU.subtract), [t1, t2], [cc])
            dv(TT(t1, cc, cc, ALU.mult), [cc], [t1])
            dv(TT(t2, ss, ss, ALU.mult), [ss], [t2])
            dv(TT(t1, t1, t2, ALU.add), [t1, t2], [t1])
            dv(TS(t1, t1, -0.5, 1.5, ALU.mult, ALU.add), [t1], [t1])
            dv(TT(cc, cc, t1, ALU.mult), [cc, t1], [cc])
            dv(TT(ss, ss, t1, ALU.mult), [ss, t1], [ss])
            dv(lambda e, pr=pr, m1=m1, cc=cc: e.tensor_tensor(out=pr[:, 0, :], in0=m1[:, :], in1=cc[:, :], op=ALU.mult), [m1, cc], [pr])
            dv(TT(ni, m1, ss, ALU.mult), [m1, ss], [ni])
            dv(lambda e, pq=pq, ni=ni: e.tensor_scalar(pq[:, 0, :], ni[:, :], sgn[:, 1:2], None, ALU.mult), [ni, sgn], [pq])
            dv(lambda e, nr=nr, pr=pr: e.tensor_scalar(nr[:, :], pr[:, 0, :], -1.0, None, ALU.add), [pr], [nr])
            dv(TT(t1, are, are, ALU.mult), [are], [t1])
            dv(TT(t2, aim, aim, ALU.mult), [aim], [t2])
            dv(TT(den, t1, t2, ALU.add), [t1, t2], [den])
            dv(lambda e, den=den: e.reciprocal(out=den[:, :], in_=den[:, :]), [den], [den])
            dv(TT(t1, nr, are, ALU.mult), [nr, are], [t1])
            dv(TT(t2, ni, aim, ALU.mult), [ni, aim], [t2])
            dv(TT(cr, t1, t2, ALU.add), [t1, t2], [cr])
            dv(TT(cr, cr, den, ALU.mult), [cr, den], [cr])
            dv(TT(t1, ni, are, ALU.mult), [ni, are], [t1])
            dv(TT(t2, nr, aim, ALU.mult), [nr, aim], [t2])
            dv(TT(ci, t1, t2, ALU.subtract), [t1, t2], [ci])
            dv(TT(ci, ci, den, ALU.mult), [ci, den], [ci])
            dv(lambda e, ci=ci: e.tensor_scalar(ci[:, :], ci[:, :], sgn[:, 0:1], None, ALU.mult), [ci, sgn], [ci])
            bc = lambda t_: t_[:, :].unsqueeze(2).to_broadcast([128, 32, 16])
            dv(lambda e, Bst=Bst, Xb=Xb, cr=cr: e.tensor_tensor(out=Bst[:, :, :], in0=Xb[:, :, :], in1=bc(cr), op=ALU.mult), [Xb, cr], [Bst])
            dv(lambda e, Tb=Tb, Yb=Yb, ci=ci: e.tensor_tensor(out=Tb[:, :, :], in0=Yb[:, :, :], in1=bc(ci), op=ALU.mult), [Yb, ci], [Tb])
            dv(lambda e, Bst=Bst, Tb=Tb: e.tensor_tensor(out=Bst[:, :, :], in0=Bst[:, :, :], in1=Tb[:, :, :], op=ALU.add), [Bst, Tb], [Bst])
            for k in range(11):
                dv(lambda e, k=k, pr=pr, t1=t1: e.tensor_tensor(out=t1[:, :], in0=pr[:, k, :], in1=pr[:, k, :], op=ALU.mult), [pr], [t1])
                dv(lambda e, k=k, pq=pq, t2=t2: e.tensor_tensor(out=t2[:, :], in0=pq[:, k, :], in1=pq[:, k, :], op=ALU.mult), [pq], [t2])
                dv(lambda e, k=k, pr=pr, pq=pq: e.scalar_tensor_tensor(out=pq[:, k + 1, :], in0=pr[:, k, :], scalar=2.0, in1=pq[:, k, :], op0=ALU.mult, op1=ALU.mult), [pr, pq], [pq])
                dv(lambda e, k=k, pr=pr, t1=t1, t2=t2: e.tensor_tensor(out=pr[:, k + 1, :], in0=t1[:, :], in1=t2[:, :], op=ALU.subtract), [t1, t2], [pr])
            PR.append(pr)
            PQ.append(pq)
            if d == 0:
                for nm, tl_ in (("are", are), ("aim", aim), ("dt", dt), ("m1", m1), ("cc", cc), ("ss", ss), ("cr", cr), ("ci", ci)):
                    self.dump(nm, tl_, tl_[:, :], [128, 32])
                self.dump("pr", pr, pr[:, :, :], [128, 12, 32])
                self.dump("pq", pq, pq[:, :, :], [128, 12, 32])
                self.dump("Bst", Bst, Bst[:, :, :], [128, 32, 16])
            for b in range(4):
                ps = self.bank(0, 4)
                p.op("pe", lambda e, ps=ps, Bst=Bst, b=b: e.transpose(ps[:, 0:128], Bst[:, b * 8:(b + 1) * 8, :].rearrange("q g c -> q (g c)"), self.identf[:, :]), reads=[Bst, self.identf], writes=[ps])
                p.op("act", lambda e, ps=ps, d=d, b=b: e.copy(out=BT[d][b][:, :], in_=ps[:, 0:128]), reads=[ps], writes=[BT[d][b]])
                p.dma("sp", Cn[:, 0:64], W["s5_c_re"][l, d, b * 8:(b + 1) * 8, :, :].rearrange("g c q -> (g c) q"), writes=[Cn])
                p.dma("sp", Cn[:, 64:128], W["s5_c_im"][l, d, b * 8:(b + 1) * 8, :, :].rearrange("g c q -> (g c) q"), writes=[Cn])
                ps = self.bank(0, 4)
                p.op("pe", lambda e, ps=ps: e.transpose(ps[:, 0:128], Cn[:, :], self.identf[:, :]), reads=[Cn, self.identf], writes=[ps])
                p.op("dve", lambda e, ps=ps, d=d, b=b: e.tensor_scalar(CST[d][b][:, :], ps[:, 0:128], sgn[:, 1:2], None, ALU.mult), reads=[ps, sgn], writes=[CST[d][b]])

        for b_ in (0, 3):
            self.dump(f"BT{b_}", BT[0][b_], BT[0][b_][:, :], [128, 128])
            self.dump(f"CST{b_}", CST[0][b_], CST[0][b_][:, :], [128, 128])
        p.barrier()
        pst.close()
        uT = p.sb("s_uT", [128, S], BF16, st)
        X = [[p.sb(f"s_X{d}{i}", [128, S], BF16, st, nb=8) for i in range(2)] for d in range(2)]
        yacc = p.sb("s_yacc", [128, S], F32, st, nb=8)
        ygT = p.sb("s_ygT", [128, 4, S], BF16, st)
        Mk = [p.sb(f"s_Mk{i}", [128, 128], BF16, st) for i in range(6)]
        Mt = [p.sb(f"s_Mt{i}", [128, 128], F32, st) for i in range(4)]
        Mt2 = [p.sb(f"s_Mt2{i}", [128, 128], F32, st) for i in range(4)]
        LB = [p.sb(f"s_LB{i}", [128, 128], BF16, st) for i in range(4)]
        LC = [p.sb(f"s_LC{i}", [128, 128], BF16, st) for i in range(4)]
        yt = [p.sb(f"s_yt{i}", [128, 512], F32, st) for i in range(2)]
        ob = [p.sb(f"s_ob{i}", [128, 512], BF16, st) for i in range(2)]
        dirs = self.dbg.get("s5_dirs", (0, 1))
        mki = 0
        gi = 0
        bk = [0]

        def nb(lo, hi):
            b_ = self.PS[lo + bk[0] % (hi - lo)]
            bk[0] += 1
            return b_

        for b in self.dbg.get("s5_tiles", range(4)):
            c0 = COL["u"] + b * 128
            self.wload(stg, wu, wu[:, :, :], W["w_in"][l, :, c0:c0 + 128], 8, 128)
            for ct in range(8):
                ps = self.bank(0, 4)
                for c in range(8):
                    self.mm(ps[:, :], wu[:, c, :], xT[:, c, ct * 512:(ct + 1) * 512], c == 0, c == 7, [wu] + xTr[ct * 4:ct * 4 + 4], [ps])
                p.op("act", lambda e, ps=ps, ct=ct: e.copy(out=uT[:, ct * 512:(ct + 1) * 512], in_=ps[:, :]), reads=[ps], writes=[uT])
            first_acc = True
            for gl in range(8):
                g = b * 8 + gl
                lbs, lcs = {}, {}
                for d in dirs:
                    lb = LB[gi % 4]
                    lc = LC[gi % 4]
                    gi += 1
                    lbs[d], lcs[d] = lb, lc
                    p.op("dve", lambda e, lb=lb, d=d, gl=gl, b=b: e.tensor_scalar(lb[:, :], BT[d][b][:, :], rowm[:, gl:gl + 1], None, ALU.mult), reads=[BT[d][b], rowm], writes=[lb])
                    p.op("pool", lambda e, lc=lc: e.memset(lc[:, :], 0.0), writes=[lc])
                    p.op("pool", lambda e, lc=lc, d=d, gl=gl, b=b: e.tensor_copy(out=lc[:, gl * 16:(gl + 1) * 16], in_=CST[d][b][:, gl * 16:(gl + 1) * 16]), reads=[CST[d][b], lc], writes=[lc])
                    for ct in range(8):
                        ps = nb(0, 8)
                        self.mm(ps[:, :], lb[:, :], uT[:, ct * 512:(ct + 1) * 512], True, True, [lb, uT], [ps])
                        if ct % 2 == 0:
                            p.op("act", lambda e, ps=ps, ct=ct, d=d: e.copy(out=X[d][0][:, ct * 512:(ct + 1) * 512], in_=ps[:, :]), reads=[ps], writes=[X[d][0].bs[ct]])
                        else:
                            p.op("dve", lambda e, ps=ps, ct=ct, d=d: e.tensor_copy(out=X[d][0][:, ct * 512:(ct + 1) * 512], in_=ps[:, :]), reads=[ps], writes=[X[d][0].bs[ct]])
                cur = 0
                for k in range(12):
                    sh = 1 << k
                    mks = {}
                    for d in dirs:
                        mt = Mt[mki % 4]
                        mt2 = Mt2[mki % 4]
                        mk = Mk[mki % 6]
                        mki += 1
                        mks[d] = mk
                        p.op("act", lambda e, mt=mt, d=d, k=k, g=g: e.mul(out=mt[:, :], in_=jx[:, :], mul=PQ[d][:, k, g:g + 1]), reads=[jx, PQ[d]], writes=[mt])
                        p.op("dve", lambda e, mt=mt, mk=mk, d=d, k=k, g=g: e.scalar_tensor_tensor(out=mk[:, :], in0=self.identf[:, :], scalar=PR[d][:, k, g:g + 1], in1=mt[:, :], op0=ALU.mult, op1=ALU.add),
                             reads=[self.identf, PR[d], mt], writes=[mk])
                    for ct in range(8):
                        for d in dirs:
                            mk = mks[d]
                            Xs, Xd = X[d][cur], X[d][1 - cur]
                            t0 = ct * 512
                            if d == 0:
                                lo = max(0, sh - t0)
                                hi = 512
                                s0 = t0 + lo - sh
                            else:
                                lo = 0
                                hi = min(512, S - sh - t0)
                                s0 = t0 + sh
                            has = hi > lo
                            srcb = []
                            if has:
                                a0, a1 = s0, s0 + (hi - lo)
                                srcb = [Xs.bs[i] for i in range(a0 // 512, (a1 - 1) // 512 + 1)]
                            use_pe = ((ct + d) % 2 == 0) or not has
                            ps = nb(0, 8)
                            if use_pe:
                                self.mm(ps[:, :], identb[:, :], Xs[:, t0:t0 + 512], True, not has, [identb, Xs.bs[ct]], [ps])
                                if has:
                                    self.mm(ps[:, lo:hi], mk[:, :], Xs[:, s0:s0 + hi - lo], False, True, [mk] + srcb, [ps])
                                p.op("act", lambda e, ps=ps, Xd=Xd, t0=t0: e.copy(out=Xd[:, t0:t0 + 512], in_=ps[:, :]), reads=[ps], writes=[Xd.bs[ct]])
                            else:
                                self.mm(ps[:, lo:hi], mk[:, :], Xs[:, s0:s0 + hi - lo], True, True, [mk] + srcb, [ps])
                                p.op("dve", lambda e, ps=ps, Xd=Xd, Xs=Xs, t0=t0, lo=lo, hi=hi: e.tensor_tensor(out=Xd[:, t0 + lo:t0 + hi], in0=ps[:, lo:hi], in1=Xs[:, t0 + lo:t0 + hi], op=ALU.add),
                                     reads=[ps, Xs.bs[ct]], writes=[Xd.bs[ct]])
                                if lo > 0:
                                    p.op("pool", lambda e, Xd=Xd, Xs=Xs, t0=t0, lo=lo: e.tensor_copy(out=Xd[:, t0:t0 + lo], in_=Xs[:, t0:t0 + lo]), reads=[Xs.bs[ct]], writes=[Xd.bs[ct]])
                                if hi < 512:
                                    p.op("pool", lambda e, Xd=Xd, Xs=Xs, t0=t0, hi=hi: e.tensor_copy(out=Xd[:, t0 + hi:t0 + 512], in_=Xs[:, t0 + hi:t0 + 512]), reads=[Xs.bs[ct]], writes=[Xd.bs[ct]])
                    cur = 1 - cur
                for ct in range(8):
                    ps = nb(0, 8)
                    for i_, d in enumerate(dirs):
                        self.mm(ps[:, :], lcs[d][:, :], X[d][cur][:, ct * 512:(ct + 1) * 512], i_ == 0, i_ == len(dirs) - 1, [lcs[d], X[d][cur].bs[ct]], [ps])
                    ya = yacc[:, ct * 512:(ct + 1) * 512]
                    if first_acc:
                        p.op("dve", lambda e, ps=ps, ya=ya: e.tensor_copy(out=ya, in_=ps[:, :]), reads=[ps], writes=[yacc.bs[ct]])
                    else:
                        p.op("dve", lambda e, ps=ps, ya=ya: e.tensor_tensor(out=ya, in0=ps[:, :], in1=ya, op=ALU.add), reads=[ps], writes=[yacc.bs[ct]])
                first_acc = False
            for ct in range(8):
                y_ = yt[ct % 2]
                sl = slice(ct * 512, (ct + 1) * 512)
                p.op("dve", lambda e, y_=y_, sl=sl, b=b: e.scalar_tensor_tensor(out=y_[:, :], in0=uT[:, sl], scalar=dcol[:, b:b + 1], in1=yacc[:, sl], op0=ALU.mult, op1=ALU.add),
                     reads=[uT, dcol, yacc.bs[ct]], writes=[y_])
                if self.dbg.get("s5y"):
                    p.dma("pool", self.dbgy[b, :, sl], y_[:, :], reads=[y_], writes=[self.dbgy])
                    p.dma("pool", self.dbgy[4 + b, :, sl], yacc[:, sl], reads=[yacc.bs[ct]], writes=[self.dbgy])
                p.op("act", lambda e, y_=y_, sl=sl, b=b: e.activation(out=ygT[:, b, sl], in_=y_[:, :], func=AF.Gelu_apprx_tanh), reads=[y_], writes=[ygT])
        for m in range(4):
            for ct in range(8):
                sl = slice(ct * 512, (ct + 1) * 512)
                ps = self.bank(0, 8)
                for b in range(4):
                    self.mm(ps[:, :], wglu[:, b, m * 128:(m + 1) * 128], ygT[:, b, sl], b == 0, b == 3, [wglu, ygT], [ps])
                y_ = yt[ct % 2]
                o_ = ob[ct % 2]
                p.op("act", lambda e, ps=ps, y_=y_, m=m: e.activation(out=y_[:, :], in_=ps[:, :], func=AF.Sigmoid, bias=bglu[:, m:m + 1]), reads=[ps, bglu], writes=[y_])
                p.op("dve", lambda e, y_=y_, o_=o_, m=m, sl=sl: e.tensor_tensor(out=o_[:, :], in0=ygT[:, m, sl], in1=y_[:, :], op=ALU.mult), reads=[ygT, y_], writes=[o_])
                p.dma("pool", self.oT[1, m, :, sl], o_[:, :], reads=[o_], writes=[self.oT.bs[4 + m]])
        p.barrier()
        st.close()


def build_model():
    M = Model()
    M.phase_ln0()
    for l in range(DEPTH):
        M.phase_attn(l)
        M.phase_s5(l)
        M.phase_gla(l)
        M.phase_merge1(l)
        M.phase_merge2(l)
        M.phase_moe(l, last=(l == DEPTH - 1))
    M.p.emit()
    return M


def kernel(**inputs):
    M = build_model()
    consts = host_consts()
    shared = {k: np.ascontiguousarray(np.asarray(inputs[k], dtype=np.float32)) for k in PARAM_SHAPES}
    shared.update(consts)
    x = np.asarray(inputs["x"], dtype=np.float32)
    in_maps = []
    for b in range(8):
        m = dict(shared)
        m["x"] = np.ascontiguousarray(x[b])
        in_maps.append(m)
    res = run_bass_kernel_spmd(M.nc, in_maps, core_ids=list(range(8)))
    return np.stack([np.asarray(r["out"], dtype=np.float32) for r in res.results], axis=0)
```

```python
import math
from contextlib import ExitStack
import numpy as np
import ml_dtypes
import concourse.bass as bass
import concourse.mybir as mybir
from concourse.bass_utils import run_bass_kernel_spmd

F32 = mybir.dt.float32
BF16 = mybir.dt.bfloat16
AF = mybir.ActivationFunctionType
ALU = mybir.AluOpType
AX = mybir.AxisListType

ENGS = ("pe", "act", "dve", "pool", "sp")
NDMA_SEM = 6


class Buf:
    __slots__ = ("name", "w", "r", "excl")

    def __init__(self, name=""):
        self.name = name
        self.w = None
        self.r = {}
        self.excl = False


class Tl:
    def __init__(self, t, name, nb=0):
        self.t = t
        self.b = Buf(name)
        self.bs = [Buf(f"{name}{i}") for i in range(nb)]

    def __getitem__(self, k):
        return self.t[k]


class Sub(Tl):
    def __init__(self, ap, buf):
        self.t = ap
        self.b = buf
        self.bs = []


class Prog:
    def __init__(self, nc):
        self.nc = nc
        self.stack = ExitStack()
        self.lists = {e: [] for e in ENGS}
        self.cnt = {e: 0 for e in ENGS}
        self.known = {e: {} for e in ENGS}
        self.sem = {}
        for e in ("pe", "act", "dve", "pool"):
            self.sem["c_" + e] = self.stack.enter_context(nc.semaphore("c_" + e))
        self.dq = {}
        for q in ("sp", "pool", "act"):
            names = []
            for i in range(NDMA_SEM):
                n = f"d_{q}{i}"
                self.sem[n] = self.stack.enter_context(nc.semaphore(n))
                names.append(n)
            self.dq[q] = dict(sems=names, issued=[0] * NDMA_SEM, rr=0)
        self.nwaits = 0

    uid = 0

    def sb(self, name, shape, dtype, stack=None, nb=0):
        Prog.uid += 1
        name = f"{name}_{Prog.uid}"
        t = (stack or self.stack).enter_context(self.nc.sbuf_tensor(name, list(shape), dtype))
        return Tl(t, name, nb)

    def ps(self, name, shape, dtype=F32, stack=None, nb=0):
        t = (stack or self.stack).enter_context(self.nc.psum_tensor(name, list(shape), dtype))
        tl = Tl(t, name, nb)
        tl.b.excl = True
        return tl

    def dram(self, name, shape, dtype, kind="Internal", nb=0):
        t = self.nc.dram_tensor(name, list(shape), dtype, kind=kind).ap()
        return Tl(t, name, nb)

    @staticmethod
    def _bufs(xs):
        out = []
        for x in xs:
            if x is None:
                continue
            out.append(x.b if isinstance(x, Tl) else x)
        return out

    def _need(self, reads, writes):
        need = {}

        def add(sv):
            s, v = sv
            if need.get(s, 0) < v:
                need[s] = v

        for b in reads:
            if b.w:
                add(b.w)
        for b in writes:
            if b.w:
                add(b.w)
            for sv in b.r.items():
                add(sv)
        return need

    def _waits(self, e, need, skip=None):
        kn = self.known[e]
        for s, v in need.items():
            if s == skip:
                continue
            if kn.get(s, 0) < v:
                self.lists[e].append(("wait", s, v))
                kn[s] = v
                self.nwaits += 1

    def op(self, e, fn, reads=(), writes=(), serial=False):
        reads = self._bufs(reads)
        writes = self._bufs(writes)
        ex = [b for b in reads if b.excl and b not in writes]
        if ex:
            reads = [b for b in reads if not b.excl]
            writes = writes + ex
        s = "c_" + e
        self._waits(e, self._need(reads, writes), skip=(s if (e == "pe" and not serial) else None))
        self.cnt[e] += 1
        v = self.cnt[e]
        self.lists[e].append(("op", fn, s))
        for b in reads:
            b.r[s] = v
        for b in writes:
            b.w = (s, v)
            b.r = {}

    def dma(self, q, out, in_, reads=(), writes=(), **kw):
        reads = self._bufs(reads)
        writes = self._bufs(writes)
        d = self.dq[q]
        i = d["rr"]
        d["rr"] = (i + 1) % NDMA_SEM
        s = d["sems"][i]
        need = self._need(reads, writes)
        if d["issued"][i] > 0:
            pv = 16 * d["issued"][i]
            if need.get(s, 0) < pv:
                need[s] = pv
        self._waits(q, need)
        d["issued"][i] += 1
        v = 16 * d["issued"][i]
        self.lists[q].append(("dma", out, in_, s, kw))
        for b in reads:
            b.r[s] = v
        for b in writes:
            b.w = (s, v)
            b.r = {}

    def barrier(self, engines=ENGS):
        need = {}
        for e in ("pe", "act", "dve", "pool"):
            if self.cnt[e]:
                need["c_" + e] = self.cnt[e]
        for q, d in self.dq.items():
            for s, n in zip(d["sems"], d["issued"]):
                if n:
                    need[s] = 16 * n
        for e in engines:
            self._waits(e, dict(need), skip=("c_" + e if e in ("pe",) else None))

    def emit(self):
        nc = self.nc
        self.barrier(engines=("sp",))
        lists = self.lists
        sem = self.sem

        def run(eng, lst):
            for it in lst:
                if it[0] == "wait":
                    eng.wait_ge(sem[it[1]], it[2])
                elif it[0] == "op":
                    it[1](eng).then_inc(sem[it[2]], 1)
                else:
                    eng.dma_start(out=it[1], in_=it[2], **it[4]).then_inc(sem[it[3]], 16)

        with nc.Block() as block:
            @block.tensor
            def _(eng):
                run(eng, lists["pe"])

            @block.scalar
            def _(eng):
                run(eng, lists["act"])

            @block.vector
            def _(eng):
                run(eng, lists["dve"])

            @block.gpsimd
            def _(eng):
                run(eng, lists["pool"])

            @block.sync
            def _(eng):
                run(eng, lists["sp"])
        self.stack.close()


S = 4096
D = 1024
NT = 32
DEPTH = 2
N_IN = 6688
COL = dict(qa=0, ka=512, va=1024, u=1536, qc=2048, kc=2304, vc=2560, rc=3072, zf=3584, zb=3600, gate=3616)
ALPHA = (2.0 * DEPTH) ** 0.25
EPS = 1e-5
PI = math.pi

PARAM_SHAPES = dict(
    ln0_g=[1024], ln0_b=[1024], w_in=[2, 1024, 6688], da_lambda=[2, 4, 64], da_norm_g=[2, 128],
    s5_a_re=[2, 2, 32, 64], s5_a_im=[2, 2, 32, 64], s5_log_dt=[2, 2, 32], s5_b_re=[2, 2, 32, 64, 16],
    s5_b_im=[2, 2, 32, 64, 16], s5_c_re=[2, 2, 32, 16, 64], s5_c_im=[2, 2, 32, 16, 64], s5_d=[2, 512],
    s5_w_glu=[2, 512, 512], s5_b_glu=[2, 512], gla_w_gate=[2, 2, 16, 256], gla_b_gate=[2, 2, 256],
    gla_norm_g=[2, 128], merge_w_up=[2, 3, 512, 1024], merge_b=[2, 3, 1024], w_out=[2, 1024, 1024],
    ln1_g=[2, 1024], ln1_b=[2, 1024], router_w=[1024, 16], router_bias=[16],
    moe_w_gate=[2, 16, 1024, 512], moe_w_up=[2, 16, 1024, 512], moe_w_down=[2, 16, 512, 1024],
    ln2_g=[2, 1024], ln2_b=[2, 1024])


def host_consts():
    c = {}
    c["identf"] = np.eye(128, dtype=np.float32)
    i = np.arange(128)
    c["absdiff"] = np.abs(i[:, None] - i[None, :]).astype(np.float32)
    pos = np.arange(S)
    hi, lo = pos // 64, pos % 64
    c["qaug"] = np.stack([64.0 * hi, lo, np.ones(S), np.ones(S)]).astype(ml_dtypes.bfloat16)
    ka = np.zeros((4, 2, 4, S), np.float32)
    for h in range(4):
        sl = 2.0 ** (-2.0 * (h + 1))
        plus = np.stack([np.full(S, sl), np.full(S, sl), -sl * 64.0 * hi, -sl * lo])
        ka[h, 0] = plus
        ka[h, 1] = -plus
    c["kaug"] = ka.astype(ml_dtypes.bfloat16)
    j = np.arange(64)
    c["gmask"] = np.stack([(j[:, None] <= j[None, :]), (j[:, None] > j[None, :])]).astype(np.float32)
    jj = np.repeat(np.arange(8), 16)
    c["s5mask"] = np.stack([(jj[None, :] >= jj[:, None]), (jj[None, :] <= jj[:, None])]).astype(np.float32)
    jx = np.zeros((128, 128), np.float32)
    for q in range(64):
        jx[q, 64 + q] = 1.0
        jx[64 + q, q] = 1.0
    c["jx"] = jx
    c["rowmask"] = (np.arange(128)[:, None] // 16 == np.arange(8)[None, :]).astype(np.float32)
    sg = np.ones((128, 2), np.float32)
    sg[:64, 0] = -1.0
    sg[64:, 1] = -1.0
    c["sgn"] = sg
    return c


CONST_SHAPES = dict(jx=([128, 128], F32), rowmask=([128, 8], F32), sgn=([128, 2], F32), identf=([128, 128], F32), absdiff=([128, 128], F32), qaug=([4, S], BF16), kaug=([4, 2, 4, S], BF16),
                    gmask=([2, 64, 64], F32), s5mask=([2, 128, 128], F32))


class Model:
    def __init__(self, dbg=None):
        self.dbg = dbg or {}
        nc = bass.Bass("TRN2", target_bir_lowering=False)
        self.nc = nc
        p = Prog(nc)
        self.p = p
        kinds = self.dbg.get("kinds", {})
        self.x_in = p.dram("x", [S, D], F32, kind="ExternalInput")
        self.out = p.dram("out", [S, D], F32, kind="ExternalOutput", nb=NT)
        self.W = {k: p.dram(k, shp, F32, kind="ExternalInput") for k, shp in PARAM_SHAPES.items()}
        self.C = {k: p.dram(k, shp, dt, kind="ExternalInput") for k, (shp, dt) in CONST_SHAPES.items()}
        self.xres = p.dram("xres", [S, D], F32, kind=kinds.get("xres", "Internal"), nb=NT)
        self.oT = p.dram("oT", [3, 4, 128, S], BF16, kind=kinds.get("oT", "Internal"), nb=12)
        self.mT = p.dram("mT", [128, 8, S], BF16, kind=kinds.get("mT", "Internal"), nb=16)
        if self.dbg.get("s5y"):
            self.dbgy = p.dram("dbgy", [8, 128, S], F32, kind="ExternalOutput")
        self.xT = p.sb("xT", [128, 8, S], BF16, nb=NT)
        self.identf = p.sb("identf_sb", [128, 128], F32)
        self.call = p.sb("call", [128, NT, 16], F32, nb=NT)
        self.PS = [p.ps(f"ps{i}", [128, 512], F32) for i in range(8)]
        self.psi = 0
        self.psc = {}
        p.dma("sp", self.identf[:], self.C["identf"][:, :], writes=[self.identf])

    def dump(self, name, tl, ap, shape):
        if not self.dbg.get("dump"):
            return
        d = self.p.dram("dump_" + name, list(shape), F32, kind="ExternalOutput")
        self.p.dma("pool", d.t, ap, reads=[tl], writes=[d])

    def bank(self, lo=0, hi=8):
        n = hi - lo
        c = self.psc.get((lo, hi), 0)
        self.psc[(lo, hi)] = c + 1
        return self.PS[lo + (c % n)]

    def mm(self, out, lhsT, rhs, start, stop, reads, writes, serial=False):
        self.p.op("pe", lambda e: e.matmul(out, lhsT, rhs, start=start, stop=stop, skip_group_check=True), reads=reads, writes=writes, serial=serial)

    def ln_tile(self, z, xn, stats, mv, rstd):
        p = self.p
        for k in range(2):
            p.op("dve", lambda e, k=k: e.bn_stats(out=stats[:, k, :], in_=z[:, k * 512:(k + 1) * 512]), reads=[z], writes=[stats])
        p.op("dve", lambda e: e.bn_aggr(out=mv[:, :], in_=stats[:, :, :].rearrange("p a b -> p (a b)")), reads=[stats], writes=[mv])
        self.rsqrt(rstd, rstd[:, :], mv, mv[:, 1:2], 1.0)
        p.op("dve", lambda e: e.tensor_scalar(xn[:, :], z[:, :], mv[:, 0:1], rstd[:, 0:1], ALU.subtract, ALU.mult), reads=[z, mv, rstd], writes=[xn])
        p.op("pool", lambda e, g_=self.gbc: e.tensor_tensor(out=xn[:, :], in0=xn[:, :], in1=g_[:, :], op=ALU.mult), reads=[xn, self.gbc], writes=[xn])
        p.op("pool", lambda e, b_=self.bbc: e.tensor_tensor(out=xn[:, :], in0=xn[:, :], in1=b_[:, :], op=ALU.add), reads=[xn, self.bbc], writes=[xn])

    def rsqrt(self, dst_tl, dst, src_tl, src, scale):
        p = self.p
        p.op("dve", lambda e: e.tensor_scalar(dst, src, scale, EPS, ALU.mult, ALU.add), reads=[src_tl], writes=[dst_tl])
        p.op("act", lambda e: e.sqrt(out=dst, in_=dst), reads=[dst_tl], writes=[dst_tl])
        p.op("dve", lambda e: e.reciprocal(out=dst, in_=dst), reads=[dst_tl], writes=[dst_tl])

    def load_ln_params(self, g_ap, b_ap, st):
        p = self.p
        self.gbc = p.sb("gbc", [128, D], F32, st)
        self.bbc = p.sb("bbc", [128, D], F32, st)
        p.dma("sp", self.gbc[:], g_ap.partition_broadcast(128), writes=[self.gbc])
        p.dma("sp", self.bbc[:], b_ap.partition_broadcast(128), writes=[self.bbc])

    def to_xT(self, xn, t, xT32=None):
        p = self.p
        for c0 in (0, 4):
            ps = self.bank(0, 4)
            for j in range(4):
                c = c0 + j
                p.op("pe", lambda e, ps=ps, j=j, c=c: e.transpose(ps[:, j * 128:(j + 1) * 128], xn[:, c * 128:(c + 1) * 128], self.identf[:, :]),
                     reads=[xn, self.identf], writes=[ps])
            src = ps[:, :].rearrange("p (j n) -> p j n", j=4)
            p.op("act", lambda e, src=src, c0=c0: e.copy(out=self.xT[:, c0:c0 + 4, t * 128:(t + 1) * 128], in_=src), reads=[ps], writes=[self.xT.bs[t]])
            if xT32 is not None:
                p.op("dve", lambda e, src=src, c0=c0: e.tensor_copy(out=xT32[:, c0:c0 + 4, :], in_=src), reads=[ps], writes=[xT32])

    def phase_ln0(self):
        p = self.p
        st = ExitStack()
        self.load_ln_params(self.W["ln0_g"].t.rearrange("(o n) -> o n", o=1), self.W["ln0_b"].t.rearrange("(o n) -> o n", o=1), st)
        zs = [p.sb(f"l0z{i}", [128, D], F32, st) for i in range(2)]
        stats = p.sb("l0stats", [128, 2, 6], F32, st)
        mv = p.sb("l0mv", [128, 2], F32, st)
        rstd = p.sb("l0rstd", [128, 1], F32, st)
        for t in range(NT):
            z = zs[t % 2]
            p.dma("sp", z[:], self.x_in[t * 128:(t + 1) * 128, :], writes=[z])
            self.ln_tile(z, z, stats, mv, rstd)
            p.dma("pool", self.xres[t * 128:(t + 1) * 128, :], z[:], reads=[z], writes=[self.xres.bs[t]])
            self.to_xT(z, t)
        p.barrier()
        st.close()

    def wload(self, stg, dst_tl, dst_ap, src_ap, kc, ncols, cast="act", wbuf=None):
        p = self.p
        cap = stg[0].t.shape[1]
        kcp = max(1, min(kc, cap // ncols))
        wb = [wbuf if wbuf is not None else dst_tl]
        for k0 in range(0, kc, kcp):
            st = stg[self.wl_i % len(stg)]
            self.wl_i += 1
            view = st[:, 0:kcp * ncols].rearrange("p (c n) -> p c n", c=kcp)
            p.dma("sp", view, src_ap[k0 * 128:(k0 + kcp) * 128, :].rearrange("(c p) n -> p c n", p=128), writes=[st])
            if cast == "act":
                p.op(cast, lambda e, view=view, k0=k0: e.copy(out=dst_ap[:, k0:k0 + kcp, :], in_=view), reads=[st], writes=wb)
            else:
                p.op(cast, lambda e, view=view, k0=k0: e.tensor_copy(out=dst_ap[:, k0:k0 + kcp, :], in_=view), reads=[st], writes=wb)

    wl_i = 0

    def phase_attn(self, l):
        p = self.p
        W = self.W
        st = ExitStack()
        stg = [p.sb(f"a_stg{i}", [128, 8 * 128], F32, st) for i in range(2)]
        wA = [p.sb(f"a_w{i}", [128, 8, 384], BF16, st) for i in range(2)]
        QT = [p.sb(f"a_qt{m}", [68, S], BF16, st) for m in range(2)]
        KTp = [p.sb(f"a_ktp{m}", [68, S], BF16, st) for m in range(2)]
        KTm = [p.sb(f"a_ktm{m}", [68, S], BF16, st) for m in range(2)]
        V = p.sb("a_v", [128, NT, 129], BF16, st)
        PT = [p.sb(f"a_pt{i}", [128, 512], BF16, st) for i in range(4)]
        oaT = [p.sb(f"a_oat{i}", [128, S], BF16, st) for i in range(1)]
        on = [p.sb(f"a_on{m}", [128, 4, 128], F32, st) for m in range(2)]
        rs = p.sb("a_rs", [128, 4], F32, st)
        diff = p.sb("a_diff", [128, 4, 128], F32, st)
        sq = p.sb("a_sq", [128, 4, 128], F32, st)
        ss = p.sb("a_ss", [128, 4], F32, st)
        rstd = p.sb("a_rstd", [128, 4], F32, st)
        oo = p.sb("a_oo", [128, 4, 128], F32, st)
        absd = p.sb("a_absd", [128, 128], F32, st)
        lamt = p.sb("a_lamt", [128, 256], F32, st)
        lsm = p.sb("a_lsm", [128, 8], F32, st)
        gA = p.sb("a_gA", [128, 128], F32, st)
        lam_init = 0.8 - 0.6 * math.exp(-0.3 * l)
        p.dma("sp", absd[:], self.C["absdiff"][:, :], writes=[absd])
        for m in range(2):
            p.dma("sp", QT[m][64:68, :], self.C["qaug"][:, :], writes=[QT[m]])
        p.dma("sp", lamt[:], W["da_lambda"][l:l + 1, :, :].rearrange("o a b -> o (a b)").partition_broadcast(128), writes=[lamt])
        p.dma("sp", gA[:], W["da_norm_g"][l:l + 1, :].partition_broadcast(128), writes=[gA])
        p.op("dve", lambda e: e.tensor_scalar(gA[:, :], gA[:, :], 1.0 - lam_init, None, ALU.mult), reads=[gA], writes=[gA])
        p.op("dve", lambda e: e.tensor_tensor(out=lamt[:, 0:64], in0=lamt[:, 0:64], in1=lamt[:, 64:128], op=ALU.mult), reads=[lamt], writes=[lamt])
        p.op("dve", lambda e: e.tensor_tensor(out=lamt[:, 128:192], in0=lamt[:, 128:192], in1=lamt[:, 192:256], op=ALU.mult), reads=[lamt], writes=[lamt])
        p.op("dve", lambda e: e.tensor_reduce(out=lsm[:, 0:1], in_=lamt[:, 0:64], axis=AX.X, op=ALU.add), reads=[lamt], writes=[lsm])
        p.op("dve", lambda e: e.tensor_reduce(out=lsm[:, 1:2], in_=lamt[:, 128:192], axis=AX.X, op=ALU.add), reads=[lamt], writes=[lsm])
        p.op("act", lambda e: e.activation(out=lsm[:, 2:4], in_=lsm[:, 0:2], func=AF.Exp), reads=[lsm], writes=[lsm])
        p.op("dve", lambda e: e.tensor_tensor(out=lsm[:, 4:5], in0=lsm[:, 3:4], in1=lsm[:, 2:3], op=ALU.subtract), reads=[lsm], writes=[lsm])
        p.op("dve", lambda e: e.tensor_scalar(lsm[:, 5:6], lsm[:, 4:5], -lam_init, None, ALU.add), reads=[lsm], writes=[lsm])
        p.op("dve", lambda e: e.memset(V[:, :, 128:129], 1.0), writes=[V])
        xT = self.xT
        xTr = list(xT.bs)

        def load_head_w(h):
            w = wA[h % 2]
            for k, nm in enumerate(("qa", "ka", "va")):
                c0 = COL[nm] + h * 128
                self.wload(stg, w, w[:, :, k * 128:(k + 1) * 128], W["w_in"][l, :, c0:c0 + 128], 8, 128, cast="dve")

        load_head_w(0)
        for h in range(self.dbg.get("heads", 4)):
            slope = 2.0 ** (-2.0 * (h + 1))
            w = wA[h % 2]
            if h + 1 < self.dbg.get("heads", 4):
                load_head_w(h + 1)
            for m in range(2):
                p.dma("sp", KTp[m][64:68, :], self.C["kaug"][h, 0, :, :], writes=[KTp[m]])
                p.dma("sp", KTm[m][64:68, :], self.C["kaug"][h, 1, :, :], writes=[KTm[m]])
            for m in range(2):
                for tb in range(8):
                    ps = self.bank(0, 4)
                    for c in range(8):
                        self.mm(ps[0:64, :], w[:, c, m * 64:(m + 1) * 64], xT[:, c, tb * 512:(tb + 1) * 512], c == 0, c == 7, [w] + xTr[tb * 4:tb * 4 + 4], [ps])
                    p.op("act", lambda e, ps=ps, m=m, tb=tb: e.mul(out=QT[m][0:64, tb * 512:(tb + 1) * 512], in_=ps[0:64, :], mul=0.125), reads=[ps], writes=[QT[m]])
                    ps = self.bank(0, 4)
                    for c in range(8):
                        self.mm(ps[0:64, :], w[:, c, 128 + m * 64:128 + (m + 1) * 64], xT[:, c, tb * 512:(tb + 1) * 512], c == 0, c == 7, [w] + xTr[tb * 4:tb * 4 + 4], [ps])
                    p.op("act", lambda e, ps=ps, m=m, tb=tb: e.copy(out=KTp[m][0:64, tb * 512:(tb + 1) * 512], in_=ps[0:64, :]), reads=[ps], writes=[KTp[m]])
                    p.op("dve", lambda e, ps=ps, m=m, tb=tb: e.tensor_copy(out=KTm[m][0:64, tb * 512:(tb + 1) * 512], in_=ps[0:64, :]), reads=[ps], writes=[KTm[m]])
            for t4 in range(8):
                ps = self.bank(0, 4)
                for j in range(4):
                    t = t4 * 4 + j
                    for c in range(8):
                        self.mm(ps[:, j * 128:(j + 1) * 128], xT[:, c, t * 128:(t + 1) * 128], w[:, c, 256:384], c == 0, c == 7, [w, xTr[t]], [ps])
                p.op("act", lambda e, ps=ps, t4=t4: e.copy(out=V[:, t4 * 4:(t4 + 1) * 4, 0:128], in_=ps[:, :].rearrange("p (j n) -> p j n", j=4)), reads=[ps], writes=[V])
            oa = oaT[0]
            pti = 0
            for Q in range(self.dbg.get("nQ", 8)):
                stages = [(m, kt) for m in range(2) for kt in range(NT)]
                stbank = {}
                ptbuf = {}

                def st_S(i, Q=Q):
                    m, kt = stages[i]
                    ps = self.PS[i % 4]
                    stbank[i] = ps
                    rel = kt - 4 * Q
                    ksl = slice(kt * 128, (kt + 1) * 128)
                    if rel < 0:
                        self.mm(ps[:, :], KTm[m][0:68, ksl], QT[m][0:68, Q * 512:(Q + 1) * 512], True, True, [KTm[m], QT[m]], [ps])
                    elif rel > 3:
                        self.mm(ps[:, :], KTp[m][0:68, ksl], QT[m][0:68, Q * 512:(Q + 1) * 512], True, True, [KTp[m], QT[m]], [ps])
                    else:
                        q0 = Q * 512
                        first = True
                        if rel > 0:
                            self.mm(ps[:, 0:rel * 128], KTp[m][0:68, ksl], QT[m][0:68, q0:q0 + rel * 128], first, True, [KTp[m], QT[m]], [ps])
                            first = False
                        self.mm(ps[:, rel * 128:(rel + 1) * 128], KTp[m][0:64, ksl], QT[m][0:64, q0 + rel * 128:q0 + (rel + 1) * 128], first, True, [KTp[m], QT[m]], [ps])
                        if rel < 3:
                            self.mm(ps[:, (rel + 1) * 128:512], KTm[m][0:68, ksl], QT[m][0:68, q0 + (rel + 1) * 128:q0 + 512], False, True, [KTm[m], QT[m]], [ps])
                        p.op("dve", lambda e, ps=ps, rel=rel, slope=slope: e.scalar_tensor_tensor(
                            out=ps[:, rel * 128:(rel + 1) * 128], in0=absd[:, :], scalar=-slope, in1=ps[:, rel * 128:(rel + 1) * 128], op0=ALU.mult, op1=ALU.add),
                            reads=[absd, ps], writes=[ps])

                def st_E(i):
                    ps = stbank[i]
                    pt = PT[i % 4]
                    ptbuf[i] = pt
                    p.op("act", lambda e, ps=ps, pt=pt: e.activation(out=pt[:, :], in_=ps[:, :], func=AF.Exp), reads=[ps], writes=[pt])

                def st_P(i):
                    m, kt = stages[i]
                    pt = ptbuf[i]
                    OB = (self.PS[4 + 2 * m], self.PS[5 + 2 * m])
                    for j in range(4):
                        ob = OB[j // 2]
                        oc = (j % 2) * 256
                        self.mm(ob[:, oc:oc + 129], pt[:, j * 128:(j + 1) * 128], V[:, kt, :], (kt == 0 and j % 2 == 0), kt == NT - 1, [pt, V], [ob])
                    if kt == NT - 1:
                        for j in range(4):
                            ob = OB[j // 2]
                            oc = (j % 2) * 256
                            p.op("dve", lambda e, ob=ob, oc=oc, j=j: e.reciprocal(out=rs[:, j:j + 1], in_=ob[:, oc + 128:oc + 129]), reads=[ob], writes=[rs])
                            p.op("dve", lambda e, ob=ob, oc=oc, j=j, m=m: e.tensor_scalar(on[m][:, j, :], ob[:, oc:oc + 128], rs[:, j:j + 1], None, ALU.mult), reads=[ob, rs], writes=[on[m]])

                NS = len(stages)
                st_S(0)
                st_S(1)
                for i in range(NS):
                    st_E(i)
                    if i + 2 < NS:
                        st_S(i + 2)
                    st_P(i)
                p.op("dve", lambda e: e.scalar_tensor_tensor(out=diff[:, :, :], in0=on[1][:, :, :], scalar=lsm[:, 5:6], in1=on[0][:, :, :], op0=ALU.mult, op1=ALU.add),
                     reads=[on[0], on[1], lsm], writes=[diff])
                p.op("pool", lambda e: e.tensor_tensor(out=sq[:, :, :], in0=diff[:, :, :], in1=diff[:, :, :], op=ALU.mult), reads=[diff], writes=[sq])
                p.op("dve", lambda e: e.tensor_reduce(out=ss[:, :], in_=sq[:, :, :], axis=AX.X, op=ALU.add), reads=[sq], writes=[ss])
                self.rsqrt(rstd, rstd[:, :], ss, ss[:, :], 1.0 / 128.0)
                for j in range(4):
                    p.op("dve", lambda e, j=j: e.scalar_tensor_tensor(out=oo[:, j, :], in0=diff[:, j, :], scalar=rstd[:, j:j + 1], in1=gA[:, :], op0=ALU.mult, op1=ALU.mult),
                         reads=[diff, rstd, gA], writes=[oo])
                ps = self.bank(0, 4)
                for j in range(4):
                    p.op("pe", lambda e, ps=ps, j=j: e.transpose(ps[:, j * 128:(j + 1) * 128], oo[:, j, :], self.identf[:, :]), reads=[oo, self.identf], writes=[ps])
                p.op("act", lambda e, ps=ps, Q=Q, oa=oa: e.copy(out=oa[:, Q * 512:(Q + 1) * 512], in_=ps[:, :]), reads=[ps], writes=[oa])
            p.dma("pool", self.oT[0, h, :, :], oa[:, :], reads=[oa], writes=[self.oT.bs[h]])
        p.barrier()
        st.close()

    def phase_merge1(self, l):
        for dh in range(2):
            self._merge1_dh(l, dh)

    def _merge1_dh(self, l, dh):
        p = self.p
        W = self.W
        xT = self.xT
        if True:
            st = ExitStack()
            stg = [p.sb(f"m_stg{i}", [128, 2048], F32, st) for i in range(2)]
            wg = p.sb("m_wg", [128, 8, 1536], BF16, st, nb=3)
            wup = p.sb("m_wup", [128, 12, 512], BF16, st, nb=3)
            mb = p.sb("m_mb", [128, 3, 512], F32, st)
            ot = [p.sb(f"m_ot{i}", [128, 12, 512], BF16, st) for i in range(2)]
            sg = [p.sb(f"m_sg{i}", [128, 512], F32, st) for i in range(2)]
            acc = [p.sb(f"m_acc{i}", [128, 512], F32, st) for i in range(2)]
            tmp = [p.sb(f"m_tmp{i}", [128, 512], F32, st) for i in range(2)]
            mtb = [p.sb(f"m_mtb{i}", [128, 4, 512], BF16, st) for i in range(2)]
            d0 = dh * 512
            for n in range(3):
                c0 = COL["gate"] + n * 1024 + d0
                self.wload(stg, wg, wg[:, :, n * 512:(n + 1) * 512], W["w_in"][l, :, c0:c0 + 512], 8, 512, wbuf=wg.bs[n])
                self.wload(stg, wup, wup[:, n * 4:(n + 1) * 4, :], W["merge_w_up"][l, n, :, d0:d0 + 512], 4, 512, wbuf=wup.bs[n])
            p.dma("sp", mb[:], W["merge_b"][l:l + 1, :, d0:d0 + 512].partition_broadcast(128), writes=[mb])
            k = 0
            for tb in range(8):
                o = ot[tb % 2]
                p.dma("sp", o[:], self.oT[:, :, :, tb * 512:(tb + 1) * 512].rearrange("n c p t -> p (n c) t"), reads=self.oT.bs, writes=[o])
                mt = mtb[tb % 2]
                for tt in range(4):
                    t = tb * 4 + tt
                    a = acc[k % 2]
                    for n in range(3):
                        g = sg[(k * 3 + n) % 2]
                        psg = self.bank(0, 3)
                        for c in range(8):
                            self.mm(psg[:, :], xT[:, c, t * 128:(t + 1) * 128], wg[:, c, n * 512:(n + 1) * 512], c == 0, c == 7, [xT.bs[t], wg.bs[n]], [psg])
                        p.op("dve", lambda e, g=g, psg=psg, n=n, mb=mb: e.tensor_tensor(out=g[:, :], in0=psg[:, :], in1=mb[:, n, :], op=ALU.add), reads=[psg, mb], writes=[g])
                        p.op("act", lambda e, g=g: e.activation(out=g[:, :], in_=g[:, :], func=AF.Sigmoid), reads=[g], writes=[g])
                        psu = self.bank(3, 6)
                        for c in range(4):
                            self.mm(psu[:, :], o[:, n * 4 + c, tt * 128:(tt + 1) * 128], wup[:, n * 4 + c, :], c == 0, c == 3, [o, wup.bs[n]], [psu])
                        if n == 0:
                            p.op("dve", lambda e, a=a, g=g, psu=psu: e.tensor_tensor(out=a[:, :], in0=g[:, :], in1=psu[:, :], op=ALU.mult), reads=[g, psu], writes=[a])
                        else:
                            tm = tmp[n % 2]
                            p.op("dve", lambda e, tm=tm, g=g, psu=psu: e.tensor_tensor(out=tm[:, :], in0=g[:, :], in1=psu[:, :], op=ALU.mult), reads=[g, psu], writes=[tm])
                            p.op("pool", lambda e, tm=tm, a=a: e.tensor_tensor(out=a[:, :], in0=a[:, :], in1=tm[:, :], op=ALU.add), reads=[a, tm], writes=[a])
                    pst = self.bank(6, 8)
                    for j in range(4):
                        p.op("pe", lambda e, pst=pst, j=j, a=a: e.transpose(pst[:, j * 128:(j + 1) * 128], a[:, j * 128:(j + 1) * 128], self.identf[:, :]), reads=[a, self.identf], writes=[pst])
                    p.op("act", lambda e, pst=pst, mt=mt, tt=tt: e.copy(out=mt[:, :, tt * 128:(tt + 1) * 128], in_=pst[:, :].rearrange("p (j n) -> p j n", j=4)), reads=[pst], writes=[mt])
                    k += 1
                p.dma("pool", self.mT[:, dh * 4:(dh + 1) * 4, tb * 512:(tb + 1) * 512], mt[:, :, :], reads=[mt], writes=[self.mT.bs[dh * 8 + tb]])
            p.barrier()
            st.close()

    def phase_merge2(self, l):
        p = self.p
        W = self.W
        st = ExitStack()
        stg = [p.sb(f"n_stg{i}", [128, 2048], F32, st) for i in range(2)]
        wo = p.sb("n_wo", [128, 8, 1024], BF16, st)
        rw = p.sb("n_rw", [128, 8, 16], F32, st)
        rb = p.sb("n_rb", [128, 16], F32, st)
        mtl = [p.sb(f"n_mt{i}", [128, 8, 512], BF16, st) for i in range(2)]
        xr = [p.sb(f"n_xr{i}", [128, D], F32, st) for i in range(2)]
        z = [p.sb(f"n_z{i}", [128, D], F32, st) for i in range(2)]
        xT32 = p.sb("n_xT32", [128, 8, 128], F32, st)
        stats = p.sb("n_stats", [128, 2, 6], F32, st)
        mv = p.sb("n_mv", [128, 2], F32, st)
        rstd = p.sb("n_rstd", [128, 1], F32, st)
        R = {k: p.sb("n_r_" + k, shp, F32, st) for k, shp in dict(sc=[128, 16], bi=[128, 16], m1=[128, 4], eq=[128, 16], t2=[128, 16], m2=[128, 4],
                                                                  gs=[128, 4], gm=[128, 1], gsel=[128, 4], ge=[128, 16], w=[128, 16], ws=[128, 1]).items()}
        for h2 in range(2):
            self.wload(stg, wo, wo[:, :, h2 * 512:(h2 + 1) * 512], W["w_out"][l, :, h2 * 512:(h2 + 1) * 512], 8, 512)
        p.dma("sp", rw[:], W["router_w"].t.rearrange("(c p) n -> p c n", p=128), writes=[rw])
        p.dma("sp", rb[:], W["router_bias"].t.rearrange("(o n) -> o n", o=1).partition_broadcast(128), writes=[rb])
        self.load_ln_params(W["ln1_g"][l:l + 1, :], W["ln1_b"][l:l + 1, :], st)
        for tb in range(8):
            mt = mtl[tb % 2]
            p.dma("sp", mt[:], self.mT[:, :, tb * 512:(tb + 1) * 512], reads=[self.mT.bs[tb], self.mT.bs[8 + tb]], writes=[mt])
            for tt in range(4):
                t = tb * 4 + tt
                x_ = xr[t % 2]
                z_ = z[t % 2]
                p.dma("sp", x_[:], self.xres[t * 128:(t + 1) * 128, :], reads=[self.xres.bs[t]], writes=[x_])
                for h2 in range(2):
                    ps = self.bank(0, 4)
                    for c in range(8):
                        self.mm(ps[:, :], mt[:, c, tt * 128:(tt + 1) * 128], wo[:, c, h2 * 512:(h2 + 1) * 512], c == 0, c == 7, [mt, wo], [ps])
                    p.op("dve", lambda e, ps=ps, x_=x_, z_=z_, h2=h2: e.scalar_tensor_tensor(out=z_[:, h2 * 512:(h2 + 1) * 512], in0=x_[:, h2 * 512:(h2 + 1) * 512], scalar=ALPHA,
                                                                                    in1=ps[:, :], op0=ALU.mult, op1=ALU.add), reads=[ps, x_], writes=[z_])
                self.ln_tile(z_, z_, stats, mv, rstd)
                p.dma("pool", self.xres[t * 128:(t + 1) * 128, :], z_[:], reads=[z_], writes=[self.xres.bs[t]])
                self.to_xT(z_, t, xT32=xT32)
                self.router(t, xT32, rw, rb, R)
        p.barrier()
        st.close()

    def router(self, t, xT32, rw, rb, R):
        p = self.p
        ps = self.bank(4, 8)
        for c in range(8):
            self.mm(ps[:, 0:16], xT32[:, c, :], rw[:, c, :], c == 0, c == 7, [xT32, rw], [ps])
        sc, bi, m1, eq, t2, m2, gs, gm, gsel, ge, w, ws = (R[k] for k in ("sc", "bi", "m1", "eq", "t2", "m2", "gs", "gm", "gsel", "ge", "w", "ws"))
        v3 = lambda tl: tl[:, :].rearrange("p (g e) -> p g e", g=4)
        b3 = lambda tl: tl[:, :].unsqueeze(2).to_broadcast([128, 4, 4])
        p.op("act", lambda e: e.activation(out=sc[:, :], in_=ps[:, 0:16], func=AF.Sigmoid), reads=[ps], writes=[sc])
        p.op("dve", lambda e: e.tensor_tensor(out=bi[:, :], in0=sc[:, :], in1=rb[:, :], op=ALU.add), reads=[sc, rb], writes=[bi])
        p.op("dve", lambda e: e.tensor_reduce(out=m1[:, :], in_=v3(bi), axis=AX.X, op=ALU.max), reads=[bi], writes=[m1])
        p.op("dve", lambda e: e.tensor_tensor(out=v3(eq), in0=v3(bi), in1=b3(m1), op=ALU.is_equal), reads=[bi, m1], writes=[eq])
        p.op("dve", lambda e: e.scalar_tensor_tensor(out=t2[:, :], in0=eq[:, :], scalar=-1e30, in1=bi[:, :], op0=ALU.mult, op1=ALU.add), reads=[eq, bi], writes=[t2])
        p.op("dve", lambda e: e.tensor_reduce(out=m2[:, :], in_=v3(t2), axis=AX.X, op=ALU.max), reads=[t2], writes=[m2])
        p.op("dve", lambda e: e.tensor_tensor(out=gs[:, :], in0=m1[:, :], in1=m2[:, :], op=ALU.add), reads=[m1, m2], writes=[gs])
        p.op("dve", lambda e: e.tensor_reduce(out=gm[:, :], in_=gs[:, :], axis=AX.X, op=ALU.max), reads=[gs], writes=[gm])
        p.op("dve", lambda e: e.tensor_scalar(gsel[:, :], gs[:, :], gm[:, 0:1], None, ALU.is_equal), reads=[gs, gm], writes=[gsel])
        p.op("dve", lambda e: e.tensor_tensor(out=v3(ge), in0=v3(bi), in1=b3(m2), op=ALU.is_ge), reads=[bi, m2], writes=[ge])
        p.op("dve", lambda e: e.tensor_tensor(out=v3(ge), in0=v3(ge), in1=b3(gsel), op=ALU.mult), reads=[ge, gsel], writes=[ge])
        p.op("dve", lambda e: e.tensor_tensor(out=w[:, :], in0=ge[:, :], in1=sc[:, :], op=ALU.mult), reads=[ge, sc], writes=[w])
        p.op("dve", lambda e: e.tensor_reduce(out=ws[:, :], in_=w[:, :], axis=AX.X, op=ALU.add), reads=[w], writes=[ws])
        p.op("dve", lambda e: e.reciprocal(out=ws[:, :], in_=ws[:, :]), reads=[ws], writes=[ws])
        p.op("dve", lambda e: e.tensor_scalar(self.call[:, t, :], w[:, :], ws[:, 0:1], None, ALU.mult), reads=[w, ws], writes=[self.call.bs[t]])

    def phase_moe(self, l, last):
        p = self.p
        W = self.W
        xT = self.xT
        st = ExitStack()
        stg = [p.sb(f"e_stg{i}", [128, 2048], F32, st) for i in range(2)]
        wg = [p.sb(f"e_wg{i}", [128, 8, 512], BF16, st) for i in range(2)]
        wu = [p.sb(f"e_wu{i}", [128, 8, 512], BF16, st) for i in range(2)]
        wd = [p.sb(f"e_wd{i}", [128, 4, 1024], BF16, st) for i in range(2)]
        yacc = p.sb("e_yacc", [128, 8, D], F32, st, nb=8)
        hT = [p.sb(f"e_hT{i}", [128, 4, 512], BF16, st) for i in range(2)]
        sgl = [p.sb(f"e_sg{i}", [128, 512], F32, st) for i in range(2)]
        xr = [p.sb(f"e_xr{i}", [128, D], F32, st) for i in range(1)]
        stats = p.sb("e_stats", [128, 2, 6], F32, st)
        mv = p.sb("e_mv", [128, 2], F32, st)
        rstd = p.sb("e_rstd", [128, 1], F32, st)
        self.load_ln_params(W["ln2_g"][l:l + 1, :], W["ln2_b"][l:l + 1, :], st)
        cast_i = 0
        k = 0
        for q4 in range(4):
            for ex in range(16):
                g_, u_, d_ = wg[ex % 2], wu[ex % 2], wd[ex % 2]
                for h2 in range(2):
                    self.wload(stg, g_, g_[:, h2 * 4:(h2 + 1) * 4, :], W["moe_w_gate"][l, ex, h2 * 512:(h2 + 1) * 512, :], 4, 512, cast=("pool", "dve")[h2])
                    self.wload(stg, u_, u_[:, h2 * 4:(h2 + 1) * 4, :], W["moe_w_up"][l, ex, h2 * 512:(h2 + 1) * 512, :], 4, 512, cast=("pool", "dve")[h2])
                    self.wload(stg, d_, d_[:, :, h2 * 512:(h2 + 1) * 512], W["moe_w_down"][l, ex, :, h2 * 512:(h2 + 1) * 512], 4, 512, cast=("pool", "dve")[h2])
                for tb2 in range(2):
                    tb = q4 * 2 + tb2
                    h_ = hT[k % 2]
                    k += 1
                    xr_ = [xT.bs[tb * 4 + i] for i in range(4)]
                    for fc in range(4):
                        pg = self.bank(0, 2)
                        for c in range(8):
                            self.mm(pg[:, :], g_[:, c, fc * 128:(fc + 1) * 128], xT[:, c, tb * 512:(tb + 1) * 512], c == 0, c == 7, [g_] + xr_, [pg])
                        pu = self.bank(2, 4)
                        for c in range(8):
                            self.mm(pu[:, :], u_[:, c, fc * 128:(fc + 1) * 128], xT[:, c, tb * 512:(tb + 1) * 512], c == 0, c == 7, [u_] + xr_, [pu])
                        s_ = sgl[fc % 2]
                        p.op("act", lambda e, s_=s_, pg=pg: e.activation(out=s_[:, :], in_=pg[:, :], func=AF.Silu), reads=[pg], writes=[s_])
                        p.op("dve", lambda e, s_=s_, pu=pu, h_=h_, fc=fc: e.tensor_tensor(out=h_[:, fc, :], in0=s_[:, :], in1=pu[:, :], op=ALU.mult), reads=[s_, pu], writes=[h_])
                    for tt in range(4):
                        t = tb * 4 + tt
                        tl = tb2 * 4 + tt
                        for h2 in range(2):
                            py = self.bank(4, 8)
                            for fc in range(4):
                                self.mm(py[:, :], h_[:, fc, tt * 128:(tt + 1) * 128], d_[:, fc, h2 * 512:(h2 + 1) * 512], fc == 0, fc == 3, [h_, d_], [py])
                            ya = yacc[:, tl, h2 * 512:(h2 + 1) * 512]
                            if ex == 0:
                                p.op("dve", lambda e, ya=ya, py=py, t=t, ex=ex: e.tensor_scalar(ya, py[:, :], self.call[:, t, ex:ex + 1], None, ALU.mult),
                                     reads=[py, self.call.bs[t]], writes=[yacc.bs[tl]])
                            else:
                                p.op("dve", lambda e, ya=ya, py=py, t=t, ex=ex: e.scalar_tensor_tensor(out=ya, in0=py[:, :], scalar=self.call[:, t, ex:ex + 1], in1=ya, op0=ALU.mult, op1=ALU.add),
                                     reads=[py, self.call.bs[t]], writes=[yacc.bs[tl]])
            for tl in range(8):
                t = q4 * 8 + tl
                x_ = xr[0]
                yv = Sub(yacc[:, tl, :], yacc.bs[tl])
                p.dma("sp", x_[:], self.xres[t * 128:(t + 1) * 128, :], reads=[self.xres.bs[t]], writes=[x_])
                p.op("dve", lambda e, x_=x_, tl=tl: e.scalar_tensor_tensor(out=yacc[:, tl, :], in0=x_[:, :], scalar=ALPHA, in1=yacc[:, tl, :], op0=ALU.mult, op1=ALU.add),
                     reads=[x_, yacc.bs[tl]], writes=[yacc.bs[tl]])
                self.ln_tile(yv, yv, stats, mv, rstd)
                if last:
                    p.dma("pool", self.out[t * 128:(t + 1) * 128, :], yv[:, :], reads=[yv], writes=[self.out.bs[t]])
                else:
                    p.dma("pool", self.xres[t * 128:(t + 1) * 128, :], yv[:, :], reads=[yv], writes=[self.xres.bs[t]])
                    self.to_xT(yv, t)
        p.barrier()
        st.close()

    def phase_gla(self, l):
        for hp in range(2):
            self._gla_hp(l, hp)

    def _gla_hp(self, l, hp):
        p = self.p
        W = self.W
        xT = self.xT
        xTr = list(xT.bs)
        if True:
            st = ExitStack()
            stg = [p.sb(f"g_stg{i}", [128, 1024], F32, st) for i in range(2)]
            wq = p.sb("g_wq", [128, 8, 128], BF16, st)
            wk = p.sb("g_wk", [128, 8, 128], BF16, st)
            wv = p.sb("g_wv", [128, 8, 256], BF16, st)
            wr = p.sb("g_wr", [128, 8, 256], BF16, st)
            wz = p.sb("g_wz", [128, 8, 32], BF16, st)
            wgf = p.sb("g_wgf", [16, 2, 128], F32, st)
            wgt = p.sb("g_wgt", [16, 2, 128], BF16, st)
            bg = p.sb("g_bg", [128, 2], F32, st)
            gG = p.sb("g_gG", [128, 128], F32, st)
            ones = p.sb("g_ones", [128, 1], F32, st)
            m4 = p.sb("g_m4", [128, 4, 64], F32, st)
            msk = p.sb("g_msk", [128, 8, 64], F32, st)
            qA1 = p.sb("g_qA1", [128, S], BF16, st)
            kA1 = p.sb("g_kA1", [128, S], BF16, st)
            qi1 = p.sb("g_qi1", [128, S], BF16, st)
            qA0 = [p.sb(f"g_qA0{i}", [128, 512], BF16, st) for i in range(2)]
            kA0 = [p.sb(f"g_kA0{i}", [128, 512], BF16, st) for i in range(2)]
            qi0 = [p.sb(f"g_qi0{i}", [128, 512], BF16, st) for i in range(2)]
            dec = [p.sb(f"g_dec{d}", [128, 64], F32, st) for d in range(2)]
            sts1 = p.sb("g_st1", [128, 64, 128], BF16, st)
            sts0 = [p.sb(f"g_st0{i}", [128, 8, 128], BF16, st) for i in range(2)]
            S32 = [p.sb(f"g_S32{d}", [128, 128], F32, st) for d in range(2)]
            v = p.sb("g_v", [128, NT, 256], BF16, st)
            zt = p.sb("g_zt", [16, 512], BF16, st)
            T1 = p.sb("g_T1", [128, 512], F32, st)
            T2 = p.sb("g_T2", [128, 512], F32, st)
            T3 = p.sb("g_T3", [128, 512], F32, st)
            T4 = p.sb("g_T4", [128, 512], F32, st)
            T5 = p.sb("g_T5", [128, 512], F32, st)
            kltr = [p.sb(f"g_klt{i}", [128, 4, 128], BF16, st) for i in range(2)]
            scT = [p.sb(f"g_scT{i}", [128, 4, 64], BF16, st) for i in range(2)]
            sr = p.sb("g_sr", [128, 256], F32, st)
            sq = p.sb("g_sq", [128, 2, 128], F32, st)
            ssq = p.sb("g_ssq", [128, 2], F32, st)
            oc = p.sb("g_oc", [128, 256], F32, st)
            ocT = [p.sb(f"g_ocT{i}", [128, 2, 512], BF16, st) for i in range(2)]
            c0 = COL["qc"] + hp * 128
            self.wload(stg, wq, wq[:, :, :], W["w_in"][l, :, c0:c0 + 128], 8, 128)
            p.op("pool", lambda e: e.tensor_scalar(wq[:, :, :], wq[:, :, :], 0.125, None, ALU.mult), reads=[wq], writes=[wq])
            c0 = COL["kc"] + hp * 128
            self.wload(stg, wk, wk[:, :, :], W["w_in"][l, :, c0:c0 + 128], 8, 128)
            for k2 in range(2):
                c0 = COL["vc"] + hp * 256 + k2 * 128
                self.wload(stg, wv, wv[:, :, k2 * 128:(k2 + 1) * 128], W["w_in"][l, :, c0:c0 + 128], 8, 128)
                c0 = COL["rc"] + hp * 256 + k2 * 128
                self.wload(stg, wr, wr[:, :, k2 * 128:(k2 + 1) * 128], W["w_in"][l, :, c0:c0 + 128], 8, 128)
            self.wload(stg, wz, wz[:, :, :], W["w_in"][l, :, COL["zf"]:COL["zf"] + 32], 8, 32)
            p.dma("sp", wgf[:], W["gla_w_gate"][l, :, :, hp * 128:(hp + 1) * 128].rearrange("d r n -> r d n"), writes=[wgf])
            p.op("dve", lambda e: e.tensor_copy(out=wgt[:, :, :], in_=wgf[:, :, :]), reads=[wgf], writes=[wgt])
            p.dma("sp", bg[:], W["gla_b_gate"][l, :, hp * 128:(hp + 1) * 128].rearrange("d n -> n d"), writes=[bg], allow_slow_non_contiguous=True)
            p.dma("sp", gG[:], W["gla_norm_g"][l:l + 1, :].partition_broadcast(128), writes=[gG])
            p.op("pool", lambda e: e.memset(ones[:, :], 1.0), writes=[ones])
            for d in range(2):
                for hh in range(2):
                    for half in range(2):
                        p.dma("sp", m4[half * 64:(half + 1) * 64, d * 2 + hh, :], self.C["gmask"][d, :, :], writes=[m4])
            p.op("pool", lambda e: e.memset(msk[:, :, :], 1.0), writes=[msk])
            p.op("pool", lambda e: e.memset(msk[:, :, 0:1], 0.0), reads=[msk], writes=[msk])
            for t in range(NT):
                ps = self.bank(0, 4)
                for c in range(8):
                    self.mm(ps[:, 0:256], xT[:, c, t * 128:(t + 1) * 128], wv[:, c, :], c == 0, c == 7, [wv, xTr[t]], [ps])
                p.op("act", lambda e, ps=ps, t=t: e.copy(out=v[:, t, :], in_=ps[:, 0:256]), reads=[ps], writes=[v])
            def arr(d, tb):
                if d == 1:
                    sl = slice(tb * 512, (tb + 1) * 512)
                    return (qA1, qA1[:, sl]), (kA1, kA1[:, sl]), (qi1, qi1[:, sl])
                i = tb % 2
                return (qA0[i], qA0[i][:, :]), (kA0[i], kA0[i][:, :]), (qi0[i], qi0[i][:, :])

            def chunk_view(d, which, n, hh):
                hs = slice(hh * 64, (hh + 1) * 64)
                if d == 1:
                    tl = (qA1, kA1, qi1)[which]
                    return tl, tl[hs, n * 64:(n + 1) * 64]
                tl = (qA0, kA0, qi0)[which][(n // 8) % 2]
                return tl, tl[hs, (n % 8) * 64:(n % 8 + 1) * 64]

            def state_view(d, n, hh):
                hs = slice(hh * 64, (hh + 1) * 64)
                if d == 1:
                    return sts1, sts1[hs, n, :]
                tl = sts0[(n // 8) % 2]
                return tl, tl[hs, n % 8, :]

            def sweep_A(d, tb):
                klt = kltr[tb % 2]
                xr_ = xTr[tb * 4:tb * 4 + 4]
                tsl = slice(tb * 512, (tb + 1) * 512)
                (qA_t, qA_ap), (kA_t, kA_ap), (qi_t, qi_ap) = arr(d, tb)
                pz = self.bank(0, 4)
                for c in range(8):
                    self.mm(pz[0:16, :], wz[:, c, d * 16:(d + 1) * 16], xT[:, c, tsl], c == 0, c == 7, [wz] + xr_, [pz])
                p.op("act", lambda e: e.copy(out=zt[:, :], in_=pz[0:16, :]), reads=[pz], writes=[zt])
                pg = self.bank(0, 4)
                self.mm(pg[:, :], wgt[0:16, d, :], zt[0:16, :], True, True, [wgt, zt], [pg])
                pq = self.bank(4, 6)
                for c in range(8):
                    self.mm(pq[:, :], wq[:, c, :], xT[:, c, tsl], c == 0, c == 7, [wq] + xr_, [pq])
                pk = self.bank(6, 8)
                for c in range(8):
                    self.mm(pk[:, :], wk[:, c, :], xT[:, c, tsl], c == 0, c == 7, [wk] + xr_, [pk])
                p.op("dve", lambda e: e.tensor_scalar(T1[:, :], pg[:, :], bg[:, d:d + 1], None, ALU.add), reads=[pg, bg], writes=[T1])
                p.op("dve", lambda e: e.scalar_tensor_tensor(out=T2[:, :], in0=T1[:, :], scalar=-1.0, in1=T1[:, :], op0=ALU.mult, op1=ALU.max), reads=[T1], writes=[T2])
                p.op("act", lambda e: e.activation(out=T2[:, :], in_=T2[:, :], func=AF.Exp, scale=-1.0), reads=[T2], writes=[T2])
                p.op("act", lambda e: e.activation(out=T2[:, :], in_=T2[:, :], func=AF.Ln, bias=ones[:, 0:1]), reads=[T2, ones], writes=[T2])
                p.op("dve", lambda e: e.scalar_tensor_tensor(out=T1[:, :], in0=T1[:, :], scalar=0.0, in1=T2[:, :], op0=ALU.min, op1=ALU.subtract), reads=[T1, T2], writes=[T1])
                p.op("act", lambda e: e.mul(out=T1[:, :], in_=T1[:, :], mul=1.0 / 16.0), reads=[T1], writes=[T1])
                p.op("dve", lambda e: e.tensor_tensor_scan(T3[:, :], msk[:, :, :].rearrange("p a b -> p (a b)"), T1[:, :], 0.0, ALU.mult, ALU.add), reads=[msk, T1], writes=[T3])
                c3 = T3[:, :].rearrange("p (a b) -> p a b", a=8)
                v4 = lambda tl: tl[:, :].rearrange("p (a b) -> p a b", a=8)
                if d == 1:
                    p.op("dve", lambda e: e.tensor_tensor(out=v4(T2), in0=c3[:, :, 63:64].to_broadcast([128, 8, 64]), in1=c3, op=ALU.subtract), reads=[T3], writes=[T2])
                    p.op("dve", lambda e: e.tensor_tensor(out=T3[:, :], in0=T2[:, :], in1=T1[:, :], op=ALU.add), reads=[T2, T1], writes=[T3])
                    ref, last = c3[:, :, 31:32], c3[:, :, 0:1]
                else:
                    ref, last = c3[:, :, 32:33], c3[:, :, 63:64]
                refb = ref.to_broadcast([128, 8, 64])
                lastb = last.to_broadcast([128, 8, 64])
                p.op("act", lambda e: e.activation(out=dec[d][:, tb * 8:(tb + 1) * 8].unsqueeze(2), in_=last, func=AF.Exp), reads=[T3], writes=[dec[d]])
                p.op("dve", lambda e: e.tensor_tensor(out=v4(T4), in0=c3, in1=refb, op=ALU.subtract), reads=[T3], writes=[T4])
                p.op("act", lambda e: e.activation(out=T5[:, :], in_=T4[:, :], func=AF.Exp), reads=[T4], writes=[T5])
                p.op("dve", lambda e: e.tensor_tensor(out=qA_ap, in0=pq[:, :], in1=T5[:, :], op=ALU.mult), reads=[pq, T5], writes=[qA_t])
                p.op("act", lambda e: e.activation(out=T5[:, :], in_=T4[:, :], func=AF.Exp, scale=-1.0), reads=[T4], writes=[T5])
                p.op("dve", lambda e: e.tensor_tensor(out=kA_ap, in0=pk[:, :], in1=T5[:, :], op=ALU.mult), reads=[pk, T5], writes=[kA_t])
                p.op("act", lambda e: e.activation(out=T5[:, :], in_=T3[:, :], func=AF.Exp), reads=[T3], writes=[T5])
                p.op("dve", lambda e: e.tensor_tensor(out=qi_ap, in0=pq[:, :], in1=T5[:, :], op=ALU.mult), reads=[pq, T5], writes=[qi_t])
                p.op("dve", lambda e: e.tensor_tensor(out=v4(T4), in0=lastb, in1=c3, op=ALU.subtract), reads=[T3], writes=[T4])
                p.op("act", lambda e: e.activation(out=T5[:, :], in_=T4[:, :], func=AF.Exp), reads=[T4], writes=[T5])
                p.op("dve", lambda e: e.tensor_tensor(out=T4[:, :], in0=pk[:, :], in1=T5[:, :], op=ALU.mult), reads=[pk, T5], writes=[T4])
                pt = self.bank(0, 4)
                for j in range(4):
                    p.op("pe", lambda e, j=j: e.transpose(pt[:, j * 128:(j + 1) * 128], T4[:, j * 128:(j + 1) * 128], self.identf[:, :]), reads=[T4, self.identf], writes=[pt])
                p.op("act", lambda e: e.copy(out=klt[:, :, :], in_=pt[:, :].rearrange("p (j n) -> p j n", j=4)), reads=[pt], writes=[klt])

            def sweep_B(d, tb):
                klt = kltr[tb % 2]
                cs = range(8) if d == 0 else range(7, -1, -1)
                for ci in cs:
                    n = tb * 8 + ci
                    tt, half = ci // 2, ci % 2
                    t = tb * 4 + tt
                    stl, _ = state_view(d, n, 0)
                    sap = sts1[:, n, :] if d == 1 else stl[:, n % 8, :]
                    p.op("act", lambda e, sap=sap: e.copy(out=sap, in_=S32[d][:, :]), reads=[S32[d]], writes=[stl])
                    pd = self.bank(0, 4)
                    for hh in range(2):
                        self.mm(pd[hh * 64:(hh + 1) * 64, 0:128], klt[half * 64:(half + 1) * 64, tt, hh * 64:(hh + 1) * 64],
                                v[half * 64:(half + 1) * 64, t, hh * 128:(hh + 1) * 128], True, True, [klt, v], [pd], serial=True)
                    p.op("dve", lambda e, pd=pd, n=n: e.scalar_tensor_tensor(out=S32[d][:, :], in0=S32[d][:, :], scalar=dec[d][:, n:n + 1], in1=pd[:, 0:128], op0=ALU.mult, op1=ALU.add),
                         reads=[pd, dec[d], S32[d]], writes=[S32[d]])

            def out_block(tb):
                ot_ = ocT[tb % 2]
                for tt in range(4):
                    t = tb * 4 + tt
                    pss = self.bank(0, 2)
                    for half in range(2):
                        n = t * 2 + half
                        first = True
                        for d in range(2):
                            for hh in range(2):
                                kt_, kap = chunk_view(d, 1, n, hh)
                                qt_, qap = chunk_view(d, 0, n, hh)
                                self.mm(pss[half * 64:(half + 1) * 64, (d * 2 + hh) * 64:(d * 2 + hh + 1) * 64], kap, qap, first, True, [kt_, qt_], [pss], serial=True)
                                first = False
                    sc_ = scT[t % 2]
                    p.op("dve", lambda e, pss=pss, sc_=sc_: e.tensor_tensor(out=sc_[:, :, :], in0=pss[:, 0:256].rearrange("p (a b) -> p a b", a=4), in1=m4[:, :, :], op=ALU.mult), reads=[pss, m4], writes=[sc_])
                    po = self.bank(2, 4)
                    for half in range(2):
                        n = t * 2 + half
                        hs = slice(half * 64, (half + 1) * 64)
                        first = True
                        for hh in range(2):
                            osl = po[hs, hh * 128:(hh + 1) * 128]
                            for d in range(2):
                                self.mm(osl, sc_[hs, d * 2 + hh, :], v[hs, t, hh * 128:(hh + 1) * 128], first, False, [sc_, v], [po], serial=True)
                                first = False
                            for d in range(2):
                                it_, iap = chunk_view(d, 2, n, hh)
                                st_, sap = state_view(d, n, hh)
                                self.mm(osl, iap, sap, False, d == 1, [it_, st_], [po], serial=True)
                    pr = self.bank(4, 8)
                    for c in range(8):
                        self.mm(pr[:, 0:256], xT[:, c, t * 128:(t + 1) * 128], wr[:, c, :], c == 0, c == 7, [wr, xTr[t]], [pr])
                    p.op("act", lambda e, pr=pr: e.activation(out=sr[:, :], in_=pr[:, 0:256], func=AF.Silu), reads=[pr], writes=[sr])
                    po3 = po[:, 0:256].rearrange("p (a b) -> p a b", a=2)
                    p.op("act", lambda e, po3=po3: e.activation(out=sq[:, :, :], in_=po3, func=AF.Square), reads=[po], writes=[sq])
                    p.op("dve", lambda e: e.tensor_reduce(out=ssq[:, :], in_=sq[:, :, :], axis=AX.X, op=ALU.add), reads=[sq], writes=[ssq])
                    self.rsqrt(ssq, ssq[:, :], ssq, ssq[:, :], 1.0 / 128.0)
                    for hh in range(2):
                        p.op("dve", lambda e, po=po, hh=hh: e.scalar_tensor_tensor(out=oc[:, hh * 128:(hh + 1) * 128], in0=po[:, hh * 128:(hh + 1) * 128], scalar=ssq[:, hh:hh + 1], in1=gG[:, :],
                                                                                  op0=ALU.mult, op1=ALU.mult), reads=[po, ssq, gG], writes=[oc])
                    p.op("dve", lambda e: e.tensor_tensor(out=oc[:, :], in0=oc[:, :], in1=sr[:, :], op=ALU.mult), reads=[oc, sr], writes=[oc])
                    pt = self.bank(4, 8)
                    for hh in range(2):
                        p.op("pe", lambda e, pt=pt, hh=hh: e.transpose(pt[:, hh * 128:(hh + 1) * 128], oc[:, hh * 128:(hh + 1) * 128], self.identf[:, :]), reads=[oc, self.identf], writes=[pt])
                    p.op("act", lambda e, pt=pt, tt=tt: e.copy(out=ot_[:, :, tt * 128:(tt + 1) * 128], in_=pt[:, 0:256].rearrange("p (a b) -> p a b", a=2)), reads=[pt], writes=[ot_])
                p.dma("pool", self.oT[2, hp * 2:(hp + 1) * 2, :, tb * 512:(tb + 1) * 512].rearrange("c p t -> p c t"), ot_[:, :, :], reads=[ot_], writes=[self.oT.bs[8 + hp * 2], self.oT.bs[8 + hp * 2 + 1]])

            for d in (1, 0):
                p.op("pool", lambda e, d=d: e.memset(S32[d][:, :], 0.0), writes=[S32[d]])
            order1 = list(range(7, -1, -1))
            sweep_A(1, order1[0])
            for i, tb in enumerate(order1):
                if i + 1 < 8:
                    sweep_A(1, order1[i + 1])
                sweep_B(1, tb)
            sweep_A(0, 0)
            for tb in range(8):
                if tb + 1 < 8:
                    sweep_A(0, tb + 1)
                sweep_B(0, tb)
                out_block(tb)
            p.barrier()
            st.close()

    def phase_s5(self, l):
        p = self.p
        W = self.W
        xT = self.xT
        xTr = list(xT.bs)
        st = ExitStack()
        sm = lambda n, shp=(128, 32): p.sb("s_" + n, list(shp), F32, st)
        stg = [p.sb(f"s_stg{i}", [128, 1024], F32, st) for i in range(2)]
        identb = p.sb("s_identb", [128, 128], BF16, st)
        jx = sm("jx", (128, 128))
        rowm = sm("rowm", (128, 8))
        sgn = sm("sgn", (128, 2))
        hpi = sm("hpi", (128, 1))
        wu = p.sb("s_wu", [128, 8, 128], BF16, st)
        wglu = p.sb("s_wglu", [128, 4, 512], BF16, st)
        bglu = sm("bglu", (128, 4))
        dcol = sm("dcol", (128, 4))
        CST = [[p.sb(f"s_cst{d}{b}", [128, 128], F32, st) for b in range(4)] for d in range(2)]
        BT = [[p.sb(f"s_bt{d}{b}", [128, 128], F32, st) for b in range(4)] for d in range(2)]
        PRt = [sm(f"pr{d}", (128, 12, 32)) for d in range(2)]
        PQt = [sm(f"pq{d}", (128, 12, 32)) for d in range(2)]
        pst = ExitStack()
        smp = lambda n, shp=(128, 32): p.sb("s_" + n, list(shp), F32, pst)
        p.dma("sp", jx[:], self.C["jx"][:, :], writes=[jx])
        p.dma("sp", rowm[:], self.C["rowmask"][:, :], writes=[rowm])
        p.dma("sp", sgn[:], self.C["sgn"][:, :], writes=[sgn])
        p.op("dve", lambda e: e.tensor_copy(out=identb[:, :], in_=self.identf[:, :]), reads=[self.identf], writes=[identb])
        p.op("pool", lambda e: e.memset(hpi[:, :], PI / 2), writes=[hpi])
        for b in range(4):
            self.wload(stg, wglu, wglu[:, b:b + 1, :], W["s5_w_glu"][l, b * 128:(b + 1) * 128, :], 1, 512)
        p.dma("sp", bglu[:], W["s5_b_glu"][l, :].rearrange("(m q) -> q m", q=128), writes=[bglu], allow_slow_non_contiguous=True)
        p.dma("sp", dcol[:], W["s5_d"][l, :].rearrange("(m q) -> q m", q=128), writes=[dcol], allow_slow_non_contiguous=True)

        PR, PQ, BST = [], [], []
        Cn = smp("Cn", (128, 128))
        dv = lambda fn, r, w: p.op("dve", fn, reads=r, writes=w)
        for d in range(2):
            are, aim, dt, lr, li, m1, cc, ss, t1, t2, nr, ni, den, cr, ci = (smp(f"{n}{d}") for n in
                                                                             ("are", "aim", "dt", "lr", "li", "m1", "cc", "ss", "t1", "t2", "nr", "ni", "den", "cr", "ci"))
            Xb = smp(f"Xb{d}", (128, 32, 16))
            Yb = smp(f"Yb{d}", (128, 32, 16))
            Bst = smp(f"Bst{d}", (128, 32, 16))
            Tb = smp(f"Tb{d}", (128, 32, 16))
            pr = PRt[d]
            pq = PQt[d]
            for hf in range(2):
                hs = slice(hf * 64, (hf + 1) * 64)
                p.dma("sp", are[hs, :], W["s5_a_re"][l, d, :, :].rearrange("g q -> q g"), writes=[are], allow_slow_non_contiguous=True)
                p.dma("sp", aim[hs, :], W["s5_a_im"][l, d, :, :].rearrange("g q -> q g"), writes=[aim], allow_slow_non_contiguous=True)
                own, oth = ("s5_b_re", "s5_b_im") if hf == 0 else ("s5_b_im", "s5_b_re")
                p.dma("sp", Xb[hs, :, :], W[own][l, d, :, :, :].rearrange("g q c -> q g c"), writes=[Xb])
                p.dma("sp", Yb[hs, :, :], W[oth][l, d, :, :, :].rearrange("g q c -> q g c"), writes=[Yb])
            p.dma("sp", dt[:], W["s5_log_dt"][l, d:d + 1, :].partition_broadcast(128), writes=[dt])
            p.op("act", lambda e, dt=dt: e.activation(out=dt[:, :], in_=dt[:, :], func=AF.Exp), reads=[dt], writes=[dt])
            TT = lambda o, a, b_, op: (lambda e: e.tensor_tensor(out=o[:, :], in0=a[:, :], in1=b_[:, :], op=op))
            dv(TT(lr, are, dt, ALU.mult), [are, dt], [lr])
            dv(TT(li, aim, dt, ALU.mult), [aim, dt], [li])
            TS = lambda o, a, s1, s2, o0, o1=None: (lambda e: e.tensor_scalar(o[:, :], a[:, :], s1, s2, o0, o1) if o1 is not None else e.tensor_scalar(o[:, :], a[:, :], s1, None, o0))
            dv(TS(m1, lr, 0.25, 1.0, ALU.mult, ALU.add), [lr], [m1])
            dv(TT(m1, m1, lr, ALU.mult), [m1, lr], [m1])
            dv(TS(m1, m1, 1.0 / 3.0, 1.0, ALU.mult, ALU.add), [m1], [m1])
            dv(TT(m1, m1, lr, ALU.mult), [m1, lr], [m1])
            dv(TS(m1, m1, 0.5, 1.0, ALU.mult, ALU.add), [m1], [m1])
            dv(TT(m1, m1, lr, ALU.mult), [m1, lr], [m1])
            dv(TS(m1, m1, 1.0, None, ALU.add), [m1], [m1])
            dv(TS(nr, li, 1.0 / 256.0, None, ALU.mult), [li], [nr])
            dv(TT(t1, nr, nr, ALU.mult), [nr], [t1])
            dv(TS(ss, t1, 1.0 / 120.0, -1.0 / 6.0, ALU.mult, ALU.add), [t1], [ss])
            dv(TT(ss, ss, t1, ALU.mult), [ss, t1], [ss])
            dv(TS(ss, ss, 1.0, None, ALU.add), [ss], [ss])
            dv(TT(ss, ss, nr, ALU.mult), [ss, nr], [ss])
            dv(TS(cc, t1, -1.0 / 720.0, 1.0 / 24.0, ALU.mult, ALU.add), [t1], [cc])
            dv(TT(cc, cc, t1, ALU.mult), [cc, t1], [cc])
            dv(TS(cc, cc, -0.5, None, ALU.add), [cc], [cc])
            dv(TT(cc, cc, t1, ALU.mult), [cc, t1], [cc])
            dv(TS(cc, cc, 1.0, None, ALU.add), [cc], [cc])
            for _ in range(8):
                dv(TT(t1, cc, cc, ALU.mult), [cc], [t1])
                dv(TT(t2, ss, ss, ALU.mult), [ss], [t2])
                dv(lambda e, cc=cc, ss=ss: e.scalar_tensor_tensor(out=ss[:, :], in0=cc[:, :], scalar=2.0, in1=ss[:, :], op0=ALU.mult, op1=ALU.mult), [cc, ss], [ss])
                dv(TT(cc, t1, t2, ALU.subtract), [t1, t2], [cc])
            dv(TT(t1, cc, cc, ALU.mult), [cc], [t1])
            dv(TT(t2, ss, ss, ALU.mult), [ss], [t2])
            dv(TT(t1, t1, t2, ALU.add), [t1, t2], [t1])
            dv(TS(t1, t1, -0.5, 1.5, ALU.mult, ALU.add), [t1], [t1])
            dv(TT(cc, cc, t1, ALU.mult), [cc, t1], [cc])
            dv(TT(ss, ss, t1, ALU.mult), [ss, t1], [ss])
            dv(lambda e, pr=pr, m1=m1, cc=cc: e.tensor_tensor(out=pr[:, 0, :], in0=m1[:, :], in1=cc[:, :], op=ALU.mult), [m1, cc], [pr])
            dv(TT(ni, m1, ss, ALU.mult), [m1, ss], [ni])
            dv(lambda e, pq=pq, ni=ni: e.tensor_scalar(pq[:, 0, :], ni[:, :], sgn[:, 1:2], None, ALU.mult), [ni, sgn], [pq])
            dv(lambda e, nr=nr, pr=pr: e.tensor_scalar(nr[:, :], pr[:, 0, :], -1.0, None, ALU.add), [pr], [nr])
            dv(TT(t1, are, are, ALU.mult), [are], [t1])
            dv(TT(t2, aim, aim, ALU.mult), [aim], [t2])
            dv(TT(den, t1, t2, ALU.add), [t1, t2], [den])
            dv(lambda e, den=den: e.reciprocal(out=den[:, :], in_=den[:, :]), [den], [den])
            dv(TT(t1, nr, are, ALU.mult), [nr, are], [t1])
            dv(TT(t2, ni, aim, ALU.mult), [ni, aim], [t2])
            dv(TT(cr, t1, t2, ALU.add), [t1, t2], [cr])
            dv(TT(cr, cr, den, ALU.mult), [cr, den], [cr])
            dv(TT(t1, ni, are, ALU.mult), [ni, are], [t1])
            dv(TT(t2, nr, aim, ALU.mult), [nr, aim], [t2])
            dv(TT(ci, t1, t2, ALU.subtract), [t1, t2], [ci])
            dv(TT(ci, ci, den, ALU.mult), [ci, den], [ci])
            dv(lambda e, ci=ci: e.tensor_scalar(ci[:, :], ci[:, :], sgn[:, 0:1], None, ALU.mult), [ci, sgn], [ci])
            bc = lambda t_: t_[:, :].unsqueeze(2).to_broadcast([128, 32, 16])
            dv(lambda e, Bst=Bst, Xb=Xb, cr=cr: e.tensor_tensor(out=Bst[:, :, :], in0=Xb[:, :, :], in1=bc(cr), op=ALU.mult), [Xb, cr], [Bst])
            dv(lambda e, Tb=Tb, Yb=Yb, ci=ci: e.tensor_tensor(out=Tb[:, :, :], in0=Yb[:, :, :], in1=bc(ci), op=ALU.mult), [Yb, ci], [Tb])
            dv(lambda e, Bst=Bst, Tb=Tb: e.tensor_tensor(out=Bst[:, :, :], in0=Bst[:, :, :], in1=Tb[:, :, :], op=ALU.add), [Bst, Tb], [Bst])
            for k in range(11):
                dv(lambda e, k=k, pr=pr, t1=t1: e.tensor_tensor(out=t1[:, :], in0=pr[:, k, :], in1=pr[:, k, :], op=ALU.mult), [pr], [t1])
                dv(lambda e, k=k, pq=pq, t2=t2: e.tensor_tensor(out=t2[:, :], in0=pq[:, k, :], in1=pq[:, k, :], op=ALU.mult), [pq], [t2])
                dv(lambda e, k=k, pr=pr, pq=pq: e.scalar_tensor_tensor(out=pq[:, k + 1, :], in0=pr[:, k, :], scalar=2.0, in1=pq[:, k, :], op0=ALU.mult, op1=ALU.mult), [pr, pq], [pq])
                dv(lambda e, k=k, pr=pr, t1=t1, t2=t2: e.tensor_tensor(out=pr[:, k + 1, :], in0=t1[:, :], in1=t2[:, :], op=ALU.subtract), [t1, t2], [pr])
            PR.append(pr)
            PQ.append(pq)
            if d == 0:
                for nm, tl_ in (("are", are), ("aim", aim), ("dt", dt), ("m1", m1), ("cc", cc), ("ss", ss), ("cr", cr), ("ci", ci)):
                    self.dump(nm, tl_, tl_[:, :], [128, 32])
                self.dump("pr", pr, pr[:, :, :], [128, 12, 32])
                self.dump("pq", pq, pq[:, :, :], [128, 12, 32])
                self.dump("Bst", Bst, Bst[:, :, :], [128, 32, 16])
            for b in range(4):
                ps = self.bank(0, 4)
                p.op("pe", lambda e, ps=ps, Bst=Bst, b=b: e.transpose(ps[:, 0:128], Bst[:, b * 8:(b + 1) * 8, :].rearrange("q g c -> q (g c)"), self.identf[:, :]), reads=[Bst, self.identf], writes=[ps])
                p.op("act", lambda e, ps=ps, d=d, b=b: e.copy(out=BT[d][b][:, :], in_=ps[:, 0:128]), reads=[ps], writes=[BT[d][b]])
                p.dma("sp", Cn[:, 0:64], W["s5_c_re"][l, d, b * 8:(b + 1) * 8, :, :].rearrange("g c q -> (g c) q"), writes=[Cn])
                p.dma("sp", Cn[:, 64:128], W["s5_c_im"][l, d, b * 8:(b + 1) * 8, :, :].rearrange("g c q -> (g c) q"), writes=[Cn])
                ps = self.bank(0, 4)
                p.op("pe", lambda e, ps=ps: e.transpose(ps[:, 0:128], Cn[:, :], self.identf[:, :]), reads=[Cn, self.identf], writes=[ps])
                p.op("dve", lambda e, ps=ps, d=d, b=b: e.tensor_scalar(CST[d][b][:, :], ps[:, 0:128], sgn[:, 1:2], None, ALU.mult), reads=[ps, sgn], writes=[CST[d][b]])

        for b_ in (0, 3):
            self.dump(f"BT{b_}", BT[0][b_], BT[0][b_][:, :], [128, 128])
            self.dump(f"CST{b_}", CST[0][b_], CST[0][b_][:, :], [128, 128])
        p.barrier()
        pst.close()
        uT = p.sb("s_uT", [128, S], BF16, st)
        X = [[p.sb(f"s_X{d}{i}", [128, S], BF16, st, nb=8) for i in range(2)] for d in range(2)]
        yacc = p.sb("s_yacc", [128, S], F32, st, nb=8)
        ygT = p.sb("s_ygT", [128, 4, S], BF16, st)
        Mk = [p.sb(f"s_Mk{i}", [128, 128], BF16, st) for i in range(8)]
        Mt = [p.sb(f"s_Mt{i}", [128, 128], F32, st) for i in range(4)]
        Mt2 = [p.sb(f"s_Mt2{i}", [128, 128], F32, st) for i in range(4)]
        LB = [p.sb(f"s_LB{i}", [128, 128], BF16, st) for i in range(4)]
        LC = [p.sb(f"s_LC{i}", [128, 128], BF16, st) for i in range(4)]
        yt = [p.sb(f"s_yt{i}", [128, 512], F32, st) for i in range(2)]
        ob = [p.sb(f"s_ob{i}", [128, 512], BF16, st) for i in range(2)]
        dirs = self.dbg.get("s5_dirs", (0, 1))
        mki = 0
        gi = 0
        bk = [0]

        def nb(lo, hi):
            b_ = self.PS[lo + bk[0] % (hi - lo)]
            bk[0] += 1
            return b_

        for b in self.dbg.get("s5_tiles", range(4)):
            c0 = COL["u"] + b * 128
            self.wload(stg, wu, wu[:, :, :], W["w_in"][l, :, c0:c0 + 128], 8, 128)
            for ct in range(8):
                ps = self.bank(0, 4)
                for c in range(8):
                    self.mm(ps[:, :], wu[:, c, :], xT[:, c, ct * 512:(ct + 1) * 512], c == 0, c == 7, [wu] + xTr[ct * 4:ct * 4 + 4], [ps])
                p.op("act", lambda e, ps=ps, ct=ct: e.copy(out=uT[:, ct * 512:(ct + 1) * 512], in_=ps[:, :]), reads=[ps], writes=[uT])
            first_acc = True
            for gl in range(8):
                g = b * 8 + gl
                lbs, lcs = {}, {}
                for d in dirs:
                    lb = LB[gi % 4]
                    lc = LC[gi % 4]
                    gi += 1
                    lbs[d], lcs[d] = lb, lc
                    p.op("dve", lambda e, lb=lb, d=d, gl=gl, b=b: e.tensor_scalar(lb[:, :], BT[d][b][:, :], rowm[:, gl:gl + 1], None, ALU.mult), reads=[BT[d][b], rowm], writes=[lb])
                    p.op("pool", lambda e, lc=lc: e.memset(lc[:, :], 0.0), writes=[lc])
                    p.op("pool", lambda e, lc=lc, d=d, gl=gl, b=b: e.tensor_copy(out=lc[:, gl * 16:(gl + 1) * 16], in_=CST[d][b][:, gl * 16:(gl + 1) * 16]), reads=[CST[d][b], lc], writes=[lc])
                    for ct in range(8):
                        ps = nb(0, 8)
                        self.mm(ps[:, :], lb[:, :], uT[:, ct * 512:(ct + 1) * 512], True, True, [lb, uT], [ps])
                        if ct % 2 == 0:
                            p.op("act", lambda e, ps=ps, ct=ct, d=d: e.copy(out=X[d][0][:, ct * 512:(ct + 1) * 512], in_=ps[:, :]), reads=[ps], writes=[X[d][0].bs[ct]])
                        else:
                            p.op("dve", lambda e, ps=ps, ct=ct, d=d: e.tensor_copy(out=X[d][0][:, ct * 512:(ct + 1) * 512], in_=ps[:, :]), reads=[ps], writes=[X[d][0].bs[ct]])
                cur = 0
                mkq = {}

                def build_mk(k, g=g):
                    nonlocal mki
                    for d in dirs:
                        mt = Mt[mki % 4]
                        mk = Mk[mki % 8]
                        mki += 1
                        mkq[(k, d)] = mk
                        p.op("act", lambda e, mt=mt, d=d, k=k, g=g: e.mul(out=mt[:, :], in_=jx[:, :], mul=PQ[d][:, k, g:g + 1]), reads=[jx, PQ[d]], writes=[mt])
                        p.op("dve", lambda e, mt=mt, mk=mk, d=d, k=k, g=g: e.scalar_tensor_tensor(out=mk[:, :], in0=self.identf[:, :], scalar=PR[d][:, k, g:g + 1], in1=mt[:, :], op0=ALU.mult, op1=ALU.add),
                             reads=[self.identf, PR[d], mt], writes=[mk])

                build_mk(0)
                build_mk(1)
                for k in range(12):
                    sh = 1 << k
                    if k + 2 < 12:
                        build_mk(k + 2)
                    mks = {d: mkq[(k, d)] for d in dirs}
                    for ct in range(8):
                        for d in dirs:
                            mk = mks[d]
                            Xs, Xd = X[d][cur], X[d][1 - cur]
                            t0 = ct * 512
                            if d == 0:
                                lo = max(0, sh - t0)
                                hi = 512
                                s0 = t0 + lo - sh
                            else:
                                lo = 0
                                hi = min(512, S - sh - t0)
                                s0 = t0 + sh
                            has = hi > lo
                            srcb = []
                            if has:
                                a0, a1 = s0, s0 + (hi - lo)
                                srcb = [Xs.bs[i] for i in range(a0 // 512, (a1 - 1) // 512 + 1)]
                            use_pe = ((ct + d) % 2 == 0) or not has
                            ps = nb(0, 8)
                            if use_pe:
                                self.mm(ps[:, :], identb[:, :], Xs[:, t0:t0 + 512], True, not has, [identb, Xs.bs[ct]], [ps])
                                if has:
                                    self.mm(ps[:, lo:hi], mk[:, :], Xs[:, s0:s0 + hi - lo], False, True, [mk] + srcb, [ps])
                                p.op("act", lambda e, ps=ps, Xd=Xd, t0=t0: e.copy(out=Xd[:, t0:t0 + 512], in_=ps[:, :]), reads=[ps], writes=[Xd.bs[ct]])
                            else:
                                self.mm(ps[:, lo:hi], mk[:, :], Xs[:, s0:s0 + hi - lo], True, True, [mk] + srcb, [ps])
                                p.op("dve", lambda e, ps=ps, Xd=Xd, Xs=Xs, t0=t0, lo=lo, hi=hi: e.tensor_tensor(out=Xd[:, t0 + lo:t0 + hi], in0=ps[:, lo:hi], in1=Xs[:, t0 + lo:t0 + hi], op=ALU.add),
                                     reads=[ps, Xs.bs[ct]], writes=[Xd.bs[ct]])
                                if lo > 0:
                                    p.op("pool", lambda e, Xd=Xd, Xs=Xs, t0=t0, lo=lo: e.tensor_copy(out=Xd[:, t0:t0 + lo], in_=Xs[:, t0:t0 + lo]), reads=[Xs.bs[ct]], writes=[Xd.bs[ct]])
                                if hi < 512:
                                    p.op("pool", lambda e, Xd=Xd, Xs=Xs, t0=t0, hi=hi: e.tensor_copy(out=Xd[:, t0 + hi:t0 + 512], in_=Xs[:, t0 + hi:t0 + 512]), reads=[Xs.bs[ct]], writes=[Xd.bs[ct]])
                    cur = 1 - cur
                for ct in range(8):
                    ps = nb(0, 8)
                    for i_, d in enumerate(dirs):
                        self.mm(ps[:, :], lcs[d][:, :], X[d][cur][:, ct * 512:(ct + 1) * 512], i_ == 0, i_ == len(dirs) - 1, [lcs[d], X[d][cur].bs[ct]], [ps])
                    ya = yacc[:, ct * 512:(ct + 1) * 512]
                    if first_acc:
                        p.op("dve", lambda e, ps=ps, ya=ya: e.tensor_copy(out=ya, in_=ps[:, :]), reads=[ps], writes=[yacc.bs[ct]])
                    else:
                        p.op("dve", lambda e, ps=ps, ya=ya: e.tensor_tensor(out=ya, in0=ps[:, :], in1=ya, op=ALU.add), reads=[ps], writes=[yacc.bs[ct]])
                first_acc = False
            for ct in range(8):
                y_ = yt[ct % 2]
                sl = slice(ct * 512, (ct + 1) * 512)
                p.op("dve", lambda e, y_=y_, sl=sl, b=b: e.scalar_tensor_tensor(out=y_[:, :], in0=uT[:, sl], scalar=dcol[:, b:b + 1], in1=yacc[:, sl], op0=ALU.mult, op1=ALU.add),
                     reads=[uT, dcol, yacc.bs[ct]], writes=[y_])
                if self.dbg.get("s5y"):
                    p.dma("pool", self.dbgy[b, :, sl], y_[:, :], reads=[y_], writes=[self.dbgy])
                    p.dma("pool", self.dbgy[4 + b, :, sl], yacc[:, sl], reads=[yacc.bs[ct]], writes=[self.dbgy])
                p.op("act", lambda e, y_=y_, sl=sl, b=b: e.activation(out=ygT[:, b, sl], in_=y_[:, :], func=AF.Gelu_apprx_tanh), reads=[y_], writes=[ygT])
        for m in range(4):
            for ct in range(8):
                sl = slice(ct * 512, (ct + 1) * 512)
                ps = self.bank(0, 8)
                for b in range(4):
                    self.mm(ps[:, :], wglu[:, b, m * 128:(m + 1) * 128], ygT[:, b, sl], b == 0, b == 3, [wglu, ygT], [ps])
                y_ = yt[ct % 2]
                o_ = ob[ct % 2]
                p.op("act", lambda e, ps=ps, y_=y_, m=m: e.activation(out=y_[:, :], in_=ps[:, :], func=AF.Sigmoid, bias=bglu[:, m:m + 1]), reads=[ps, bglu], writes=[y_])
                p.op("dve", lambda e, y_=y_, o_=o_, m=m, sl=sl: e.tensor_tensor(out=o_[:, :], in0=ygT[:, m, sl], in1=y_[:, :], op=ALU.mult), reads=[ygT, y_], writes=[o_])
                p.dma("pool", self.oT[1, m, :, sl], o_[:, :], reads=[o_], writes=[self.oT.bs[4 + m]])
        p.barrier()
        st.close()


def build_model():
    M = Model()
    M.phase_ln0()
    for l in range(DEPTH):
        M.phase_attn(l)
        M.phase_s5(l)
        M.phase_gla(l)
        M.phase_merge1(l)
        M.phase_merge2(l)
        M.phase_moe(l, last=(l == DEPTH - 1))
    M.p.emit()
    return M


def kernel(**inputs):
    M = build_model()
    consts = host_consts()
    shared = {k: np.ascontiguousarray(np.asarray(inputs[k], dtype=np.float32)) for k in PARAM_SHAPES}
    shared.update(consts)
    x = np.asarray(inputs["x"], dtype=np.float32)
    in_maps = []
    for b in range(8):
        m = dict(shared)
        m["x"] = np.ascontiguousarray(x[b])
        in_maps.append(m)
    res = run_bass_kernel_spmd(M.nc, in_maps, core_ids=list(range(8)))
    return np.stack([np.asarray(r["out"], dtype=np.float32) for r in res.results], axis=0)
```

```python
import math
from contextlib import ExitStack
import numpy as np
import ml_dtypes
import concourse.bass as bass
import concourse.mybir as mybir
from concourse.bass_utils import run_bass_kernel_spmd

F32 = mybir.dt.float32
BF16 = mybir.dt.bfloat16
AF = mybir.ActivationFunctionType
ALU = mybir.AluOpType
AX = mybir.AxisListType

ENGS = ("pe", "act", "dve", "pool", "sp")
NDMA_SEM = 6


class Buf:
    __slots__ = ("name", "w", "r", "excl")

    def __init__(self, name=""):
        self.name = name
        self.w = None
        self.r = {}
        self.excl = False


class Tl:
    def __init__(self, t, name, nb=0):
        self.t = t
        self.b = Buf(name)
        self.bs = [Buf(f"{name}{i}") for i in range(nb)]

    def __getitem__(self, k):
        return self.t[k]


class Sub(Tl):
    def __init__(self, ap, buf):
        self.t = ap
        self.b = buf
        self.bs = []


class Prog:
    def __init__(self, nc):
        self.nc = nc
        self.stack = ExitStack()
        self.lists = {e: [] for e in ENGS}
        self.cnt = {e: 0 for e in ENGS}
        self.known = {e: {} for e in ENGS}
        self.sem = {}
        for e in ("pe", "act", "dve", "pool"):
            self.sem["c_" + e] = self.stack.enter_context(nc.semaphore("c_" + e))
        self.dq = {}
        for q in ("sp", "pool", "act"):
            names = []
            for i in range(NDMA_SEM):
                n = f"d_{q}{i}"
                self.sem[n] = self.stack.enter_context(nc.semaphore(n))
                names.append(n)
            self.dq[q] = dict(sems=names, issued=[0] * NDMA_SEM, rr=0)
        self.nwaits = 0

    uid = 0

    def sb(self, name, shape, dtype, stack=None, nb=0):
        Prog.uid += 1
        name = f"{name}_{Prog.uid}"
        t = (stack or self.stack).enter_context(self.nc.sbuf_tensor(name, list(shape), dtype))
        return Tl(t, name, nb)

    def ps(self, name, shape, dtype=F32, stack=None, nb=0):
        t = (stack or self.stack).enter_context(self.nc.psum_tensor(name, list(shape), dtype))
        tl = Tl(t, name, nb)
        tl.b.excl = True
        return tl

    def dram(self, name, shape, dtype, kind="Internal", nb=0):
        t = self.nc.dram_tensor(name, list(shape), dtype, kind=kind).ap()
        return Tl(t, name, nb)

    @staticmethod
    def _bufs(xs):
        out = []
        for x in xs:
            if x is None:
                continue
            out.append(x.b if isinstance(x, Tl) else x)
        return out

    def _need(self, reads, writes):
        need = {}

        def add(sv):
            s, v = sv
            if need.get(s, 0) < v:
                need[s] = v

        for b in reads:
            if b.w:
                add(b.w)
        for b in writes:
            if b.w:
                add(b.w)
            for sv in b.r.items():
                add(sv)
        return need

    def _waits(self, e, need, skip=None):
        kn = self.known[e]
        for s, v in need.items():
            if s == skip:
                continue
            if kn.get(s, 0) < v:
                self.lists[e].append(("wait", s, v))
                kn[s] = v
                self.nwaits += 1

    def op(self, e, fn, reads=(), writes=(), serial=False):
        reads = self._bufs(reads)
        writes = self._bufs(writes)
        ex = [b for b in reads if b.excl and b not in writes]
        if ex:
            reads = [b for b in reads if not b.excl]
            writes = writes + ex
        s = "c_" + e
        self._waits(e, self._need(reads, writes), skip=(s if (e == "pe" and not serial) else None))
        self.cnt[e] += 1
        v = self.cnt[e]
        self.lists[e].append(("op", fn, s))
        for b in reads:
            b.r[s] = v
        for b in writes:
            b.w = (s, v)
            b.r = {}

    def dma(self, q, out, in_, reads=(), writes=(), **kw):
        reads = self._bufs(reads)
        writes = self._bufs(writes)
        d = self.dq[q]
        i = d["rr"]
        d["rr"] = (i + 1) % NDMA_SEM
        s = d["sems"][i]
        need = self._need(reads, writes)
        if d["issued"][i] > 0:
            pv = 16 * d["issued"][i]
            if need.get(s, 0) < pv:
                need[s] = pv
        self._waits(q, need)
        d["issued"][i] += 1
        v = 16 * d["issued"][i]
        self.lists[q].append(("dma", out, in_, s, kw))
        for b in reads:
            b.r[s] = v
        for b in writes:
            b.w = (s, v)
            b.r = {}

    def barrier(self, engines=ENGS):
        need = {}
        for e in ("pe", "act", "dve", "pool"):
            if self.cnt[e]:
                need["c_" + e] = self.cnt[e]
        for q, d in self.dq.items():
            for s, n in zip(d["sems"], d["issued"]):
                if n:
                    need[s] = 16 * n
        for e in engines:
            self._waits(e, dict(need), skip=("c_" + e if e in ("pe",) else None))

    def emit(self):
        nc = self.nc
        self.barrier(engines=("sp",))
        lists = self.lists
        sem = self.sem

        def run(eng, lst):
            for it in lst:
                if it[0] == "wait":
                    eng.wait_ge(sem[it[1]], it[2])
                elif it[0] == "op":
                    it[1](eng).then_inc(sem[it[2]], 1)
                else:
                    eng.dma_start(out=it[1], in_=it[2], **it[4]).then_inc(sem[it[3]], 16)

        with nc.Block() as block:
            @block.tensor
            def _(eng):
                run(eng, lists["pe"])

            @block.scalar
            def _(eng):
                run(eng, lists["act"])

            @block.vector
            def _(eng):
                run(eng, lists["dve"])

            @block.gpsimd
            def _(eng):
                run(eng, lists["pool"])

            @block.sync
            def _(eng):
                run(eng, lists["sp"])
        self.stack.close()


S = 4096
D = 1024
NT = 32
DEPTH = 2
N_IN = 6688
COL = dict(qa=0, ka=512, va=1024, u=1536, qc=2048, kc=2304, vc=2560, rc=3072, zf=3584, zb=3600, gate=3616)
ALPHA = (2.0 * DEPTH) ** 0.25
EPS = 1e-5
PI = math.pi

PARAM_SHAPES = dict(
    ln0_g=[1024], ln0_b=[1024], w_in=[2, 1024, 6688], da_lambda=[2, 4, 64], da_norm_g=[2, 128],
    s5_a_re=[2, 2, 32, 64], s5_a_im=[2, 2, 32, 64], s5_log_dt=[2, 2, 32], s5_b_re=[2, 2, 32, 64, 16],
    s5_b_im=[2, 2, 32, 64, 16], s5_c_re=[2, 2, 32, 16, 64], s5_c_im=[2, 2, 32, 16, 64], s5_d=[2, 512],
    s5_w_glu=[2, 512, 512], s5_b_glu=[2, 512], gla_w_gate=[2, 2, 16, 256], gla_b_gate=[2, 2, 256],
    gla_norm_g=[2, 128], merge_w_up=[2, 3, 512, 1024], merge_b=[2, 3, 1024], w_out=[2, 1024, 1024],
    ln1_g=[2, 1024], ln1_b=[2, 1024], router_w=[1024, 16], router_bias=[16],
    moe_w_gate=[2, 16, 1024, 512], moe_w_up=[2, 16, 1024, 512], moe_w_down=[2, 16, 512, 1024],
    ln2_g=[2, 1024], ln2_b=[2, 1024])


def host_consts():
    c = {}
    c["identf"] = np.eye(128, dtype=np.float32)
    i = np.arange(128)
    c["absdiff"] = np.abs(i[:, None] - i[None, :]).astype(np.float32)
    pos = np.arange(S)
    hi, lo = pos // 64, pos % 64
    c["qaug"] = np.stack([64.0 * hi, lo, np.ones(S), np.ones(S)]).astype(ml_dtypes.bfloat16)
    ka = np.zeros((4, 2, 4, S), np.float32)
    for h in range(4):
        sl = 2.0 ** (-2.0 * (h + 1))
        plus = np.stack([np.full(S, sl), np.full(S, sl), -sl * 64.0 * hi, -sl * lo])
        ka[h, 0] = plus
        ka[h, 1] = -plus
    c["kaug"] = ka.astype(ml_dtypes.bfloat16)
    j = np.arange(64)
    c["gmask"] = np.stack([(j[:, None] <= j[None, :]), (j[:, None] > j[None, :])]).astype(np.float32)
    jj = np.repeat(np.arange(8), 16)
    c["s5mask"] = np.stack([(jj[None, :] >= jj[:, None]), (jj[None, :] <= jj[:, None])]).astype(np.float32)
    jx = np.zeros((128, 128), np.float32)
    for q in range(64):
        jx[q, 64 + q] = 1.0
        jx[64 + q, q] = 1.0
    c["jx"] = jx
    c["rowmask"] = (np.arange(128)[:, None] // 16 == np.arange(8)[None, :]).astype(np.float32)
    sg = np.ones((128, 2), np.float32)
    sg[:64, 0] = -1.0
    sg[64:, 1] = -1.0
    c["sgn"] = sg
    return c


CONST_SHAPES = dict(jx=([128, 128], F32), rowmask=([128, 8], F32), sgn=([128, 2], F32), identf=([128, 128], F32), absdiff=([128, 128], F32), qaug=([4, S], BF16), kaug=([4, 2, 4, S], BF16),
                    gmask=([2, 64, 64], F32), s5mask=([2, 128, 128], F32))


class Model:
    def __init__(self, dbg=None):
        self.dbg = dbg or {}
        nc = bass.Bass("TRN2", target_bir_lowering=False)
        self.nc = nc
        p = Prog(nc)
        self.p = p
        kinds = self.dbg.get("kinds", {})
        self.x_in = p.dram("x", [S, D], F32, kind="ExternalInput")
        self.out = p.dram("out", [S, D], F32, kind="ExternalOutput", nb=NT)
        self.W = {k: p.dram(k, shp, F32, kind="ExternalInput") for k, shp in PARAM_SHAPES.items()}
        self.C = {k: p.dram(k, shp, dt, kind="ExternalInput") for k, (shp, dt) in CONST_SHAPES.items()}
        self.xres = p.dram("xres", [S, D], F32, kind=kinds.get("xres", "Internal"), nb=NT)
        self.oT = p.dram("oT", [3, 4, 128, S], BF16, kind=kinds.get("oT", "Internal"), nb=12)
        self.mT = p.dram("mT", [128, 8, S], BF16, kind=kinds.get("mT", "Internal"), nb=16)
        if self.dbg.get("s5y"):
            self.dbgy = p.dram("dbgy", [8, 128, S], F32, kind="ExternalOutput")
        self.xT = p.sb("xT", [128, 8, S], BF16, nb=NT)
        self.identf = p.sb("identf_sb", [128, 128], F32)
        self.call = p.sb("call", [128, NT, 16], F32, nb=NT)
        self.PS = [p.ps(f"ps{i}", [128, 512], F32) for i in range(8)]
        self.psi = 0
        self.psc = {}
        p.dma("sp", self.identf[:], self.C["identf"][:, :], writes=[self.identf])

    def dump(self, name, tl, ap, shape):
        if not self.dbg.get("dump"):
            return
        d = self.p.dram("dump_" + name, list(shape), F32, kind="ExternalOutput")
        self.p.dma("pool", d.t, ap, reads=[tl], writes=[d])

    def bank(self, lo=0, hi=8):
        n = hi - lo
        c = self.psc.get((lo, hi), 0)
        self.psc[(lo, hi)] = c + 1
        return self.PS[lo + (c % n)]

    def mm(self, out, lhsT, rhs, start, stop, reads, writes, serial=False):
        self.p.op("pe", lambda e: e.matmul(out, lhsT, rhs, start=start, stop=stop, skip_group_check=True), reads=reads, writes=writes, serial=serial)

    def ln_tile(self, z, xn, stats, mv, rstd):
        p = self.p
        for k in range(2):
            p.op("dve", lambda e, k=k: e.bn_stats(out=stats[:, k, :], in_=z[:, k * 512:(k + 1) * 512]), reads=[z], writes=[stats])
        p.op("dve", lambda e: e.bn_aggr(out=mv[:, :], in_=stats[:, :, :].rearrange("p a b -> p (a b)")), reads=[stats], writes=[mv])
        self.rsqrt(rstd, rstd[:, :], mv, mv[:, 1:2], 1.0)
        p.op("dve", lambda e: e.tensor_scalar(xn[:, :], z[:, :], mv[:, 0:1], rstd[:, 0:1], ALU.subtract, ALU.mult), reads=[z, mv, rstd], writes=[xn])
        p.op("pool", lambda e, g_=self.gbc: e.tensor_tensor(out=xn[:, :], in0=xn[:, :], in1=g_[:, :], op=ALU.mult), reads=[xn, self.gbc], writes=[xn])
        p.op("pool", lambda e, b_=self.bbc: e.tensor_tensor(out=xn[:, :], in0=xn[:, :], in1=b_[:, :], op=ALU.add), reads=[xn, self.bbc], writes=[xn])

    def rsqrt(self, dst_tl, dst, src_tl, src, scale):
        p = self.p
        p.op("dve", lambda e: e.tensor_scalar(dst, src, scale, EPS, ALU.mult, ALU.add), reads=[src_tl], writes=[dst_tl])
        p.op("act", lambda e: e.sqrt(out=dst, in_=dst), reads=[dst_tl], writes=[dst_tl])
        p.op("dve", lambda e: e.reciprocal(out=dst, in_=dst), reads=[dst_tl], writes=[dst_tl])

    def load_ln_params(self, g_ap, b_ap, st):
        p = self.p
        self.gbc = p.sb("gbc", [128, D], F32, st)
        self.bbc = p.sb("bbc", [128, D], F32, st)
        p.dma("sp", self.gbc[:], g_ap.partition_broadcast(128), writes=[self.gbc])
        p.dma("sp", self.bbc[:], b_ap.partition_broadcast(128), writes=[self.bbc])

    def to_xT(self, xn, t, xT32=None):
        p = self.p
        for c0 in (0, 4):
            ps = self.bank(0, 4)
            for j in range(4):
                c = c0 + j
                p.op("pe", lambda e, ps=ps, j=j, c=c: e.transpose(ps[:, j * 128:(j + 1) * 128], xn[:, c * 128:(c + 1) * 128], self.identf[:, :]),
                     reads=[xn, self.identf], writes=[ps])
            src = ps[:, :].rearrange("p (j n) -> p j n", j=4)
            p.op("act", lambda e, src=src, c0=c0: e.copy(out=self.xT[:, c0:c0 + 4, t * 128:(t + 1) * 128], in_=src), reads=[ps], writes=[self.xT.bs[t]])
            if xT32 is not None:
                p.op("dve", lambda e, src=src, c0=c0: e.tensor_copy(out=xT32[:, c0:c0 + 4, :], in_=src), reads=[ps], writes=[xT32])

    def phase_ln0(self):
        p = self.p
        st = ExitStack()
        self.load_ln_params(self.W["ln0_g"].t.rearrange("(o n) -> o n", o=1), self.W["ln0_b"].t.rearrange("(o n) -> o n", o=1), st)
        zs = [p.sb(f"l0z{i}", [128, D], F32, st) for i in range(2)]
        stats = p.sb("l0stats", [128, 2, 6], F32, st)
        mv = p.sb("l0mv", [128, 2], F32, st)
        rstd = p.sb("l0rstd", [128, 1], F32, st)
        for t in range(NT):
            z = zs[t % 2]
            p.dma("sp", z[:], self.x_in[t * 128:(t + 1) * 128, :], writes=[z])
            self.ln_tile(z, z, stats, mv, rstd)
            p.dma("pool", self.xres[t * 128:(t + 1) * 128, :], z[:], reads=[z], writes=[self.xres.bs[t]])
            self.to_xT(z, t)
        p.barrier()
        st.close()

    def wload(self, stg, dst_tl, dst_ap, src_ap, kc, ncols, cast="act", wbuf=None):
        p = self.p
        cap = stg[0].t.shape[1]
        kcp = max(1, min(kc, cap // ncols))
        wb = [wbuf if wbuf is not None else dst_tl]
        for k0 in range(0, kc, kcp):
            st = stg[self.wl_i % len(stg)]
            self.wl_i += 1
            view = st[:, 0:kcp * ncols].rearrange("p (c n) -> p c n", c=kcp)
            p.dma("sp", view, src_ap[k0 * 128:(k0 + kcp) * 128, :].rearrange("(c p) n -> p c n", p=128), writes=[st])
            if cast == "act":
                p.op(cast, lambda e, view=view, k0=k0: e.copy(out=dst_ap[:, k0:k0 + kcp, :], in_=view), reads=[st], writes=wb)
            else:
                p.op(cast, lambda e, view=view, k0=k0: e.tensor_copy(out=dst_ap[:, k0:k0 + kcp, :], in_=view), reads=[st], writes=wb)

    wl_i = 0

    def phase_attn(self, l):
        p = self.p
        W = self.W
        st = ExitStack()
        stg = [p.sb(f"a_stg{i}", [128, 8 * 128], F32, st) for i in range(2)]
        wA = [p.sb(f"a_w{i}", [128, 8, 384], BF16, st) for i in range(2)]
        QT = [p.sb(f"a_qt{m}", [68, S], BF16, st) for m in range(2)]
        KTp = [p.sb(f"a_ktp{m}", [68, S], BF16, st) for m in range(2)]
        KTm = [p.sb(f"a_ktm{m}", [68, S], BF16, st) for m in range(2)]
        V = p.sb("a_v", [128, NT, 129], BF16, st)
        PT = [p.sb(f"a_pt{i}", [128, 512], BF16, st) for i in range(4)]
        oaT = [p.sb(f"a_oat{i}", [128, S], BF16, st) for i in range(1)]
        on = [p.sb(f"a_on{m}", [128, 4, 128], F32, st) for m in range(2)]
        rs = p.sb("a_rs", [128, 4], F32, st)
        diff = p.sb("a_diff", [128, 4, 128], F32, st)
        sq = p.sb("a_sq", [128, 4, 128], F32, st)
        ss = p.sb("a_ss", [128, 4], F32, st)
        rstd = p.sb("a_rstd", [128, 4], F32, st)
        oo = p.sb("a_oo", [128, 4, 128], F32, st)
        absd = p.sb("a_absd", [128, 128], F32, st)
        lamt = p.sb("a_lamt", [128, 256], F32, st)
        lsm = p.sb("a_lsm", [128, 8], F32, st)
        gA = p.sb("a_gA", [128, 128], F32, st)
        lam_init = 0.8 - 0.6 * math.exp(-0.3 * l)
        p.dma("sp", absd[:], self.C["absdiff"][:, :], writes=[absd])
        for m in range(2):
            p.dma("sp", QT[m][64:68, :], self.C["qaug"][:, :], writes=[QT[m]])
        p.dma("sp", lamt[:], W["da_lambda"][l:l + 1, :, :].rearrange("o a b -> o (a b)").partition_broadcast(128), writes=[lamt])
        p.dma("sp", gA[:], W["da_norm_g"][l:l + 1, :].partition_broadcast(128), writes=[gA])
        p.op("dve", lambda e: e.tensor_scalar(gA[:, :], gA[:, :], 1.0 - lam_init, None, ALU.mult), reads=[gA], writes=[gA])
        p.op("dve", lambda e: e.tensor_tensor(out=lamt[:, 0:64], in0=lamt[:, 0:64], in1=lamt[:, 64:128], op=ALU.mult), reads=[lamt], writes=[lamt])
        p.op("dve", lambda e: e.tensor_tensor(out=lamt[:, 128:192], in0=lamt[:, 128:192], in1=lamt[:, 192:256], op=ALU.mult), reads=[lamt], writes=[lamt])
        p.op("dve", lambda e: e.tensor_reduce(out=lsm[:, 0:1], in_=lamt[:, 0:64], axis=AX.X, op=ALU.add), reads=[lamt], writes=[lsm])
        p.op("dve", lambda e: e.tensor_reduce(out=lsm[:, 1:2], in_=lamt[:, 128:192], axis=AX.X, op=ALU.add), reads=[lamt], writes=[lsm])
        p.op("act", lambda e: e.activation(out=lsm[:, 2:4], in_=lsm[:, 0:2], func=AF.Exp), reads=[lsm], writes=[lsm])
        p.op("dve", lambda e: e.tensor_tensor(out=lsm[:, 4:5], in0=lsm[:, 3:4], in1=lsm[:, 2:3], op=ALU.subtract), reads=[lsm], writes=[lsm])
        p.op("dve", lambda e: e.tensor_scalar(lsm[:, 5:6], lsm[:, 4:5], -lam_init, None, ALU.add), reads=[lsm], writes=[lsm])
        p.op("dve", lambda e: e.memset(V[:, :, 128:129], 1.0), writes=[V])
        xT = self.xT
        xTr = list(xT.bs)

        def load_head_w(h):
            w = wA[h % 2]
            for k, nm in enumerate(("qa", "ka", "va")):
                c0 = COL[nm] + h * 128
                self.wload(stg, w, w[:, :, k * 128:(k + 1) * 128], W["w_in"][l, :, c0:c0 + 128], 8, 128, cast="dve")

        load_head_w(0)
        for h in range(self.dbg.get("heads", 4)):
            slope = 2.0 ** (-2.0 * (h + 1))
            w = wA[h % 2]
            if h + 1 < self.dbg.get("heads", 4):
                load_head_w(h + 1)
            for m in range(2):
                p.dma("sp", KTp[m][64:68, :], self.C["kaug"][h, 0, :, :], writes=[KTp[m]])
                p.dma("sp", KTm[m][64:68, :], self.C["kaug"][h, 1, :, :], writes=[KTm[m]])
            for m in range(2):
                for tb in range(8):
                    ps = self.bank(0, 4)
                    for c in range(8):
                        self.mm(ps[0:64, :], w[:, c, m * 64:(m + 1) * 64], xT[:, c, tb * 512:(tb + 1) * 512], c == 0, c == 7, [w] + xTr[tb * 4:tb * 4 + 4], [ps])
                    p.op("act", lambda e, ps=ps, m=m, tb=tb: e.mul(out=QT[m][0:64, tb * 512:(tb + 1) * 512], in_=ps[0:64, :], mul=0.125), reads=[ps], writes=[QT[m]])
                    ps = self.bank(0, 4)
                    for c in range(8):
                        self.mm(ps[0:64, :], w[:, c, 128 + m * 64:128 + (m + 1) * 64], xT[:, c, tb * 512:(tb + 1) * 512], c == 0, c == 7, [w] + xTr[tb * 4:tb * 4 + 4], [ps])
                    p.op("act", lambda e, ps=ps, m=m, tb=tb: e.copy(out=KTp[m][0:64, tb * 512:(tb + 1) * 512], in_=ps[0:64, :]), reads=[ps], writes=[KTp[m]])
                    p.op("dve", lambda e, ps=ps, m=m, tb=tb: e.tensor_copy(out=KTm[m][0:64, tb * 512:(tb + 1) * 512], in_=ps[0:64, :]), reads=[ps], writes=[KTm[m]])
            for t4 in range(8):
                ps = self.bank(0, 4)
                for j in range(4):
                    t = t4 * 4 + j
                    for c in range(8):
                        self.mm(ps[:, j * 128:(j + 1) * 128], xT[:, c, t * 128:(t + 1) * 128], w[:, c, 256:384], c == 0, c == 7, [w, xTr[t]], [ps])
                p.op("act", lambda e, ps=ps, t4=t4: e.copy(out=V[:, t4 * 4:(t4 + 1) * 4, 0:128], in_=ps[:, :].rearrange("p (j n) -> p j n", j=4)), reads=[ps], writes=[V])
            oa = oaT[0]
            pti = 0
            for Q in range(self.dbg.get("nQ", 8)):
                stages = [(m, kt) for m in range(2) for kt in range(NT)]
                stbank = {}
                ptbuf = {}

                def st_S(i, Q=Q):
                    m, kt = stages[i]
                    ps = self.PS[i % 4]
                    stbank[i] = ps
                    rel = kt - 4 * Q
                    ksl = slice(kt * 128, (kt + 1) * 128)
                    if rel < 0:
                        self.mm(ps[:, :], KTm[m][0:68, ksl], QT[m][0:68, Q * 512:(Q + 1) * 512], True, True, [KTm[m], QT[m]], [ps])
                    elif rel > 3:
                        self.mm(ps[:, :], KTp[m][0:68, ksl], QT[m][0:68, Q * 512:(Q + 1) * 512], True, True, [KTp[m], QT[m]], [ps])
                    else:
                        q0 = Q * 512
                        first = True
                        if rel > 0:
                            self.mm(ps[:, 0:rel * 128], KTp[m][0:68, ksl], QT[m][0:68, q0:q0 + rel * 128], first, True, [KTp[m], QT[m]], [ps])
                            first = False
                        self.mm(ps[:, rel * 128:(rel + 1) * 128], KTp[m][0:64, ksl], QT[m][0:64, q0 + rel * 128:q0 + (rel + 1) * 128], first, True, [KTp[m], QT[m]], [ps])
                        if rel < 3:
                            self.mm(ps[:, (rel + 1) * 128:512], KTm[m][0:68, ksl], QT[m][0:68, q0 + (rel + 1) * 128:q0 + 512], False, True, [KTm[m], QT[m]], [ps])
                        p.op("dve", lambda e, ps=ps, rel=rel, slope=slope: e.scalar_tensor_tensor(
                            out=ps[:, rel * 128:(rel + 1) * 128], in0=absd[:, :], scalar=-slope, in1=ps[:, rel * 128:(rel + 1) * 128], op0=ALU.mult, op1=ALU.add),
                            reads=[absd, ps], writes=[ps])

                def st_E(i):
                    ps = stbank[i]
                    pt = PT[i % 4]
                    ptbuf[i] = pt
                    p.op("act", lambda e, ps=ps, pt=pt: e.activation(out=pt[:, :], in_=ps[:, :], func=AF.Exp), reads=[ps], writes=[pt])

                def st_P(i):
                    m, kt = stages[i]
                    pt = ptbuf[i]
                    OB = (self.PS[4 + 2 * m], self.PS[5 + 2 * m])
                    for j in range(4):
                        ob = OB[j // 2]
                        oc = (j % 2) * 256
                        self.mm(ob[:, oc:oc + 129], pt[:, j * 128:(j + 1) * 128], V[:, kt, :], (kt == 0 and j % 2 == 0), kt == NT - 1, [pt, V], [ob])
                    if kt == NT - 1:
                        for j in range(4):
                            ob = OB[j // 2]
                            oc = (j % 2) * 256
                            p.op("dve", lambda e, ob=ob, oc=oc, j=j: e.reciprocal(out=rs[:, j:j + 1], in_=ob[:, oc + 128:oc + 129]), reads=[ob], writes=[rs])
                            p.op("dve", lambda e, ob=ob, oc=oc, j=j, m=m: e.tensor_scalar(on[m][:, j, :], ob[:, oc:oc + 128], rs[:, j:j + 1], None, ALU.mult), reads=[ob, rs], writes=[on[m]])

                NS = len(stages)
                st_S(0)
                st_S(1)
                for i in range(NS):
                    st_E(i)
                    if i + 2 < NS:
                        st_S(i + 2)
                    st_P(i)
                p.op("dve", lambda e: e.scalar_tensor_tensor(out=diff[:, :, :], in0=on[1][:, :, :], scalar=lsm[:, 5:6], in1=on[0][:, :, :], op0=ALU.mult, op1=ALU.add),
                     reads=[on[0], on[1], lsm], writes=[diff])
                p.op("pool", lambda e: e.tensor_tensor(out=sq[:, :, :], in0=diff[:, :, :], in1=diff[:, :, :], op=ALU.mult), reads=[diff], writes=[sq])
                p.op("dve", lambda e: e.tensor_reduce(out=ss[:, :], in_=sq[:, :, :], axis=AX.X, op=ALU.add), reads=[sq], writes=[ss])
                self.rsqrt(rstd, rstd[:, :], ss, ss[:, :], 1.0 / 128.0)
                for j in range(4):
                    p.op("dve", lambda e, j=j: e.scalar_tensor_tensor(out=oo[:, j, :], in0=diff[:, j, :], scalar=rstd[:, j:j + 1], in1=gA[:, :], op0=ALU.mult, op1=ALU.mult),
                         reads=[diff, rstd, gA], writes=[oo])
                ps = self.bank(0, 4)
                for j in range(4):
                    p.op("pe", lambda e, ps=ps, j=j: e.transpose(ps[:, j * 128:(j + 1) * 128], oo[:, j, :], self.identf[:, :]), reads=[oo, self.identf], writes=[ps])
                p.op("act", lambda e, ps=ps, Q=Q, oa=oa: e.copy(out=oa[:, Q * 512:(Q + 1) * 512], in_=ps[:, :]), reads=[ps], writes=[oa])
            p.dma("pool", self.oT[0, h, :, :], oa[:, :], reads=[oa], writes=[self.oT.bs[h]])
        p.barrier()
        st.close()

    def phase_merge1(self, l):
        for dh in range(2):
            self._merge1_dh(l, dh)

    def _merge1_dh(self, l, dh):
        p = self.p
        W = self.W
        xT = self.xT
        if True:
            st = ExitStack()
            stg = [p.sb(f"m_stg{i}", [128, 2048], F32, st) for i in range(2)]
            wg = p.sb("m_wg", [128, 8, 1536], BF16, st, nb=3)
            wup = p.sb("m_wup", [128, 12, 512], BF16, st, nb=3)
            mb = p.sb("m_mb", [128, 3, 512], F32, st)
            ot = [p.sb(f"m_ot{i}", [128, 12, 512], BF16, st) for i in range(2)]
            sg = [p.sb(f"m_sg{i}", [128, 512], F32, st) for i in range(2)]
            acc = [p.sb(f"m_acc{i}", [128, 512], F32, st) for i in range(2)]
            tmp = [p.sb(f"m_tmp{i}", [128, 512], F32, st) for i in range(2)]
            mtb = [p.sb(f"m_mtb{i}", [128, 4, 512], BF16, st) for i in range(2)]
            d0 = dh * 512
            for n in range(3):
                c0 = COL["gate"] + n * 1024 + d0
                self.wload(stg, wg, wg[:, :, n * 512:(n + 1) * 512], W["w_in"][l, :, c0:c0 + 512], 8, 512, wbuf=wg.bs[n])
                self.wload(stg, wup, wup[:, n * 4:(n + 1) * 4, :], W["merge_w_up"][l, n, :, d0:d0 + 512], 4, 512, wbuf=wup.bs[n])
            p.dma("sp", mb[:], W["merge_b"][l:l + 1, :, d0:d0 + 512].partition_broadcast(128), writes=[mb])
            def stage_A(k):
                tb, tt = k // 4, k % 4
                t = k
                o = ot[tb % 2]
                if tt == 0:
                    p.dma("sp", o[:], self.oT[:, :, :, tb * 512:(tb + 1) * 512].rearrange("n c p t -> p (n c) t"), reads=self.oT.bs, writes=[o])
                a = acc[k % 2]
                for n in range(3):
                    g = sg[(k * 3 + n) % 2]
                    psg = self.bank(0, 3)
                    for c in range(8):
                        self.mm(psg[:, :], xT[:, c, t * 128:(t + 1) * 128], wg[:, c, n * 512:(n + 1) * 512], c == 0, c == 7, [xT.bs[t], wg.bs[n]], [psg])
                    p.op("dve", lambda e, g=g, psg=psg, n=n, mb=mb: e.tensor_tensor(out=g[:, :], in0=psg[:, :], in1=mb[:, n, :], op=ALU.add), reads=[psg, mb], writes=[g])
                    p.op("act", lambda e, g=g: e.activation(out=g[:, :], in_=g[:, :], func=AF.Sigmoid), reads=[g], writes=[g])
                    psu = self.bank(3, 6)
                    for c in range(4):
                        self.mm(psu[:, :], o[:, n * 4 + c, tt * 128:(tt + 1) * 128], wup[:, n * 4 + c, :], c == 0, c == 3, [o, wup.bs[n]], [psu])
                    if n == 0:
                        p.op("dve", lambda e, a=a, g=g, psu=psu: e.tensor_tensor(out=a[:, :], in0=g[:, :], in1=psu[:, :], op=ALU.mult), reads=[g, psu], writes=[a])
                    else:
                        tm = tmp[n % 2]
                        p.op("dve", lambda e, tm=tm, g=g, psu=psu: e.tensor_tensor(out=tm[:, :], in0=g[:, :], in1=psu[:, :], op=ALU.mult), reads=[g, psu], writes=[tm])
                        p.op("dve", lambda e, tm=tm, a=a: e.tensor_tensor(out=a[:, :], in0=a[:, :], in1=tm[:, :], op=ALU.add), reads=[a, tm], writes=[a])

            def stage_B(k):
                tb, tt = k // 4, k % 4
                a = acc[k % 2]
                mt = mtb[tb % 2]
                pst = self.bank(6, 8)
                for j in range(4):
                    p.op("pe", lambda e, pst=pst, j=j, a=a: e.transpose(pst[:, j * 128:(j + 1) * 128], a[:, j * 128:(j + 1) * 128], self.identf[:, :]), reads=[a, self.identf], writes=[pst])
                p.op("act", lambda e, pst=pst, mt=mt, tt=tt: e.copy(out=mt[:, :, tt * 128:(tt + 1) * 128], in_=pst[:, :].rearrange("p (j n) -> p j n", j=4)), reads=[pst], writes=[mt])
                if tt == 3:
                    p.dma("pool", self.mT[:, dh * 4:(dh + 1) * 4, tb * 512:(tb + 1) * 512], mt[:, :, :], reads=[mt], writes=[self.mT.bs[dh * 8 + tb]])

            stage_A(0)
            for k in range(NT):
                if k + 1 < NT:
                    stage_A(k + 1)
                stage_B(k)
            p.barrier()
            st.close()

    def phase_merge2(self, l):
        p = self.p
        W = self.W
        st = ExitStack()
        stg = [p.sb(f"n_stg{i}", [128, 2048], F32, st) for i in range(2)]
        wo = p.sb("n_wo", [128, 8, 1024], BF16, st)
        rw = p.sb("n_rw", [128, 8, 16], F32, st)
        rb = p.sb("n_rb", [128, 16], F32, st)
        mtl = [p.sb(f"n_mt{i}", [128, 8, 512], BF16, st) for i in range(2)]
        xr = [p.sb(f"n_xr{i}", [128, D], F32, st) for i in range(2)]
        z = [p.sb(f"n_z{i}", [128, D], F32, st) for i in range(2)]
        xT32 = p.sb("n_xT32", [128, 8, 128], F32, st)
        stats = p.sb("n_stats", [128, 2, 6], F32, st)
        mv = p.sb("n_mv", [128, 2], F32, st)
        rstd = p.sb("n_rstd", [128, 1], F32, st)
        R = {k: p.sb("n_r_" + k, shp, F32, st) for k, shp in dict(sc=[128, 16], bi=[128, 16], m1=[128, 4], eq=[128, 16], t2=[128, 16], m2=[128, 4],
                                                                  gs=[128, 4], gm=[128, 1], gsel=[128, 4], ge=[128, 16], w=[128, 16], ws=[128, 1]).items()}
        for h2 in range(2):
            self.wload(stg, wo, wo[:, :, h2 * 512:(h2 + 1) * 512], W["w_out"][l, :, h2 * 512:(h2 + 1) * 512], 8, 512)
        p.dma("sp", rw[:], W["router_w"].t.rearrange("(c p) n -> p c n", p=128), writes=[rw])
        p.dma("sp", rb[:], W["router_bias"].t.rearrange("(o n) -> o n", o=1).partition_broadcast(128), writes=[rb])
        self.load_ln_params(W["ln1_g"][l:l + 1, :], W["ln1_b"][l:l + 1, :], st)
        def stage_A(t):
            tb, tt = t // 4, t % 4
            mt = mtl[tb % 2]
            if tt == 0:
                p.dma("sp", mt[:], self.mT[:, :, tb * 512:(tb + 1) * 512], reads=[self.mT.bs[tb], self.mT.bs[8 + tb]], writes=[mt])
            x_ = xr[t % 2]
            z_ = z[t % 2]
            p.dma("sp", x_[:], self.xres[t * 128:(t + 1) * 128, :], reads=[self.xres.bs[t]], writes=[x_])
            for h2 in range(2):
                ps = self.bank(0, 4)
                for c in range(8):
                    self.mm(ps[:, :], mt[:, c, tt * 128:(tt + 1) * 128], wo[:, c, h2 * 512:(h2 + 1) * 512], c == 0, c == 7, [mt, wo], [ps])
                p.op("dve", lambda e, ps=ps, x_=x_, z_=z_, h2=h2: e.scalar_tensor_tensor(out=z_[:, h2 * 512:(h2 + 1) * 512], in0=x_[:, h2 * 512:(h2 + 1) * 512], scalar=ALPHA,
                                                                                in1=ps[:, :], op0=ALU.mult, op1=ALU.add), reads=[ps, x_], writes=[z_])
            self.ln_tile(z_, z_, stats, mv, rstd)
            p.dma("pool", self.xres[t * 128:(t + 1) * 128, :], z_[:], reads=[z_], writes=[self.xres.bs[t]])

        def stage_B(t):
            self.to_xT(z[t % 2], t, xT32=xT32)
            self.router(t, xT32, rw, rb, R)

        stage_A(0)
        for t in range(NT):
            if t + 1 < NT:
                stage_A(t + 1)
            stage_B(t)
        p.barrier()
        st.close()

    def router(self, t, xT32, rw, rb, R):
        p = self.p
        ps = self.bank(4, 8)
        for c in range(8):
            self.mm(ps[:, 0:16], xT32[:, c, :], rw[:, c, :], c == 0, c == 7, [xT32, rw], [ps])
        sc, bi, m1, eq, t2, m2, gs, gm, gsel, ge, w, ws = (R[k] for k in ("sc", "bi", "m1", "eq", "t2", "m2", "gs", "gm", "gsel", "ge", "w", "ws"))
        v3 = lambda tl: tl[:, :].rearrange("p (g e) -> p g e", g=4)
        b3 = lambda tl: tl[:, :].unsqueeze(2).to_broadcast([128, 4, 4])
        p.op("act", lambda e: e.activation(out=sc[:, :], in_=ps[:, 0:16], func=AF.Sigmoid), reads=[ps], writes=[sc])
        p.op("dve", lambda e: e.tensor_tensor(out=bi[:, :], in0=sc[:, :], in1=rb[:, :], op=ALU.add), reads=[sc, rb], writes=[bi])
        p.op("dve", lambda e: e.tensor_reduce(out=m1[:, :], in_=v3(bi), axis=AX.X, op=ALU.max), reads=[bi], writes=[m1])
        p.op("dve", lambda e: e.tensor_tensor(out=v3(eq), in0=v3(bi), in1=b3(m1), op=ALU.is_equal), reads=[bi, m1], writes=[eq])
        p.op("dve", lambda e: e.scalar_tensor_tensor(out=t2[:, :], in0=eq[:, :], scalar=-1e30, in1=bi[:, :], op0=ALU.mult, op1=ALU.add), reads=[eq, bi], writes=[t2])
        p.op("dve", lambda e: e.tensor_reduce(out=m2[:, :], in_=v3(t2), axis=AX.X, op=ALU.max), reads=[t2], writes=[m2])
        p.op("dve", lambda e: e.tensor_tensor(out=gs[:, :], in0=m1[:, :], in1=m2[:, :], op=ALU.add), reads=[m1, m2], writes=[gs])
        p.op("dve", lambda e: e.tensor_reduce(out=gm[:, :], in_=gs[:, :], axis=AX.X, op=ALU.max), reads=[gs], writes=[gm])
        p.op("dve", lambda e: e.tensor_scalar(gsel[:, :], gs[:, :], gm[:, 0:1], None, ALU.is_equal), reads=[gs, gm], writes=[gsel])
        p.op("dve", lambda e: e.tensor_tensor(out=v3(ge), in0=v3(bi), in1=b3(m2), op=ALU.is_ge), reads=[bi, m2], writes=[ge])
        p.op("dve", lambda e: e.tensor_tensor(out=v3(ge), in0=v3(ge), in1=b3(gsel), op=ALU.mult), reads=[ge, gsel], writes=[ge])
        p.op("dve", lambda e: e.tensor_tensor(out=w[:, :], in0=ge[:, :], in1=sc[:, :], op=ALU.mult), reads=[ge, sc], writes=[w])
        p.op("dve", lambda e: e.tensor_reduce(out=ws[:, :], in_=w[:, :], axis=AX.X, op=ALU.add), reads=[w], writes=[ws])
        p.op("dve", lambda e: e.reciprocal(out=ws[:, :], in_=ws[:, :]), reads=[ws], writes=[ws])
        p.op("dve", lambda e: e.tensor_scalar(self.call[:, t, :], w[:, :], ws[:, 0:1], None, ALU.mult), reads=[w, ws], writes=[self.call.bs[t]])

    def phase_moe(self, l, last):
        p = self.p
        W = self.W
        xT = self.xT
        st = ExitStack()
        stg = [p.sb(f"e_stg{i}", [128, 2048], F32, st) for i in range(2)]
        wg = [p.sb(f"e_wg{i}", [128, 8, 512], BF16, st) for i in range(2)]
        wu = [p.sb(f"e_wu{i}", [128, 8, 512], BF16, st) for i in range(2)]
        wd = [p.sb(f"e_wd{i}", [128, 4, 1024], BF16, st) for i in range(2)]
        yacc = p.sb("e_yacc", [128, 8, D], F32, st, nb=8)
        hT = [p.sb(f"e_hT{i}", [128, 4, 512], BF16, st) for i in range(2)]
        sgl = [p.sb(f"e_sg{i}", [128, 512], F32, st) for i in range(2)]
        xr = [p.sb(f"e_xr{i}", [128, D], F32, st) for i in range(1)]
        stats = p.sb("e_stats", [128, 2, 6], F32, st)
        mv = p.sb("e_mv", [128, 2], F32, st)
        rstd = p.sb("e_rstd", [128, 1], F32, st)
        self.load_ln_params(W["ln2_g"][l:l + 1, :], W["ln2_b"][l:l + 1, :], st)
        cast_i = 0
        k = 0
        for q4 in range(4):
            for ex in range(16):
                g_, u_, d_ = wg[ex % 2], wu[ex % 2], wd[ex % 2]
                for h2 in range(2):
                    self.wload(stg, g_, g_[:, h2 * 4:(h2 + 1) * 4, :], W["moe_w_gate"][l, ex, h2 * 512:(h2 + 1) * 512, :], 4, 512, cast=("pool", "dve")[h2])
                    self.wload(stg, u_, u_[:, h2 * 4:(h2 + 1) * 4, :], W["moe_w_up"][l, ex, h2 * 512:(h2 + 1) * 512, :], 4, 512, cast=("pool", "dve")[h2])
                    self.wload(stg, d_, d_[:, :, h2 * 512:(h2 + 1) * 512], W["moe_w_down"][l, ex, :, h2 * 512:(h2 + 1) * 512], 4, 512, cast=("pool", "dve")[h2])
                for tb2 in range(2):
                    tb = q4 * 2 + tb2
                    h_ = hT[k % 2]
                    k += 1
                    xr_ = [xT.bs[tb * 4 + i] for i in range(4)]
                    for fc in range(4):
                        pg = self.bank(0, 2)
                        for c in range(8):
                            self.mm(pg[:, :], g_[:, c, fc * 128:(fc + 1) * 128], xT[:, c, tb * 512:(tb + 1) * 512], c == 0, c == 7, [g_] + xr_, [pg])
                        pu = self.bank(2, 4)
                        for c in range(8):
                            self.mm(pu[:, :], u_[:, c, fc * 128:(fc + 1) * 128], xT[:, c, tb * 512:(tb + 1) * 512], c == 0, c == 7, [u_] + xr_, [pu])
                        s_ = sgl[fc % 2]
                        p.op("act", lambda e, s_=s_, pg=pg: e.activation(out=s_[:, :], in_=pg[:, :], func=AF.Silu), reads=[pg], writes=[s_])
                        p.op("dve", lambda e, s_=s_, pu=pu, h_=h_, fc=fc: e.tensor_tensor(out=h_[:, fc, :], in0=s_[:, :], in1=pu[:, :], op=ALU.mult), reads=[s_, pu], writes=[h_])
                    for tt in range(4):
                        t = tb * 4 + tt
                        tl = tb2 * 4 + tt
                        for h2 in range(2):
                            py = self.bank(4, 8)
                            for fc in range(4):
                                self.mm(py[:, :], h_[:, fc, tt * 128:(tt + 1) * 128], d_[:, fc, h2 * 512:(h2 + 1) * 512], fc == 0, fc == 3, [h_, d_], [py])
                            ya = yacc[:, tl, h2 * 512:(h2 + 1) * 512]
                            if ex == 0:
                                p.op("dve", lambda e, ya=ya, py=py, t=t, ex=ex: e.tensor_scalar(ya, py[:, :], self.call[:, t, ex:ex + 1], None, ALU.mult),
                                     reads=[py, self.call.bs[t]], writes=[yacc.bs[tl]])
                            else:
                                p.op("dve", lambda e, ya=ya, py=py, t=t, ex=ex: e.scalar_tensor_tensor(out=ya, in0=py[:, :], scalar=self.call[:, t, ex:ex + 1], in1=ya, op0=ALU.mult, op1=ALU.add),
                                     reads=[py, self.call.bs[t]], writes=[yacc.bs[tl]])
            for tl in range(8):
                t = q4 * 8 + tl
                x_ = xr[0]
                yv = Sub(yacc[:, tl, :], yacc.bs[tl])
                p.dma("sp", x_[:], self.xres[t * 128:(t + 1) * 128, :], reads=[self.xres.bs[t]], writes=[x_])
                p.op("dve", lambda e, x_=x_, tl=tl: e.scalar_tensor_tensor(out=yacc[:, tl, :], in0=x_[:, :], scalar=ALPHA, in1=yacc[:, tl, :], op0=ALU.mult, op1=ALU.add),
                     reads=[x_, yacc.bs[tl]], writes=[yacc.bs[tl]])
                self.ln_tile(yv, yv, stats, mv, rstd)
                if last:
                    p.dma("pool", self.out[t * 128:(t + 1) * 128, :], yv[:, :], reads=[yv], writes=[self.out.bs[t]])
                else:
                    p.dma("pool", self.xres[t * 128:(t + 1) * 128, :], yv[:, :], reads=[yv], writes=[self.xres.bs[t]])
                    self.to_xT(yv, t)
        p.barrier()
        st.close()

    def phase_gla(self, l):
        for hp in range(2):
            self._gla_hp(l, hp)

    def _gla_hp(self, l, hp):
        p = self.p
        W = self.W
        xT = self.xT
        xTr = list(xT.bs)
        if True:
            st = ExitStack()
            stg = [p.sb(f"g_stg{i}", [128, 1024], F32, st) for i in range(2)]
            wq = p.sb("g_wq", [128, 8, 128], BF16, st)
            wk = p.sb("g_wk", [128, 8, 128], BF16, st)
            wv = p.sb("g_wv", [128, 8, 256], BF16, st)
            wr = p.sb("g_wr", [128, 8, 256], BF16, st)
            wz = p.sb("g_wz", [128, 8, 32], BF16, st)
            wgf = p.sb("g_wgf", [16, 2, 128], F32, st)
            wgt = p.sb("g_wgt", [16, 2, 128], BF16, st)
            bg = p.sb("g_bg", [128, 2], F32, st)
            gG = p.sb("g_gG", [128, 128], F32, st)
            ones = p.sb("g_ones", [128, 1], F32, st)
            m4 = p.sb("g_m4", [128, 4, 64], F32, st)
            msk = p.sb("g_msk", [128, 8, 64], F32, st)
            qA1 = p.sb("g_qA1", [128, S], BF16, st)
            kA1 = p.sb("g_kA1", [128, S], BF16, st)
            qi1 = p.sb("g_qi1", [128, S], BF16, st)
            qA0 = [p.sb(f"g_qA0{i}", [128, 512], BF16, st) for i in range(2)]
            kA0 = [p.sb(f"g_kA0{i}", [128, 512], BF16, st) for i in range(2)]
            qi0 = [p.sb(f"g_qi0{i}", [128, 512], BF16, st) for i in range(2)]
            dec = [p.sb(f"g_dec{d}", [128, 64], F32, st) for d in range(2)]
            sts1 = p.sb("g_st1", [128, 64, 128], BF16, st)
            sts0 = [p.sb(f"g_st0{i}", [128, 8, 128], BF16, st) for i in range(2)]
            S32 = [p.sb(f"g_S32{d}", [128, 128], F32, st) for d in range(2)]
            v = p.sb("g_v", [128, NT, 256], BF16, st)
            zt = p.sb("g_zt", [16, 512], BF16, st)
            T1 = p.sb("g_T1", [128, 512], F32, st)
            T2 = p.sb("g_T2", [128, 512], F32, st)
            T3 = p.sb("g_T3", [128, 512], F32, st)
            T4 = p.sb("g_T4", [128, 512], F32, st)
            T5 = p.sb("g_T5", [128, 512], F32, st)
            kltr = [p.sb(f"g_klt{i}", [128, 4, 128], BF16, st) for i in range(2)]
            scT = [p.sb(f"g_scT{i}", [128, 4, 64], BF16, st) for i in range(2)]
            sr = p.sb("g_sr", [128, 256], F32, st)
            sq = p.sb("g_sq", [128, 2, 128], F32, st)
            ssq = p.sb("g_ssq", [128, 2], F32, st)
            oc = p.sb("g_oc", [128, 256], F32, st)
            ocT = [p.sb(f"g_ocT{i}", [128, 2, 512], BF16, st) for i in range(2)]
            c0 = COL["qc"] + hp * 128
            self.wload(stg, wq, wq[:, :, :], W["w_in"][l, :, c0:c0 + 128], 8, 128)
            p.op("pool", lambda e: e.tensor_scalar(wq[:, :, :], wq[:, :, :], 0.125, None, ALU.mult), reads=[wq], writes=[wq])
            c0 = COL["kc"] + hp * 128
            self.wload(stg, wk, wk[:, :, :], W["w_in"][l, :, c0:c0 + 128], 8, 128)
            for k2 in range(2):
                c0 = COL["vc"] + hp * 256 + k2 * 128
                self.wload(stg, wv, wv[:, :, k2 * 128:(k2 + 1) * 128], W["w_in"][l, :, c0:c0 + 128], 8, 128)
                c0 = COL["rc"] + hp * 256 + k2 * 128
                self.wload(stg, wr, wr[:, :, k2 * 128:(k2 + 1) * 128], W["w_in"][l, :, c0:c0 + 128], 8, 128)
            self.wload(stg, wz, wz[:, :, :], W["w_in"][l, :, COL["zf"]:COL["zf"] + 32], 8, 32)
            p.dma("sp", wgf[:], W["gla_w_gate"][l, :, :, hp * 128:(hp + 1) * 128].rearrange("d r n -> r d n"), writes=[wgf])
            p.op("dve", lambda e: e.tensor_copy(out=wgt[:, :, :], in_=wgf[:, :, :]), reads=[wgf], writes=[wgt])
            p.dma("sp", bg[:], W["gla_b_gate"][l, :, hp * 128:(hp + 1) * 128].rearrange("d n -> n d"), writes=[bg], allow_slow_non_contiguous=True)
            p.dma("sp", gG[:], W["gla_norm_g"][l:l + 1, :].partition_broadcast(128), writes=[gG])
            p.op("pool", lambda e: e.memset(ones[:, :], 1.0), writes=[ones])
            for d in range(2):
                for hh in range(2):
                    for half in range(2):
                        p.dma("sp", m4[half * 64:(half + 1) * 64, d * 2 + hh, :], self.C["gmask"][d, :, :], writes=[m4])
            p.op("pool", lambda e: e.memset(msk[:, :, :], 1.0), writes=[msk])
            p.op("pool", lambda e: e.memset(msk[:, :, 0:1], 0.0), reads=[msk], writes=[msk])
            for t in range(NT):
                ps = self.bank(0, 4)
                for c in range(8):
                    self.mm(ps[:, 0:256], xT[:, c, t * 128:(t + 1) * 128], wv[:, c, :], c == 0, c == 7, [wv, xTr[t]], [ps])
                p.op("act", lambda e, ps=ps, t=t: e.copy(out=v[:, t, :], in_=ps[:, 0:256]), reads=[ps], writes=[v])
            def arr(d, tb):
                if d == 1:
                    sl = slice(tb * 512, (tb + 1) * 512)
                    return (qA1, qA1[:, sl]), (kA1, kA1[:, sl]), (qi1, qi1[:, sl])
                i = tb % 2
                return (qA0[i], qA0[i][:, :]), (kA0[i], kA0[i][:, :]), (qi0[i], qi0[i][:, :])

            def chunk_view(d, which, n, hh):
                hs = slice(hh * 64, (hh + 1) * 64)
                if d == 1:
                    tl = (qA1, kA1, qi1)[which]
                    return tl, tl[hs, n * 64:(n + 1) * 64]
                tl = (qA0, kA0, qi0)[which][(n // 8) % 2]
                return tl, tl[hs, (n % 8) * 64:(n % 8 + 1) * 64]

            def state_view(d, n, hh):
                hs = slice(hh * 64, (hh + 1) * 64)
                if d == 1:
                    return sts1, sts1[hs, n, :]
                tl = sts0[(n // 8) % 2]
                return tl, tl[hs, n % 8, :]

            def sweep_A(d, tb):
                klt = kltr[tb % 2]
                xr_ = xTr[tb * 4:tb * 4 + 4]
                tsl = slice(tb * 512, (tb + 1) * 512)
                (qA_t, qA_ap), (kA_t, kA_ap), (qi_t, qi_ap) = arr(d, tb)
                pz = self.bank(0, 4)
                for c in range(8):
                    self.mm(pz[0:16, :], wz[:, c, d * 16:(d + 1) * 16], xT[:, c, tsl], c == 0, c == 7, [wz] + xr_, [pz])
                p.op("act", lambda e: e.copy(out=zt[:, :], in_=pz[0:16, :]), reads=[pz], writes=[zt])
                pg = self.bank(0, 4)
                self.mm(pg[:, :], wgt[0:16, d, :], zt[0:16, :], True, True, [wgt, zt], [pg])
                pq = self.bank(4, 6)
                for c in range(8):
                    self.mm(pq[:, :], wq[:, c, :], xT[:, c, tsl], c == 0, c == 7, [wq] + xr_, [pq])
                pk = self.bank(6, 8)
                for c in range(8):
                    self.mm(pk[:, :], wk[:, c, :], xT[:, c, tsl], c == 0, c == 7, [wk] + xr_, [pk])
                p.op("dve", lambda e: e.tensor_scalar(T1[:, :], pg[:, :], bg[:, d:d + 1], None, ALU.add), reads=[pg, bg], writes=[T1])
                p.op("dve", lambda e: e.scalar_tensor_tensor(out=T2[:, :], in0=T1[:, :], scalar=-1.0, in1=T1[:, :], op0=ALU.mult, op1=ALU.max), reads=[T1], writes=[T2])
                p.op("act", lambda e: e.activation(out=T2[:, :], in_=T2[:, :], func=AF.Exp, scale=-1.0), reads=[T2], writes=[T2])
                p.op("act", lambda e: e.activation(out=T2[:, :], in_=T2[:, :], func=AF.Ln, bias=ones[:, 0:1]), reads=[T2, ones], writes=[T2])
                p.op("dve", lambda e: e.scalar_tensor_tensor(out=T1[:, :], in0=T1[:, :], scalar=0.0, in1=T2[:, :], op0=ALU.min, op1=ALU.subtract), reads=[T1, T2], writes=[T1])
                p.op("act", lambda e: e.mul(out=T1[:, :], in_=T1[:, :], mul=1.0 / 16.0), reads=[T1], writes=[T1])
                p.op("dve", lambda e: e.tensor_tensor_scan(T3[:, :], msk[:, :, :].rearrange("p a b -> p (a b)"), T1[:, :], 0.0, ALU.mult, ALU.add), reads=[msk, T1], writes=[T3])
                c3 = T3[:, :].rearrange("p (a b) -> p a b", a=8)
                v4 = lambda tl: tl[:, :].rearrange("p (a b) -> p a b", a=8)
                if d == 1:
                    p.op("dve", lambda e: e.tensor_tensor(out=v4(T2), in0=c3[:, :, 63:64].to_broadcast([128, 8, 64]), in1=c3, op=ALU.subtract), reads=[T3], writes=[T2])
                    p.op("dve", lambda e: e.tensor_tensor(out=T3[:, :], in0=T2[:, :], in1=T1[:, :], op=ALU.add), reads=[T2, T1], writes=[T3])
                    ref, last = c3[:, :, 31:32], c3[:, :, 0:1]
                else:
                    ref, last = c3[:, :, 32:33], c3[:, :, 63:64]
                refb = ref.to_broadcast([128, 8, 64])
                lastb = last.to_broadcast([128, 8, 64])
                p.op("act", lambda e: e.activation(out=dec[d][:, tb * 8:(tb + 1) * 8].unsqueeze(2), in_=last, func=AF.Exp), reads=[T3], writes=[dec[d]])
                p.op("dve", lambda e: e.tensor_tensor(out=v4(T4), in0=c3, in1=refb, op=ALU.subtract), reads=[T3], writes=[T4])
                p.op("act", lambda e: e.activation(out=T5[:, :], in_=T4[:, :], func=AF.Exp), reads=[T4], writes=[T5])
                p.op("dve", lambda e: e.tensor_tensor(out=qA_ap, in0=pq[:, :], in1=T5[:, :], op=ALU.mult), reads=[pq, T5], writes=[qA_t])
                p.op("act", lambda e: e.activation(out=T5[:, :], in_=T4[:, :], func=AF.Exp, scale=-1.0), reads=[T4], writes=[T5])
                p.op("dve", lambda e: e.tensor_tensor(out=kA_ap, in0=pk[:, :], in1=T5[:, :], op=ALU.mult), reads=[pk, T5], writes=[kA_t])
                p.op("act", lambda e: e.activation(out=T5[:, :], in_=T3[:, :], func=AF.Exp), reads=[T3], writes=[T5])
                p.op("dve", lambda e: e.tensor_tensor(out=qi_ap, in0=pq[:, :], in1=T5[:, :], op=ALU.mult), reads=[pq, T5], writes=[qi_t])
                p.op("dve", lambda e: e.tensor_tensor(out=v4(T4), in0=lastb, in1=c3, op=ALU.subtract), reads=[T3], writes=[T4])
                p.op("act", lambda e: e.activation(out=T5[:, :], in_=T4[:, :], func=AF.Exp), reads=[T4], writes=[T5])
                p.op("dve", lambda e: e.tensor_tensor(out=T4[:, :], in0=pk[:, :], in1=T5[:, :], op=ALU.mult), reads=[pk, T5], writes=[T4])
                pt = self.bank(0, 4)
                for j in range(4):
                    p.op("pe", lambda e, j=j: e.transpose(pt[:, j * 128:(j + 1) * 128], T4[:, j * 128:(j + 1) * 128], self.identf[:, :]), reads=[T4, self.identf], writes=[pt])
                p.op("act", lambda e: e.copy(out=klt[:, :, :], in_=pt[:, :].rearrange("p (j n) -> p j n", j=4)), reads=[pt], writes=[klt])

            def sweep_B(d, tb):
                klt = kltr[tb % 2]
                cs = range(8) if d == 0 else range(7, -1, -1)
                for ci in cs:
                    n = tb * 8 + ci
                    tt, half = ci // 2, ci % 2
                    t = tb * 4 + tt
                    stl, _ = state_view(d, n, 0)
                    sap = sts1[:, n, :] if d == 1 else stl[:, n % 8, :]
                    p.op("act", lambda e, sap=sap: e.copy(out=sap, in_=S32[d][:, :]), reads=[S32[d]], writes=[stl])
                    pd = self.bank(0, 4)
                    for hh in range(2):
                        self.mm(pd[hh * 64:(hh + 1) * 64, 0:128], klt[half * 64:(half + 1) * 64, tt, hh * 64:(hh + 1) * 64],
                                v[half * 64:(half + 1) * 64, t, hh * 128:(hh + 1) * 128], True, True, [klt, v], [pd], serial=True)
                    p.op("dve", lambda e, pd=pd, n=n: e.scalar_tensor_tensor(out=S32[d][:, :], in0=S32[d][:, :], scalar=dec[d][:, n:n + 1], in1=pd[:, 0:128], op0=ALU.mult, op1=ALU.add),
                         reads=[pd, dec[d], S32[d]], writes=[S32[d]])

            def out_block(tb):
                ot_ = ocT[tb % 2]
                for tt in range(4):
                    t = tb * 4 + tt
                    pss = self.bank(0, 2)
                    for half in range(2):
                        n = t * 2 + half
                        first = True
                        for d in range(2):
                            for hh in range(2):
                                kt_, kap = chunk_view(d, 1, n, hh)
                                qt_, qap = chunk_view(d, 0, n, hh)
                                self.mm(pss[half * 64:(half + 1) * 64, (d * 2 + hh) * 64:(d * 2 + hh + 1) * 64], kap, qap, first, True, [kt_, qt_], [pss], serial=True)
                                first = False
                    sc_ = scT[t % 2]
                    p.op("dve", lambda e, pss=pss, sc_=sc_: e.tensor_tensor(out=sc_[:, :, :], in0=pss[:, 0:256].rearrange("p (a b) -> p a b", a=4), in1=m4[:, :, :], op=ALU.mult), reads=[pss, m4], writes=[sc_])
                    po = self.bank(2, 4)
                    for half in range(2):
                        n = t * 2 + half
                        hs = slice(half * 64, (half + 1) * 64)
                        first = True
                        for hh in range(2):
                            osl = po[hs, hh * 128:(hh + 1) * 128]
                            for d in range(2):
                                self.mm(osl, sc_[hs, d * 2 + hh, :], v[hs, t, hh * 128:(hh + 1) * 128], first, False, [sc_, v], [po], serial=True)
                                first = False
                            for d in range(2):
                                it_, iap = chunk_view(d, 2, n, hh)
                                st_, sap = state_view(d, n, hh)
                                self.mm(osl, iap, sap, False, d == 1, [it_, st_], [po], serial=True)
                    pr = self.bank(4, 8)
                    for c in range(8):
                        self.mm(pr[:, 0:256], xT[:, c, t * 128:(t + 1) * 128], wr[:, c, :], c == 0, c == 7, [wr, xTr[t]], [pr])
                    p.op("act", lambda e, pr=pr: e.activation(out=sr[:, :], in_=pr[:, 0:256], func=AF.Silu), reads=[pr], writes=[sr])
                    po3 = po[:, 0:256].rearrange("p (a b) -> p a b", a=2)
                    p.op("act", lambda e, po3=po3: e.activation(out=sq[:, :, :], in_=po3, func=AF.Square), reads=[po], writes=[sq])
                    p.op("dve", lambda e: e.tensor_reduce(out=ssq[:, :], in_=sq[:, :, :], axis=AX.X, op=ALU.add), reads=[sq], writes=[ssq])
                    self.rsqrt(ssq, ssq[:, :], ssq, ssq[:, :], 1.0 / 128.0)
                    for hh in range(2):
                        p.op("dve", lambda e, po=po, hh=hh: e.scalar_tensor_tensor(out=oc[:, hh * 128:(hh + 1) * 128], in0=po[:, hh * 128:(hh + 1) * 128], scalar=ssq[:, hh:hh + 1], in1=gG[:, :],
                                                                                  op0=ALU.mult, op1=ALU.mult), reads=[po, ssq, gG], writes=[oc])
                    p.op("dve", lambda e: e.tensor_tensor(out=oc[:, :], in0=oc[:, :], in1=sr[:, :], op=ALU.mult), reads=[oc, sr], writes=[oc])
                    pt = self.bank(4, 8)
                    for hh in range(2):
                        p.op("pe", lambda e, pt=pt, hh=hh: e.transpose(pt[:, hh * 128:(hh + 1) * 128], oc[:, hh * 128:(hh + 1) * 128], self.identf[:, :]), reads=[oc, self.identf], writes=[pt])
                    p.op("act", lambda e, pt=pt, tt=tt: e.copy(out=ot_[:, :, tt * 128:(tt + 1) * 128], in_=pt[:, 0:256].rearrange("p (a b) -> p a b", a=2)), reads=[pt], writes=[ot_])
                p.dma("pool", self.oT[2, hp * 2:(hp + 1) * 2, :, tb * 512:(tb + 1) * 512].rearrange("c p t -> p c t"), ot_[:, :, :], reads=[ot_], writes=[self.oT.bs[8 + hp * 2], self.oT.bs[8 + hp * 2 + 1]])

            for d in (1, 0):
                p.op("pool", lambda e, d=d: e.memset(S32[d][:, :], 0.0), writes=[S32[d]])
            order1 = list(range(7, -1, -1))
            sweep_A(1, order1[0])
            for i, tb in enumerate(order1):
                if i + 1 < 8:
                    sweep_A(1, order1[i + 1])
                sweep_B(1, tb)
            sweep_A(0, 0)
            for tb in range(8):
                if tb + 1 < 8:
                    sweep_A(0, tb + 1)
                sweep_B(0, tb)
                out_block(tb)
            p.barrier()
            st.close()

    def phase_s5(self, l):
        p = self.p
        W = self.W
        xT = self.xT
        xTr = list(xT.bs)
        st = ExitStack()
        sm = lambda n, shp=(128, 32): p.sb("s_" + n, list(shp), F32, st)
        stg = [p.sb(f"s_stg{i}", [128, 1024], F32, st) for i in range(2)]
        identb = p.sb("s_identb", [128, 128], BF16, st)
        jx = sm("jx", (128, 128))
        rowm = sm("rowm", (128, 8))
        sgn = sm("sgn", (128, 2))
        hpi = sm("hpi", (128, 1))
        wu = p.sb("s_wu", [128, 8, 128], BF16, st)
        wglu = p.sb("s_wglu", [128, 4, 512], BF16, st)
        bglu = sm("bglu", (128, 4))
        dcol = sm("dcol", (128, 4))
        CST = [[p.sb(f"s_cst{d}{b}", [128, 128], F32, st) for b in range(4)] for d in range(2)]
        BT = [[p.sb(f"s_bt{d}{b}", [128, 128], F32, st) for b in range(4)] for d in range(2)]
        PRt = [sm(f"pr{d}", (128, 12, 32)) for d in range(2)]
        PQt = [sm(f"pq{d}", (128, 12, 32)) for d in range(2)]
        pst = ExitStack()
        smp = lambda n, shp=(128, 32): p.sb("s_" + n, list(shp), F32, pst)
        p.dma("sp", jx[:], self.C["jx"][:, :], writes=[jx])
        p.dma("sp", rowm[:], self.C["rowmask"][:, :], writes=[rowm])
        p.dma("sp", sgn[:], self.C["sgn"][:, :], writes=[sgn])
        p.op("dve", lambda e: e.tensor_copy(out=identb[:, :], in_=self.identf[:, :]), reads=[self.identf], writes=[identb])
        p.op("pool", lambda e: e.memset(hpi[:, :], PI / 2), writes=[hpi])
        for b in range(4):
            self.wload(stg, wglu, wglu[:, b:b + 1, :], W["s5_w_glu"][l, b * 128:(b + 1) * 128, :], 1, 512)
        p.dma("sp", bglu[:], W["s5_b_glu"][l, :].rearrange("(m q) -> q m", q=128), writes=[bglu], allow_slow_non_contiguous=True)
        p.dma("sp", dcol[:], W["s5_d"][l, :].rearrange("(m q) -> q m", q=128), writes=[dcol], allow_slow_non_contiguous=True)

        PR, PQ, BST = [], [], []
        Cn = smp("Cn", (128, 128))
        dv = lambda fn, r, w: p.op("dve", fn, reads=r, writes=w)
        for d in range(2):
            are, aim, dt, lr, li, m1, cc, ss, t1, t2, nr, ni, den, cr, ci = (smp(f"{n}{d}") for n in
                                                                             ("are", "aim", "dt", "lr", "li", "m1", "cc", "ss", "t1", "t2", "nr", "ni", "den", "cr", "ci"))
            Xb = smp(f"Xb{d}", (128, 32, 16))
            Yb = smp(f"Yb{d}", (128, 32, 16))
            Bst = smp(f"Bst{d}", (128, 32, 16))
            Tb = smp(f"Tb{d}", (128, 32, 16))
            pr = PRt[d]
            pq = PQt[d]
            for hf in range(2):
                hs = slice(hf * 64, (hf + 1) * 64)
                p.dma("sp", are[hs, :], W["s5_a_re"][l, d, :, :].rearrange("g q -> q g"), writes=[are], allow_slow_non_contiguous=True)
                p.dma("sp", aim[hs, :], W["s5_a_im"][l, d, :, :].rearrange("g q -> q g"), writes=[aim], allow_slow_non_contiguous=True)
                own, oth = ("s5_b_re", "s5_b_im") if hf == 0 else ("s5_b_im", "s5_b_re")
                p.dma("sp", Xb[hs, :, :], W[own][l, d, :, :, :].rearrange("g q c -> q g c"), writes=[Xb])
                p.dma("sp", Yb[hs, :, :], W[oth][l, d, :, :, :].rearrange("g q c -> q g c"), writes=[Yb])
            p.dma("sp", dt[:], W["s5_log_dt"][l, d:d + 1, :].partition_broadcast(128), writes=[dt])
            p.op("act", lambda e, dt=dt: e.activation(out=dt[:, :], in_=dt[:, :], func=AF.Exp), reads=[dt], writes=[dt])
            TT = lambda o, a, b_, op: (lambda e: e.tensor_tensor(out=o[:, :], in0=a[:, :], in1=b_[:, :], op=op))
            dv(TT(lr, are, dt, ALU.mult), [are, dt], [lr])
            dv(TT(li, aim, dt, ALU.mult), [aim, dt], [li])
            TS = lambda o, a, s1, s2, o0, o1=None: (lambda e: e.tensor_scalar(o[:, :], a[:, :], s1, s2, o0, o1) if o1 is not None else e.tensor_scalar(o[:, :], a[:, :], s1, None, o0))
            dv(TS(m1, lr, 0.25, 1.0, ALU.mult, ALU.add), [lr], [m1])
            dv(TT(m1, m1, lr, ALU.mult), [m1, lr], [m1])
            dv(TS(m1, m1, 1.0 / 3.0, 1.0, ALU.mult, ALU.add), [m1], [m1])
            dv(TT(m1, m1, lr, ALU.mult), [m1, lr], [m1])
            dv(TS(m1, m1, 0.5, 1.0, ALU.mult, ALU.add), [m1], [m1])
            dv(TT(m1, m1, lr, ALU.mult), [m1, lr], [m1])
            dv(TS(m1, m1, 1.0, None, ALU.add), [m1], [m1])
            dv(TS(nr, li, 1.0 / 256.0, None, ALU.mult), [li], [nr])
            dv(TT(t1, nr, nr, ALU.mult), [nr], [t1])
            dv(TS(ss, t1, 1.0 / 120.0, -1.0 / 6.0, ALU.mult, ALU.add), [t1], [ss])
            dv(TT(ss, ss, t1, ALU.mult), [ss, t1], [ss])
            dv(TS(ss, ss, 1.0, None, ALU.add), [ss], [ss])
            dv(TT(ss, ss, nr, ALU.mult), [ss, nr], [ss])
            dv(TS(cc, t1, -1.0 / 720.0, 1.0 / 24.0, ALU.mult, ALU.add), [t1], [cc])
            dv(TT(cc, cc, t1, ALU.mult), [cc, t1], [cc])
            dv(TS(cc, cc, -0.5, None, ALU.add), [cc], [cc])
            dv(TT(cc, cc, t1, ALU.mult), [cc, t1], [cc])
            dv(TS(cc, cc, 1.0, None, ALU.add), [cc], [cc])
            for _ in range(8):
                dv(TT(t1, cc, cc, ALU.mult), [cc], [t1])
                dv(TT(t2, ss, ss, ALU.mult), [ss], [t2])
                dv(lambda e, cc=cc, ss=ss: e.scalar_tensor_tensor(out=ss[:, :], in0=cc[:, :], scalar=2.0, in1=ss[:, :], op0=ALU.mult, op1=ALU.mult), [cc, ss], [ss])
                dv(TT(cc, t1, t2, ALU.subtract), [t1, t2], [cc])
            dv(TT(t1, cc, cc, ALU.mult), [cc], [t1])
            dv(TT(t2, ss, ss, ALU.mult), [ss], [t2])
            dv(TT(t1, t1, t2, ALU.add), [t1, t2], [t1])
            dv(TS(t1, t1, -0.5, 1.5, ALU.mult, ALU.add), [t1], [t1])
            dv(TT(cc, cc, t1, ALU.mult), [cc, t1], [cc])
            dv(TT(ss, ss, t1, ALU.mult), [ss, t1], [ss])
            dv(lambda e, pr=pr, m1=m1, cc=cc: e.tensor_tensor(out=pr[:, 0, :], in0=m1[:, :], in1=cc[:, :], op=ALU.mult), [m1, cc], [pr])
            dv(TT(ni, m1, ss, ALU.mult), [m1, ss], [ni])
            dv(lambda e, pq=pq, ni=ni: e.tensor_scalar(pq[:, 0, :], ni[:, :], sgn[:, 1:2], None, ALU.mult), [ni, sgn], [pq])
            dv(lambda e, nr=nr, pr=pr: e.tensor_scalar(nr[:, :], pr[:, 0, :], -1.0, None, ALU.add), [pr], [nr])
            dv(TT(t1, are, are, ALU.mult), [are], [t1])
            dv(TT(t2, aim, aim, ALU.mult), [aim], [t2])
            dv(TT(den, t1, t2, ALU.add), [t1, t2], [den])
            dv(lambda e, den=den: e.reciprocal(out=den[:, :], in_=den[:, :]), [den], [den])
            dv(TT(t1, nr, are, ALU.mult), [nr, are], [t1])
            dv(TT(t2, ni, aim, ALU.mult), [ni, aim], [t2])
            dv(TT(cr, t1, t2, ALU.add), [t1, t2], [cr])
            dv(TT(cr, cr, den, ALU.mult), [cr, den], [cr])
            dv(TT(t1, ni, are, ALU.mult), [ni, are], [t1])
            dv(TT(t2, nr, aim, ALU.mult), [nr, aim], [t2])
            dv(TT(ci, t1, t2, ALU.subtract), [t1, t2], [ci])
            dv(TT(ci, ci, den, ALU.mult), [ci, den], [ci])
            dv(lambda e, ci=ci: e.tensor_scalar(ci[:, :], ci[:, :], sgn[:, 0:1], None, ALU.mult), [ci, sgn], [ci])
            bc = lambda t_: t_[:, :].unsqueeze(2).to_broadcast([128, 32, 16])
            dv(lambda e, Bst=Bst, Xb=Xb, cr=cr: e.tensor_tensor(out=Bst[:, :, :], in0=Xb[:, :, :], in1=bc(cr), op=ALU.mult), [Xb, cr], [Bst])
            dv(lambda e, Tb=Tb, Yb=Yb, ci=ci: e.tensor_tensor(out=Tb[:, :, :], in0=Yb[:, :, :], in1=bc(ci), op=ALU.mult), [Yb, ci], [Tb])
            dv(lambda e, Bst=Bst, Tb=Tb: e.tensor_tensor(out=Bst[:, :, :], in0=Bst[:, :, :], in1=Tb[:, :, :], op=ALU.add), [Bst, Tb], [Bst])
            for k in range(11):
                dv(lambda e, k=k, pr=pr, t1=t1: e.tensor_tensor(out=t1[:, :], in0=pr[:, k, :], in1=pr[:, k, :], op=ALU.mult), [pr], [t1])
                dv(lambda e, k=k, pq=pq, t2=t2: e.tensor_tensor(out=t2[:, :], in0=pq[:, k, :], in1=pq[:, k, :], op=ALU.mult), [pq], [t2])
                dv(lambda e, k=k, pr=pr, pq=pq: e.scalar_tensor_tensor(out=pq[:, k + 1, :], in0=pr[:, k, :], scalar=2.0, in1=pq[:, k, :], op0=ALU.mult, op1=ALU.mult), [pr, pq], [pq])
                dv(lambda e, k=k, pr=pr, t1=t1, t2=t2: e.tensor_tensor(out=pr[:, k + 1, :], in0=t1[:, :], in1=t2[:, :], op=ALU.subtract), [t1, t2], [pr])
            PR.append(pr)
            PQ.append(pq)
            if d == 0:
                for nm, tl_ in (("are", are), ("aim", aim), ("dt", dt), ("m1", m1), ("cc", cc), ("ss", ss), ("cr", cr), ("ci", ci)):
                    self.dump(nm, tl_, tl_[:, :], [128, 32])
                self.dump("pr", pr, pr[:, :, :], [128, 12, 32])
                self.dump("pq", pq, pq[:, :, :], [128, 12, 32])
                self.dump("Bst", Bst, Bst[:, :, :], [128, 32, 16])
            for b in range(4):
                ps = self.bank(0, 4)
                p.op("pe", lambda e, ps=ps, Bst=Bst, b=b: e.transpose(ps[:, 0:128], Bst[:, b * 8:(b + 1) * 8, :].rearrange("q g c -> q (g c)"), self.identf[:, :]), reads=[Bst, self.identf], writes=[ps])
                p.op("act", lambda e, ps=ps, d=d, b=b: e.copy(out=BT[d][b][:, :], in_=ps[:, 0:128]), reads=[ps], writes=[BT[d][b]])
                p.dma("sp", Cn[:, 0:64], W["s5_c_re"][l, d, b * 8:(b + 1) * 8, :, :].rearrange("g c q -> (g c) q"), writes=[Cn])
                p.dma("sp", Cn[:, 64:128], W["s5_c_im"][l, d, b * 8:(b + 1) * 8, :, :].rearrange("g c q -> (g c) q"), writes=[Cn])
                ps = self.bank(0, 4)
                p.op("pe", lambda e, ps=ps: e.transpose(ps[:, 0:128], Cn[:, :], self.identf[:, :]), reads=[Cn, self.identf], writes=[ps])
                p.op("dve", lambda e, ps=ps, d=d, b=b: e.tensor_scalar(CST[d][b][:, :], ps[:, 0:128], sgn[:, 1:2], None, ALU.mult), reads=[ps, sgn], writes=[CST[d][b]])

        for b_ in (0, 3):
            self.dump(f"BT{b_}", BT[0][b_], BT[0][b_][:, :], [128, 128])
            self.dump(f"CST{b_}", CST[0][b_], CST[0][b_][:, :], [128, 128])
        p.barrier()
        pst.close()
        uT = p.sb("s_uT", [128, S], BF16, st)
        X = [[p.sb(f"s_X{d}{i}", [128, S], BF16, st, nb=8) for i in range(2)] for d in range(2)]
        yacc = p.sb("s_yacc", [128, S], F32, st, nb=8)
        ygT = p.sb("s_ygT", [128, 4, S], BF16, st)
        Mk = [p.sb(f"s_Mk{i}", [128, 128], BF16, st) for i in range(8)]
        Mt = [p.sb(f"s_Mt{i}", [128, 128], F32, st) for i in range(4)]
        Mt2 = [p.sb(f"s_Mt2{i}", [128, 128], F32, st) for i in range(4)]
        LB = [p.sb(f"s_LB{i}", [128, 128], BF16, st) for i in range(4)]
        LC = [p.sb(f"s_LC{i}", [128, 128], BF16, st) for i in range(4)]
        yt = [p.sb(f"s_yt{i}", [128, 512], F32, st) for i in range(2)]
        ob = [p.sb(f"s_ob{i}", [128, 512], BF16, st) for i in range(2)]
        dirs = self.dbg.get("s5_dirs", (0, 1))
        mki = 0
        gi = 0
        bk = [0]

        def nb(lo, hi):
            b_ = self.PS[lo + bk[0] % (hi - lo)]
            bk[0] += 1
            return b_

        for b in self.dbg.get("s5_tiles", range(4)):
            c0 = COL["u"] + b * 128
            self.wload(stg, wu, wu[:, :, :], W["w_in"][l, :, c0:c0 + 128], 8, 128)
            for ct in range(8):
                ps = self.bank(0, 4)
                for c in range(8):
                    self.mm(ps[:, :], wu[:, c, :], xT[:, c, ct * 512:(ct + 1) * 512], c == 0, c == 7, [wu] + xTr[ct * 4:ct * 4 + 4], [ps])
                p.op("act", lambda e, ps=ps, ct=ct: e.copy(out=uT[:, ct * 512:(ct + 1) * 512], in_=ps[:, :]), reads=[ps], writes=[uT])
            first_acc = True
            for gl in range(8):
                g = b * 8 + gl
                lbs, lcs = {}, {}
                for d in dirs:
                    lb = LB[gi % 4]
                    lc = LC[gi % 4]
                    gi += 1
                    lbs[d], lcs[d] = lb, lc
                    p.op("dve", lambda e, lb=lb, d=d, gl=gl, b=b: e.tensor_scalar(lb[:, :], BT[d][b][:, :], rowm[:, gl:gl + 1], None, ALU.mult), reads=[BT[d][b], rowm], writes=[lb])
                    p.op("pool", lambda e, lc=lc: e.memset(lc[:, :], 0.0), writes=[lc])
                    p.op("pool", lambda e, lc=lc, d=d, gl=gl, b=b: e.tensor_copy(out=lc[:, gl * 16:(gl + 1) * 16], in_=CST[d][b][:, gl * 16:(gl + 1) * 16]), reads=[CST[d][b], lc], writes=[lc])
                    for ct in range(8):
                        ps = nb(0, 8)
                        self.mm(ps[:, :], lb[:, :], uT[:, ct * 512:(ct + 1) * 512], True, True, [lb, uT], [ps])
                        if ct % 2 == 0:
                            p.op("act", lambda e, ps=ps, ct=ct, d=d: e.copy(out=X[d][0][:, ct * 512:(ct + 1) * 512], in_=ps[:, :]), reads=[ps], writes=[X[d][0].bs[ct]])
                        else:
                            p.op("dve", lambda e, ps=ps, ct=ct, d=d: e.tensor_copy(out=X[d][0][:, ct * 512:(ct + 1) * 512], in_=ps[:, :]), reads=[ps], writes=[X[d][0].bs[ct]])
                cur = 0
                mkq = {}

                def build_mk(k, g=g):
                    nonlocal mki
                    for d in dirs:
                        mt = Mt[mki % 4]
                        mk = Mk[mki % 8]
                        mki += 1
                        mkq[(k, d)] = mk
                        p.op("act", lambda e, mt=mt, d=d, k=k, g=g: e.mul(out=mt[:, :], in_=jx[:, :], mul=PQ[d][:, k, g:g + 1]), reads=[jx, PQ[d]], writes=[mt])
                        p.op("dve", lambda e, mt=mt, mk=mk, d=d, k=k, g=g: e.scalar_tensor_tensor(out=mk[:, :], in0=self.identf[:, :], scalar=PR[d][:, k, g:g + 1], in1=mt[:, :], op0=ALU.mult, op1=ALU.add),
                             reads=[self.identf, PR[d], mt], writes=[mk])

                build_mk(0)
                build_mk(1)
                for k in range(12):
                    sh = 1 << k
                    if k + 2 < 12:
                        build_mk(k + 2)
                    mks = {d: mkq[(k, d)] for d in dirs}
                    for ct in range(8):
                        for d in dirs:
                            mk = mks[d]
                            Xs, Xd = X[d][cur], X[d][1 - cur]
                            t0 = ct * 512
                            if d == 0:
                                lo = max(0, sh - t0)
                                hi = 512
                                s0 = t0 + lo - sh
                            else:
                                lo = 0
                                hi = min(512, S - sh - t0)
                                s0 = t0 + sh
                            has = hi > lo
                            srcb = []
                            if has:
                                a0, a1 = s0, s0 + (hi - lo)
                                srcb = [Xs.bs[i] for i in range(a0 // 512, (a1 - 1) // 512 + 1)]
                            use_pe = ((ct + d) % 2 == 0) or not has
                            ps = nb(0, 8)
                            if use_pe:
                                self.mm(ps[:, :], identb[:, :], Xs[:, t0:t0 + 512], True, not has, [identb, Xs.bs[ct]], [ps])
                                if has:
                                    self.mm(ps[:, lo:hi], mk[:, :], Xs[:, s0:s0 + hi - lo], False, True, [mk] + srcb, [ps])
                                p.op("act", lambda e, ps=ps, Xd=Xd, t0=t0: e.copy(out=Xd[:, t0:t0 + 512], in_=ps[:, :]), reads=[ps], writes=[Xd.bs[ct]])
                            else:
                                self.mm(ps[:, lo:hi], mk[:, :], Xs[:, s0:s0 + hi - lo], True, True, [mk] + srcb, [ps])
                                p.op("dve", lambda e, ps=ps, Xd=Xd, Xs=Xs, t0=t0, lo=lo, hi=hi: e.tensor_tensor(out=Xd[:, t0 + lo:t0 + hi], in0=ps[:, lo:hi], in1=Xs[:, t0 + lo:t0 + hi], op=ALU.add),
                                     reads=[ps, Xs.bs[ct]], writes=[Xd.bs[ct]])
                                if lo > 0:
                                    p.op("pool", lambda e, Xd=Xd, Xs=Xs, t0=t0, lo=lo: e.tensor_copy(out=Xd[:, t0:t0 + lo], in_=Xs[:, t0:t0 + lo]), reads=[Xs.bs[ct]], writes=[Xd.bs[ct]])
                                if hi < 512:
                                    p.op("pool", lambda e, Xd=Xd, Xs=Xs, t0=t0, hi=hi: e.tensor_copy(out=Xd[:, t0 + hi:t0 + 512], in_=Xs[:, t0 + hi:t0 + 512]), reads=[Xs.bs[ct]], writes=[Xd.bs[ct]])
                    cur = 1 - cur
                for ct in range(8):
                    ps = nb(0, 8)
                    for i_, d in enumerate(dirs):
                        self.mm(ps[:, :], lcs[d][:, :], X[d][cur][:, ct * 512:(ct + 1) * 512], i_ == 0, i_ == len(dirs) - 1, [lcs[d], X[d][cur].bs[ct]], [ps])
                    ya = yacc[:, ct * 512:(ct + 1) * 512]
                    if first_acc:
                        p.op("dve", lambda e, ps=ps, ya=ya: e.tensor_copy(out=ya, in_=ps[:, :]), reads=[ps], writes=[yacc.bs[ct]])
                    else:
                        p.op("dve", lambda e, ps=ps, ya=ya: e.tensor_tensor(out=ya, in0=ps[:, :], in1=ya, op=ALU.add), reads=[ps], writes=[yacc.bs[ct]])
                first_acc = False
            for ct in range(8):
                y_ = yt[ct % 2]
                sl = slice(ct * 512, (ct + 1) * 512)
                p.op("dve", lambda e, y_=y_, sl=sl, b=b: e.scalar_tensor_tensor(out=y_[:, :], in0=uT[:, sl], scalar=dcol[:, b:b + 1], in1=yacc[:, sl], op0=ALU.mult, op1=ALU.add),
                     reads=[uT, dcol, yacc.bs[ct]], writes=[y_])
                if self.dbg.get("s5y"):
                    p.dma("pool", self.dbgy[b, :, sl], y_[:, :], reads=[y_], writes=[self.dbgy])
                    p.dma("pool", self.dbgy[4 + b, :, sl], yacc[:, sl], reads=[yacc.bs[ct]], writes=[self.dbgy])
                p.op("act", lambda e, y_=y_, sl=sl, b=b: e.activation(out=ygT[:, b, sl], in_=y_[:, :], func=AF.Gelu_apprx_tanh), reads=[y_], writes=[ygT])
        for m in range(4):
            for ct in range(8):
                sl = slice(ct * 512, (ct + 1) * 512)
                ps = self.bank(0, 8)
                for b in range(4):
                    self.mm(ps[:, :], wglu[:, b, m * 128:(m + 1) * 128], ygT[:, b, sl], b == 0, b == 3, [wglu, ygT], [ps])
                y_ = yt[ct % 2]
                o_ = ob[ct % 2]
                p.op("act", lambda e, ps=ps, y_=y_, m=m: e.activation(out=y_[:, :], in_=ps[:, :], func=AF.Sigmoid, bias=bglu[:, m:m + 1]), reads=[ps, bglu], writes=[y_])
                p.op("dve", lambda e, y_=y_, o_=o_, m=m, sl=sl: e.tensor_tensor(out=o_[:, :], in0=ygT[:, m, sl], in1=y_[:, :], op=ALU.mult), reads=[ygT, y_], writes=[o_])
                p.dma("pool", self.oT[1, m, :, sl], o_[:, :], reads=[o_], writes=[self.oT.bs[4 + m]])
        p.barrier()
        st.close()


def build_model():
    M = Model()
    M.phase_ln0()
    for l in range(DEPTH):
        M.phase_attn(l)
        M.phase_s5(l)
        M.phase_gla(l)
        M.phase_merge1(l)
        M.phase_merge2(l)
        M.phase_moe(l, last=(l == DEPTH - 1))
    M.p.emit()
    return M


def kernel(**inputs):
    M = build_model()
    consts = host_consts()
    shared = {k: np.ascontiguousarray(np.asarray(inputs[k], dtype=np.float32)) for k in PARAM_SHAPES}
    shared.update(consts)
    x = np.asarray(inputs["x"], dtype=np.float32)
    in_maps = []
    for b in range(8):
        m = dict(shared)
        m["x"] = np.ascontiguousarray(x[b])
        in_maps.append(m)
    res = run_bass_kernel_spmd(M.nc, in_maps, core_ids=list(range(8)))
    return np.stack([np.asarray(r["out"], dtype=np.float32) for r in res.results], axis=0)
```

```python
import math
from contextlib import ExitStack
import numpy as np
import ml_dtypes
import concourse.bass as bass
import concourse.mybir as mybir
from concourse.bass_utils import run_bass_kernel_spmd

F32 = mybir.dt.float32
BF16 = mybir.dt.bfloat16
AF = mybir.ActivationFunctionType
ALU = mybir.AluOpType
AX = mybir.AxisListType

ENGS = ("pe", "act", "dve", "pool", "sp")
NDMA_SEM = 6


class Buf:
    __slots__ = ("name", "w", "r", "excl")

    def __init__(self, name=""):
        self.name = name
        self.w = None
        self.r = {}
        self.excl = False


class Tl:
    def __init__(self, t, name, nb=0):
        self.t = t
        self.b = Buf(name)
        self.bs = [Buf(f"{name}{i}") for i in range(nb)]

    def __getitem__(self, k):
        return self.t[k]


class Sub(Tl):
    def __init__(self, ap, buf):
        self.t = ap
        self.b = buf
        self.bs = []


class Prog:
    def __init__(self, nc):
        self.nc = nc
        self.stack = ExitStack()
        self.lists = {e: [] for e in ENGS}
        self.cnt = {e: 0 for e in ENGS}
        self.known = {e: {} for e in ENGS}
        self.sem = {}
        for e in ("pe", "act", "dve", "pool"):
            self.sem["c_" + e] = self.stack.enter_context(nc.semaphore("c_" + e))
        self.dq = {}
        for q in ("sp", "pool", "act"):
            names = []
            for i in range(NDMA_SEM):
                n = f"d_{q}{i}"
                self.sem[n] = self.stack.enter_context(nc.semaphore(n))
                names.append(n)
            self.dq[q] = dict(sems=names, issued=[0] * NDMA_SEM, rr=0)
        self.nwaits = 0

    uid = 0

    def sb(self, name, shape, dtype, stack=None, nb=0):
        Prog.uid += 1
        name = f"{name}_{Prog.uid}"
        t = (stack or self.stack).enter_context(self.nc.sbuf_tensor(name, list(shape), dtype))
        return Tl(t, name, nb)

    def ps(self, name, shape, dtype=F32, stack=None, nb=0):
        t = (stack or self.stack).enter_context(self.nc.psum_tensor(name, list(shape), dtype))
        tl = Tl(t, name, nb)
        tl.b.excl = True
        return tl

    def dram(self, name, shape, dtype, kind="Internal", nb=0):
        t = self.nc.dram_tensor(name, list(shape), dtype, kind=kind).ap()
        return Tl(t, name, nb)

    @staticmethod
    def _bufs(xs):
        out = []
        for x in xs:
            if x is None:
                continue
            out.append(x.b if isinstance(x, Tl) else x)
        return out

    def _need(self, reads, writes):
        need = {}

        def add(sv):
            s, v = sv
            if need.get(s, 0) < v:
                need[s] = v

        for b in reads:
            if b.w:
                add(b.w)
        for b in writes:
            if b.w:
                add(b.w)
            for sv in b.r.items():
                add(sv)
        return need

    def _waits(self, e, need, skip=None):
        kn = self.known[e]
        for s, v in need.items():
            if s == skip:
                continue
            if kn.get(s, 0) < v:
                self.lists[e].append(("wait", s, v))
                kn[s] = v
                self.nwaits += 1

    def op(self, e, fn, reads=(), writes=(), serial=False):
        reads = self._bufs(reads)
        writes = self._bufs(writes)
        ex = [b for b in reads if b.excl and b not in writes]
        if ex:
            reads = [b for b in reads if not b.excl]
            writes = writes + ex
        s = "c_" + e
        self._waits(e, self._need(reads, writes), skip=(s if (e == "pe" and not serial) else None))
        self.cnt[e] += 1
        v = self.cnt[e]
        self.lists[e].append(("op", fn, s))
        for b in reads:
            b.r[s] = v
        for b in writes:
            b.w = (s, v)
            b.r = {}

    def dma(self, q, out, in_, reads=(), writes=(), **kw):
        reads = self._bufs(reads)
        writes = self._bufs(writes)
        d = self.dq[q]
        i = d["rr"]
        d["rr"] = (i + 1) % NDMA_SEM
        s = d["sems"][i]
        need = self._need(reads, writes)
        if d["issued"][i] > 0:
            pv = 16 * d["issued"][i]
            if need.get(s, 0) < pv:
                need[s] = pv
        self._waits(q, need)
        d["issued"][i] += 1
        v = 16 * d["issued"][i]
        self.lists[q].append(("dma", out, in_, s, kw))
        for b in reads:
            b.r[s] = v
        for b in writes:
            b.w = (s, v)
            b.r = {}

    def barrier(self, engines=ENGS):
        need = {}
        for e in ("pe", "act", "dve", "pool"):
            if self.cnt[e]:
                need["c_" + e] = self.cnt[e]
        for q, d in self.dq.items():
            for s, n in zip(d["sems"], d["issued"]):
                if n:
                    need[s] = 16 * n
        for e in engines:
            self._waits(e, dict(need), skip=("c_" + e if e in ("pe",) else None))

    def emit(self):
        nc = self.nc
        self.barrier(engines=("sp",))
        lists = self.lists
        sem = self.sem

        def run(eng, lst):
            for it in lst:
                if it[0] == "wait":
                    eng.wait_ge(sem[it[1]], it[2])
                elif it[0] == "op":
                    it[1](eng).then_inc(sem[it[2]], 1)
                else:
                    eng.dma_start(out=it[1], in_=it[2], **it[4]).then_inc(sem[it[3]], 16)

        with nc.Block() as block:
            @block.tensor
            def _(eng):
                run(eng, lists["pe"])

            @block.scalar
            def _(eng):
                run(eng, lists["act"])

            @block.vector
            def _(eng):
                run(eng, lists["dve"])

            @block.gpsimd
            def _(eng):
                run(eng, lists["pool"])

            @block.sync
            def _(eng):
                run(eng, lists["sp"])
        self.stack.close()


S = 4096
D = 1024
NT = 32
DEPTH = 2
N_IN = 6688
COL = dict(qa=0, ka=512, va=1024, u=1536, qc=2048, kc=2304, vc=2560, rc=3072, zf=3584, zb=3600, gate=3616)
ALPHA = (2.0 * DEPTH) ** 0.25
EPS = 1e-5
PI = math.pi

PARAM_SHAPES = dict(
    ln0_g=[1024], ln0_b=[1024], w_in=[2, 1024, 6688], da_lambda=[2, 4, 64], da_norm_g=[2, 128],
    s5_a_re=[2, 2, 32, 64], s5_a_im=[2, 2, 32, 64], s5_log_dt=[2, 2, 32], s5_b_re=[2, 2, 32, 64, 16],
    s5_b_im=[2, 2, 32, 64, 16], s5_c_re=[2, 2, 32, 16, 64], s5_c_im=[2, 2, 32, 16, 64], s5_d=[2, 512],
    s5_w_glu=[2, 512, 512], s5_b_glu=[2, 512], gla_w_gate=[2, 2, 16, 256], gla_b_gate=[2, 2, 256],
    gla_norm_g=[2, 128], merge_w_up=[2, 3, 512, 1024], merge_b=[2, 3, 1024], w_out=[2, 1024, 1024],
    ln1_g=[2, 1024], ln1_b=[2, 1024], router_w=[1024, 16], router_bias=[16],
    moe_w_gate=[2, 16, 1024, 512], moe_w_up=[2, 16, 1024, 512], moe_w_down=[2, 16, 512, 1024],
    ln2_g=[2, 1024], ln2_b=[2, 1024])


def host_consts():
    c = {}
    c["identf"] = np.eye(128, dtype=np.float32)
    i = np.arange(128)
    c["absdiff"] = np.abs(i[:, None] - i[None, :]).astype(np.float32)
    pos = np.arange(S)
    hi, lo = pos // 64, pos % 64
    c["qaug"] = np.stack([64.0 * hi, lo, np.ones(S), np.ones(S)]).astype(ml_dtypes.bfloat16)
    ka = np.zeros((4, 2, 4, S), np.float32)
    for h in range(4):
        sl = 2.0 ** (-2.0 * (h + 1))
        plus = np.stack([np.full(S, sl), np.full(S, sl), -sl * 64.0 * hi, -sl * lo])
        ka[h, 0] = plus
        ka[h, 1] = -plus
    c["kaug"] = ka.astype(ml_dtypes.bfloat16)
    j = np.arange(64)
    c["gmask"] = np.stack([(j[:, None] <= j[None, :]), (j[:, None] > j[None, :])]).astype(np.float32)
    jj = np.repeat(np.arange(8), 16)
    c["s5mask"] = np.stack([(jj[None, :] >= jj[:, None]), (jj[None, :] <= jj[:, None])]).astype(np.float32)
    jx = np.zeros((128, 128), np.float32)
    for q in range(64):
        jx[q, 64 + q] = 1.0
        jx[64 + q, q] = 1.0
    c["jx"] = jx
    c["rowmask"] = (np.arange(128)[:, None] // 16 == np.arange(8)[None, :]).astype(np.float32)
    sg = np.ones((128, 2), np.float32)
    sg[:64, 0] = -1.0
    sg[64:, 1] = -1.0
    c["sgn"] = sg
    return c


CONST_SHAPES = dict(jx=([128, 128], F32), rowmask=([128, 8], F32), sgn=([128, 2], F32), identf=([128, 128], F32), absdiff=([128, 128], F32), qaug=([4, S], BF16), kaug=([4, 2, 4, S], BF16),
                    gmask=([2, 64, 64], F32), s5mask=([2, 128, 128], F32))


class Model:
    def __init__(self, dbg=None):
        self.dbg = dbg or {}
        nc = bass.Bass("TRN2", target_bir_lowering=False)
        self.nc = nc
        p = Prog(nc)
        self.p = p
        kinds = self.dbg.get("kinds", {})
        self.x_in = p.dram("x", [S, D], F32, kind="ExternalInput")
        self.out = p.dram("out", [S, D], F32, kind="ExternalOutput", nb=NT)
        self.W = {k: p.dram(k, shp, F32, kind="ExternalInput") for k, shp in PARAM_SHAPES.items()}
        self.C = {k: p.dram(k, shp, dt, kind="ExternalInput") for k, (shp, dt) in CONST_SHAPES.items()}
        self.xres = p.dram("xres", [S, D], F32, kind=kinds.get("xres", "Internal"), nb=NT)
        self.oT = p.dram("oT", [3, 4, 128, S], BF16, kind=kinds.get("oT", "Internal"), nb=12)
        self.mT = p.dram("mT", [128, 8, S], BF16, kind=kinds.get("mT", "Internal"), nb=16)
        if self.dbg.get("s5y"):
            self.dbgy = p.dram("dbgy", [8, 128, S], F32, kind="ExternalOutput")
        self.xT = p.sb("xT", [128, 8, S], BF16, nb=NT)
        self.identf = p.sb("identf_sb", [128, 128], F32)
        self.call = p.sb("call", [128, NT, 16], F32, nb=NT)
        self.PS = [p.ps(f"ps{i}", [128, 512], F32) for i in range(8)]
        self.psi = 0
        self.psc = {}
        p.dma("sp", self.identf[:], self.C["identf"][:, :], writes=[self.identf])

    def dump(self, name, tl, ap, shape):
        if not self.dbg.get("dump"):
            return
        d = self.p.dram("dump_" + name, list(shape), F32, kind="ExternalOutput")
        self.p.dma("pool", d.t, ap, reads=[tl], writes=[d])

    def bank(self, lo=0, hi=8):
        n = hi - lo
        c = self.psc.get((lo, hi), 0)
        self.psc[(lo, hi)] = c + 1
        return self.PS[lo + (c % n)]

    def mm(self, out, lhsT, rhs, start, stop, reads, writes, serial=False):
        self.p.op("pe", lambda e: e.matmul(out, lhsT, rhs, start=start, stop=stop, skip_group_check=True), reads=reads, writes=writes, serial=serial)

    def ln_tile(self, z, xn, stats, mv, rstd):
        p = self.p
        for k in range(2):
            p.op("dve", lambda e, k=k: e.bn_stats(out=stats[:, k, :], in_=z[:, k * 512:(k + 1) * 512]), reads=[z], writes=[stats])
        p.op("dve", lambda e: e.bn_aggr(out=mv[:, :], in_=stats[:, :, :].rearrange("p a b -> p (a b)")), reads=[stats], writes=[mv])
        self.rsqrt(rstd, rstd[:, :], mv, mv[:, 1:2], 1.0)
        p.op("dve", lambda e: e.tensor_scalar(xn[:, :], z[:, :], mv[:, 0:1], rstd[:, 0:1], ALU.subtract, ALU.mult), reads=[z, mv, rstd], writes=[xn])
        p.op("pool", lambda e, g_=self.gbc: e.tensor_tensor(out=xn[:, :], in0=xn[:, :], in1=g_[:, :], op=ALU.mult), reads=[xn, self.gbc], writes=[xn])
        p.op("pool", lambda e, b_=self.bbc: e.tensor_tensor(out=xn[:, :], in0=xn[:, :], in1=b_[:, :], op=ALU.add), reads=[xn, self.bbc], writes=[xn])

    def rsqrt(self, dst_tl, dst, src_tl, src, scale):
        p = self.p
        p.op("dve", lambda e: e.tensor_scalar(dst, src, scale, EPS, ALU.mult, ALU.add), reads=[src_tl], writes=[dst_tl])
        p.op("act", lambda e: e.sqrt(out=dst, in_=dst), reads=[dst_tl], writes=[dst_tl])
        p.op("dve", lambda e: e.reciprocal(out=dst, in_=dst), reads=[dst_tl], writes=[dst_tl])

    def load_ln_params(self, g_ap, b_ap, st):
        p = self.p
        self.gbc = p.sb("gbc", [128, D], F32, st)
        self.bbc = p.sb("bbc", [128, D], F32, st)
        p.dma("sp", self.gbc[:], g_ap.partition_broadcast(128), writes=[self.gbc])
        p.dma("sp", self.bbc[:], b_ap.partition_broadcast(128), writes=[self.bbc])

    def to_xT(self, xn, t, xT32=None):
        p = self.p
        for c0 in (0, 4):
            ps = self.bank(0, 4)
            for j in range(4):
                c = c0 + j
                p.op("pe", lambda e, ps=ps, j=j, c=c: e.transpose(ps[:, j * 128:(j + 1) * 128], xn[:, c * 128:(c + 1) * 128], self.identf[:, :]),
                     reads=[xn, self.identf], writes=[ps])
            src = ps[:, :].rearrange("p (j n) -> p j n", j=4)
            p.op("act", lambda e, src=src, c0=c0: e.copy(out=self.xT[:, c0:c0 + 4, t * 128:(t + 1) * 128], in_=src), reads=[ps], writes=[self.xT.bs[t]])
            if xT32 is not None:
                p.op("dve", lambda e, src=src, c0=c0: e.tensor_copy(out=xT32[:, c0:c0 + 4, :], in_=src), reads=[ps], writes=[xT32])

    def phase_ln0(self):
        p = self.p
        st = ExitStack()
        self.load_ln_params(self.W["ln0_g"].t.rearrange("(o n) -> o n", o=1), self.W["ln0_b"].t.rearrange("(o n) -> o n", o=1), st)
        zs = [p.sb(f"l0z{i}", [128, D], F32, st) for i in range(2)]
        stats = p.sb("l0stats", [128, 2, 6], F32, st)
        mv = p.sb("l0mv", [128, 2], F32, st)
        rstd = p.sb("l0rstd", [128, 1], F32, st)
        for t in range(NT):
            z = zs[t % 2]
            p.dma("sp", z[:], self.x_in[t * 128:(t + 1) * 128, :], writes=[z])
            self.ln_tile(z, z, stats, mv, rstd)
            p.dma("pool", self.xres[t * 128:(t + 1) * 128, :], z[:], reads=[z], writes=[self.xres.bs[t]])
            self.to_xT(z, t)
        p.barrier()
        st.close()

    def wload(self, stg, dst_tl, dst_ap, src_ap, kc, ncols, cast="act", wbuf=None):
        p = self.p
        cap = stg[0].t.shape[1]
        kcp = max(1, min(kc, cap // ncols))
        wb = [wbuf if wbuf is not None else dst_tl]
        for k0 in range(0, kc, kcp):
            st = stg[self.wl_i % len(stg)]
            self.wl_i += 1
            view = st[:, 0:kcp * ncols].rearrange("p (c n) -> p c n", c=kcp)
            p.dma("sp", view, src_ap[k0 * 128:(k0 + kcp) * 128, :].rearrange("(c p) n -> p c n", p=128), writes=[st])
            if cast == "act":
                p.op(cast, lambda e, view=view, k0=k0: e.copy(out=dst_ap[:, k0:k0 + kcp, :], in_=view), reads=[st], writes=wb)
            else:
                p.op(cast, lambda e, view=view, k0=k0: e.tensor_copy(out=dst_ap[:, k0:k0 + kcp, :], in_=view), reads=[st], writes=wb)

    wl_i = 0

    def phase_attn(self, l):
        p = self.p
        W = self.W
        st = ExitStack()
        stg = [p.sb(f"a_stg{i}", [128, 8 * 128], F32, st) for i in range(2)]
        wA = [p.sb(f"a_w{i}", [128, 8, 384], BF16, st) for i in range(2)]
        QT = [p.sb(f"a_qt{m}", [68, S], BF16, st) for m in range(2)]
        KTp = [p.sb(f"a_ktp{m}", [68, S], BF16, st) for m in range(2)]
        KTm = [p.sb(f"a_ktm{m}", [68, S], BF16, st) for m in range(2)]
        V = p.sb("a_v", [128, NT, 129], BF16, st)
        PT = [p.sb(f"a_pt{i}", [128, 512], BF16, st) for i in range(4)]
        oaT = [p.sb(f"a_oat{i}", [128, S], BF16, st) for i in range(1)]
        on = [p.sb(f"a_on{m}", [128, 4, 128], F32, st) for m in range(2)]
        rs = p.sb("a_rs", [128, 4], F32, st)
        diff = p.sb("a_diff", [128, 4, 128], F32, st)
        sq = p.sb("a_sq", [128, 4, 128], F32, st)
        ss = p.sb("a_ss", [128, 4], F32, st)
        rstd = p.sb("a_rstd", [128, 4], F32, st)
        oo = p.sb("a_oo", [128, 4, 128], F32, st)
        absd = p.sb("a_absd", [128, 128], F32, st)
        lamt = p.sb("a_lamt", [128, 256], F32, st)
        lsm = p.sb("a_lsm", [128, 8], F32, st)
        gA = p.sb("a_gA", [128, 128], F32, st)
        lam_init = 0.8 - 0.6 * math.exp(-0.3 * l)
        p.dma("sp", absd[:], self.C["absdiff"][:, :], writes=[absd])
        for m in range(2):
            p.dma("sp", QT[m][64:68, :], self.C["qaug"][:, :], writes=[QT[m]])
        p.dma("sp", lamt[:], W["da_lambda"][l:l + 1, :, :].rearrange("o a b -> o (a b)").partition_broadcast(128), writes=[lamt])
        p.dma("sp", gA[:], W["da_norm_g"][l:l + 1, :].partition_broadcast(128), writes=[gA])
        p.op("dve", lambda e: e.tensor_scalar(gA[:, :], gA[:, :], 1.0 - lam_init, None, ALU.mult), reads=[gA], writes=[gA])
        p.op("dve", lambda e: e.tensor_tensor(out=lamt[:, 0:64], in0=lamt[:, 0:64], in1=lamt[:, 64:128], op=ALU.mult), reads=[lamt], writes=[lamt])
        p.op("dve", lambda e: e.tensor_tensor(out=lamt[:, 128:192], in0=lamt[:, 128:192], in1=lamt[:, 192:256], op=ALU.mult), reads=[lamt], writes=[lamt])
        p.op("dve", lambda e: e.tensor_reduce(out=lsm[:, 0:1], in_=lamt[:, 0:64], axis=AX.X, op=ALU.add), reads=[lamt], writes=[lsm])
        p.op("dve", lambda e: e.tensor_reduce(out=lsm[:, 1:2], in_=lamt[:, 128:192], axis=AX.X, op=ALU.add), reads=[lamt], writes=[lsm])
        p.op("act", lambda e: e.activation(out=lsm[:, 2:4], in_=lsm[:, 0:2], func=AF.Exp), reads=[lsm], writes=[lsm])
        p.op("dve", lambda e: e.tensor_tensor(out=lsm[:, 4:5], in0=lsm[:, 3:4], in1=lsm[:, 2:3], op=ALU.subtract), reads=[lsm], writes=[lsm])
        p.op("dve", lambda e: e.tensor_scalar(lsm[:, 5:6], lsm[:, 4:5], -lam_init, None, ALU.add), reads=[lsm], writes=[lsm])
        p.op("dve", lambda e: e.memset(V[:, :, 128:129], 1.0), writes=[V])
        xT = self.xT
        xTr = list(xT.bs)

        def load_head_w(h):
            w = wA[h % 2]
            for k, nm in enumerate(("qa", "ka", "va")):
                c0 = COL[nm] + h * 128
                self.wload(stg, w, w[:, :, k * 128:(k + 1) * 128], W["w_in"][l, :, c0:c0 + 128], 8, 128, cast="dve")

        load_head_w(0)
        for h in range(self.dbg.get("heads", 4)):
            slope = 2.0 ** (-2.0 * (h + 1))
            w = wA[h % 2]
            if h + 1 < self.dbg.get("heads", 4):
                load_head_w(h + 1)
            for m in range(2):
                p.dma("sp", KTp[m][64:68, :], self.C["kaug"][h, 0, :, :], writes=[KTp[m]])
                p.dma("sp", KTm[m][64:68, :], self.C["kaug"][h, 1, :, :], writes=[KTm[m]])
            for m in range(2):
                for tb in range(8):
                    ps = self.bank(0, 4)
                    for c in range(8):
                        self.mm(ps[0:64, :], w[:, c, m * 64:(m + 1) * 64], xT[:, c, tb * 512:(tb + 1) * 512], c == 0, c == 7, [w] + xTr[tb * 4:tb * 4 + 4], [ps])
                    p.op("act", lambda e, ps=ps, m=m, tb=tb: e.mul(out=QT[m][0:64, tb * 512:(tb + 1) * 512], in_=ps[0:64, :], mul=0.125), reads=[ps], writes=[QT[m]])
                    ps = self.bank(0, 4)
                    for c in range(8):
                        self.mm(ps[0:64, :], w[:, c, 128 + m * 64:128 + (m + 1) * 64], xT[:, c, tb * 512:(tb + 1) * 512], c == 0, c == 7, [w] + xTr[tb * 4:tb * 4 + 4], [ps])
                    p.op("act", lambda e, ps=ps, m=m, tb=tb: e.copy(out=KTp[m][0:64, tb * 512:(tb + 1) * 512], in_=ps[0:64, :]), reads=[ps], writes=[KTp[m]])
                    p.op("dve", lambda e, ps=ps, m=m, tb=tb: e.tensor_copy(out=KTm[m][0:64, tb * 512:(tb + 1) * 512], in_=ps[0:64, :]), reads=[ps], writes=[KTm[m]])
            for t4 in range(8):
                ps = self.bank(0, 4)
                for j in range(4):
                    t = t4 * 4 + j
                    for c in range(8):
                        self.mm(ps[:, j * 128:(j + 1) * 128], xT[:, c, t * 128:(t + 1) * 128], w[:, c, 256:384], c == 0, c == 7, [w, xTr[t]], [ps])
                p.op("act", lambda e, ps=ps, t4=t4: e.copy(out=V[:, t4 * 4:(t4 + 1) * 4, 0:128], in_=ps[:, :].rearrange("p (j n) -> p j n", j=4)), reads=[ps], writes=[V])
            oa = oaT[0]
            pti = 0
            for Q in range(self.dbg.get("nQ", 8)):
                stages = [(m, kt) for m in range(2) for kt in range(NT)]
                stbank = {}
                ptbuf = {}

                def st_S(i, Q=Q):
                    m, kt = stages[i]
                    ps = self.PS[i % 4]
                    stbank[i] = ps
                    rel = kt - 4 * Q
                    ksl = slice(kt * 128, (kt + 1) * 128)
                    if rel < 0:
                        self.mm(ps[:, :], KTm[m][0:68, ksl], QT[m][0:68, Q * 512:(Q + 1) * 512], True, True, [KTm[m], QT[m]], [ps])
                    elif rel > 3:
                        self.mm(ps[:, :], KTp[m][0:68, ksl], QT[m][0:68, Q * 512:(Q + 1) * 512], True, True, [KTp[m], QT[m]], [ps])
                    else:
                        q0 = Q * 512
                        first = True
                        if rel > 0:
                            self.mm(ps[:, 0:rel * 128], KTp[m][0:68, ksl], QT[m][0:68, q0:q0 + rel * 128], first, True, [KTp[m], QT[m]], [ps])
                            first = False
                        self.mm(ps[:, rel * 128:(rel + 1) * 128], KTp[m][0:64, ksl], QT[m][0:64, q0 + rel * 128:q0 + (rel + 1) * 128], first, True, [KTp[m], QT[m]], [ps])
                        if rel < 3:
                            self.mm(ps[:, (rel + 1) * 128:512], KTm[m][0:68, ksl], QT[m][0:68, q0 + (rel + 1) * 128:q0 + 512], False, True, [KTm[m], QT[m]], [ps])
                        p.op("dve", lambda e, ps=ps, rel=rel, slope=slope: e.scalar_tensor_tensor(
                            out=ps[:, rel * 128:(rel + 1) * 128], in0=absd[:, :], scalar=-slope, in1=ps[:, rel * 128:(rel + 1) * 128], op0=ALU.mult, op1=ALU.add),
                            reads=[absd, ps], writes=[ps])

                def st_E(i):
                    ps = stbank[i]
                    pt = PT[i % 4]
                    ptbuf[i] = pt
                    p.op("act", lambda e, ps=ps, pt=pt: e.activation(out=pt[:, :], in_=ps[:, :], func=AF.Exp), reads=[ps], writes=[pt])

                def st_P(i):
                    m, kt = stages[i]
                    pt = ptbuf[i]
                    OB = (self.PS[4 + 2 * m], self.PS[5 + 2 * m])
                    for j in range(4):
                        ob = OB[j // 2]
                        oc = (j % 2) * 256
                        self.mm(ob[:, oc:oc + 129], pt[:, j * 128:(j + 1) * 128], V[:, kt, :], (kt == 0 and j % 2 == 0), kt == NT - 1, [pt, V], [ob])
                    if kt == NT - 1:
                        for j in range(4):
                            ob = OB[j // 2]
                            oc = (j % 2) * 256
                            p.op("dve", lambda e, ob=ob, oc=oc, j=j: e.reciprocal(out=rs[:, j:j + 1], in_=ob[:, oc + 128:oc + 129]), reads=[ob], writes=[rs])
                            p.op("dve", lambda e, ob=ob, oc=oc, j=j, m=m: e.tensor_scalar(on[m][:, j, :], ob[:, oc:oc + 128], rs[:, j:j + 1], None, ALU.mult), reads=[ob, rs], writes=[on[m]])

                NS = len(stages)
                st_S(0)
                st_S(1)
                for i in range(NS):
                    st_E(i)
                    if i + 2 < NS:
                        st_S(i + 2)
                    st_P(i)
                p.op("dve", lambda e: e.scalar_tensor_tensor(out=diff[:, :, :], in0=on[1][:, :, :], scalar=lsm[:, 5:6], in1=on[0][:, :, :], op0=ALU.mult, op1=ALU.add),
                     reads=[on[0], on[1], lsm], writes=[diff])
                p.op("pool", lambda e: e.tensor_tensor(out=sq[:, :, :], in0=diff[:, :, :], in1=diff[:, :, :], op=ALU.mult), reads=[diff], writes=[sq])
                p.op("dve", lambda e: e.tensor_reduce(out=ss[:, :], in_=sq[:, :, :], axis=AX.X, op=ALU.add), reads=[sq], writes=[ss])
                self.rsqrt(rstd, rstd[:, :], ss, ss[:, :], 1.0 / 128.0)
                for j in range(4):
                    p.op("dve", lambda e, j=j: e.scalar_tensor_tensor(out=oo[:, j, :], in0=diff[:, j, :], scalar=rstd[:, j:j + 1], in1=gA[:, :], op0=ALU.mult, op1=ALU.mult),
                         reads=[diff, rstd, gA], writes=[oo])
                ps = self.bank(0, 4)
                for j in range(4):
                    p.op("pe", lambda e, ps=ps, j=j: e.transpose(ps[:, j * 128:(j + 1) * 128], oo[:, j, :], self.identf[:, :]), reads=[oo, self.identf], writes=[ps])
                p.op("act", lambda e, ps=ps, Q=Q, oa=oa: e.copy(out=oa[:, Q * 512:(Q + 1) * 512], in_=ps[:, :]), reads=[ps], writes=[oa])
            p.dma("pool", self.oT[0, h, :, :], oa[:, :], reads=[oa], writes=[self.oT.bs[h]])
        p.barrier()
        st.close()

    def phase_merge1(self, l):
        for dh in range(2):
            self._merge1_dh(l, dh)

    def _merge1_dh(self, l, dh):
        p = self.p
        W = self.W
        xT = self.xT
        if True:
            st = ExitStack()
            stg = [p.sb(f"m_stg{i}", [128, 2048], F32, st) for i in range(2)]
            wg = p.sb("m_wg", [128, 8, 1536], BF16, st, nb=3)
            wup = p.sb("m_wup", [128, 12, 512], BF16, st, nb=3)
            mb = p.sb("m_mb", [128, 3, 512], F32, st)
            ot = [p.sb(f"m_ot{i}", [128, 12, 512], BF16, st) for i in range(2)]
            sg = [p.sb(f"m_sg{i}", [128, 512], F32, st) for i in range(2)]
            acc = [p.sb(f"m_acc{i}", [128, 512], F32, st) for i in range(2)]
            tmp = [p.sb(f"m_tmp{i}", [128, 512], F32, st) for i in range(2)]
            mtb = [p.sb(f"m_mtb{i}", [128, 4, 512], BF16, st) for i in range(2)]
            d0 = dh * 512
            for n in range(3):
                c0 = COL["gate"] + n * 1024 + d0
                self.wload(stg, wg, wg[:, :, n * 512:(n + 1) * 512], W["w_in"][l, :, c0:c0 + 512], 8, 512, wbuf=wg.bs[n])
                self.wload(stg, wup, wup[:, n * 4:(n + 1) * 4, :], W["merge_w_up"][l, n, :, d0:d0 + 512], 4, 512, wbuf=wup.bs[n])
            p.dma("sp", mb[:], W["merge_b"][l:l + 1, :, d0:d0 + 512].partition_broadcast(128), writes=[mb])
            def stage_A(k):
                tb, tt = k // 4, k % 4
                t = k
                o = ot[tb % 2]
                if tt == 0:
                    p.dma("sp", o[:], self.oT[:, :, :, tb * 512:(tb + 1) * 512].rearrange("n c p t -> p (n c) t"), reads=self.oT.bs, writes=[o])
                a = acc[k % 2]
                for n in range(3):
                    g = sg[(k * 3 + n) % 2]
                    psg = self.bank(0, 3)
                    for c in range(8):
                        self.mm(psg[:, :], xT[:, c, t * 128:(t + 1) * 128], wg[:, c, n * 512:(n + 1) * 512], c == 0, c == 7, [xT.bs[t], wg.bs[n]], [psg])
                    p.op("dve", lambda e, g=g, psg=psg, n=n, mb=mb: e.tensor_tensor(out=g[:, :], in0=psg[:, :], in1=mb[:, n, :], op=ALU.add), reads=[psg, mb], writes=[g])
                    p.op("act", lambda e, g=g: e.activation(out=g[:, :], in_=g[:, :], func=AF.Sigmoid), reads=[g], writes=[g])
                    psu = self.bank(3, 6)
                    for c in range(4):
                        self.mm(psu[:, :], o[:, n * 4 + c, tt * 128:(tt + 1) * 128], wup[:, n * 4 + c, :], c == 0, c == 3, [o, wup.bs[n]], [psu])
                    if n == 0:
                        p.op("dve", lambda e, a=a, g=g, psu=psu: e.tensor_tensor(out=a[:, :], in0=g[:, :], in1=psu[:, :], op=ALU.mult), reads=[g, psu], writes=[a])
                    else:
                        tm = tmp[n % 2]
                        p.op("dve", lambda e, tm=tm, g=g, psu=psu: e.tensor_tensor(out=tm[:, :], in0=g[:, :], in1=psu[:, :], op=ALU.mult), reads=[g, psu], writes=[tm])
                        p.op("dve", lambda e, tm=tm, a=a: e.tensor_tensor(out=a[:, :], in0=a[:, :], in1=tm[:, :], op=ALU.add), reads=[a, tm], writes=[a])

            def stage_B(k):
                tb, tt = k // 4, k % 4
                a = acc[k % 2]
                mt = mtb[tb % 2]
                pst = self.bank(6, 8)
                for j in range(4):
                    p.op("pe", lambda e, pst=pst, j=j, a=a: e.transpose(pst[:, j * 128:(j + 1) * 128], a[:, j * 128:(j + 1) * 128], self.identf[:, :]), reads=[a, self.identf], writes=[pst])
                p.op("act", lambda e, pst=pst, mt=mt, tt=tt: e.copy(out=mt[:, :, tt * 128:(tt + 1) * 128], in_=pst[:, :].rearrange("p (j n) -> p j n", j=4)), reads=[pst], writes=[mt])
                if tt == 3:
                    p.dma("pool", self.mT[:, dh * 4:(dh + 1) * 4, tb * 512:(tb + 1) * 512], mt[:, :, :], reads=[mt], writes=[self.mT.bs[dh * 8 + tb]])

            stage_A(0)
            for k in range(NT):
                if k + 1 < NT:
                    stage_A(k + 1)
                stage_B(k)
            p.barrier()
            st.close()

    def phase_merge2(self, l):
        p = self.p
        W = self.W
        st = ExitStack()
        stg = [p.sb(f"n_stg{i}", [128, 2048], F32, st) for i in range(2)]
        wo = p.sb("n_wo", [128, 8, 1024], BF16, st)
        rw = p.sb("n_rw", [128, 8, 16], F32, st)
        rb = p.sb("n_rb", [128, 16], F32, st)
        mtl = [p.sb(f"n_mt{i}", [128, 8, 512], BF16, st) for i in range(2)]
        xr = [p.sb(f"n_xr{i}", [128, D], F32, st) for i in range(2)]
        z = [p.sb(f"n_z{i}", [128, D], F32, st) for i in range(2)]
        xT32 = p.sb("n_xT32", [128, 8, 128], F32, st)
        stats = p.sb("n_stats", [128, 2, 6], F32, st)
        mv = p.sb("n_mv", [128, 2], F32, st)
        rstd = p.sb("n_rstd", [128, 1], F32, st)
        R = {k: p.sb("n_r_" + k, shp, F32, st) for k, shp in dict(sc=[128, 16], bi=[128, 16], m1=[128, 4], eq=[128, 16], t2=[128, 16], m2=[128, 4],
                                                                  gs=[128, 4], gm=[128, 1], gsel=[128, 4], ge=[128, 16], w=[128, 16], ws=[128, 1]).items()}
        for h2 in range(2):
            self.wload(stg, wo, wo[:, :, h2 * 512:(h2 + 1) * 512], W["w_out"][l, :, h2 * 512:(h2 + 1) * 512], 8, 512)
        p.dma("sp", rw[:], W["router_w"].t.rearrange("(c p) n -> p c n", p=128), writes=[rw])
        p.dma("sp", rb[:], W["router_bias"].t.rearrange("(o n) -> o n", o=1).partition_broadcast(128), writes=[rb])
        self.load_ln_params(W["ln1_g"][l:l + 1, :], W["ln1_b"][l:l + 1, :], st)
        def stage_A(t):
            tb, tt = t // 4, t % 4
            mt = mtl[tb % 2]
            if tt == 0:
                p.dma("sp", mt[:], self.mT[:, :, tb * 512:(tb + 1) * 512], reads=[self.mT.bs[tb], self.mT.bs[8 + tb]], writes=[mt])
            x_ = xr[t % 2]
            z_ = z[t % 2]
            p.dma("sp", x_[:], self.xres[t * 128:(t + 1) * 128, :], reads=[self.xres.bs[t]], writes=[x_])
            for h2 in range(2):
                ps = self.bank(0, 4)
                for c in range(8):
                    self.mm(ps[:, :], mt[:, c, tt * 128:(tt + 1) * 128], wo[:, c, h2 * 512:(h2 + 1) * 512], c == 0, c == 7, [mt, wo], [ps])
                p.op("dve", lambda e, ps=ps, x_=x_, z_=z_, h2=h2: e.scalar_tensor_tensor(out=z_[:, h2 * 512:(h2 + 1) * 512], in0=x_[:, h2 * 512:(h2 + 1) * 512], scalar=ALPHA,
                                                                                in1=ps[:, :], op0=ALU.mult, op1=ALU.add), reads=[ps, x_], writes=[z_])
            self.ln_tile(z_, z_, stats, mv, rstd)
            p.dma("pool", self.xres[t * 128:(t + 1) * 128, :], z_[:], reads=[z_], writes=[self.xres.bs[t]])

        def stage_B(t):
            self.to_xT(z[t % 2], t, xT32=xT32)
            self.router(t, xT32, rw, rb, R)

        stage_A(0)
        for t in range(NT):
            if t + 1 < NT:
                stage_A(t + 1)
            stage_B(t)
        p.barrier()
        st.close()

    def router(self, t, xT32, rw, rb, R):
        p = self.p
        ps = self.bank(4, 8)
        for c in range(8):
            self.mm(ps[:, 0:16], xT32[:, c, :], rw[:, c, :], c == 0, c == 7, [xT32, rw], [ps])
        sc, bi, m1, eq, t2, m2, gs, gm, gsel, ge, w, ws = (R[k] for k in ("sc", "bi", "m1", "eq", "t2", "m2", "gs", "gm", "gsel", "ge", "w", "ws"))
        v3 = lambda tl: tl[:, :].rearrange("p (g e) -> p g e", g=4)
        b3 = lambda tl: tl[:, :].unsqueeze(2).to_broadcast([128, 4, 4])
        p.op("act", lambda e: e.activation(out=sc[:, :], in_=ps[:, 0:16], func=AF.Sigmoid), reads=[ps], writes=[sc])
        p.op("dve", lambda e: e.tensor_tensor(out=bi[:, :], in0=sc[:, :], in1=rb[:, :], op=ALU.add), reads=[sc, rb], writes=[bi])
        p.op("dve", lambda e: e.tensor_reduce(out=m1[:, :], in_=v3(bi), axis=AX.X, op=ALU.max), reads=[bi], writes=[m1])
        p.op("dve", lambda e: e.tensor_tensor(out=v3(eq), in0=v3(bi), in1=b3(m1), op=ALU.is_equal), reads=[bi, m1], writes=[eq])
        p.op("dve", lambda e: e.scalar_tensor_tensor(out=t2[:, :], in0=eq[:, :], scalar=-1e30, in1=bi[:, :], op0=ALU.mult, op1=ALU.add), reads=[eq, bi], writes=[t2])
        p.op("dve", lambda e: e.tensor_reduce(out=m2[:, :], in_=v3(t2), axis=AX.X, op=ALU.max), reads=[t2], writes=[m2])
        p.op("dve", lambda e: e.tensor_tensor(out=gs[:, :], in0=m1[:, :], in1=m2[:, :], op=ALU.add), reads=[m1, m2], writes=[gs])
        p.op("dve", lambda e: e.tensor_reduce(out=gm[:, :], in_=gs[:, :], axis=AX.X, op=ALU.max), reads=[gs], writes=[gm])
        p.op("dve", lambda e: e.tensor_scalar(gsel[:, :], gs[:, :], gm[:, 0:1], None, ALU.is_equal), reads=[gs, gm], writes=[gsel])
        p.op("dve", lambda e: e.tensor_tensor(out=v3(ge), in0=v3(bi), in1=b3(m2), op=ALU.is_ge), reads=[bi, m2], writes=[ge])
        p.op("dve", lambda e: e.tensor_tensor(out=v3(ge), in0=v3(ge), in1=b3(gsel), op=ALU.mult), reads=[ge, gsel], writes=[ge])
        p.op("dve", lambda e: e.tensor_tensor(out=w[:, :], in0=ge[:, :], in1=sc[:, :], op=ALU.mult), reads=[ge, sc], writes=[w])
        p.op("dve", lambda e: e.tensor_reduce(out=ws[:, :], in_=w[:, :], axis=AX.X, op=ALU.add), reads=[w], writes=[ws])
        p.op("dve", lambda e: e.reciprocal(out=ws[:, :], in_=ws[:, :]), reads=[ws], writes=[ws])
        p.op("dve", lambda e: e.tensor_scalar(self.call[:, t, :], w[:, :], ws[:, 0:1], None, ALU.mult), reads=[w, ws], writes=[self.call.bs[t]])

    def phase_moe(self, l, last):
        p = self.p
        W = self.W
        xT = self.xT
        st = ExitStack()
        stg = [p.sb(f"e_stg{i}", [128, 2048], F32, st) for i in range(2)]
        wg = [p.sb(f"e_wg{i}", [128, 8, 512], BF16, st) for i in range(2)]
        wu = [p.sb(f"e_wu{i}", [128, 8, 512], BF16, st) for i in range(2)]
        wd = [p.sb(f"e_wd{i}", [128, 4, 1024], BF16, st) for i in range(2)]
        yacc = p.sb("e_yacc", [128, 8, D], F32, st, nb=8)
        hT = [p.sb(f"e_hT{i}", [128, 4, 512], BF16, st) for i in range(2)]
        sgl = [p.sb(f"e_sg{i}", [128, 512], F32, st) for i in range(2)]
        xr = [p.sb(f"e_xr{i}", [128, D], F32, st) for i in range(1)]
        stats = p.sb("e_stats", [128, 2, 6], F32, st)
        mv = p.sb("e_mv", [128, 2], F32, st)
        rstd = p.sb("e_rstd", [128, 1], F32, st)
        self.load_ln_params(W["ln2_g"][l:l + 1, :], W["ln2_b"][l:l + 1, :], st)
        kk = [0]

        def wl(ex):
            g_, u_, d_ = wg[ex % 2], wu[ex % 2], wd[ex % 2]
            for h2 in range(2):
                self.wload(stg, g_, g_[:, h2 * 4:(h2 + 1) * 4, :], W["moe_w_gate"][l, ex, h2 * 512:(h2 + 1) * 512, :], 4, 512, cast=("pool", "dve")[h2])
                self.wload(stg, u_, u_[:, h2 * 4:(h2 + 1) * 4, :], W["moe_w_up"][l, ex, h2 * 512:(h2 + 1) * 512, :], 4, 512, cast=("pool", "dve")[h2])
                self.wload(stg, d_, d_[:, :, h2 * 512:(h2 + 1) * 512], W["moe_w_down"][l, ex, :, h2 * 512:(h2 + 1) * 512], 4, 512, cast=("pool", "dve")[h2])

        def gu(q4, ex, tb2):
            g_, u_ = wg[ex % 2], wu[ex % 2]
            tb = q4 * 2 + tb2
            h_ = hT[kk[0] % 2]
            kk[0] += 1
            xr_ = [xT.bs[tb * 4 + i] for i in range(4)]
            for fc in range(4):
                pg = self.bank(0, 2)
                for c in range(8):
                    self.mm(pg[:, :], g_[:, c, fc * 128:(fc + 1) * 128], xT[:, c, tb * 512:(tb + 1) * 512], c == 0, c == 7, [g_] + xr_, [pg])
                pu = self.bank(2, 4)
                for c in range(8):
                    self.mm(pu[:, :], u_[:, c, fc * 128:(fc + 1) * 128], xT[:, c, tb * 512:(tb + 1) * 512], c == 0, c == 7, [u_] + xr_, [pu])
                s_ = sgl[fc % 2]
                p.op("act", lambda e, s_=s_, pg=pg: e.activation(out=s_[:, :], in_=pg[:, :], func=AF.Silu), reads=[pg], writes=[s_])
                p.op("dve", lambda e, s_=s_, pu=pu, h_=h_, fc=fc: e.tensor_tensor(out=h_[:, fc, :], in0=s_[:, :], in1=pu[:, :], op=ALU.mult), reads=[s_, pu], writes=[h_])
            return h_

        def down(q4, ex, tb2, h_):
            d_ = wd[ex % 2]
            tb = q4 * 2 + tb2
            for tt in range(4):
                t = tb * 4 + tt
                tl = tb2 * 4 + tt
                for h2 in range(2):
                    py = self.bank(4, 8)
                    for fc in range(4):
                        self.mm(py[:, :], h_[:, fc, tt * 128:(tt + 1) * 128], d_[:, fc, h2 * 512:(h2 + 1) * 512], fc == 0, fc == 3, [h_, d_], [py])
                    ya = yacc[:, tl, h2 * 512:(h2 + 1) * 512]
                    if ex == 0:
                        p.op("dve", lambda e, ya=ya, py=py, t=t, ex=ex: e.tensor_scalar(ya, py[:, :], self.call[:, t, ex:ex + 1], None, ALU.mult),
                             reads=[py, self.call.bs[t]], writes=[yacc.bs[tl]])
                    else:
                        p.op("dve", lambda e, ya=ya, py=py, t=t, ex=ex: e.scalar_tensor_tensor(out=ya, in0=py[:, :], scalar=self.call[:, t, ex:ex + 1], in1=ya, op0=ALU.mult, op1=ALU.add),
                             reads=[py, self.call.bs[t]], writes=[yacc.bs[tl]])

        def ln2(q4):
            for tl in range(8):
                t = q4 * 8 + tl
                x_ = xr[0]
                yv = Sub(yacc[:, tl, :], yacc.bs[tl])
                p.dma("sp", x_[:], self.xres[t * 128:(t + 1) * 128, :], reads=[self.xres.bs[t]], writes=[x_])
                p.op("dve", lambda e, x_=x_, tl=tl: e.scalar_tensor_tensor(out=yacc[:, tl, :], in0=x_[:, :], scalar=ALPHA, in1=yacc[:, tl, :], op0=ALU.mult, op1=ALU.add),
                     reads=[x_, yacc.bs[tl]], writes=[yacc.bs[tl]])
                self.ln_tile(yv, yv, stats, mv, rstd)
                if last:
                    p.dma("pool", self.out[t * 128:(t + 1) * 128, :], yv[:, :], reads=[yv], writes=[self.out.bs[t]])
                else:
                    p.dma("pool", self.xres[t * 128:(t + 1) * 128, :], yv[:, :], reads=[yv], writes=[self.xres.bs[t]])
                    self.to_xT(yv, t)

        pre = None
        for q4 in range(4):
            for ex in range(16):
                if ex == 0 and pre is not None:
                    for tb2 in range(2):
                        down(q4, 0, tb2, pre[tb2])
                    pre = None
                else:
                    wl(ex)
                    for tb2 in range(2):
                        h_ = gu(q4, ex, tb2)
                        down(q4, ex, tb2, h_)
            if q4 < 3:
                wl(0)
                pre = [gu(q4 + 1, 0, 0), gu(q4 + 1, 0, 1)]
            ln2(q4)
        p.barrier()
        st.close()

    def phase_gla(self, l):
        for hp in range(2):
            self._gla_hp(l, hp)

    def _gla_hp(self, l, hp):
        p = self.p
        W = self.W
        xT = self.xT
        xTr = list(xT.bs)
        if True:
            st = ExitStack()
            stg = [p.sb(f"g_stg{i}", [128, 1024], F32, st) for i in range(2)]
            wq = p.sb("g_wq", [128, 8, 128], BF16, st)
            wk = p.sb("g_wk", [128, 8, 128], BF16, st)
            wv = p.sb("g_wv", [128, 8, 256], BF16, st)
            wr = p.sb("g_wr", [128, 8, 256], BF16, st)
            wz = p.sb("g_wz", [128, 8, 32], BF16, st)
            wgf = p.sb("g_wgf", [16, 2, 128], F32, st)
            wgt = p.sb("g_wgt", [16, 2, 128], BF16, st)
            bg = p.sb("g_bg", [128, 2], F32, st)
            gG = p.sb("g_gG", [128, 128], F32, st)
            ones = p.sb("g_ones", [128, 1], F32, st)
            m4 = p.sb("g_m4", [128, 4, 64], F32, st)
            msk = p.sb("g_msk", [128, 8, 64], F32, st)
            qA1 = p.sb("g_qA1", [128, S], BF16, st)
            kA1 = p.sb("g_kA1", [128, S], BF16, st)
            qi1 = p.sb("g_qi1", [128, S], BF16, st)
            qA0 = [p.sb(f"g_qA0{i}", [128, 512], BF16, st) for i in range(2)]
            kA0 = [p.sb(f"g_kA0{i}", [128, 512], BF16, st) for i in range(2)]
            qi0 = [p.sb(f"g_qi0{i}", [128, 512], BF16, st) for i in range(2)]
            dec = [p.sb(f"g_dec{d}", [128, 64], F32, st) for d in range(2)]
            sts1 = p.sb("g_st1", [128, 64, 128], BF16, st)
            sts0 = [p.sb(f"g_st0{i}", [128, 8, 128], BF16, st) for i in range(2)]
            S32 = [p.sb(f"g_S32{d}", [128, 128], F32, st) for d in range(2)]
            v = p.sb("g_v", [128, NT, 256], BF16, st)
            zt = p.sb("g_zt", [16, 512], BF16, st)
            T1 = p.sb("g_T1", [128, 512], F32, st)
            T2 = p.sb("g_T2", [128, 512], F32, st)
            T3 = p.sb("g_T3", [128, 512], F32, st)
            T4 = p.sb("g_T4", [128, 512], F32, st)
            T5 = p.sb("g_T5", [128, 512], F32, st)
            kltr = [p.sb(f"g_klt{i}", [128, 4, 128], BF16, st) for i in range(2)]
            scT = [p.sb(f"g_scT{i}", [128, 4, 64], BF16, st) for i in range(2)]
            sr = p.sb("g_sr", [128, 256], F32, st)
            sq = p.sb("g_sq", [128, 2, 128], F32, st)
            ssq = p.sb("g_ssq", [128, 2], F32, st)
            oc = p.sb("g_oc", [128, 256], F32, st)
            ocT = [p.sb(f"g_ocT{i}", [128, 2, 512], BF16, st) for i in range(2)]
            c0 = COL["qc"] + hp * 128
            self.wload(stg, wq, wq[:, :, :], W["w_in"][l, :, c0:c0 + 128], 8, 128)
            p.op("pool", lambda e: e.tensor_scalar(wq[:, :, :], wq[:, :, :], 0.125, None, ALU.mult), reads=[wq], writes=[wq])
            c0 = COL["kc"] + hp * 128
            self.wload(stg, wk, wk[:, :, :], W["w_in"][l, :, c0:c0 + 128], 8, 128)
            for k2 in range(2):
                c0 = COL["vc"] + hp * 256 + k2 * 128
                self.wload(stg, wv, wv[:, :, k2 * 128:(k2 + 1) * 128], W["w_in"][l, :, c0:c0 + 128], 8, 128)
                c0 = COL["rc"] + hp * 256 + k2 * 128
                self.wload(stg, wr, wr[:, :, k2 * 128:(k2 + 1) * 128], W["w_in"][l, :, c0:c0 + 128], 8, 128)
            self.wload(stg, wz, wz[:, :, :], W["w_in"][l, :, COL["zf"]:COL["zf"] + 32], 8, 32)
            p.dma("sp", wgf[:], W["gla_w_gate"][l, :, :, hp * 128:(hp + 1) * 128].rearrange("d r n -> r d n"), writes=[wgf])
            p.op("dve", lambda e: e.tensor_copy(out=wgt[:, :, :], in_=wgf[:, :, :]), reads=[wgf], writes=[wgt])
            p.dma("sp", bg[:], W["gla_b_gate"][l, :, hp * 128:(hp + 1) * 128].rearrange("d n -> n d"), writes=[bg], allow_slow_non_contiguous=True)
            p.dma("sp", gG[:], W["gla_norm_g"][l:l + 1, :].partition_broadcast(128), writes=[gG])
            p.op("pool", lambda e: e.memset(ones[:, :], 1.0), writes=[ones])
            for d in range(2):
                for hh in range(2):
                    for half in range(2):
                        p.dma("sp", m4[half * 64:(half + 1) * 64, d * 2 + hh, :], self.C["gmask"][d, :, :], writes=[m4])
            p.op("pool", lambda e: e.memset(msk[:, :, :], 1.0), writes=[msk])
            p.op("pool", lambda e: e.memset(msk[:, :, 0:1], 0.0), reads=[msk], writes=[msk])
            for t in range(NT):
                ps = self.bank(0, 4)
                for c in range(8):
                    self.mm(ps[:, 0:256], xT[:, c, t * 128:(t + 1) * 128], wv[:, c, :], c == 0, c == 7, [wv, xTr[t]], [ps])
                p.op("act", lambda e, ps=ps, t=t: e.copy(out=v[:, t, :], in_=ps[:, 0:256]), reads=[ps], writes=[v])
            def arr(d, tb):
                if d == 1:
                    sl = slice(tb * 512, (tb + 1) * 512)
                    return (qA1, qA1[:, sl]), (kA1, kA1[:, sl]), (qi1, qi1[:, sl])
                i = tb % 2
                return (qA0[i], qA0[i][:, :]), (kA0[i], kA0[i][:, :]), (qi0[i], qi0[i][:, :])

            def chunk_view(d, which, n, hh):
                hs = slice(hh * 64, (hh + 1) * 64)
                if d == 1:
                    tl = (qA1, kA1, qi1)[which]
                    return tl, tl[hs, n * 64:(n + 1) * 64]
                tl = (qA0, kA0, qi0)[which][(n // 8) % 2]
                return tl, tl[hs, (n % 8) * 64:(n % 8 + 1) * 64]

            def state_view(d, n, hh):
                hs = slice(hh * 64, (hh + 1) * 64)
                if d == 1:
                    return sts1, sts1[hs, n, :]
                tl = sts0[(n // 8) % 2]
                return tl, tl[hs, n % 8, :]

            def sweep_A(d, tb):
                klt = kltr[tb % 2]
                xr_ = xTr[tb * 4:tb * 4 + 4]
                tsl = slice(tb * 512, (tb + 1) * 512)
                (qA_t, qA_ap), (kA_t, kA_ap), (qi_t, qi_ap) = arr(d, tb)
                pz = self.bank(0, 4)
                for c in range(8):
                    self.mm(pz[0:16, :], wz[:, c, d * 16:(d + 1) * 16], xT[:, c, tsl], c == 0, c == 7, [wz] + xr_, [pz])
                p.op("act", lambda e: e.copy(out=zt[:, :], in_=pz[0:16, :]), reads=[pz], writes=[zt])
                pg = self.bank(0, 4)
                self.mm(pg[:, :], wgt[0:16, d, :], zt[0:16, :], True, True, [wgt, zt], [pg])
                pq = self.bank(4, 6)
                for c in range(8):
                    self.mm(pq[:, :], wq[:, c, :], xT[:, c, tsl], c == 0, c == 7, [wq] + xr_, [pq])
                pk = self.bank(6, 8)
                for c in range(8):
                    self.mm(pk[:, :], wk[:, c, :], xT[:, c, tsl], c == 0, c == 7, [wk] + xr_, [pk])
                p.op("dve", lambda e: e.tensor_scalar(T1[:, :], pg[:, :], bg[:, d:d + 1], None, ALU.add), reads=[pg, bg], writes=[T1])
                p.op("dve", lambda e: e.scalar_tensor_tensor(out=T2[:, :], in0=T1[:, :], scalar=-1.0, in1=T1[:, :], op0=ALU.mult, op1=ALU.max), reads=[T1], writes=[T2])
                p.op("act", lambda e: e.activation(out=T2[:, :], in_=T2[:, :], func=AF.Exp, scale=-1.0), reads=[T2], writes=[T2])
                p.op("act", lambda e: e.activation(out=T2[:, :], in_=T2[:, :], func=AF.Ln, bias=ones[:, 0:1]), reads=[T2, ones], writes=[T2])
                p.op("dve", lambda e: e.scalar_tensor_tensor(out=T1[:, :], in0=T1[:, :], scalar=0.0, in1=T2[:, :], op0=ALU.min, op1=ALU.subtract), reads=[T1, T2], writes=[T1])
                p.op("act", lambda e: e.mul(out=T1[:, :], in_=T1[:, :], mul=1.0 / 16.0), reads=[T1], writes=[T1])
                p.op("dve", lambda e: e.tensor_tensor_scan(T3[:, :], msk[:, :, :].rearrange("p a b -> p (a b)"), T1[:, :], 0.0, ALU.mult, ALU.add), reads=[msk, T1], writes=[T3])
                c3 = T3[:, :].rearrange("p (a b) -> p a b", a=8)
                v4 = lambda tl: tl[:, :].rearrange("p (a b) -> p a b", a=8)
                if d == 1:
                    p.op("dve", lambda e: e.tensor_tensor(out=v4(T2), in0=c3[:, :, 63:64].to_broadcast([128, 8, 64]), in1=c3, op=ALU.subtract), reads=[T3], writes=[T2])
                    p.op("dve", lambda e: e.tensor_tensor(out=T3[:, :], in0=T2[:, :], in1=T1[:, :], op=ALU.add), reads=[T2, T1], writes=[T3])
                    ref, last = c3[:, :, 31:32], c3[:, :, 0:1]
                else:
                    ref, last = c3[:, :, 32:33], c3[:, :, 63:64]
                refb = ref.to_broadcast([128, 8, 64])
                lastb = last.to_broadcast([128, 8, 64])
                p.op("act", lambda e: e.activation(out=dec[d][:, tb * 8:(tb + 1) * 8].unsqueeze(2), in_=last, func=AF.Exp), reads=[T3], writes=[dec[d]])
                p.op("dve", lambda e: e.tensor_tensor(out=v4(T4), in0=c3, in1=refb, op=ALU.subtract), reads=[T3], writes=[T4])
                p.op("act", lambda e: e.activation(out=T5[:, :], in_=T4[:, :], func=AF.Exp), reads=[T4], writes=[T5])
                p.op("dve", lambda e: e.tensor_tensor(out=qA_ap, in0=pq[:, :], in1=T5[:, :], op=ALU.mult), reads=[pq, T5], writes=[qA_t])
                p.op("act", lambda e: e.activation(out=T5[:, :], in_=T4[:, :], func=AF.Exp, scale=-1.0), reads=[T4], writes=[T5])
                p.op("dve", lambda e: e.tensor_tensor(out=kA_ap, in0=pk[:, :], in1=T5[:, :], op=ALU.mult), reads=[pk, T5], writes=[kA_t])
                p.op("act", lambda e: e.activation(out=T5[:, :], in_=T3[:, :], func=AF.Exp), reads=[T3], writes=[T5])
                p.op("dve", lambda e: e.tensor_tensor(out=qi_ap, in0=pq[:, :], in1=T5[:, :], op=ALU.mult), reads=[pq, T5], writes=[qi_t])
                p.op("dve", lambda e: e.tensor_tensor(out=v4(T4), in0=lastb, in1=c3, op=ALU.subtract), reads=[T3], writes=[T4])
                p.op("act", lambda e: e.activation(out=T5[:, :], in_=T4[:, :], func=AF.Exp), reads=[T4], writes=[T5])
                p.op("dve", lambda e: e.tensor_tensor(out=T4[:, :], in0=pk[:, :], in1=T5[:, :], op=ALU.mult), reads=[pk, T5], writes=[T4])
                pt = self.bank(0, 4)
                for j in range(4):
                    p.op("pe", lambda e, j=j: e.transpose(pt[:, j * 128:(j + 1) * 128], T4[:, j * 128:(j + 1) * 128], self.identf[:, :]), reads=[T4, self.identf], writes=[pt])
                p.op("act", lambda e: e.copy(out=klt[:, :, :], in_=pt[:, :].rearrange("p (j n) -> p j n", j=4)), reads=[pt], writes=[klt])

            def sweep_B(d, tb):
                klt = kltr[tb % 2]
                cs = range(8) if d == 0 else range(7, -1, -1)
                for ci in cs:
                    n = tb * 8 + ci
                    tt, half = ci // 2, ci % 2
                    t = tb * 4 + tt
                    stl, _ = state_view(d, n, 0)
                    sap = sts1[:, n, :] if d == 1 else stl[:, n % 8, :]
                    p.op("act", lambda e, sap=sap: e.copy(out=sap, in_=S32[d][:, :]), reads=[S32[d]], writes=[stl])
                    pd = self.bank(0, 4)
                    for hh in range(2):
                        self.mm(pd[hh * 64:(hh + 1) * 64, 0:128], klt[half * 64:(half + 1) * 64, tt, hh * 64:(hh + 1) * 64],
                                v[half * 64:(half + 1) * 64, t, hh * 128:(hh + 1) * 128], True, True, [klt, v], [pd], serial=True)
                    p.op("dve", lambda e, pd=pd, n=n: e.scalar_tensor_tensor(out=S32[d][:, :], in0=S32[d][:, :], scalar=dec[d][:, n:n + 1], in1=pd[:, 0:128], op0=ALU.mult, op1=ALU.add),
                         reads=[pd, dec[d], S32[d]], writes=[S32[d]])

            def out_block(tb):
                ot_ = ocT[tb % 2]
                for tt in range(4):
                    t = tb * 4 + tt
                    pss = self.bank(0, 2)
                    for half in range(2):
                        n = t * 2 + half
                        first = True
                        for d in range(2):
                            for hh in range(2):
                                kt_, kap = chunk_view(d, 1, n, hh)
                                qt_, qap = chunk_view(d, 0, n, hh)
                                self.mm(pss[half * 64:(half + 1) * 64, (d * 2 + hh) * 64:(d * 2 + hh + 1) * 64], kap, qap, first, True, [kt_, qt_], [pss], serial=True)
                                first = False
                    sc_ = scT[t % 2]
                    p.op("dve", lambda e, pss=pss, sc_=sc_: e.tensor_tensor(out=sc_[:, :, :], in0=pss[:, 0:256].rearrange("p (a b) -> p a b", a=4), in1=m4[:, :, :], op=ALU.mult), reads=[pss, m4], writes=[sc_])
                    po = self.bank(2, 4)
                    for half in range(2):
                        n = t * 2 + half
                        hs = slice(half * 64, (half + 1) * 64)
                        first = True
                        for hh in range(2):
                            osl = po[hs, hh * 128:(hh + 1) * 128]
                            for d in range(2):
                                self.mm(osl, sc_[hs, d * 2 + hh, :], v[hs, t, hh * 128:(hh + 1) * 128], first, False, [sc_, v], [po], serial=True)
                                first = False
                            for d in range(2):
                                it_, iap = chunk_view(d, 2, n, hh)
                                st_, sap = state_view(d, n, hh)
                                self.mm(osl, iap, sap, False, d == 1, [it_, st_], [po], serial=True)
                    pr = self.bank(4, 8)
                    for c in range(8):
                        self.mm(pr[:, 0:256], xT[:, c, t * 128:(t + 1) * 128], wr[:, c, :], c == 0, c == 7, [wr, xTr[t]], [pr])
                    p.op("act", lambda e, pr=pr: e.activation(out=sr[:, :], in_=pr[:, 0:256], func=AF.Silu), reads=[pr], writes=[sr])
                    po3 = po[:, 0:256].rearrange("p (a b) -> p a b", a=2)
                    p.op("act", lambda e, po3=po3: e.activation(out=sq[:, :, :], in_=po3, func=AF.Square), reads=[po], writes=[sq])
                    p.op("dve", lambda e: e.tensor_reduce(out=ssq[:, :], in_=sq[:, :, :], axis=AX.X, op=ALU.add), reads=[sq], writes=[ssq])
                    self.rsqrt(ssq, ssq[:, :], ssq, ssq[:, :], 1.0 / 128.0)
                    for hh in range(2):
                        p.op("dve", lambda e, po=po, hh=hh: e.scalar_tensor_tensor(out=oc[:, hh * 128:(hh + 1) * 128], in0=po[:, hh * 128:(hh + 1) * 128], scalar=ssq[:, hh:hh + 1], in1=gG[:, :],
                                                                                  op0=ALU.mult, op1=ALU.mult), reads=[po, ssq, gG], writes=[oc])
                    p.op("dve", lambda e: e.tensor_tensor(out=oc[:, :], in0=oc[:, :], in1=sr[:, :], op=ALU.mult), reads=[oc, sr], writes=[oc])
                    pt = self.bank(4, 8)
                    for hh in range(2):
                        p.op("pe", lambda e, pt=pt, hh=hh: e.transpose(pt[:, hh * 128:(hh + 1) * 128], oc[:, hh * 128:(hh + 1) * 128], self.identf[:, :]), reads=[oc, self.identf], writes=[pt])
                    p.op("act", lambda e, pt=pt, tt=tt: e.copy(out=ot_[:, :, tt * 128:(tt + 1) * 128], in_=pt[:, 0:256].rearrange("p (a b) -> p a b", a=2)), reads=[pt], writes=[ot_])
                p.dma("pool", self.oT[2, hp * 2:(hp + 1) * 2, :, tb * 512:(tb + 1) * 512].rearrange("c p t -> p c t"), ot_[:, :, :], reads=[ot_], writes=[self.oT.bs[8 + hp * 2], self.oT.bs[8 + hp * 2 + 1]])

            for d in (1, 0):
                p.op("pool", lambda e, d=d: e.memset(S32[d][:, :], 0.0), writes=[S32[d]])
            order1 = list(range(7, -1, -1))
            sweep_A(1, order1[0])
            for i, tb in enumerate(order1):
                if i + 1 < 8:
                    sweep_A(1, order1[i + 1])
                sweep_B(1, tb)
            sweep_A(0, 0)
            for tb in range(8):
                if tb + 1 < 8:
                    sweep_A(0, tb + 1)
                sweep_B(0, tb)
                out_block(tb)
            p.barrier()
            st.close()

    def phase_s5(self, l):
        p = self.p
        W = self.W
        xT = self.xT
        xTr = list(xT.bs)
        st = ExitStack()
        sm = lambda n, shp=(128, 32): p.sb("s_" + n, list(shp), F32, st)
        stg = [p.sb(f"s_stg{i}", [128, 1024], F32, st) for i in range(2)]
        identb = p.sb("s_identb", [128, 128], BF16, st)
        jx = sm("jx", (128, 128))
        rowm = sm("rowm", (128, 8))
        sgn = sm("sgn", (128, 2))
        hpi = sm("hpi", (128, 1))
        wu = p.sb("s_wu", [128, 8, 128], BF16, st)
        wglu = p.sb("s_wglu", [128, 4, 512], BF16, st)
        bglu = sm("bglu", (128, 4))
        dcol = sm("dcol", (128, 4))
        CST = [[p.sb(f"s_cst{d}{b}", [128, 128], F32, st) for b in range(4)] for d in range(2)]
        BT = [[p.sb(f"s_bt{d}{b}", [128, 128], F32, st) for b in range(4)] for d in range(2)]
        PRt = [sm(f"pr{d}", (128, 12, 32)) for d in range(2)]
        PQt = [sm(f"pq{d}", (128, 12, 32)) for d in range(2)]
        pst = ExitStack()
        smp = lambda n, shp=(128, 32): p.sb("s_" + n, list(shp), F32, pst)
        p.dma("sp", jx[:], self.C["jx"][:, :], writes=[jx])
        p.dma("sp", rowm[:], self.C["rowmask"][:, :], writes=[rowm])
        p.dma("sp", sgn[:], self.C["sgn"][:, :], writes=[sgn])
        p.op("dve", lambda e: e.tensor_copy(out=identb[:, :], in_=self.identf[:, :]), reads=[self.identf], writes=[identb])
        p.op("pool", lambda e: e.memset(hpi[:, :], PI / 2), writes=[hpi])
        for b in range(4):
            self.wload(stg, wglu, wglu[:, b:b + 1, :], W["s5_w_glu"][l, b * 128:(b + 1) * 128, :], 1, 512)
        p.dma("sp", bglu[:], W["s5_b_glu"][l, :].rearrange("(m q) -> q m", q=128), writes=[bglu], allow_slow_non_contiguous=True)
        p.dma("sp", dcol[:], W["s5_d"][l, :].rearrange("(m q) -> q m", q=128), writes=[dcol], allow_slow_non_contiguous=True)

        PR, PQ, BST = [], [], []
        Cn = smp("Cn", (128, 128))
        dv = lambda fn, r, w: p.op("dve", fn, reads=r, writes=w)
        for d in range(2):
            are, aim, dt, lr, li, m1, cc, ss, t1, t2, nr, ni, den, cr, ci = (smp(f"{n}{d}") for n in
                                                                             ("are", "aim", "dt", "lr", "li", "m1", "cc", "ss", "t1", "t2", "nr", "ni", "den", "cr", "ci"))
            Xb = smp(f"Xb{d}", (128, 32, 16))
            Yb = smp(f"Yb{d}", (128, 32, 16))
            Bst = smp(f"Bst{d}", (128, 32, 16))
            Tb = smp(f"Tb{d}", (128, 32, 16))
            pr = PRt[d]
            pq = PQt[d]
            for hf in range(2):
                hs = slice(hf * 64, (hf + 1) * 64)
                p.dma("sp", are[hs, :], W["s5_a_re"][l, d, :, :].rearrange("g q -> q g"), writes=[are], allow_slow_non_contiguous=True)
                p.dma("sp", aim[hs, :], W["s5_a_im"][l, d, :, :].rearrange("g q -> q g"), writes=[aim], allow_slow_non_contiguous=True)
                own, oth = ("s5_b_re", "s5_b_im") if hf == 0 else ("s5_b_im", "s5_b_re")
                p.dma("sp", Xb[hs, :, :], W[own][l, d, :, :, :].rearrange("g q c -> q g c"), writes=[Xb])
                p.dma("sp", Yb[hs, :, :], W[oth][l, d, :, :, :].rearrange("g q c -> q g c"), writes=[Yb])
            p.dma("sp", dt[:], W["s5_log_dt"][l, d:d + 1, :].partition_broadcast(128), writes=[dt])
            p.op("act", lambda e, dt=dt: e.activation(out=dt[:, :], in_=dt[:, :], func=AF.Exp), reads=[dt], writes=[dt])
            TT = lambda o, a, b_, op: (lambda e: e.tensor_tensor(out=o[:, :], in0=a[:, :], in1=b_[:, :], op=op))
            dv(TT(lr, are, dt, ALU.mult), [are, dt], [lr])
            dv(TT(li, aim, dt, ALU.mult), [aim, dt], [li])
            TS = lambda o, a, s1, s2, o0, o1=None: (lambda e: e.tensor_scalar(o[:, :], a[:, :], s1, s2, o0, o1) if o1 is not None else e.tensor_scalar(o[:, :], a[:, :], s1, None, o0))
            dv(TS(m1, lr, 0.25, 1.0, ALU.mult, ALU.add), [lr], [m1])
            dv(TT(m1, m1, lr, ALU.mult), [m1, lr], [m1])
            dv(TS(m1, m1, 1.0 / 3.0, 1.0, ALU.mult, ALU.add), [m1], [m1])
            dv(TT(m1, m1, lr, ALU.mult), [m1, lr], [m1])
            dv(TS(m1, m1, 0.5, 1.0, ALU.mult, ALU.add), [m1], [m1])
            dv(TT(m1, m1, lr, ALU.mult), [m1, lr], [m1])
            dv(TS(m1, m1, 1.0, None, ALU.add), [m1], [m1])
            dv(TS(nr, li, 1.0 / 256.0, None, ALU.mult), [li], [nr])
            dv(TT(t1, nr, nr, ALU.mult), [nr], [t1])
            dv(TS(ss, t1, 1.0 / 120.0, -1.0 / 6.0, ALU.mult, ALU.add), [t1], [ss])
            dv(TT(ss, ss, t1, ALU.mult), [ss, t1], [ss])
            dv(TS(ss, ss, 1.0, None, ALU.add), [ss], [ss])
            dv(TT(ss, ss, nr, ALU.mult), [ss, nr], [ss])
            dv(TS(cc, t1, -1.0 / 720.0, 1.0 / 24.0, ALU.mult, ALU.add), [t1], [cc])
            dv(TT(cc, cc, t1, ALU.mult), [cc, t1], [cc])
            dv(TS(cc, cc, -0.5, None, ALU.add), [cc], [cc])
            dv(TT(cc, cc, t1, ALU.mult), [cc, t1], [cc])
            dv(TS(cc, cc, 1.0, None, ALU.add), [cc], [cc])
            for _ in range(8):
                dv(TT(t1, cc, cc, ALU.mult), [cc], [t1])
                dv(TT(t2, ss, ss, ALU.mult), [ss], [t2])
                dv(lambda e, cc=cc, ss=ss: e.scalar_tensor_tensor(out=ss[:, :], in0=cc[:, :], scalar=2.0, in1=ss[:, :], op0=ALU.mult, op1=ALU.mult), [cc, ss], [ss])
                dv(TT(cc, t1, t2, ALU.subtract), [t1, t2], [cc])
            dv(TT(t1, cc, cc, ALU.mult), [cc], [t1])
            dv(TT(t2, ss, ss, ALU.mult), [ss], [t2])
            dv(TT(t1, t1, t2, ALU.add), [t1, t2], [t1])
            dv(TS(t1, t1, -0.5, 1.5, ALU.mult, ALU.add), [t1], [t1])
            dv(TT(cc, cc, t1, ALU.mult), [cc, t1], [cc])
            dv(TT(ss, ss, t1, ALU.mult), [ss, t1], [ss])
            dv(lambda e, pr=pr, m1=m1, cc=cc: e.tensor_tensor(out=pr[:, 0, :], in0=m1[:, :], in1=cc[:, :], op=ALU.mult), [m1, cc], [pr])
            dv(TT(ni, m1, ss, ALU.mult), [m1, ss], [ni])
            dv(lambda e, pq=pq, ni=ni: e.tensor_scalar(pq[:, 0, :], ni[:, :], sgn[:, 1:2], None, ALU.mult), [ni, sgn], [pq])
            dv(lambda e, nr=nr, pr=pr: e.tensor_scalar(nr[:, :], pr[:, 0, :], -1.0, None, ALU.add), [pr], [nr])
            dv(TT(t1, are, are, ALU.mult), [are], [t1])
            dv(TT(t2, aim, aim, ALU.mult), [aim], [t2])
            dv(TT(den, t1, t2, ALU.add), [t1, t2], [den])
            dv(lambda e, den=den: e.reciprocal(out=den[:, :], in_=den[:, :]), [den], [den])
            dv(TT(t1, nr, are, ALU.mult), [nr, are], [t1])
            dv(TT(t2, ni, aim, ALU.mult), [ni, aim], [t2])
            dv(TT(cr, t1, t2, ALU.add), [t1, t2], [cr])
            dv(TT(cr, cr, den, ALU.mult), [cr, den], [cr])
            dv(TT(t1, ni, are, ALU.mult), [ni, are], [t1])
            dv(TT(t2, nr, aim, ALU.mult), [nr, aim], [t2])
            dv(TT(ci, t1, t2, ALU.subtract), [t1, t2], [ci])
            dv(TT(ci, ci, den, ALU.mult), [ci, den], [ci])
            dv(lambda e, ci=ci: e.tensor_scalar(ci[:, :], ci[:, :], sgn[:, 0:1], None, ALU.mult), [ci, sgn], [ci])
            bc = lambda t_: t_[:, :].unsqueeze(2).to_broadcast([128, 32, 16])
            dv(lambda e, Bst=Bst, Xb=Xb, cr=cr: e.tensor_tensor(out=Bst[:, :, :], in0=Xb[:, :, :], in1=bc(cr), op=ALU.mult), [Xb, cr], [Bst])
            dv(lambda e, Tb=Tb, Yb=Yb, ci=ci: e.tensor_tensor(out=Tb[:, :, :], in0=Yb[:, :, :], in1=bc(ci), op=ALU.mult), [Yb, ci], [Tb])
            dv(lambda e, Bst=Bst, Tb=Tb: e.tensor_tensor(out=Bst[:, :, :], in0=Bst[:, :, :], in1=Tb[:, :, :], op=ALU.add), [Bst, Tb], [Bst])
            for k in range(11):
                dv(lambda e, k=k, pr=pr, t1=t1: e.tensor_tensor(out=t1[:, :], in0=pr[:, k, :], in1=pr[:, k, :], op=ALU.mult), [pr], [t1])
                dv(lambda e, k=k, pq=pq, t2=t2: e.tensor_tensor(out=t2[:, :], in0=pq[:, k, :], in1=pq[:, k, :], op=ALU.mult), [pq], [t2])
                dv(lambda e, k=k, pr=pr, pq=pq: e.scalar_tensor_tensor(out=pq[:, k + 1, :], in0=pr[:, k, :], scalar=2.0, in1=pq[:, k, :], op0=ALU.mult, op1=ALU.mult), [pr, pq], [pq])
                dv(lambda e, k=k, pr=pr, t1=t1, t2=t2: e.tensor_tensor(out=pr[:, k + 1, :], in0=t1[:, :], in1=t2[:, :], op=ALU.subtract), [t1, t2], [pr])
            PR.append(pr)
            PQ.append(pq)
            if d == 0:
                for nm, tl_ in (("are", are), ("aim", aim), ("dt", dt), ("m1", m1), ("cc", cc), ("ss", ss), ("cr", cr), ("ci", ci)):
                    self.dump(nm, tl_, tl_[:, :], [128, 32])
                self.dump("pr", pr, pr[:, :, :], [128, 12, 32])
                self.dump("pq", pq, pq[:, :, :], [128, 12, 32])
                self.dump("Bst", Bst, Bst[:, :, :], [128, 32, 16])
            for b in range(4):
                ps = self.bank(0, 4)
                p.op("pe", lambda e, ps=ps, Bst=Bst, b=b: e.transpose(ps[:, 0:128], Bst[:, b * 8:(b + 1) * 8, :].rearrange("q g c -> q (g c)"), self.identf[:, :]), reads=[Bst, self.identf], writes=[ps])
                p.op("act", lambda e, ps=ps, d=d, b=b: e.copy(out=BT[d][b][:, :], in_=ps[:, 0:128]), reads=[ps], writes=[BT[d][b]])
                p.dma("sp", Cn[:, 0:64], W["s5_c_re"][l, d, b * 8:(b + 1) * 8, :, :].rearrange("g c q -> (g c) q"), writes=[Cn])
                p.dma("sp", Cn[:, 64:128], W["s5_c_im"][l, d, b * 8:(b + 1) * 8, :, :].rearrange("g c q -> (g c) q"), writes=[Cn])
                ps = self.bank(0, 4)
                p.op("pe", lambda e, ps=ps: e.transpose(ps[:, 0:128], Cn[:, :], self.identf[:, :]), reads=[Cn, self.identf], writes=[ps])
                p.op("dve", lambda e, ps=ps, d=d, b=b: e.tensor_scalar(CST[d][b][:, :], ps[:, 0:128], sgn[:, 1:2], None, ALU.mult), reads=[ps, sgn], writes=[CST[d][b]])

        for b_ in (0, 3):
            self.dump(f"BT{b_}", BT[0][b_], BT[0][b_][:, :], [128, 128])
            self.dump(f"CST{b_}", CST[0][b_], CST[0][b_][:, :], [128, 128])
        p.barrier()
        pst.close()
        uT = p.sb("s_uT", [128, S], BF16, st)
        X = [[p.sb(f"s_X{d}{i}", [128, S], BF16, st, nb=8) for i in range(2)] for d in range(2)]
        yacc = p.sb("s_yacc", [128, S], F32, st, nb=8)
        ygT = p.sb("s_ygT", [128, 4, S], BF16, st)
        Mk = [p.sb(f"s_Mk{i}", [128, 128], BF16, st) for i in range(8)]
        Mt = [p.sb(f"s_Mt{i}", [128, 128], F32, st) for i in range(4)]
        Mt2 = [p.sb(f"s_Mt2{i}", [128, 128], F32, st) for i in range(4)]
        LB = [p.sb(f"s_LB{i}", [128, 128], BF16, st) for i in range(4)]
        LC = [p.sb(f"s_LC{i}", [128, 128], BF16, st) for i in range(4)]
        yt = [p.sb(f"s_yt{i}", [128, 512], F32, st) for i in range(2)]
        ob = [p.sb(f"s_ob{i}", [128, 512], BF16, st) for i in range(2)]
        dirs = self.dbg.get("s5_dirs", (0, 1))
        mki = 0
        gi = 0
        bk = [0]

        def nb(lo, hi):
            b_ = self.PS[lo + bk[0] % (hi - lo)]
            bk[0] += 1
            return b_

        for b in self.dbg.get("s5_tiles", range(4)):
            c0 = COL["u"] + b * 128
            self.wload(stg, wu, wu[:, :, :], W["w_in"][l, :, c0:c0 + 128], 8, 128)
            for ct in range(8):
                ps = self.bank(0, 4)
                for c in range(8):
                    self.mm(ps[:, :], wu[:, c, :], xT[:, c, ct * 512:(ct + 1) * 512], c == 0, c == 7, [wu] + xTr[ct * 4:ct * 4 + 4], [ps])
                p.op("act", lambda e, ps=ps, ct=ct: e.copy(out=uT[:, ct * 512:(ct + 1) * 512], in_=ps[:, :]), reads=[ps], writes=[uT])
            first_acc = True
            for gl in range(8):
                g = b * 8 + gl
                lbs, lcs = {}, {}
                for d in dirs:
                    lb = LB[gi % 4]
                    lc = LC[gi % 4]
                    gi += 1
                    lbs[d], lcs[d] = lb, lc
                    p.op("dve", lambda e, lb=lb, d=d, gl=gl, b=b: e.tensor_scalar(lb[:, :], BT[d][b][:, :], rowm[:, gl:gl + 1], None, ALU.mult), reads=[BT[d][b], rowm], writes=[lb])
                    p.op("pool", lambda e, lc=lc: e.memset(lc[:, :], 0.0), writes=[lc])
                    p.op("pool", lambda e, lc=lc, d=d, gl=gl, b=b: e.tensor_copy(out=lc[:, gl * 16:(gl + 1) * 16], in_=CST[d][b][:, gl * 16:(gl + 1) * 16]), reads=[CST[d][b], lc], writes=[lc])
                    for ct in range(8):
                        ps = nb(0, 8)
                        self.mm(ps[:, :], lb[:, :], uT[:, ct * 512:(ct + 1) * 512], True, True, [lb, uT], [ps])
                        if ct % 2 == 0:
                            p.op("act", lambda e, ps=ps, ct=ct, d=d: e.copy(out=X[d][0][:, ct * 512:(ct + 1) * 512], in_=ps[:, :]), reads=[ps], writes=[X[d][0].bs[ct]])
                        else:
                            p.op("dve", lambda e, ps=ps, ct=ct, d=d: e.tensor_copy(out=X[d][0][:, ct * 512:(ct + 1) * 512], in_=ps[:, :]), reads=[ps], writes=[X[d][0].bs[ct]])
                cur = 0
                mkq = {}

                def build_mk(k, g=g):
                    nonlocal mki
                    for d in dirs:
                        mt = Mt[mki % 4]
                        mk = Mk[mki % 8]
                        mki += 1
                        mkq[(k, d)] = mk
                        p.op("act", lambda e, mt=mt, d=d, k=k, g=g: e.mul(out=mt[:, :], in_=jx[:, :], mul=PQ[d][:, k, g:g + 1]), reads=[jx, PQ[d]], writes=[mt])
                        p.op("dve", lambda e, mt=mt, mk=mk, d=d, k=k, g=g: e.scalar_tensor_tensor(out=mk[:, :], in0=self.identf[:, :], scalar=PR[d][:, k, g:g + 1], in1=mt[:, :], op0=ALU.mult, op1=ALU.add),
                             reads=[self.identf, PR[d], mt], writes=[mk])

                build_mk(0)
                build_mk(1)
                for k in range(12):
                    sh = 1 << k
                    if k + 2 < 12:
                        build_mk(k + 2)
                    mks = {d: mkq[(k, d)] for d in dirs}
                    for ct in range(8):
                        for d in dirs:
                            mk = mks[d]
                            Xs, Xd = X[d][cur], X[d][1 - cur]
                            t0 = ct * 512
                            if d == 0:
                                lo = max(0, sh - t0)
                                hi = 512
                                s0 = t0 + lo - sh
                            else:
                                lo = 0
                                hi = min(512, S - sh - t0)
                                s0 = t0 + sh
                            has = hi > lo
                            srcb = []
                            if has:
                                a0, a1 = s0, s0 + (hi - lo)
                                srcb = [Xs.bs[i] for i in range(a0 // 512, (a1 - 1) // 512 + 1)]
                            use_pe = ((ct + d) % 2 == 0) or not has
                            ps = nb(0, 8)
                            if use_pe:
                                self.mm(ps[:, :], identb[:, :], Xs[:, t0:t0 + 512], True, not has, [identb, Xs.bs[ct]], [ps])
                                if has:
                                    self.mm(ps[:, lo:hi], mk[:, :], Xs[:, s0:s0 + hi - lo], False, True, [mk] + srcb, [ps])
                                p.op("act", lambda e, ps=ps, Xd=Xd, t0=t0: e.copy(out=Xd[:, t0:t0 + 512], in_=ps[:, :]), reads=[ps], writes=[Xd.bs[ct]])
                            else:
                                self.mm(ps[:, lo:hi], mk[:, :], Xs[:, s0:s0 + hi - lo], True, True, [mk] + srcb, [ps])
                                p.op("dve", lambda e, ps=ps, Xd=Xd, Xs=Xs, t0=t0, lo=lo, hi=hi: e.tensor_tensor(out=Xd[:, t0 + lo:t0 + hi], in0=ps[:, lo:hi], in1=Xs[:, t0 + lo:t0 + hi], op=ALU.add),
                                     reads=[ps, Xs.bs[ct]], writes=[Xd.bs[ct]])
                                if lo > 0:
                                    p.op("pool", lambda e, Xd=Xd, Xs=Xs, t0=t0, lo=lo: e.tensor_copy(out=Xd[:, t0:t0 + lo], in_=Xs[:, t0:t0 + lo]), reads=[Xs.bs[ct]], writes=[Xd.bs[ct]])
                                if hi < 512:
                                    p.op("pool", lambda e, Xd=Xd, Xs=Xs, t0=t0, hi=hi: e.tensor_copy(out=Xd[:, t0 + hi:t0 + 512], in_=Xs[:, t0 + hi:t0 + 512]), reads=[Xs.bs[ct]], writes=[Xd.bs[ct]])
                    cur = 1 - cur
                for ct in range(8):
                    ps = nb(0, 8)
                    for i_, d in enumerate(dirs):
                        self.mm(ps[:, :], lcs[d][:, :], X[d][cur][:, ct * 512:(ct + 1) * 512], i_ == 0, i_ == len(dirs) - 1, [lcs[d], X[d][cur].bs[ct]], [ps])
                    ya = yacc[:, ct * 512:(ct + 1) * 512]
                    if first_acc:
                        p.op("dve", lambda e, ps=ps, ya=ya: e.tensor_copy(out=ya, in_=ps[:, :]), reads=[ps], writes=[yacc.bs[ct]])
                    else:
                        p.op("dve", lambda e, ps=ps, ya=ya: e.tensor_tensor(out=ya, in0=ps[:, :], in1=ya, op=ALU.add), reads=[ps], writes=[yacc.bs[ct]])
                first_acc = False
            for ct in range(8):
                y_ = yt[ct % 2]
                sl = slice(ct * 512, (ct + 1) * 512)
                p.op("dve", lambda e, y_=y_, sl=sl, b=b: e.scalar_tensor_tensor(out=y_[:, :], in0=uT[:, sl], scalar=dcol[:, b:b + 1], in1=yacc[:, sl], op0=ALU.mult, op1=ALU.add),
                     reads=[uT, dcol, yacc.bs[ct]], writes=[y_])
                if self.dbg.get("s5y"):
                    p.dma("pool", self.dbgy[b, :, sl], y_[:, :], reads=[y_], writes=[self.dbgy])
                    p.dma("pool", self.dbgy[4 + b, :, sl], yacc[:, sl], reads=[yacc.bs[ct]], writes=[self.dbgy])
                p.op("act", lambda e, y_=y_, sl=sl, b=b: e.activation(out=ygT[:, b, sl], in_=y_[:, :], func=AF.Gelu_apprx_tanh), reads=[y_], writes=[ygT])
        for m in range(4):
            for ct in range(8):
                sl = slice(ct * 512, (ct + 1) * 512)
                ps = self.bank(0, 8)
                for b in range(4):
                    self.mm(ps[:, :], wglu[:, b, m * 128:(m + 1) * 128], ygT[:, b, sl], b == 0, b == 3, [wglu, ygT], [ps])
                y_ = yt[ct % 2]
                o_ = ob[ct % 2]
                p.op("act", lambda e, ps=ps, y_=y_, m=m: e.activation(out=y_[:, :], in_=ps[:, :], func=AF.Sigmoid, bias=bglu[:, m:m + 1]), reads=[ps, bglu], writes=[y_])
                p.op("dve", lambda e, y_=y_, o_=o_, m=m, sl=sl: e.tensor_tensor(out=o_[:, :], in0=ygT[:, m, sl], in1=y_[:, :], op=ALU.mult), reads=[ygT, y_], writes=[o_])
                p.dma("pool", self.oT[1, m, :, sl], o_[:, :], reads=[o_], writes=[self.oT.bs[4 + m]])
        p.barrier()
        st.close()


def build_model():
    M = Model()
    M.phase_ln0()
    for l in range(DEPTH):
        M.phase_attn(l)
        M.phase_s5(l)
        M.phase_gla(l)
        M.phase_merge1(l)
        M.phase_merge2(l)
        M.phase_moe(l, last=(l == DEPTH - 1))
    M.p.emit()
    return M


def kernel(**inputs):
    M = build_model()
    consts = host_consts()
    shared = {k: np.ascontiguousarray(np.asarray(inputs[k], dtype=np.float32)) for k in PARAM_SHAPES}
    shared.update(consts)
    x = np.asarray(inputs["x"], dtype=np.float32)
    in_maps = []
    for b in range(8):
        m = dict(shared)
        m["x"] = np.ascontiguousarray(x[b])
        in_maps.append(m)
    res = run_bass_kernel_spmd(M.nc, in_maps, core_ids=list(range(8)))
    return np.stack([np.asarray(r["out"], dtype=np.float32) for r in res.results], axis=0)
```

```python
import math
from contextlib import ExitStack
import numpy as np
import ml_dtypes
import concourse.bass as bass
import concourse.mybir as mybir
from concourse.bass_utils import run_bass_kernel_spmd

F32 = mybir.dt.float32
BF16 = mybir.dt.bfloat16
AF = mybir.ActivationFunctionType
ALU = mybir.AluOpType
AX = mybir.AxisListType

ENGS = ("pe", "act", "dve", "pool", "sp")
NDMA_SEM = 6


class Buf:
    __slots__ = ("name", "w", "r", "excl")

    def __init__(self, name=""):
        self.name = name
        self.w = None
        self.r = {}
        self.excl = False


class Tl:
    def __init__(self, t, name, nb=0):
        self.t = t
        self.b = Buf(name)
        self.bs = [Buf(f"{name}{i}") for i in range(nb)]

    def __getitem__(self, k):
        return self.t[k]


class Sub(Tl):
    def __init__(self, ap, buf):
        self.t = ap
        self.b = buf
        self.bs = []


class Prog:
    def __init__(self, nc):
        self.nc = nc
        self.stack = ExitStack()
        self.lists = {e: [] for e in ENGS}
        self.cnt = {e: 0 for e in ENGS}
        self.known = {e: {} for e in ENGS}
        self.sem = {}
        for e in ("pe", "act", "dve", "pool"):
            self.sem["c_" + e] = self.stack.enter_context(nc.semaphore("c_" + e))
        self.dq = {}
        for q in ("sp", "pool", "act"):
            names = []
            for i in range(NDMA_SEM):
                n = f"d_{q}{i}"
                self.sem[n] = self.stack.enter_context(nc.semaphore(n))
                names.append(n)
            self.dq[q] = dict(sems=names, issued=[0] * NDMA_SEM, rr=0)
        self.nwaits = 0

    uid = 0

    def sb(self, name, shape, dtype, stack=None, nb=0):
        Prog.uid += 1
        name = f"{name}_{Prog.uid}"
        t = (stack or self.stack).enter_context(self.nc.sbuf_tensor(name, list(shape), dtype))
        return Tl(t, name, nb)

    def ps(self, name, shape, dtype=F32, stack=None, nb=0):
        t = (stack or self.stack).enter_context(self.nc.psum_tensor(name, list(shape), dtype))
        tl = Tl(t, name, nb)
        tl.b.excl = True
        return tl

    def dram(self, name, shape, dtype, kind="Internal", nb=0):
        t = self.nc.dram_tensor(name, list(shape), dtype, kind=kind).ap()
        return Tl(t, name, nb)

    @staticmethod
    def _bufs(xs):
        out = []
        for x in xs:
            if x is None:
                continue
            out.append(x.b if isinstance(x, Tl) else x)
        return out

    def _need(self, reads, writes):
        need = {}

        def add(sv):
            s, v = sv
            if need.get(s, 0) < v:
                need[s] = v

        for b in reads:
            if b.w:
                add(b.w)
        for b in writes:
            if b.w:
                add(b.w)
            for sv in b.r.items():
                add(sv)
        return need

    def _waits(self, e, need, skip=None):
        kn = self.known[e]
        for s, v in need.items():
            if s == skip:
                continue
            if kn.get(s, 0) < v:
                self.lists[e].append(("wait", s, v))
                kn[s] = v
                self.nwaits += 1

    def op(self, e, fn, reads=(), writes=(), serial=False):
        reads = self._bufs(reads)
        writes = self._bufs(writes)
        ex = [b for b in reads if b.excl and b not in writes]
        if ex:
            reads = [b for b in reads if not b.excl]
            writes = writes + ex
        s = "c_" + e
        self._waits(e, self._need(reads, writes), skip=(s if (e == "pe" and not serial) else None))
        self.cnt[e] += 1
        v = self.cnt[e]
        self.lists[e].append(("op", fn, s))
        for b in reads:
            b.r[s] = v
        for b in writes:
            b.w = (s, v)
            b.r = {}

    def dma(self, q, out, in_, reads=(), writes=(), **kw):
        reads = self._bufs(reads)
        writes = self._bufs(writes)
        d = self.dq[q]
        i = d["rr"]
        d["rr"] = (i + 1) % NDMA_SEM
        s = d["sems"][i]
        need = self._need(reads, writes)
        if d["issued"][i] > 0:
            pv = 16 * d["issued"][i]
            if need.get(s, 0) < pv:
                need[s] = pv
        self._waits(q, need)
        d["issued"][i] += 1
        v = 16 * d["issued"][i]
        self.lists[q].append(("dma", out, in_, s, kw))
        for b in reads:
            b.r[s] = v
        for b in writes:
            b.w = (s, v)
            b.r = {}

    def barrier(self, engines=ENGS):
        need = {}
        for e in ("pe", "act", "dve", "pool"):
            if self.cnt[e]:
                need["c_" + e] = self.cnt[e]
        for q, d in self.dq.items():
            for s, n in zip(d["sems"], d["issued"]):
                if n:
                    need[s] = 16 * n
        for e in engines:
            self._waits(e, dict(need), skip=("c_" + e if e in ("pe",) else None))

    def emit(self):
        nc = self.nc
        self.barrier(engines=("sp",))
        lists = self.lists
        sem = self.sem

        def run(eng, lst):
            for it in lst:
                if it[0] == "wait":
                    eng.wait_ge(sem[it[1]], it[2])
                elif it[0] == "op":
                    it[1](eng).then_inc(sem[it[2]], 1)
                else:
                    eng.dma_start(out=it[1], in_=it[2], **it[4]).then_inc(sem[it[3]], 16)

        with nc.Block() as block:
            @block.tensor
            def _(eng):
                run(eng, lists["pe"])

            @block.scalar
            def _(eng):
                run(eng, lists["act"])

            @block.vector
            def _(eng):
                run(eng, lists["dve"])

            @block.gpsimd
            def _(eng):
                run(eng, lists["pool"])

            @block.sync
            def _(eng):
                run(eng, lists["sp"])
        self.stack.close()


S = 4096
D = 1024
NT = 32
DEPTH = 2
N_IN = 6688
COL = dict(qa=0, ka=512, va=1024, u=1536, qc=2048, kc=2304, vc=2560, rc=3072, zf=3584, zb=3600, gate=3616)
ALPHA = (2.0 * DEPTH) ** 0.25
EPS = 1e-5
PI = math.pi

PARAM_SHAPES = dict(
    ln0_g=[1024], ln0_b=[1024], w_in=[2, 1024, 6688], da_lambda=[2, 4, 64], da_norm_g=[2, 128],
    s5_a_re=[2, 2, 32, 64], s5_a_im=[2, 2, 32, 64], s5_log_dt=[2, 2, 32], s5_b_re=[2, 2, 32, 64, 16],
    s5_b_im=[2, 2, 32, 64, 16], s5_c_re=[2, 2, 32, 16, 64], s5_c_im=[2, 2, 32, 16, 64], s5_d=[2, 512],
    s5_w_glu=[2, 512, 512], s5_b_glu=[2, 512], gla_w_gate=[2, 2, 16, 256], gla_b_gate=[2, 2, 256],
    gla_norm_g=[2, 128], merge_w_up=[2, 3, 512, 1024], merge_b=[2, 3, 1024], w_out=[2, 1024, 1024],
    ln1_g=[2, 1024], ln1_b=[2, 1024], router_w=[1024, 16], router_bias=[16],
    moe_w_gate=[2, 16, 1024, 512], moe_w_up=[2, 16, 1024, 512], moe_w_down=[2, 16, 512, 1024],
    ln2_g=[2, 1024], ln2_b=[2, 1024])


def host_consts():
    c = {}
    c["identf"] = np.eye(128, dtype=np.float32)
    i = np.arange(128)
    c["absdiff"] = np.abs(i[:, None] - i[None, :]).astype(np.float32)
    pos = np.arange(S)
    hi, lo = pos // 64, pos % 64
    c["qaug"] = np.stack([64.0 * hi, lo, np.ones(S), np.ones(S)]).astype(ml_dtypes.bfloat16)
    ka = np.zeros((4, 2, 4, S), np.float32)
    for h in range(4):
        sl = 2.0 ** (-2.0 * (h + 1))
        plus = np.stack([np.full(S, sl), np.full(S, sl), -sl * 64.0 * hi, -sl * lo])
        ka[h, 0] = plus
        ka[h, 1] = -plus
    c["kaug"] = ka.astype(ml_dtypes.bfloat16)
    j = np.arange(64)
    c["gmask"] = np.stack([(j[:, None] <= j[None, :]), (j[:, None] > j[None, :])]).astype(np.float32)
    jj = np.repeat(np.arange(8), 16)
    c["s5mask"] = np.stack([(jj[None, :] >= jj[:, None]), (jj[None, :] <= jj[:, None])]).astype(np.float32)
    jx = np.zeros((128, 128), np.float32)
    for q in range(64):
        jx[q, 64 + q] = 1.0
        jx[64 + q, q] = 1.0
    c["jx"] = jx
    c["rowmask"] = (np.arange(128)[:, None] // 16 == np.arange(8)[None, :]).astype(np.float32)
    sg = np.ones((128, 2), np.float32)
    sg[:64, 0] = -1.0
    sg[64:, 1] = -1.0
    c["sgn"] = sg
    return c


CONST_SHAPES = dict(jx=([128, 128], F32), rowmask=([128, 8], F32), sgn=([128, 2], F32), identf=([128, 128], F32), absdiff=([128, 128], F32), qaug=([4, S], BF16), kaug=([4, 2, 4, S], BF16),
                    gmask=([2, 64, 64], F32), s5mask=([2, 128, 128], F32))


class Model:
    def __init__(self, dbg=None):
        self.dbg = dbg or {}
        nc = bass.Bass("TRN2", target_bir_lowering=False)
        self.nc = nc
        p = Prog(nc)
        self.p = p
        kinds = self.dbg.get("kinds", {})
        self.x_in = p.dram("x", [S, D], F32, kind="ExternalInput")
        self.out = p.dram("out", [S, D], F32, kind="ExternalOutput", nb=NT)
        self.W = {k: p.dram(k, shp, F32, kind="ExternalInput") for k, shp in PARAM_SHAPES.items()}
        self.C = {k: p.dram(k, shp, dt, kind="ExternalInput") for k, (shp, dt) in CONST_SHAPES.items()}
        self.xres = p.dram("xres", [S, D], F32, kind=kinds.get("xres", "Internal"), nb=NT)
        self.oT = p.dram("oT", [3, 4, 128, S], BF16, kind=kinds.get("oT", "Internal"), nb=12)
        self.mT = p.dram("mT", [128, 8, S], BF16, kind=kinds.get("mT", "Internal"), nb=16)
        if self.dbg.get("s5y"):
            self.dbgy = p.dram("dbgy", [8, 128, S], F32, kind="ExternalOutput")
        self.xT = p.sb("xT", [128, 8, S], BF16, nb=NT)
        self.identf = p.sb("identf_sb", [128, 128], F32)
        self.call = p.sb("call", [128, NT, 16], F32, nb=NT)
        self.PS = [p.ps(f"ps{i}", [128, 512], F32) for i in range(8)]
        self.psi = 0
        self.psc = {}
        p.dma("sp", self.identf[:], self.C["identf"][:, :], writes=[self.identf])

    def dump(self, name, tl, ap, shape):
        if not self.dbg.get("dump"):
            return
        d = self.p.dram("dump_" + name, list(shape), F32, kind="ExternalOutput")
        self.p.dma("pool", d.t, ap, reads=[tl], writes=[d])

    def bank(self, lo=0, hi=8):
        n = hi - lo
        c = self.psc.get((lo, hi), 0)
        self.psc[(lo, hi)] = c + 1
        return self.PS[lo + (c % n)]

    def mm(self, out, lhsT, rhs, start, stop, reads, writes, serial=False):
        self.p.op("pe", lambda e: e.matmul(out, lhsT, rhs, start=start, stop=stop, skip_group_check=True), reads=reads, writes=writes, serial=serial)

    def ln_tile(self, z, xn, stats, mv, rstd):
        p = self.p
        for k in range(2):
            p.op("dve", lambda e, k=k: e.bn_stats(out=stats[:, k, :], in_=z[:, k * 512:(k + 1) * 512]), reads=[z], writes=[stats])
        p.op("dve", lambda e: e.bn_aggr(out=mv[:, :], in_=stats[:, :, :].rearrange("p a b -> p (a b)")), reads=[stats], writes=[mv])
        self.rsqrt(rstd, rstd[:, :], mv, mv[:, 1:2], 1.0)
        p.op("dve", lambda e: e.tensor_scalar(xn[:, :], z[:, :], mv[:, 0:1], rstd[:, 0:1], ALU.subtract, ALU.mult), reads=[z, mv, rstd], writes=[xn])
        p.op("pool", lambda e, g_=self.gbc: e.tensor_tensor(out=xn[:, :], in0=xn[:, :], in1=g_[:, :], op=ALU.mult), reads=[xn, self.gbc], writes=[xn])
        p.op("pool", lambda e, b_=self.bbc: e.tensor_tensor(out=xn[:, :], in0=xn[:, :], in1=b_[:, :], op=ALU.add), reads=[xn, self.bbc], writes=[xn])

    def rsqrt(self, dst_tl, dst, src_tl, src, scale):
        p = self.p
        p.op("dve", lambda e: e.tensor_scalar(dst, src, scale, EPS, ALU.mult, ALU.add), reads=[src_tl], writes=[dst_tl])
        p.op("act", lambda e: e.sqrt(out=dst, in_=dst), reads=[dst_tl], writes=[dst_tl])
        p.op("dve", lambda e: e.reciprocal(out=dst, in_=dst), reads=[dst_tl], writes=[dst_tl])

    def load_ln_params(self, g_ap, b_ap, st):
        p = self.p
        self.gbc = p.sb("gbc", [128, D], F32, st)
        self.bbc = p.sb("bbc", [128, D], F32, st)
        p.dma("sp", self.gbc[:], g_ap.partition_broadcast(128), writes=[self.gbc])
        p.dma("sp", self.bbc[:], b_ap.partition_broadcast(128), writes=[self.bbc])

    def to_xT(self, xn, t, xT32=None):
        p = self.p
        for c0 in (0, 4):
            ps = self.bank(0, 4)
            for j in range(4):
                c = c0 + j
                p.op("pe", lambda e, ps=ps, j=j, c=c: e.transpose(ps[:, j * 128:(j + 1) * 128], xn[:, c * 128:(c + 1) * 128], self.identf[:, :]),
                     reads=[xn, self.identf], writes=[ps])
            src = ps[:, :].rearrange("p (j n) -> p j n", j=4)
            p.op("act", lambda e, src=src, c0=c0: e.copy(out=self.xT[:, c0:c0 + 4, t * 128:(t + 1) * 128], in_=src), reads=[ps], writes=[self.xT.bs[t]])
            if xT32 is not None:
                p.op("dve", lambda e, src=src, c0=c0: e.tensor_copy(out=xT32[:, c0:c0 + 4, :], in_=src), reads=[ps], writes=[xT32])

    def phase_ln0(self):
        p = self.p
        st = ExitStack()
        self.load_ln_params(self.W["ln0_g"].t.rearrange("(o n) -> o n", o=1), self.W["ln0_b"].t.rearrange("(o n) -> o n", o=1), st)
        zs = [p.sb(f"l0z{i}", [128, D], F32, st) for i in range(2)]
        stats = p.sb("l0stats", [128, 2, 6], F32, st)
        mv = p.sb("l0mv", [128, 2], F32, st)
        rstd = p.sb("l0rstd", [128, 1], F32, st)
        def stage_A(t):
            z = zs[t % 2]
            p.dma("sp", z[:], self.x_in[t * 128:(t + 1) * 128, :], writes=[z])
            self.ln_tile(z, z, stats, mv, rstd)
            p.dma("pool", self.xres[t * 128:(t + 1) * 128, :], z[:], reads=[z], writes=[self.xres.bs[t]])

        stage_A(0)
        for t in range(NT):
            if t + 1 < NT:
                stage_A(t + 1)
            self.to_xT(zs[t % 2], t)
        p.barrier()
        st.close()

    def wload(self, stg, dst_tl, dst_ap, src_ap, kc, ncols, cast="act", wbuf=None):
        p = self.p
        cap = stg[0].t.shape[1]
        kcp = max(1, min(kc, cap // ncols))
        wb = [wbuf if wbuf is not None else dst_tl]
        for k0 in range(0, kc, kcp):
            st = stg[self.wl_i % len(stg)]
            self.wl_i += 1
            view = st[:, 0:kcp * ncols].rearrange("p (c n) -> p c n", c=kcp)
            p.dma("sp", view, src_ap[k0 * 128:(k0 + kcp) * 128, :].rearrange("(c p) n -> p c n", p=128), writes=[st])
            if cast == "act":
                p.op(cast, lambda e, view=view, k0=k0: e.copy(out=dst_ap[:, k0:k0 + kcp, :], in_=view), reads=[st], writes=wb)
            else:
                p.op(cast, lambda e, view=view, k0=k0: e.tensor_copy(out=dst_ap[:, k0:k0 + kcp, :], in_=view), reads=[st], writes=wb)

    wl_i = 0

    def phase_attn(self, l):
        p = self.p
        W = self.W
        st = ExitStack()
        stg = [p.sb(f"a_stg{i}", [128, 8 * 128], F32, st) for i in range(2)]
        wA = [p.sb(f"a_w{i}", [128, 8, 384], BF16, st) for i in range(2)]
        QT = [p.sb(f"a_qt{m}", [68, S], BF16, st) for m in range(2)]
        KTp = [p.sb(f"a_ktp{m}", [68, S], BF16, st) for m in range(2)]
        KTm = [p.sb(f"a_ktm{m}", [68, S], BF16, st) for m in range(2)]
        V = p.sb("a_v", [128, NT, 129], BF16, st)
        PT = [p.sb(f"a_pt{i}", [128, 512], BF16, st) for i in range(4)]
        oaT = [p.sb(f"a_oat{i}", [128, S], BF16, st) for i in range(1)]
        on = [p.sb(f"a_on{m}", [128, 4, 128], F32, st) for m in range(2)]
        rs = p.sb("a_rs", [128, 4], F32, st)
        diff = p.sb("a_diff", [128, 4, 128], F32, st)
        sq = p.sb("a_sq", [128, 4, 128], F32, st)
        ss = p.sb("a_ss", [128, 4], F32, st)
        rstd = p.sb("a_rstd", [128, 4], F32, st)
        oo = p.sb("a_oo", [128, 4, 128], F32, st)
        absd = p.sb("a_absd", [128, 128], F32, st)
        lamt = p.sb("a_lamt", [128, 256], F32, st)
        lsm = p.sb("a_lsm", [128, 8], F32, st)
        gA = p.sb("a_gA", [128, 128], F32, st)
        lam_init = 0.8 - 0.6 * math.exp(-0.3 * l)
        p.dma("sp", absd[:], self.C["absdiff"][:, :], writes=[absd])
        for m in range(2):
            p.dma("sp", QT[m][64:68, :], self.C["qaug"][:, :], writes=[QT[m]])
        p.dma("sp", lamt[:], W["da_lambda"][l:l + 1, :, :].rearrange("o a b -> o (a b)").partition_broadcast(128), writes=[lamt])
        p.dma("sp", gA[:], W["da_norm_g"][l:l + 1, :].partition_broadcast(128), writes=[gA])
        p.op("dve", lambda e: e.tensor_scalar(gA[:, :], gA[:, :], 1.0 - lam_init, None, ALU.mult), reads=[gA], writes=[gA])
        p.op("dve", lambda e: e.tensor_tensor(out=lamt[:, 0:64], in0=lamt[:, 0:64], in1=lamt[:, 64:128], op=ALU.mult), reads=[lamt], writes=[lamt])
        p.op("dve", lambda e: e.tensor_tensor(out=lamt[:, 128:192], in0=lamt[:, 128:192], in1=lamt[:, 192:256], op=ALU.mult), reads=[lamt], writes=[lamt])
        p.op("dve", lambda e: e.tensor_reduce(out=lsm[:, 0:1], in_=lamt[:, 0:64], axis=AX.X, op=ALU.add), reads=[lamt], writes=[lsm])
        p.op("dve", lambda e: e.tensor_reduce(out=lsm[:, 1:2], in_=lamt[:, 128:192], axis=AX.X, op=ALU.add), reads=[lamt], writes=[lsm])
        p.op("act", lambda e: e.activation(out=lsm[:, 2:4], in_=lsm[:, 0:2], func=AF.Exp), reads=[lsm], writes=[lsm])
        p.op("dve", lambda e: e.tensor_tensor(out=lsm[:, 4:5], in0=lsm[:, 3:4], in1=lsm[:, 2:3], op=ALU.subtract), reads=[lsm], writes=[lsm])
        p.op("dve", lambda e: e.tensor_scalar(lsm[:, 5:6], lsm[:, 4:5], -lam_init, None, ALU.add), reads=[lsm], writes=[lsm])
        p.op("dve", lambda e: e.memset(V[:, :, 128:129], 1.0), writes=[V])
        xT = self.xT
        xTr = list(xT.bs)

        def load_head_w(h):
            w = wA[h % 2]
            for k, nm in enumerate(("qa", "ka", "va")):
                c0 = COL[nm] + h * 128
                self.wload(stg, w, w[:, :, k * 128:(k + 1) * 128], W["w_in"][l, :, c0:c0 + 128], 8, 128, cast="dve")

        load_head_w(0)
        for h in range(self.dbg.get("heads", 4)):
            slope = 2.0 ** (-2.0 * (h + 1))
            w = wA[h % 2]
            if h + 1 < self.dbg.get("heads", 4):
                load_head_w(h + 1)
            for m in range(2):
                p.dma("sp", KTp[m][64:68, :], self.C["kaug"][h, 0, :, :], writes=[KTp[m]])
                p.dma("sp", KTm[m][64:68, :], self.C["kaug"][h, 1, :, :], writes=[KTm[m]])
            for m in range(2):
                for tb in range(8):
                    ps = self.bank(0, 4)
                    for c in range(8):
                        self.mm(ps[0:64, :], w[:, c, m * 64:(m + 1) * 64], xT[:, c, tb * 512:(tb + 1) * 512], c == 0, c == 7, [w] + xTr[tb * 4:tb * 4 + 4], [ps])
                    p.op("act", lambda e, ps=ps, m=m, tb=tb: e.mul(out=QT[m][0:64, tb * 512:(tb + 1) * 512], in_=ps[0:64, :], mul=0.125), reads=[ps], writes=[QT[m]])
                    ps = self.bank(0, 4)
                    for c in range(8):
                        self.mm(ps[0:64, :], w[:, c, 128 + m * 64:128 + (m + 1) * 64], xT[:, c, tb * 512:(tb + 1) * 512], c == 0, c == 7, [w] + xTr[tb * 4:tb * 4 + 4], [ps])
                    p.op("act", lambda e, ps=ps, m=m, tb=tb: e.copy(out=KTp[m][0:64, tb * 512:(tb + 1) * 512], in_=ps[0:64, :]), reads=[ps], writes=[KTp[m]])
                    p.op("dve", lambda e, ps=ps, m=m, tb=tb: e.tensor_copy(out=KTm[m][0:64, tb * 512:(tb + 1) * 512], in_=ps[0:64, :]), reads=[ps], writes=[KTm[m]])
            for t4 in range(8):
                ps = self.bank(0, 4)
                for j in range(4):
                    t = t4 * 4 + j
                    for c in range(8):
                        self.mm(ps[:, j * 128:(j + 1) * 128], xT[:, c, t * 128:(t + 1) * 128], w[:, c, 256:384], c == 0, c == 7, [w, xTr[t]], [ps])
                p.op("act", lambda e, ps=ps, t4=t4: e.copy(out=V[:, t4 * 4:(t4 + 1) * 4, 0:128], in_=ps[:, :].rearrange("p (j n) -> p j n", j=4)), reads=[ps], writes=[V])
            oa = oaT[0]
            pti = 0
            for Q in range(self.dbg.get("nQ", 8)):
                stages = [(m, kt) for m in range(2) for kt in range(NT)]
                stbank = {}
                ptbuf = {}

                def st_S(i, Q=Q):
                    m, kt = stages[i]
                    ps = self.PS[i % 4]
                    stbank[i] = ps
                    rel = kt - 4 * Q
                    ksl = slice(kt * 128, (kt + 1) * 128)
                    if rel < 0:
                        self.mm(ps[:, :], KTm[m][0:68, ksl], QT[m][0:68, Q * 512:(Q + 1) * 512], True, True, [KTm[m], QT[m]], [ps])
                    elif rel > 3:
                        self.mm(ps[:, :], KTp[m][0:68, ksl], QT[m][0:68, Q * 512:(Q + 1) * 512], True, True, [KTp[m], QT[m]], [ps])
                    else:
                        q0 = Q * 512
                        first = True
                        if rel > 0:
                            self.mm(ps[:, 0:rel * 128], KTp[m][0:68, ksl], QT[m][0:68, q0:q0 + rel * 128], first, True, [KTp[m], QT[m]], [ps])
                            first = False
                        self.mm(ps[:, rel * 128:(rel + 1) * 128], KTp[m][0:64, ksl], QT[m][0:64, q0 + rel * 128:q0 + (rel + 1) * 128], first, True, [KTp[m], QT[m]], [ps])
                        if rel < 3:
                            self.mm(ps[:, (rel + 1) * 128:512], KTm[m][0:68, ksl], QT[m][0:68, q0 + (rel + 1) * 128:q0 + 512], False, True, [KTm[m], QT[m]], [ps])
                        p.op("dve", lambda e, ps=ps, rel=rel, slope=slope: e.scalar_tensor_tensor(
                            out=ps[:, rel * 128:(rel + 1) * 128], in0=absd[:, :], scalar=-slope, in1=ps[:, rel * 128:(rel + 1) * 128], op0=ALU.mult, op1=ALU.add),
                            reads=[absd, ps], writes=[ps])

                def st_E(i):
                    ps = stbank[i]
                    pt = PT[i % 4]
                    ptbuf[i] = pt
                    p.op("act", lambda e, ps=ps, pt=pt: e.activation(out=pt[:, :], in_=ps[:, :], func=AF.Exp), reads=[ps], writes=[pt])

                def st_P(i):
                    m, kt = stages[i]
                    pt = ptbuf[i]
                    OB = (self.PS[4 + 2 * m], self.PS[5 + 2 * m])
                    for j in range(4):
                        ob = OB[j // 2]
                        oc = (j % 2) * 256
                        self.mm(ob[:, oc:oc + 129], pt[:, j * 128:(j + 1) * 128], V[:, kt, :], (kt == 0 and j % 2 == 0), kt == NT - 1, [pt, V], [ob])
                    if kt == NT - 1:
                        for j in range(4):
                            ob = OB[j // 2]
                            oc = (j % 2) * 256
                            p.op("dve", lambda e, ob=ob, oc=oc, j=j: e.reciprocal(out=rs[:, j:j + 1], in_=ob[:, oc + 128:oc + 129]), reads=[ob], writes=[rs])
                            p.op("dve", lambda e, ob=ob, oc=oc, j=j, m=m: e.tensor_scalar(on[m][:, j, :], ob[:, oc:oc + 128], rs[:, j:j + 1], None, ALU.mult), reads=[ob, rs], writes=[on[m]])

                NS = len(stages)
                st_S(0)
                st_S(1)
                for i in range(NS):
                    st_E(i)
                    if i + 2 < NS:
                        st_S(i + 2)
                    st_P(i)
                p.op("dve", lambda e: e.scalar_tensor_tensor(out=diff[:, :, :], in0=on[1][:, :, :], scalar=lsm[:, 5:6], in1=on[0][:, :, :], op0=ALU.mult, op1=ALU.add),
                     reads=[on[0], on[1], lsm], writes=[diff])
                p.op("pool", lambda e: e.tensor_tensor(out=sq[:, :, :], in0=diff[:, :, :], in1=diff[:, :, :], op=ALU.mult), reads=[diff], writes=[sq])
                p.op("dve", lambda e: e.tensor_reduce(out=ss[:, :], in_=sq[:, :, :], axis=AX.X, op=ALU.add), reads=[sq], writes=[ss])
                self.rsqrt(rstd, rstd[:, :], ss, ss[:, :], 1.0 / 128.0)
                for j in range(4):
                    p.op("dve", lambda e, j=j: e.scalar_tensor_tensor(out=oo[:, j, :], in0=diff[:, j, :], scalar=rstd[:, j:j + 1], in1=gA[:, :], op0=ALU.mult, op1=ALU.mult),
                         reads=[diff, rstd, gA], writes=[oo])
                ps = self.bank(0, 4)
                for j in range(4):
                    p.op("pe", lambda e, ps=ps, j=j: e.transpose(ps[:, j * 128:(j + 1) * 128], oo[:, j, :], self.identf[:, :]), reads=[oo, self.identf], writes=[ps])
                p.op("act", lambda e, ps=ps, Q=Q, oa=oa: e.copy(out=oa[:, Q * 512:(Q + 1) * 512], in_=ps[:, :]), reads=[ps], writes=[oa])
            p.dma("pool", self.oT[0, h, :, :], oa[:, :], reads=[oa], writes=[self.oT.bs[h]])
        p.barrier()
        st.close()

    def phase_merge1(self, l):
        for dh in range(2):
            self._merge1_dh(l, dh)

    def _merge1_dh(self, l, dh):
        p = self.p
        W = self.W
        xT = self.xT
        if True:
            st = ExitStack()
            stg = [p.sb(f"m_stg{i}", [128, 2048], F32, st) for i in range(2)]
            wg = p.sb("m_wg", [128, 8, 1536], BF16, st, nb=3)
            wup = p.sb("m_wup", [128, 12, 512], BF16, st, nb=3)
            mb = p.sb("m_mb", [128, 3, 512], F32, st)
            ot = [p.sb(f"m_ot{i}", [128, 12, 512], BF16, st) for i in range(2)]
            sg = [p.sb(f"m_sg{i}", [128, 512], F32, st) for i in range(2)]
            acc = [p.sb(f"m_acc{i}", [128, 512], F32, st) for i in range(2)]
            tmp = [p.sb(f"m_tmp{i}", [128, 512], F32, st) for i in range(2)]
            mtb = [p.sb(f"m_mtb{i}", [128, 4, 512], BF16, st) for i in range(2)]
            d0 = dh * 512
            for n in range(3):
                c0 = COL["gate"] + n * 1024 + d0
                self.wload(stg, wg, wg[:, :, n * 512:(n + 1) * 512], W["w_in"][l, :, c0:c0 + 512], 8, 512, wbuf=wg.bs[n])
                self.wload(stg, wup, wup[:, n * 4:(n + 1) * 4, :], W["merge_w_up"][l, n, :, d0:d0 + 512], 4, 512, wbuf=wup.bs[n])
            p.dma("sp", mb[:], W["merge_b"][l:l + 1, :, d0:d0 + 512].partition_broadcast(128), writes=[mb])
            def stage_A(k):
                tb, tt = k // 4, k % 4
                t = k
                o = ot[tb % 2]
                if tt == 0:
                    p.dma("sp", o[:], self.oT[:, :, :, tb * 512:(tb + 1) * 512].rearrange("n c p t -> p (n c) t"), reads=self.oT.bs, writes=[o])
                a = acc[k % 2]
                for n in range(3):
                    g = sg[(k * 3 + n) % 2]
                    psg = self.bank(0, 3)
                    for c in range(8):
                        self.mm(psg[:, :], xT[:, c, t * 128:(t + 1) * 128], wg[:, c, n * 512:(n + 1) * 512], c == 0, c == 7, [xT.bs[t], wg.bs[n]], [psg])
                    p.op("dve", lambda e, g=g, psg=psg, n=n, mb=mb: e.tensor_tensor(out=g[:, :], in0=psg[:, :], in1=mb[:, n, :], op=ALU.add), reads=[psg, mb], writes=[g])
                    p.op("act", lambda e, g=g: e.activation(out=g[:, :], in_=g[:, :], func=AF.Sigmoid), reads=[g], writes=[g])
                    psu = self.bank(3, 6)
                    for c in range(4):
                        self.mm(psu[:, :], o[:, n * 4 + c, tt * 128:(tt + 1) * 128], wup[:, n * 4 + c, :], c == 0, c == 3, [o, wup.bs[n]], [psu])
                    if n == 0:
                        p.op("dve", lambda e, a=a, g=g, psu=psu: e.tensor_tensor(out=a[:, :], in0=g[:, :], in1=psu[:, :], op=ALU.mult), reads=[g, psu], writes=[a])
                    else:
                        tm = tmp[n % 2]
                        p.op("dve", lambda e, tm=tm, g=g, psu=psu: e.tensor_tensor(out=tm[:, :], in0=g[:, :], in1=psu[:, :], op=ALU.mult), reads=[g, psu], writes=[tm])
                        p.op("dve", lambda e, tm=tm, a=a: e.tensor_tensor(out=a[:, :], in0=a[:, :], in1=tm[:, :], op=ALU.add), reads=[a, tm], writes=[a])

            def stage_B(k):
                tb, tt = k // 4, k % 4
                a = acc[k % 2]
                mt = mtb[tb % 2]
                pst = self.bank(6, 8)
                for j in range(4):
                    p.op("pe", lambda e, pst=pst, j=j, a=a: e.transpose(pst[:, j * 128:(j + 1) * 128], a[:, j * 128:(j + 1) * 128], self.identf[:, :]), reads=[a, self.identf], writes=[pst])
                p.op("act", lambda e, pst=pst, mt=mt, tt=tt: e.copy(out=mt[:, :, tt * 128:(tt + 1) * 128], in_=pst[:, :].rearrange("p (j n) -> p j n", j=4)), reads=[pst], writes=[mt])
                if tt == 3:
                    p.dma("pool", self.mT[:, dh * 4:(dh + 1) * 4, tb * 512:(tb + 1) * 512], mt[:, :, :], reads=[mt], writes=[self.mT.bs[dh * 8 + tb]])

            stage_A(0)
            for k in range(NT):
                if k + 1 < NT:
                    stage_A(k + 1)
                stage_B(k)
            p.barrier()
            st.close()

    def phase_merge2(self, l):
        p = self.p
        W = self.W
        st = ExitStack()
        stg = [p.sb(f"n_stg{i}", [128, 2048], F32, st) for i in range(2)]
        wo = p.sb("n_wo", [128, 8, 1024], BF16, st)
        rw = p.sb("n_rw", [128, 8, 16], F32, st)
        rb = p.sb("n_rb", [128, 16], F32, st)
        mtl = [p.sb(f"n_mt{i}", [128, 8, 512], BF16, st) for i in range(2)]
        xr = [p.sb(f"n_xr{i}", [128, D], F32, st) for i in range(2)]
        z = [p.sb(f"n_z{i}", [128, D], F32, st) for i in range(2)]
        xT32 = p.sb("n_xT32", [128, 8, 128], F32, st)
        stats = p.sb("n_stats", [128, 2, 6], F32, st)
        mv = p.sb("n_mv", [128, 2], F32, st)
        rstd = p.sb("n_rstd", [128, 1], F32, st)
        R = {k: p.sb("n_r_" + k, shp, F32, st) for k, shp in dict(sc=[128, 16], bi=[128, 16], m1=[128, 4], eq=[128, 16], t2=[128, 16], m2=[128, 4],
                                                                  gs=[128, 4], gm=[128, 1], gsel=[128, 4], ge=[128, 16], w=[128, 16], ws=[128, 1]).items()}
        for h2 in range(2):
            self.wload(stg, wo, wo[:, :, h2 * 512:(h2 + 1) * 512], W["w_out"][l, :, h2 * 512:(h2 + 1) * 512], 8, 512)
        p.dma("sp", rw[:], W["router_w"].t.rearrange("(c p) n -> p c n", p=128), writes=[rw])
        p.dma("sp", rb[:], W["router_bias"].t.rearrange("(o n) -> o n", o=1).partition_broadcast(128), writes=[rb])
        self.load_ln_params(W["ln1_g"][l:l + 1, :], W["ln1_b"][l:l + 1, :], st)
        def stage_A(t):
            tb, tt = t // 4, t % 4
            mt = mtl[tb % 2]
            if tt == 0:
                p.dma("sp", mt[:], self.mT[:, :, tb * 512:(tb + 1) * 512], reads=[self.mT.bs[tb], self.mT.bs[8 + tb]], writes=[mt])
            x_ = xr[t % 2]
            z_ = z[t % 2]
            p.dma("sp", x_[:], self.xres[t * 128:(t + 1) * 128, :], reads=[self.xres.bs[t]], writes=[x_])
            for h2 in range(2):
                ps = self.bank(0, 4)
                for c in range(8):
                    self.mm(ps[:, :], mt[:, c, tt * 128:(tt + 1) * 128], wo[:, c, h2 * 512:(h2 + 1) * 512], c == 0, c == 7, [mt, wo], [ps])
                p.op("dve", lambda e, ps=ps, x_=x_, z_=z_, h2=h2: e.scalar_tensor_tensor(out=z_[:, h2 * 512:(h2 + 1) * 512], in0=x_[:, h2 * 512:(h2 + 1) * 512], scalar=ALPHA,
                                                                                in1=ps[:, :], op0=ALU.mult, op1=ALU.add), reads=[ps, x_], writes=[z_])
            self.ln_tile(z_, z_, stats, mv, rstd)
            p.dma("pool", self.xres[t * 128:(t + 1) * 128, :], z_[:], reads=[z_], writes=[self.xres.bs[t]])

        def stage_B(t):
            self.to_xT(z[t % 2], t, xT32=xT32)
            self.router(t, xT32, rw, rb, R)

        stage_A(0)
        for t in range(NT):
            if t + 1 < NT:
                stage_A(t + 1)
            stage_B(t)
        p.barrier()
        st.close()

    def router(self, t, xT32, rw, rb, R):
        p = self.p
        ps = self.bank(4, 8)
        for c in range(8):
            self.mm(ps[:, 0:16], xT32[:, c, :], rw[:, c, :], c == 0, c == 7, [xT32, rw], [ps])
        sc, bi, m1, eq, t2, m2, gs, gm, gsel, ge, w, ws = (R[k] for k in ("sc", "bi", "m1", "eq", "t2", "m2", "gs", "gm", "gsel", "ge", "w", "ws"))
        v3 = lambda tl: tl[:, :].rearrange("p (g e) -> p g e", g=4)
        b3 = lambda tl: tl[:, :].unsqueeze(2).to_broadcast([128, 4, 4])
        p.op("act", lambda e: e.activation(out=sc[:, :], in_=ps[:, 0:16], func=AF.Sigmoid), reads=[ps], writes=[sc])
        p.op("dve", lambda e: e.tensor_tensor(out=bi[:, :], in0=sc[:, :], in1=rb[:, :], op=ALU.add), reads=[sc, rb], writes=[bi])
        p.op("dve", lambda e: e.tensor_reduce(out=m1[:, :], in_=v3(bi), axis=AX.X, op=ALU.max), reads=[bi], writes=[m1])
        p.op("dve", lambda e: e.tensor_tensor(out=v3(eq), in0=v3(bi), in1=b3(m1), op=ALU.is_equal), reads=[bi, m1], writes=[eq])
        p.op("dve", lambda e: e.scalar_tensor_tensor(out=t2[:, :], in0=eq[:, :], scalar=-1e30, in1=bi[:, :], op0=ALU.mult, op1=ALU.add), reads=[eq, bi], writes=[t2])
        p.op("dve", lambda e: e.tensor_reduce(out=m2[:, :], in_=v3(t2), axis=AX.X, op=ALU.max), reads=[t2], writes=[m2])
        p.op("dve", lambda e: e.tensor_tensor(out=gs[:, :], in0=m1[:, :], in1=m2[:, :], op=ALU.add), reads=[m1, m2], writes=[gs])
        p.op("dve", lambda e: e.tensor_reduce(out=gm[:, :], in_=gs[:, :], axis=AX.X, op=ALU.max), reads=[gs], writes=[gm])
        p.op("dve", lambda e: e.tensor_scalar(gsel[:, :], gs[:, :], gm[:, 0:1], None, ALU.is_equal), reads=[gs, gm], writes=[gsel])
        p.op("dve", lambda e: e.tensor_tensor(out=v3(ge), in0=v3(bi), in1=b3(m2), op=ALU.is_ge), reads=[bi, m2], writes=[ge])
        p.op("dve", lambda e: e.tensor_tensor(out=v3(ge), in0=v3(ge), in1=b3(gsel), op=ALU.mult), reads=[ge, gsel], writes=[ge])
        p.op("dve", lambda e: e.tensor_tensor(out=w[:, :], in0=ge[:, :], in1=sc[:, :], op=ALU.mult), reads=[ge, sc], writes=[w])
        p.op("dve", lambda e: e.tensor_reduce(out=ws[:, :], in_=w[:, :], axis=AX.X, op=ALU.add), reads=[w], writes=[ws])
        p.op("dve", lambda e: e.reciprocal(out=ws[:, :], in_=ws[:, :]), reads=[ws], writes=[ws])
        p.op("dve", lambda e: e.tensor_scalar(self.call[:, t, :], w[:, :], ws[:, 0:1], None, ALU.mult), reads=[w, ws], writes=[self.call.bs[t]])

    def phase_moe(self, l, last):
        p = self.p
        W = self.W
        xT = self.xT
        st = ExitStack()
        stg = [p.sb(f"e_stg{i}", [128, 2048], F32, st) for i in range(2)]
        wg = [p.sb(f"e_wg{i}", [128, 8, 512], BF16, st) for i in range(2)]
        wu = [p.sb(f"e_wu{i}", [128, 8, 512], BF16, st) for i in range(2)]
        wd = [p.sb(f"e_wd{i}", [128, 4, 1024], BF16, st) for i in range(2)]
        yacc = p.sb("e_yacc", [128, 8, D], F32, st, nb=8)
        hT = [p.sb(f"e_hT{i}", [128, 4, 512], BF16, st) for i in range(2)]
        sgl = [p.sb(f"e_sg{i}", [128, 512], F32, st) for i in range(2)]
        xr = [p.sb(f"e_xr{i}", [128, D], F32, st) for i in range(1)]
        stats = p.sb("e_stats", [128, 2, 6], F32, st)
        mv = p.sb("e_mv", [128, 2], F32, st)
        rstd = p.sb("e_rstd", [128, 1], F32, st)
        self.load_ln_params(W["ln2_g"][l:l + 1, :], W["ln2_b"][l:l + 1, :], st)
        kk = [0]

        def wl(ex):
            g_, u_, d_ = wg[ex % 2], wu[ex % 2], wd[ex % 2]
            for h2 in range(2):
                self.wload(stg, g_, g_[:, h2 * 4:(h2 + 1) * 4, :], W["moe_w_gate"][l, ex, h2 * 512:(h2 + 1) * 512, :], 4, 512, cast=("pool", "dve")[h2])
                self.wload(stg, u_, u_[:, h2 * 4:(h2 + 1) * 4, :], W["moe_w_up"][l, ex, h2 * 512:(h2 + 1) * 512, :], 4, 512, cast=("pool", "dve")[h2])
                self.wload(stg, d_, d_[:, :, h2 * 512:(h2 + 1) * 512], W["moe_w_down"][l, ex, :, h2 * 512:(h2 + 1) * 512], 4, 512, cast=("pool", "dve")[h2])

        def gu(q4, ex, tb2):
            g_, u_ = wg[ex % 2], wu[ex % 2]
            tb = q4 * 2 + tb2
            h_ = hT[kk[0] % 2]
            kk[0] += 1
            xr_ = [xT.bs[tb * 4 + i] for i in range(4)]
            for fc in range(4):
                pg = self.bank(0, 2)
                for c in range(8):
                    self.mm(pg[:, :], g_[:, c, fc * 128:(fc + 1) * 128], xT[:, c, tb * 512:(tb + 1) * 512], c == 0, c == 7, [g_] + xr_, [pg])
                pu = self.bank(2, 4)
                for c in range(8):
                    self.mm(pu[:, :], u_[:, c, fc * 128:(fc + 1) * 128], xT[:, c, tb * 512:(tb + 1) * 512], c == 0, c == 7, [u_] + xr_, [pu])
                s_ = sgl[fc % 2]
                p.op("act", lambda e, s_=s_, pg=pg: e.activation(out=s_[:, :], in_=pg[:, :], func=AF.Silu), reads=[pg], writes=[s_])
                p.op("dve", lambda e, s_=s_, pu=pu, h_=h_, fc=fc: e.tensor_tensor(out=h_[:, fc, :], in0=s_[:, :], in1=pu[:, :], op=ALU.mult), reads=[s_, pu], writes=[h_])
            return h_

        def down(q4, ex, tb2, h_):
            d_ = wd[ex % 2]
            tb = q4 * 2 + tb2
            for tt in range(4):
                t = tb * 4 + tt
                tl = tb2 * 4 + tt
                for h2 in range(2):
                    py = self.bank(4, 8)
                    for fc in range(4):
                        self.mm(py[:, :], h_[:, fc, tt * 128:(tt + 1) * 128], d_[:, fc, h2 * 512:(h2 + 1) * 512], fc == 0, fc == 3, [h_, d_], [py])
                    ya = yacc[:, tl, h2 * 512:(h2 + 1) * 512]
                    if ex == 0:
                        p.op("dve", lambda e, ya=ya, py=py, t=t, ex=ex: e.tensor_scalar(ya, py[:, :], self.call[:, t, ex:ex + 1], None, ALU.mult),
                             reads=[py, self.call.bs[t]], writes=[yacc.bs[tl]])
                    else:
                        p.op("dve", lambda e, ya=ya, py=py, t=t, ex=ex: e.scalar_tensor_tensor(out=ya, in0=py[:, :], scalar=self.call[:, t, ex:ex + 1], in1=ya, op0=ALU.mult, op1=ALU.add),
                             reads=[py, self.call.bs[t]], writes=[yacc.bs[tl]])

        def ln2(q4):
            for tl in range(8):
                t = q4 * 8 + tl
                x_ = xr[0]
                yv = Sub(yacc[:, tl, :], yacc.bs[tl])
                p.dma("sp", x_[:], self.xres[t * 128:(t + 1) * 128, :], reads=[self.xres.bs[t]], writes=[x_])
                p.op("dve", lambda e, x_=x_, tl=tl: e.scalar_tensor_tensor(out=yacc[:, tl, :], in0=x_[:, :], scalar=ALPHA, in1=yacc[:, tl, :], op0=ALU.mult, op1=ALU.add),
                     reads=[x_, yacc.bs[tl]], writes=[yacc.bs[tl]])
                self.ln_tile(yv, yv, stats, mv, rstd)
                if last:
                    p.dma("pool", self.out[t * 128:(t + 1) * 128, :], yv[:, :], reads=[yv], writes=[self.out.bs[t]])
                else:
                    p.dma("pool", self.xres[t * 128:(t + 1) * 128, :], yv[:, :], reads=[yv], writes=[self.xres.bs[t]])
                    self.to_xT(yv, t)

        pre = None
        for q4 in range(4):
            for ex in range(16):
                if ex == 0 and pre is not None:
                    for tb2 in range(2):
                        down(q4, 0, tb2, pre[tb2])
                    pre = None
                else:
                    wl(ex)
                    for tb2 in range(2):
                        h_ = gu(q4, ex, tb2)
                        down(q4, ex, tb2, h_)
            if q4 < 3:
                wl(0)
                pre = [gu(q4 + 1, 0, 0), gu(q4 + 1, 0, 1)]
            ln2(q4)
        p.barrier()
        st.close()

    def phase_gla(self, l):
        for hp in range(2):
            self._gla_hp(l, hp)

    def _gla_hp(self, l, hp):
        p = self.p
        W = self.W
        xT = self.xT
        xTr = list(xT.bs)
        if True:
            st = ExitStack()
            stg = [p.sb(f"g_stg{i}", [128, 1024], F32, st) for i in range(2)]
            wq = p.sb("g_wq", [128, 8, 128], BF16, st)
            wk = p.sb("g_wk", [128, 8, 128], BF16, st)
            wv = p.sb("g_wv", [128, 8, 256], BF16, st)
            wr = p.sb("g_wr", [128, 8, 256], BF16, st)
            wz = p.sb("g_wz", [128, 8, 32], BF16, st)
            wgf = p.sb("g_wgf", [16, 2, 128], F32, st)
            wgt = p.sb("g_wgt", [16, 2, 128], BF16, st)
            bg = p.sb("g_bg", [128, 2], F32, st)
            gG = p.sb("g_gG", [128, 128], F32, st)
            ones = p.sb("g_ones", [128, 1], F32, st)
            m4 = p.sb("g_m4", [128, 4, 64], F32, st)
            msk = p.sb("g_msk", [128, 8, 64], F32, st)
            qA1 = p.sb("g_qA1", [128, S], BF16, st)
            kA1 = p.sb("g_kA1", [128, S], BF16, st)
            qi1 = p.sb("g_qi1", [128, S], BF16, st)
            qA0 = [p.sb(f"g_qA0{i}", [128, 512], BF16, st) for i in range(2)]
            kA0 = [p.sb(f"g_kA0{i}", [128, 512], BF16, st) for i in range(2)]
            qi0 = [p.sb(f"g_qi0{i}", [128, 512], BF16, st) for i in range(2)]
            dec = [p.sb(f"g_dec{d}", [128, 64], F32, st) for d in range(2)]
            sts1 = p.sb("g_st1", [128, 64, 128], BF16, st)
            sts0 = [p.sb(f"g_st0{i}", [128, 8, 128], BF16, st) for i in range(2)]
            S32 = [p.sb(f"g_S32{d}", [128, 128], F32, st) for d in range(2)]
            v = p.sb("g_v", [128, NT, 256], BF16, st)
            zt = p.sb("g_zt", [16, 512], BF16, st)
            T1 = p.sb("g_T1", [128, 512], F32, st)
            T2 = p.sb("g_T2", [128, 512], F32, st)
            T3 = p.sb("g_T3", [128, 512], F32, st)
            T4 = p.sb("g_T4", [128, 512], F32, st)
            T5 = p.sb("g_T5", [128, 512], F32, st)
            kltr = [p.sb(f"g_klt{i}", [128, 4, 128], BF16, st) for i in range(2)]
            scT = [p.sb(f"g_scT{i}", [128, 4, 64], BF16, st) for i in range(2)]
            sr = p.sb("g_sr", [128, 256], F32, st)
            sq = p.sb("g_sq", [128, 2, 128], F32, st)
            ssq = p.sb("g_ssq", [128, 2], F32, st)
            oc = p.sb("g_oc", [128, 256], F32, st)
            ocT = [p.sb(f"g_ocT{i}", [128, 2, 512], BF16, st) for i in range(2)]
            c0 = COL["qc"] + hp * 128
            self.wload(stg, wq, wq[:, :, :], W["w_in"][l, :, c0:c0 + 128], 8, 128)
            p.op("pool", lambda e: e.tensor_scalar(wq[:, :, :], wq[:, :, :], 0.125, None, ALU.mult), reads=[wq], writes=[wq])
            c0 = COL["kc"] + hp * 128
            self.wload(stg, wk, wk[:, :, :], W["w_in"][l, :, c0:c0 + 128], 8, 128)
            for k2 in range(2):
                c0 = COL["vc"] + hp * 256 + k2 * 128
                self.wload(stg, wv, wv[:, :, k2 * 128:(k2 + 1) * 128], W["w_in"][l, :, c0:c0 + 128], 8, 128)
                c0 = COL["rc"] + hp * 256 + k2 * 128
                self.wload(stg, wr, wr[:, :, k2 * 128:(k2 + 1) * 128], W["w_in"][l, :, c0:c0 + 128], 8, 128)
            self.wload(stg, wz, wz[:, :, :], W["w_in"][l, :, COL["zf"]:COL["zf"] + 32], 8, 32)
            p.dma("sp", wgf[:], W["gla_w_gate"][l, :, :, hp * 128:(hp + 1) * 128].rearrange("d r n -> r d n"), writes=[wgf])
            p.op("dve", lambda e: e.tensor_copy(out=wgt[:, :, :], in_=wgf[:, :, :]), reads=[wgf], writes=[wgt])
            p.dma("sp", bg[:], W["gla_b_gate"][l, :, hp * 128:(hp + 1) * 128].rearrange("d n -> n d"), writes=[bg], allow_slow_non_contiguous=True)
            p.dma("sp", gG[:], W["gla_norm_g"][l:l + 1, :].partition_broadcast(128), writes=[gG])
            p.op("pool", lambda e: e.memset(ones[:, :], 1.0), writes=[ones])
            for d in range(2):
                for hh in range(2):
                    for half in range(2):
                        p.dma("sp", m4[half * 64:(half + 1) * 64, d * 2 + hh, :], self.C["gmask"][d, :, :], writes=[m4])
            p.op("pool", lambda e: e.memset(msk[:, :, :], 1.0), writes=[msk])
            p.op("pool", lambda e: e.memset(msk[:, :, 0:1], 0.0), reads=[msk], writes=[msk])
            for t in range(NT):
                ps = self.bank(0, 4)
                for c in range(8):
                    self.mm(ps[:, 0:256], xT[:, c, t * 128:(t + 1) * 128], wv[:, c, :], c == 0, c == 7, [wv, xTr[t]], [ps])
                p.op("act", lambda e, ps=ps, t=t: e.copy(out=v[:, t, :], in_=ps[:, 0:256]), reads=[ps], writes=[v])
            def arr(d, tb):
                if d == 1:
                    sl = slice(tb * 512, (tb + 1) * 512)
                    return (qA1, qA1[:, sl]), (kA1, kA1[:, sl]), (qi1, qi1[:, sl])
                i = tb % 2
                return (qA0[i], qA0[i][:, :]), (kA0[i], kA0[i][:, :]), (qi0[i], qi0[i][:, :])

            def chunk_view(d, which, n, hh):
                hs = slice(hh * 64, (hh + 1) * 64)
                if d == 1:
                    tl = (qA1, kA1, qi1)[which]
                    return tl, tl[hs, n * 64:(n + 1) * 64]
                tl = (qA0, kA0, qi0)[which][(n // 8) % 2]
                return tl, tl[hs, (n % 8) * 64:(n % 8 + 1) * 64]

            def state_view(d, n, hh):
                hs = slice(hh * 64, (hh + 1) * 64)
                if d == 1:
                    return sts1, sts1[hs, n, :]
                tl = sts0[(n // 8) % 2]
                return tl, tl[hs, n % 8, :]

            def sweep_A(d, tb):
                klt = kltr[tb % 2]
                xr_ = xTr[tb * 4:tb * 4 + 4]
                tsl = slice(tb * 512, (tb + 1) * 512)
                (qA_t, qA_ap), (kA_t, kA_ap), (qi_t, qi_ap) = arr(d, tb)
                pz = self.bank(0, 4)
                for c in range(8):
                    self.mm(pz[0:16, :], wz[:, c, d * 16:(d + 1) * 16], xT[:, c, tsl], c == 0, c == 7, [wz] + xr_, [pz])
                p.op("act", lambda e: e.copy(out=zt[:, :], in_=pz[0:16, :]), reads=[pz], writes=[zt])
                pg = self.bank(0, 4)
                self.mm(pg[:, :], wgt[0:16, d, :], zt[0:16, :], True, True, [wgt, zt], [pg])
                pq = self.bank(4, 6)
                for c in range(8):
                    self.mm(pq[:, :], wq[:, c, :], xT[:, c, tsl], c == 0, c == 7, [wq] + xr_, [pq])
                pk = self.bank(6, 8)
                for c in range(8):
                    self.mm(pk[:, :], wk[:, c, :], xT[:, c, tsl], c == 0, c == 7, [wk] + xr_, [pk])
                p.op("dve", lambda e: e.tensor_scalar(T1[:, :], pg[:, :], bg[:, d:d + 1], None, ALU.add), reads=[pg, bg], writes=[T1])
                p.op("dve", lambda e: e.scalar_tensor_tensor(out=T2[:, :], in0=T1[:, :], scalar=-1.0, in1=T1[:, :], op0=ALU.mult, op1=ALU.max), reads=[T1], writes=[T2])
                p.op("act", lambda e: e.activation(out=T2[:, :], in_=T2[:, :], func=AF.Exp, scale=-1.0), reads=[T2], writes=[T2])
                p.op("act", lambda e: e.activation(out=T2[:, :], in_=T2[:, :], func=AF.Ln, bias=ones[:, 0:1]), reads=[T2, ones], writes=[T2])
                p.op("dve", lambda e: e.scalar_tensor_tensor(out=T1[:, :], in0=T1[:, :], scalar=0.0, in1=T2[:, :], op0=ALU.min, op1=ALU.subtract), reads=[T1, T2], writes=[T1])
                p.op("act", lambda e: e.mul(out=T1[:, :], in_=T1[:, :], mul=1.0 / 16.0), reads=[T1], writes=[T1])
                p.op("dve", lambda e: e.tensor_tensor_scan(T3[:, :], msk[:, :, :].rearrange("p a b -> p (a b)"), T1[:, :], 0.0, ALU.mult, ALU.add), reads=[msk, T1], writes=[T3])
                c3 = T3[:, :].rearrange("p (a b) -> p a b", a=8)
                v4 = lambda tl: tl[:, :].rearrange("p (a b) -> p a b", a=8)
                if d == 1:
                    p.op("dve", lambda e: e.tensor_tensor(out=v4(T2), in0=c3[:, :, 63:64].to_broadcast([128, 8, 64]), in1=c3, op=ALU.subtract), reads=[T3], writes=[T2])
                    p.op("dve", lambda e: e.tensor_tensor(out=T3[:, :], in0=T2[:, :], in1=T1[:, :], op=ALU.add), reads=[T2, T1], writes=[T3])
                    ref, last = c3[:, :, 31:32], c3[:, :, 0:1]
                else:
                    ref, last = c3[:, :, 32:33], c3[:, :, 63:64]
                refb = ref.to_broadcast([128, 8, 64])
                lastb = last.to_broadcast([128, 8, 64])
                p.op("act", lambda e: e.activation(out=dec[d][:, tb * 8:(tb + 1) * 8].unsqueeze(2), in_=last, func=AF.Exp), reads=[T3], writes=[dec[d]])
                p.op("dve", lambda e: e.tensor_tensor(out=v4(T4), in0=c3, in1=refb, op=ALU.subtract), reads=[T3], writes=[T4])
                p.op("act", lambda e: e.activation(out=T5[:, :], in_=T4[:, :], func=AF.Exp), reads=[T4], writes=[T5])
                p.op("dve", lambda e: e.tensor_tensor(out=qA_ap, in0=pq[:, :], in1=T5[:, :], op=ALU.mult), reads=[pq, T5], writes=[qA_t])
                p.op("act", lambda e: e.activation(out=T5[:, :], in_=T4[:, :], func=AF.Exp, scale=-1.0), reads=[T4], writes=[T5])
                p.op("dve", lambda e: e.tensor_tensor(out=kA_ap, in0=pk[:, :], in1=T5[:, :], op=ALU.mult), reads=[pk, T5], writes=[kA_t])
                p.op("act", lambda e: e.activation(out=T5[:, :], in_=T3[:, :], func=AF.Exp), reads=[T3], writes=[T5])
                p.op("dve", lambda e: e.tensor_tensor(out=qi_ap, in0=pq[:, :], in1=T5[:, :], op=ALU.mult), reads=[pq, T5], writes=[qi_t])
                p.op("dve", lambda e: e.tensor_tensor(out=v4(T4), in0=lastb, in1=c3, op=ALU.subtract), reads=[T3], writes=[T4])
                p.op("act", lambda e: e.activation(out=T5[:, :], in_=T4[:, :], func=AF.Exp), reads=[T4], writes=[T5])
                p.op("dve", lambda e: e.tensor_tensor(out=T4[:, :], in0=pk[:, :], in1=T5[:, :], op=ALU.mult), reads=[pk, T5], writes=[T4])
                pt = self.bank(0, 4)
                for j in range(4):
                    p.op("pe", lambda e, j=j: e.transpose(pt[:, j * 128:(j + 1) * 128], T4[:, j * 128:(j + 1) * 128], self.identf[:, :]), reads=[T4, self.identf], writes=[pt])
                p.op("act", lambda e: e.copy(out=klt[:, :, :], in_=pt[:, :].rearrange("p (j n) -> p j n", j=4)), reads=[pt], writes=[klt])

            def sweep_B(d, tb):
                klt = kltr[tb % 2]
                cs = range(8) if d == 0 else range(7, -1, -1)
                for ci in cs:
                    n = tb * 8 + ci
                    tt, half = ci // 2, ci % 2
                    t = tb * 4 + tt
                    stl, _ = state_view(d, n, 0)
                    sap = sts1[:, n, :] if d == 1 else stl[:, n % 8, :]
                    p.op("act", lambda e, sap=sap: e.copy(out=sap, in_=S32[d][:, :]), reads=[S32[d]], writes=[stl])
                    pd = self.bank(0, 4)
                    for hh in range(2):
                        self.mm(pd[hh * 64:(hh + 1) * 64, 0:128], klt[half * 64:(half + 1) * 64, tt, hh * 64:(hh + 1) * 64],
                                v[half * 64:(half + 1) * 64, t, hh * 128:(hh + 1) * 128], True, True, [klt, v], [pd], serial=True)
                    p.op("dve", lambda e, pd=pd, n=n: e.scalar_tensor_tensor(out=S32[d][:, :], in0=S32[d][:, :], scalar=dec[d][:, n:n + 1], in1=pd[:, 0:128], op0=ALU.mult, op1=ALU.add),
                         reads=[pd, dec[d], S32[d]], writes=[S32[d]])

            def out_block(tb):
                ot_ = ocT[tb % 2]
                for tt in range(4):
                    t = tb * 4 + tt
                    pss = self.bank(0, 2)
                    for half in range(2):
                        n = t * 2 + half
                        first = True
                        for d in range(2):
                            for hh in range(2):
                                kt_, kap = chunk_view(d, 1, n, hh)
                                qt_, qap = chunk_view(d, 0, n, hh)
                                self.mm(pss[half * 64:(half + 1) * 64, (d * 2 + hh) * 64:(d * 2 + hh + 1) * 64], kap, qap, first, True, [kt_, qt_], [pss], serial=True)
                                first = False
                    sc_ = scT[t % 2]
                    p.op("dve", lambda e, pss=pss, sc_=sc_: e.tensor_tensor(out=sc_[:, :, :], in0=pss[:, 0:256].rearrange("p (a b) -> p a b", a=4), in1=m4[:, :, :], op=ALU.mult), reads=[pss, m4], writes=[sc_])
                    po = self.bank(2, 4)
                    for half in range(2):
                        n = t * 2 + half
                        hs = slice(half * 64, (half + 1) * 64)
                        first = True
                        for hh in range(2):
                            osl = po[hs, hh * 128:(hh + 1) * 128]
                            for d in range(2):
                                self.mm(osl, sc_[hs, d * 2 + hh, :], v[hs, t, hh * 128:(hh + 1) * 128], first, False, [sc_, v], [po], serial=True)
                                first = False
                            for d in range(2):
                                it_, iap = chunk_view(d, 2, n, hh)
                                st_, sap = state_view(d, n, hh)
                                self.mm(osl, iap, sap, False, d == 1, [it_, st_], [po], serial=True)
                    pr = self.bank(4, 8)
                    for c in range(8):
                        self.mm(pr[:, 0:256], xT[:, c, t * 128:(t + 1) * 128], wr[:, c, :], c == 0, c == 7, [wr, xTr[t]], [pr])
                    p.op("act", lambda e, pr=pr: e.activation(out=sr[:, :], in_=pr[:, 0:256], func=AF.Silu), reads=[pr], writes=[sr])
                    po3 = po[:, 0:256].rearrange("p (a b) -> p a b", a=2)
                    p.op("act", lambda e, po3=po3: e.activation(out=sq[:, :, :], in_=po3, func=AF.Square), reads=[po], writes=[sq])
                    p.op("dve", lambda e: e.tensor_reduce(out=ssq[:, :], in_=sq[:, :, :], axis=AX.X, op=ALU.add), reads=[sq], writes=[ssq])
                    self.rsqrt(ssq, ssq[:, :], ssq, ssq[:, :], 1.0 / 128.0)
                    for hh in range(2):
                        p.op("dve", lambda e, po=po, hh=hh: e.scalar_tensor_tensor(out=oc[:, hh * 128:(hh + 1) * 128], in0=po[:, hh * 128:(hh + 1) * 128], scalar=ssq[:, hh:hh + 1], in1=gG[:, :],
                                                                                  op0=ALU.mult, op1=ALU.mult), reads=[po, ssq, gG], writes=[oc])
                    p.op("dve", lambda e: e.tensor_tensor(out=oc[:, :], in0=oc[:, :], in1=sr[:, :], op=ALU.mult), reads=[oc, sr], writes=[oc])
                    pt = self.bank(4, 8)
                    for hh in range(2):
                        p.op("pe", lambda e, pt=pt, hh=hh: e.transpose(pt[:, hh * 128:(hh + 1) * 128], oc[:, hh * 128:(hh + 1) * 128], self.identf[:, :]), reads=[oc, self.identf], writes=[pt])
                    p.op("act", lambda e, pt=pt, tt=tt: e.copy(out=ot_[:, :, tt * 128:(tt + 1) * 128], in_=pt[:, 0:256].rearrange("p (a b) -> p a b", a=2)), reads=[pt], writes=[ot_])
                p.dma("pool", self.oT[2, hp * 2:(hp + 1) * 2, :, tb * 512:(tb + 1) * 512].rearrange("c p t -> p c t"), ot_[:, :, :], reads=[ot_], writes=[self.oT.bs[8 + hp * 2], self.oT.bs[8 + hp * 2 + 1]])

            for d in (1, 0):
                p.op("pool", lambda e, d=d: e.memset(S32[d][:, :], 0.0), writes=[S32[d]])
            order1 = list(range(7, -1, -1))
            sweep_A(1, order1[0])
            for i, tb in enumerate(order1):
                if i + 1 < 8:
                    sweep_A(1, order1[i + 1])
                sweep_B(1, tb)
            sweep_A(0, 0)
            for tb in range(8):
                if tb + 1 < 8:
                    sweep_A(0, tb + 1)
                sweep_B(0, tb)
                out_block(tb)
            p.barrier()
            st.close()

    def phase_s5(self, l):
        p = self.p
        W = self.W
        xT = self.xT
        xTr = list(xT.bs)
        st = ExitStack()
        sm = lambda n, shp=(128, 32): p.sb("s_" + n, list(shp), F32, st)
        stg = [p.sb(f"s_stg{i}", [128, 1024], F32, st) for i in range(2)]
        identb = p.sb("s_identb", [128, 128], BF16, st)
        jx = sm("jx", (128, 128))
        rowm = sm("rowm", (128, 8))
        sgn = sm("sgn", (128, 2))
        hpi = sm("hpi", (128, 1))
        wu = p.sb("s_wu", [128, 8, 128], BF16, st)
        wglu = p.sb("s_wglu", [128, 4, 512], BF16, st)
        bglu = sm("bglu", (128, 4))
        dcol = sm("dcol", (128, 4))
        CST = [[p.sb(f"s_cst{d}{b}", [128, 128], F32, st) for b in range(4)] for d in range(2)]
        BT = [[p.sb(f"s_bt{d}{b}", [128, 128], F32, st) for b in range(4)] for d in range(2)]
        PRt = [sm(f"pr{d}", (128, 12, 32)) for d in range(2)]
        PQt = [sm(f"pq{d}", (128, 12, 32)) for d in range(2)]
        pst = ExitStack()
        smp = lambda n, shp=(128, 32): p.sb("s_" + n, list(shp), F32, pst)
        p.dma("sp", jx[:], self.C["jx"][:, :], writes=[jx])
        p.dma("sp", rowm[:], self.C["rowmask"][:, :], writes=[rowm])
        p.dma("sp", sgn[:], self.C["sgn"][:, :], writes=[sgn])
        p.op("dve", lambda e: e.tensor_copy(out=identb[:, :], in_=self.identf[:, :]), reads=[self.identf], writes=[identb])
        p.op("pool", lambda e: e.memset(hpi[:, :], PI / 2), writes=[hpi])
        for b in range(4):
            self.wload(stg, wglu, wglu[:, b:b + 1, :], W["s5_w_glu"][l, b * 128:(b + 1) * 128, :], 1, 512)
        p.dma("sp", bglu[:], W["s5_b_glu"][l, :].rearrange("(m q) -> q m", q=128), writes=[bglu], allow_slow_non_contiguous=True)
        p.dma("sp", dcol[:], W["s5_d"][l, :].rearrange("(m q) -> q m", q=128), writes=[dcol], allow_slow_non_contiguous=True)

        PR, PQ, BST = [], [], []
        Cn = smp("Cn", (128, 128))
        dv = lambda fn, r, w: p.op("dve", fn, reads=r, writes=w)
        for d in range(2):
            are, aim, dt, lr, li, m1, cc, ss, t1, t2, nr, ni, den, cr, ci = (smp(f"{n}{d}") for n in
                                                                             ("are", "aim", "dt", "lr", "li", "m1", "cc", "ss", "t1", "t2", "nr", "ni", "den", "cr", "ci"))
            Xb = smp(f"Xb{d}", (128, 32, 16))
            Yb = smp(f"Yb{d}", (128, 32, 16))
            Bst = smp(f"Bst{d}", (128, 32, 16))
            Tb = smp(f"Tb{d}", (128, 32, 16))
            pr = PRt[d]
            pq = PQt[d]
            for hf in range(2):
                hs = slice(hf * 64, (hf + 1) * 64)
                p.dma("sp", are[hs, :], W["s5_a_re"][l, d, :, :].rearrange("g q -> q g"), writes=[are], allow_slow_non_contiguous=True)
                p.dma("sp", aim[hs, :], W["s5_a_im"][l, d, :, :].rearrange("g q -> q g"), writes=[aim], allow_slow_non_contiguous=True)
                own, oth = ("s5_b_re", "s5_b_im") if hf == 0 else ("s5_b_im", "s5_b_re")
                p.dma("sp", Xb[hs, :, :], W[own][l, d, :, :, :].rearrange("g q c -> q g c"), writes=[Xb])
                p.dma("sp", Yb[hs, :, :], W[oth][l, d, :, :, :].rearrange("g q c -> q g c"), writes=[Yb])
            p.dma("sp", dt[:], W["s5_log_dt"][l, d:d + 1, :].partition_broadcast(128), writes=[dt])
            p.op("act", lambda e, dt=dt: e.activation(out=dt[:, :], in_=dt[:, :], func=AF.Exp), reads=[dt], writes=[dt])
            TT = lambda o, a, b_, op: (lambda e: e.tensor_tensor(out=o[:, :], in0=a[:, :], in1=b_[:, :], op=op))
            dv(TT(lr, are, dt, ALU.mult), [are, dt], [lr])
            dv(TT(li, aim, dt, ALU.mult), [aim, dt], [li])
            TS = lambda o, a, s1, s2, o0, o1=None: (lambda e: e.tensor_scalar(o[:, :], a[:, :], s1, s2, o0, o1) if o1 is not None else e.tensor_scalar(o[:, :], a[:, :], s1, None, o0))
            dv(TS(m1, lr, 0.25, 1.0, ALU.mult, ALU.add), [lr], [m1])
            dv(TT(m1, m1, lr, ALU.mult), [m1, lr], [m1])
            dv(TS(m1, m1, 1.0 / 3.0, 1.0, ALU.mult, ALU.add), [m1], [m1])
            dv(TT(m1, m1, lr, ALU.mult), [m1, lr], [m1])
            dv(TS(m1, m1, 0.5, 1.0, ALU.mult, ALU.add), [m1], [m1])
            dv(TT(m1, m1, lr, ALU.mult), [m1, lr], [m1])
            dv(TS(m1, m1, 1.0, None, ALU.add), [m1], [m1])
            dv(TS(nr, li, 1.0 / 256.0, None, ALU.mult), [li], [nr])
            dv(TT(t1, nr, nr, ALU.mult), [nr], [t1])
            dv(TS(ss, t1, 1.0 / 120.0, -1.0 / 6.0, ALU.mult, ALU.add), [t1], [ss])
            dv(TT(ss, ss, t1, ALU.mult), [ss, t1], [ss])
            dv(TS(ss, ss, 1.0, None, ALU.add), [ss], [ss])
            dv(TT(ss, ss, nr, ALU.mult), [ss, nr], [ss])
            dv(TS(cc, t1, -1.0 / 720.0, 1.0 / 24.0, ALU.mult, ALU.add), [t1], [cc])
            dv(TT(cc, cc, t1, ALU.mult), [cc, t1], [cc])
            dv(TS(cc, cc, -0.5, None, ALU.add), [cc], [cc])
            dv(TT(cc, cc, t1, ALU.mult), [cc, t1], [cc])
            dv(TS(cc, cc, 1.0, None, ALU.add), [cc], [cc])
            for _ in range(8):
                dv(TT(t1, cc, cc, ALU.mult), [cc], [t1])
                dv(TT(t2, ss, ss, ALU.mult), [ss], [t2])
                dv(lambda e, cc=cc, ss=ss: e.scalar_tensor_tensor(out=ss[:, :], in0=cc[:, :], scalar=2.0, in1=ss[:, :], op0=ALU.mult, op1=ALU.mult), [cc, ss], [ss])
                dv(TT(cc, t1, t2, ALU.subtract), [t1, t2], [cc])
            dv(TT(t1, cc, cc, ALU.mult), [cc], [t1])
            dv(TT(t2, ss, ss, ALU.mult), [ss], [t2])
            dv(TT(t1, t1, t2, ALU.add), [t1, t2], [t1])
            dv(TS(t1, t1, -0.5, 1.5, ALU.mult, ALU.add), [t1], [t1])
            dv(TT(cc, cc, t1, ALU.mult), [cc, t1], [cc])
            dv(TT(ss, ss, t1, ALU.mult), [ss, t1], [ss])
            dv(lambda e, pr=pr, m1=m1, cc=cc: e.tensor_tensor(out=pr[:, 0, :], in0=m1[:, :], in1=cc[:, :], op=ALU.mult), [m1, cc], [pr])
            dv(TT(ni, m1, ss, ALU.mult), [m1, ss], [ni])
            dv(lambda e, pq=pq, ni=ni: e.tensor_scalar(pq[:, 0, :], ni[:, :], sgn[:, 1:2], None, ALU.mult), [ni, sgn], [pq])
            dv(lambda e, nr=nr, pr=pr: e.tensor_scalar(nr[:, :], pr[:, 0, :], -1.0, None, ALU.add), [pr], [nr])
            dv(TT(t1, are, are, ALU.mult), [are], [t1])
            dv(TT(t2, aim, aim, ALU.mult), [aim], [t2])
            dv(TT(den, t1, t2, ALU.add), [t1, t2], [den])
            dv(lambda e, den=den: e.reciprocal(out=den[:, :], in_=den[:, :]), [den], [den])
            dv(TT(t1, nr, are, ALU.mult), [nr, are], [t1])
            dv(TT(t2, ni, aim, ALU.mult), [ni, aim], [t2])
            dv(TT(cr, t1, t2, ALU.add), [t1, t2], [cr])
            dv(TT(cr, cr, den, ALU.mult), [cr, den], [cr])
            dv(TT(t1, ni, are, ALU.mult), [ni, are], [t1])
            dv(TT(t2, nr, aim, ALU.mult), [nr, aim], [t2])
            dv(TT(ci, t1, t2, ALU.subtract), [t1, t2], [ci])
            dv(TT(ci, ci, den, ALU.mult), [ci, den], [ci])
            dv(lambda e, ci=ci: e.tensor_scalar(ci[:, :], ci[:, :], sgn[:, 0:1], None, ALU.mult), [ci, sgn], [ci])
            bc = lambda t_: t_[:, :].unsqueeze(2).to_broadcast([128, 32, 16])
            dv(lambda e, Bst=Bst, Xb=Xb, cr=cr: e.tensor_tensor(out=Bst[:, :, :], in0=Xb[:, :, :], in1=bc(cr), op=ALU.mult), [Xb, cr], [Bst])
            dv(lambda e, Tb=Tb, Yb=Yb, ci=ci: e.tensor_tensor(out=Tb[:, :, :], in0=Yb[:, :, :], in1=bc(ci), op=ALU.mult), [Yb, ci], [Tb])
            dv(lambda e, Bst=Bst, Tb=Tb: e.tensor_tensor(out=Bst[:, :, :], in0=Bst[:, :, :], in1=Tb[:, :, :], op=ALU.add), [Bst, Tb], [Bst])
            for k in range(11):
                dv(lambda e, k=k, pr=pr, t1=t1: e.tensor_tensor(out=t1[:, :], in0=pr[:, k, :], in1=pr[:, k, :], op=ALU.mult), [pr], [t1])
                dv(lambda e, k=k, pq=pq, t2=t2: e.tensor_tensor(out=t2[:, :], in0=pq[:, k, :], in1=pq[:, k, :], op=ALU.mult), [pq], [t2])
                dv(lambda e, k=k, pr=pr, pq=pq: e.scalar_tensor_tensor(out=pq[:, k + 1, :], in0=pr[:, k, :], scalar=2.0, in1=pq[:, k, :], op0=ALU.mult, op1=ALU.mult), [pr, pq], [pq])
                dv(lambda e, k=k, pr=pr, t1=t1, t2=t2: e.tensor_tensor(out=pr[:, k + 1, :], in0=t1[:, :], in1=t2[:, :], op=ALU.subtract), [t1, t2], [pr])
            PR.append(pr)
            PQ.append(pq)
            if d == 0:
                for nm, tl_ in (("are", are), ("aim", aim), ("dt", dt), ("m1", m1), ("cc", cc), ("ss", ss), ("cr", cr), ("ci", ci)):
                    self.dump(nm, tl_, tl_[:, :], [128, 32])
                self.dump("pr", pr, pr[:, :, :], [128, 12, 32])
                self.dump("pq", pq, pq[:, :, :], [128, 12, 32])
                self.dump("Bst", Bst, Bst[:, :, :], [128, 32, 16])
            for b in range(4):
                ps = self.bank(0, 4)
                p.op("pe", lambda e, ps=ps, Bst=Bst, b=b: e.transpose(ps[:, 0:128], Bst[:, b * 8:(b + 1) * 8, :].rearrange("q g c -> q (g c)"), self.identf[:, :]), reads=[Bst, self.identf], writes=[ps])
                p.op("act", lambda e, ps=ps, d=d, b=b: e.copy(out=BT[d][b][:, :], in_=ps[:, 0:128]), reads=[ps], writes=[BT[d][b]])
                p.dma("sp", Cn[:, 0:64], W["s5_c_re"][l, d, b * 8:(b + 1) * 8, :, :].rearrange("g c q -> (g c) q"), writes=[Cn])
                p.dma("sp", Cn[:, 64:128], W["s5_c_im"][l, d, b * 8:(b + 1) * 8, :, :].rearrange("g c q -> (g c) q"), writes=[Cn])
                ps = self.bank(0, 4)
                p.op("pe", lambda e, ps=ps: e.transpose(ps[:, 0:128], Cn[:, :], self.identf[:, :]), reads=[Cn, self.identf], writes=[ps])
                p.op("dve", lambda e, ps=ps, d=d, b=b: e.tensor_scalar(CST[d][b][:, :], ps[:, 0:128], sgn[:, 1:2], None, ALU.mult), reads=[ps, sgn], writes=[CST[d][b]])

        for b_ in (0, 3):
            self.dump(f"BT{b_}", BT[0][b_], BT[0][b_][:, :], [128, 128])
            self.dump(f"CST{b_}", CST[0][b_], CST[0][b_][:, :], [128, 128])
        p.barrier()
        pst.close()
        uT = p.sb("s_uT", [128, S], BF16, st)
        X = [[p.sb(f"s_X{d}{i}", [128, S], BF16, st, nb=8) for i in range(2)] for d in range(2)]
        yacc = p.sb("s_yacc", [128, S], F32, st, nb=8)
        ygT = p.sb("s_ygT", [128, 4, S], BF16, st)
        Mk = [p.sb(f"s_Mk{i}", [128, 128], BF16, st) for i in range(8)]
        Mt = [p.sb(f"s_Mt{i}", [128, 128], F32, st) for i in range(4)]
        Mt2 = [p.sb(f"s_Mt2{i}", [128, 128], F32, st) for i in range(4)]
        LB = [p.sb(f"s_LB{i}", [128, 128], BF16, st) for i in range(4)]
        LC = [p.sb(f"s_LC{i}", [128, 128], BF16, st) for i in range(4)]
        yt = [p.sb(f"s_yt{i}", [128, 512], F32, st) for i in range(2)]
        ob = [p.sb(f"s_ob{i}", [128, 512], BF16, st) for i in range(2)]
        dirs = self.dbg.get("s5_dirs", (0, 1))
        mki = 0
        gi = 0
        bk = [0]

        def nb(lo, hi):
            b_ = self.PS[lo + bk[0] % (hi - lo)]
            bk[0] += 1
            return b_

        for b in self.dbg.get("s5_tiles", range(4)):
            c0 = COL["u"] + b * 128
            self.wload(stg, wu, wu[:, :, :], W["w_in"][l, :, c0:c0 + 128], 8, 128)
            for ct in range(8):
                ps = self.bank(0, 4)
                for c in range(8):
                    self.mm(ps[:, :], wu[:, c, :], xT[:, c, ct * 512:(ct + 1) * 512], c == 0, c == 7, [wu] + xTr[ct * 4:ct * 4 + 4], [ps])
                p.op("act", lambda e, ps=ps, ct=ct: e.copy(out=uT[:, ct * 512:(ct + 1) * 512], in_=ps[:, :]), reads=[ps], writes=[uT])
            first_acc = True
            for gl in range(8):
                g = b * 8 + gl
                lbs, lcs = {}, {}
                for d in dirs:
                    lb = LB[gi % 4]
                    lc = LC[gi % 4]
                    gi += 1
                    lbs[d], lcs[d] = lb, lc
                    p.op("dve", lambda e, lb=lb, d=d, gl=gl, b=b: e.tensor_scalar(lb[:, :], BT[d][b][:, :], rowm[:, gl:gl + 1], None, ALU.mult), reads=[BT[d][b], rowm], writes=[lb])
                    p.op("pool", lambda e, lc=lc: e.memset(lc[:, :], 0.0), writes=[lc])
                    p.op("pool", lambda e, lc=lc, d=d, gl=gl, b=b: e.tensor_copy(out=lc[:, gl * 16:(gl + 1) * 16], in_=CST[d][b][:, gl * 16:(gl + 1) * 16]), reads=[CST[d][b], lc], writes=[lc])
                    for ct in range(8):
                        ps = nb(0, 8)
                        self.mm(ps[:, :], lb[:, :], uT[:, ct * 512:(ct + 1) * 512], True, True, [lb, uT], [ps])
                        if ct % 2 == 0:
                            p.op("act", lambda e, ps=ps, ct=ct, d=d: e.copy(out=X[d][0][:, ct * 512:(ct + 1) * 512], in_=ps[:, :]), reads=[ps], writes=[X[d][0].bs[ct]])
                        else:
                            p.op("dve", lambda e, ps=ps, ct=ct, d=d: e.tensor_copy(out=X[d][0][:, ct * 512:(ct + 1) * 512], in_=ps[:, :]), reads=[ps], writes=[X[d][0].bs[ct]])
                cur = 0
                mkq = {}

                def build_mk(k, g=g):
                    nonlocal mki
                    for d in dirs:
                        mt = Mt[mki % 4]
                        mk = Mk[mki % 8]
                        mki += 1
                        mkq[(k, d)] = mk
                        p.op("act", lambda e, mt=mt, d=d, k=k, g=g: e.mul(out=mt[:, :], in_=jx[:, :], mul=PQ[d][:, k, g:g + 1]), reads=[jx, PQ[d]], writes=[mt])
                        p.op("dve", lambda e, mt=mt, mk=mk, d=d, k=k, g=g: e.scalar_tensor_tensor(out=mk[:, :], in0=self.identf[:, :], scalar=PR[d][:, k, g:g + 1], in1=mt[:, :], op0=ALU.mult, op1=ALU.add),
                             reads=[self.identf, PR[d], mt], writes=[mk])

                build_mk(0)
                build_mk(1)
                for k in range(12):
                    sh = 1 << k
                    if k + 2 < 12:
                        build_mk(k + 2)
                    mks = {d: mkq[(k, d)] for d in dirs}
                    for ct in range(8):
                        for d in dirs:
                            mk = mks[d]
                            Xs, Xd = X[d][cur], X[d][1 - cur]
                            t0 = ct * 512
                            if d == 0:
                                lo = max(0, sh - t0)
                                hi = 512
                                s0 = t0 + lo - sh
                            else:
                                lo = 0
                                hi = min(512, S - sh - t0)
                                s0 = t0 + sh
                            has = hi > lo
                            srcb = []
                            if has:
                                a0, a1 = s0, s0 + (hi - lo)
                                srcb = [Xs.bs[i] for i in range(a0 // 512, (a1 - 1) // 512 + 1)]
                            use_pe = ((ct + d) % 2 == 0) or not has
                            ps = nb(0, 8)
                            if use_pe:
                                self.mm(ps[:, :], identb[:, :], Xs[:, t0:t0 + 512], True, not has, [identb, Xs.bs[ct]], [ps])
                                if has:
                                    self.mm(ps[:, lo:hi], mk[:, :], Xs[:, s0:s0 + hi - lo], False, True, [mk] + srcb, [ps])
                                p.op("act", lambda e, ps=ps, Xd=Xd, t0=t0: e.copy(out=Xd[:, t0:t0 + 512], in_=ps[:, :]), reads=[ps], writes=[Xd.bs[ct]])
                            else:
                                self.mm(ps[:, lo:hi], mk[:, :], Xs[:, s0:s0 + hi - lo], True, True, [mk] + srcb, [ps])
                                p.op("dve", lambda e, ps=ps, Xd=Xd, Xs=Xs, t0=t0, lo=lo, hi=hi: e.tensor_tensor(out=Xd[:, t0 + lo:t0 + hi], in0=ps[:, lo:hi], in1=Xs[:, t0 + lo:t0 + hi], op=ALU.add),
                                     reads=[ps, Xs.bs[ct]], writes=[Xd.bs[ct]])
                                if lo > 0:
                                    p.op("pool", lambda e, Xd=Xd, Xs=Xs, t0=t0, lo=lo: e.tensor_copy(out=Xd[:, t0:t0 + lo], in_=Xs[:, t0:t0 + lo]), reads=[Xs.bs[ct]], writes=[Xd.bs[ct]])
                                if hi < 512:
                                    p.op("pool", lambda e, Xd=Xd, Xs=Xs, t0=t0, hi=hi: e.tensor_copy(out=Xd[:, t0 + hi:t0 + 512], in_=Xs[:, t0 + hi:t0 + 512]), reads=[Xs.bs[ct]], writes=[Xd.bs[ct]])
                    cur = 1 - cur
                for ct in range(8):
                    ps = nb(0, 8)
                    for i_, d in enumerate(dirs):
                        self.mm(ps[:, :], lcs[d][:, :], X[d][cur][:, ct * 512:(ct + 1) * 512], i_ == 0, i_ == len(dirs) - 1, [lcs[d], X[d][cur].bs[ct]], [ps])
                    ya = yacc[:, ct * 512:(ct + 1) * 512]
                    if first_acc:
                        p.op("dve", lambda e, ps=ps, ya=ya: e.tensor_copy(out=ya, in_=ps[:, :]), reads=[ps], writes=[yacc.bs[ct]])
                    else:
                        p.op("dve", lambda e, ps=ps, ya=ya: e.tensor_tensor(out=ya, in0=ps[:, :], in1=ya, op=ALU.add), reads=[ps], writes=[yacc.bs[ct]])
                first_acc = False
            for ct in range(8):
                y_ = yt[ct % 2]
                sl = slice(ct * 512, (ct + 1) * 512)
                p.op("dve", lambda e, y_=y_, sl=sl, b=b: e.scalar_tensor_tensor(out=y_[:, :], in0=uT[:, sl], scalar=dcol[:, b:b + 1], in1=yacc[:, sl], op0=ALU.mult, op1=ALU.add),
                     reads=[uT, dcol, yacc.bs[ct]], writes=[y_])
                if self.dbg.get("s5y"):
                    p.dma("pool", self.dbgy[b, :, sl], y_[:, :], reads=[y_], writes=[self.dbgy])
                    p.dma("pool", self.dbgy[4 + b, :, sl], yacc[:, sl], reads=[yacc.bs[ct]], writes=[self.dbgy])
                p.op("act", lambda e, y_=y_, sl=sl, b=b: e.activation(out=ygT[:, b, sl], in_=y_[:, :], func=AF.Gelu_apprx_tanh), reads=[y_], writes=[ygT])
        for m in range(4):
            for ct in range(8):
                sl = slice(ct * 512, (ct + 1) * 512)
                ps = self.bank(0, 8)
                for b in range(4):
                    self.mm(ps[:, :], wglu[:, b, m * 128:(m + 1) * 128], ygT[:, b, sl], b == 0, b == 3, [wglu, ygT], [ps])
                y_ = yt[ct % 2]
                o_ = ob[ct % 2]
                p.op("act", lambda e, ps=ps, y_=y_, m=m: e.activation(out=y_[:, :], in_=ps[:, :], func=AF.Sigmoid, bias=bglu[:, m:m + 1]), reads=[ps, bglu], writes=[y_])
                p.op("dve", lambda e, y_=y_, o_=o_, m=m, sl=sl: e.tensor_tensor(out=o_[:, :], in0=ygT[:, m, sl], in1=y_[:, :], op=ALU.mult), reads=[ygT, y_], writes=[o_])
                p.dma("pool", self.oT[1, m, :, sl], o_[:, :], reads=[o_], writes=[self.oT.bs[4 + m]])
        p.barrier()
        st.close()


def build_model():
    M = Model()
    M.phase_ln0()
    for l in range(DEPTH):
        M.phase_attn(l)
        M.phase_s5(l)
        M.phase_gla(l)
        M.phase_merge1(l)
        M.phase_merge2(l)
        M.phase_moe(l, last=(l == DEPTH - 1))
    M.p.emit()
    return M


def kernel(**inputs):
    M = build_model()
    consts = host_consts()
    shared = {k: np.ascontiguousarray(np.asarray(inputs[k], dtype=np.float32)) for k in PARAM_SHAPES}
    shared.update(consts)
    x = np.asarray(inputs["x"], dtype=np.float32)
    in_maps = []
    for b in range(8):
        m = dict(shared)
        m["x"] = np.ascontiguousarray(x[b])
        in_maps.append(m)
    res = run_bass_kernel_spmd(M.nc, in_maps, core_ids=list(range(8)))
    return np.stack([np.asarray(r["out"], dtype=np.float32) for r in res.results], axis=0)
```
